# Optimizing a Trainium2 kernel written in Bass

```python
import math
import jax, jax.numpy as jnp
from jax import lax
import numpy as np

D_MODEL = 1024
BATCH = 32
SEQ = 2048
DEPTH = 4
DEC_BATCH = 8
DEC_SEQ = 16
PAST_LEN = 4096

CHUNK = 64
A_GROUPS = 4
A_GDIM = 64
A_WIDTH = A_GROUPS * A_GDIM
SGU_LEN = 128
B_HEADS = 4
B_DK = 64
B_DV = 2 * B_DK
B_WIDTH = B_HEADS * B_DV
B_QK = B_HEADS * 2 * B_DK
QBLK = 128
C_HEADS = 4
C_DK = 64
C_DV = 64
C_WIDTH = C_HEADS * C_DV
CONV_W = 4
C_QKV = C_HEADS * (2 * C_DK + C_DV)
D_MIX = A_WIDTH + B_WIDTH + C_WIDTH
P_A = 2 * A_WIDTH
P_B = 2 * B_QK + B_WIDTH
P_C = C_QKV + C_WIDTH + 2 * C_HEADS
P_IN = P_A + P_B + P_C
PEER_HEADS = 8
PEER_NKEYS = 128
PEER_N = PEER_NKEYS * PEER_NKEYS
PEER_DQ = 256
PEER_TOPK = 16
PEER_BLOCK = 512
DN_ALPHA = (2 * DEPTH) ** 0.25
DN_BETA = (8 * DEPTH) ** -0.25
LN_EPS = 1e-5

kernel_name = "hybrid_stream_encoder_step"


def layer_norm(x, g=None, b=None):
    xf = x.astype(jnp.float32)
    mu = jnp.mean(xf, -1, keepdims=True)
    var = jnp.mean(jnp.square(xf - mu), -1, keepdims=True)
    y = (xf - mu) * lax.rsqrt(var + LN_EPS)
    if g is not None:
        y = y * g.astype(jnp.float32) + b.astype(jnp.float32)
    return y.astype(x.dtype)


def rms_norm(x, g):
    xf = x.astype(jnp.float32)
    y = xf * lax.rsqrt(jnp.mean(xf * xf, -1, keepdims=True) + LN_EPS) * g.astype(jnp.float32)
    return y.astype(x.dtype)


def l2norm(x):
    return x * lax.rsqrt(jnp.sum(x * x, -1, keepdims=True) + 1e-6)


def alibi_slopes():
    return jnp.exp2(-(8.0 / B_HEADS) * jnp.arange(1, B_HEADS + 1, dtype=jnp.float32))


def spatial_gating(u, v, ln_g, ln_b, w_s, b_s):
    nb, t, _ = u.shape
    v = layer_norm(v.reshape(nb, t, A_GROUPS, A_GDIM), ln_g, ln_b)
    pad = -t % SGU_LEN
    vc = jnp.pad(v, ((0, 0), (0, pad), (0, 0), (0, 0))).reshape(nb, -1, SGU_LEN, A_GROUPS, A_GDIM)
    i = jnp.arange(SGU_LEN)
    mask = (i[None, :] // CHUNK) <= (i[:, None] // CHUNK)
    w = jnp.where(mask[None], w_s, 0.0).astype(vc.dtype)
    s = jnp.einsum('gij,bnjgd->bnigd', w, vc) + b_s.T[None, None, :, :, None]
    s = s.reshape(nb, -1, A_GROUPS, A_GDIM)[:, :t]
    y = (u.reshape(nb, t, A_GROUPS, A_GDIM) * s).reshape(nb, t, A_WIDTH)
    return y, v.reshape(nb, t, A_WIDTH)


def diff_core(q, k, v, qpos, kpos, lam):
    s = jnp.einsum('bqhmd,bkhmd->bhmqk', q, k, preferred_element_type=jnp.float32) * (B_DK ** -0.5)
    dist = jnp.abs(qpos[:, None] - kpos[None, :]).astype(jnp.float32)
    s = s - alibi_slopes()[None, :, None, None, None] * dist
    mask = (kpos[None, :] // CHUNK) <= (qpos[:, None] // CHUNK)
    s = jnp.where(mask, s, -jnp.inf)
    p = jax.nn.softmax(s, axis=-1)
    a = p[:, :, 0] - lam * p[:, :, 1]
    return jnp.einsum('bhqk,bkhd->bqhd', a.astype(v.dtype), v)


def diff_attention_prompt(q, k, v, lam):
    t = q.shape[1]
    pos = jnp.arange(t)
    outs = []
    for qb in range(t // QBLK):
        lo, hi = qb * QBLK, (qb + 1) * QBLK
        outs.append(diff_core(q[:, lo:hi], k[:, :hi], v[:, :hi], pos[lo:hi], pos[:hi], lam))
    return jnp.concatenate(outs, axis=1)


def diff_attention_cached(q, k, v, past_k, past_v, lam):
    p_len, t = past_k.shape[1], q.shape[1]
    kk = jnp.concatenate([past_k.astype(k.dtype), k], axis=1)
    vv = jnp.concatenate([past_v.astype(v.dtype), v], axis=1)
    return diff_core(q, kk, vv, p_len + jnp.arange(t), jnp.arange(p_len + t), lam)


def gated_delta_rule(q, k, v, g, beta, s0):
    f32 = jnp.float32
    nb, t, nh, _ = q.shape
    dv = v.shape[-1]
    pad = -t % CHUNK
    def padt(z):
        return jnp.pad(z.astype(f32), ((0, 0), (0, pad)) + ((0, 0),) * (z.ndim - 2))
    n = (t + pad) // CHUNK
    def blk(z):
        return jnp.moveaxis(padt(z).reshape(nb, n, CHUNK, nh, -1), 3, 1)
    q, k, v = blk(q), blk(k), blk(v)
    g, beta = blk(g)[..., 0], blk(beta)[..., 0]
    gc = jnp.cumsum(g, axis=-1)
    idx = jnp.arange(CHUNK)
    lower = idx[:, None] >= idx[None, :]
    strict = idx[:, None] > idx[None, :]
    gamma = jnp.where(lower, jnp.exp(jnp.where(lower, gc[..., :, None] - gc[..., None, :], 0.0)), 0.0)
    kb = k * beta[..., None]
    m = jnp.where(strict, jnp.einsum('bhnid,bhnjd->bhnij', kb, k) * gamma, 0.0)
    rhs = jnp.concatenate([v * beta[..., None], kb * jnp.exp(gc)[..., None]], axis=-1)
    sol = lax.linalg.triangular_solve(jnp.eye(CHUNK, dtype=f32) + m, rhs, left_side=True, lower=True)
    u, w = sol[..., :dv], sol[..., dv:]
    aqk = jnp.einsum('bhnid,bhnjd->bhnij', q, k) * gamma
    qg = q * jnp.exp(gc)[..., None]
    kdec = k * jnp.exp(gc[..., -1:] - gc)[..., None]
    glast = jnp.exp(gc[..., -1])

    def step(s, inp):
        u_n, w_n, qg_n, aqk_n, kdec_n, gl_n = inp
        v_new = u_n - jnp.einsum('bhcd,bhde->bhce', w_n, s)
        o_n = jnp.einsum('bhcd,bhde->bhce', qg_n, s) + jnp.einsum('bhij,bhje->bhie', aqk_n, v_new)
        s = s * gl_n[..., None, None] + jnp.einsum('bhcd,bhce->bhde', kdec_n, v_new)
        return s, o_n

    xs = tuple(jnp.moveaxis(z, 2, 0) for z in (u, w, qg, aqk, kdec, glast))
    s_fin, o = lax.scan(step, s0.astype(f32), xs)
    o = jnp.transpose(o, (1, 0, 3, 2, 4)).reshape(nb, n * CHUNK, nh, dv)[:, :t]
    return o, s_fin


def gated_deltanet(pc, conv_w, a_log, dt_bias, norm_g, conv_state, s0):
    nb, t, _ = pc.shape
    qkv = pc[..., :C_QKV]
    z = pc[..., C_QKV:C_QKV + C_WIDTH]
    a = pc[..., C_QKV + C_WIDTH:C_QKV + C_WIDTH + C_HEADS]
    bb = pc[..., C_QKV + C_WIDTH + C_HEADS:]
    if conv_state is None:
        conv_state = jnp.zeros((nb, CONV_W - 1, C_QKV), qkv.dtype)
    if s0 is None:
        s0 = jnp.zeros((nb, C_HEADS, C_DK, C_DV), jnp.float32)
    xp = jnp.concatenate([conv_state.astype(qkv.dtype), qkv], axis=1)
    conv_tail = xp[:, -(CONV_W - 1):]
    conv = xp[:, :t] * conv_w[0]
    for j in range(1, CONV_W):
        conv = conv + xp[:, j:j + t] * conv_w[j]
    conv = jax.nn.silu(conv).astype(jnp.float32)
    q = conv[..., :C_HEADS * C_DK].reshape(nb, t, C_HEADS, C_DK)
    k = conv[..., C_HEADS * C_DK:2 * C_HEADS * C_DK].reshape(nb, t, C_HEADS, C_DK)
    v = conv[..., 2 * C_HEADS * C_DK:].reshape(nb, t, C_HEADS, C_DV)
    q = l2norm(q) * (C_DK ** -0.5)
    k = l2norm(k)
    g = -jnp.exp(a_log.astype(jnp.float32)) * jax.nn.softplus(a.astype(jnp.float32) + dt_bias.astype(jnp.float32))
    beta = jax.nn.sigmoid(bb.astype(jnp.float32))
    o, s_fin = gated_delta_rule(q, k, v, g, beta, s0)
    o = rms_norm(o.astype(pc.dtype), norm_g) * jax.nn.silu(z.reshape(nb, t, C_HEADS, C_DV))
    return o.reshape(nb, t, C_WIDTH), s_fin, conv_tail


def peer_block(h, w_query, sub_keys, expert_u, expert_v):
    n = h.shape[0]
    q = (h @ w_query).reshape(n, PEER_HEADS, 2, PEER_DQ // 2)
    s = jnp.einsum('nhpd,hpkd->nhpk', q, sub_keys, preferred_element_type=jnp.float32)
    top_s, top_i = lax.top_k(s, PEER_TOPK)
    cand = top_s[:, :, 0, :, None] + top_s[:, :, 1, None, :]
    best_s, best_c = lax.top_k(cand.reshape(n, PEER_HEADS, PEER_TOPK * PEER_TOPK), PEER_TOPK)
    i1 = jnp.take_along_axis(top_i[:, :, 0], best_c // PEER_TOPK, axis=-1)
    i2 = jnp.take_along_axis(top_i[:, :, 1], best_c % PEER_TOPK, axis=-1)
    ids = (i1 * PEER_NKEYS + i2).reshape(n, PEER_HEADS * PEER_TOPK)
    gates = jax.nn.softmax(best_s, axis=-1).reshape(n, PEER_HEADS * PEER_TOPK)
    act = jax.nn.gelu(jnp.einsum('nkd,nd->nk', expert_u[ids], h))
    return jnp.einsum('nk,nkd->nd', (gates * act).astype(h.dtype), expert_v[ids])


def peer(h, w_query, sub_keys, expert_u, expert_v):
    nb, t, d = h.shape
    flat = h.reshape(nb * t, d)
    n = flat.shape[0]
    flat = jnp.pad(flat, ((0, -n % PEER_BLOCK), (0, 0)))
    out = lax.map(lambda hb: peer_block(hb, w_query, sub_keys, expert_u, expert_v),
                  flat.reshape(-1, PEER_BLOCK, d))
    return out.reshape(-1, d)[:n].reshape(nb, t, d)


def run_group(x, c, past_k, past_v, gdn_state, conv_state,
              w_ada, b_ada, w_in, sgu_ln_g, sgu_ln_b, sgu_w, sgu_b,
              lam_q1, lam_k1, lam_q2, lam_k2, diff_norm_g,
              conv_w, gdn_a_log, gdn_dt_bias, gdn_norm_g,
              w_out, ln1_g, ln1_b, peer_wq, peer_keys, expert_u, expert_v, ln2_g, ln2_b):
    nb, t, _ = x.shape
    cached = past_k is not None
    new_k, new_v, new_s, new_conv, new_sgu_v = [], [], [], [], []
    for l in range(DEPTH):
        mod = (jax.nn.silu(c) @ w_ada[l] + b_ada[l]).reshape(nb, 6, 1, D_MODEL)
        shift1, scale1, gate1, shift2, scale2, gate2 = (mod[:, i] for i in range(6))
        h = layer_norm(x) * (1 + scale1) + shift1
        p = h @ w_in[l]
        pa, pb, pc = p[..., :P_A], p[..., P_A:P_A + P_B], p[..., P_A + P_B:]
        ya, va = spatial_gating(jax.nn.gelu(pa[..., :A_WIDTH]), jax.nn.gelu(pa[..., A_WIDTH:]),
                                sgu_ln_g[l], sgu_ln_b[l], sgu_w[l], sgu_b[l])
        qb = pb[..., :B_QK].reshape(nb, t, B_HEADS, 2, B_DK)
        kb = pb[..., B_QK:2 * B_QK].reshape(nb, t, B_HEADS, 2, B_DK)
        vb = pb[..., 2 * B_QK:].reshape(nb, t, B_HEADS, B_DV)
        lam_init = 0.8 - 0.6 * math.exp(-0.3 * l)
        lam = (jnp.exp(jnp.sum(lam_q1[l].astype(jnp.float32) * lam_k1[l].astype(jnp.float32)))
               - jnp.exp(jnp.sum(lam_q2[l].astype(jnp.float32) * lam_k2[l].astype(jnp.float32)))
               + lam_init)
        if cached:
            ob = diff_attention_cached(qb, kb, vb, past_k[l].reshape(nb, -1, B_HEADS, 2, B_DK), past_v[l], lam)
        else:
            ob = diff_attention_prompt(qb, kb, vb, lam)
        yb = (rms_norm(ob, diff_norm_g[l]) * (1.0 - lam_init)).reshape(nb, t, B_WIDTH)
        yc, s_fin, conv_tail = gated_deltanet(pc, conv_w[l], gdn_a_log[l], gdn_dt_bias[l], gdn_norm_g[l],
                                              conv_state[l] if cached else None,
                                              gdn_state[l] if cached else None)
        y = jnp.concatenate([ya, yb, yc], axis=-1) @ w_out[l]
        x = layer_norm(DN_ALPHA * x + gate1 * y, ln1_g[l], ln1_b[l])
        h2 = layer_norm(x) * (1 + scale2) + shift2
        f = peer(h2, peer_wq[l], peer_keys[l], expert_u[l], expert_v[l])
        x = layer_norm(DN_ALPHA * x + gate2 * f, ln2_g[l], ln2_b[l])
        new_k.append(kb.reshape(nb, t, B_HEADS, 2 * B_DK))
        new_v.append(vb)
        new_s.append(s_fin)
        new_conv.append(conv_tail)
        if cached:
            new_sgu_v.append(va)
    sgu_state = jnp.stack(new_sgu_v) if cached else None
    return (x, jnp.stack(new_k), jnp.stack(new_v), jnp.stack(new_s), jnp.stack(new_conv), sgu_state)


def setup_inputs(seed: int = 0) -> dict:
    key = jax.random.key(seed)
    k = jax.random.split(key, 34)
    f32 = jnp.float32
    def nrm(kk, shape, scale):
        return jax.random.normal(kk, shape, f32) * scale
    dt = jnp.exp(jax.random.uniform(k[22], (DEPTH, C_HEADS), f32, math.log(1e-3), math.log(1e-1)))
    return {
        "x_prompt": nrm(k[0], (BATCH, SEQ, D_MODEL), 1.0),
        "x_sample": nrm(k[1], (DEC_BATCH, DEC_SEQ, D_MODEL), 1.0),
        "cache_k": nrm(k[2], (DEPTH, DEC_BATCH, PAST_LEN, B_HEADS, 2 * B_DK), 1.0),
        "cache_v": nrm(k[3], (DEPTH, DEC_BATCH, PAST_LEN, B_HEADS, B_DV), 1.0),
        "state_gdn": nrm(k[4], (DEPTH, DEC_BATCH, C_HEADS, C_DK, C_DV), 0.3),
        "state_conv": nrm(k[5], (DEPTH, DEC_BATCH, CONV_W - 1, C_QKV), 1.0),
        "c_prompt": nrm(k[6], (BATCH, D_MODEL), 1.0),
        "c_sample": nrm(k[7], (DEC_BATCH, D_MODEL), 1.0),
        "w_ada": nrm(k[8], (DEPTH, D_MODEL, 6 * D_MODEL), 0.5 * D_MODEL ** -0.5),
        "b_ada": nrm(k[9], (DEPTH, 6 * D_MODEL), 0.02),
        "w_in": nrm(k[10], (DEPTH, D_MODEL, P_IN), D_MODEL ** -0.5),
        "sgu_ln_g": 1.0 + nrm(k[11], (DEPTH, A_GROUPS, A_GDIM), 0.02),
        "sgu_ln_b": nrm(k[12], (DEPTH, A_GROUPS, A_GDIM), 0.02),
        "sgu_w": nrm(k[13], (DEPTH, A_GROUPS, SGU_LEN, SGU_LEN), 0.5 * SGU_LEN ** -0.5),
        "sgu_b": 1.0 + nrm(k[14], (DEPTH, A_GROUPS, SGU_LEN), 0.1),
        "lam_q1": nrm(k[15], (DEPTH, B_DK), 0.1),
        "lam_k1": nrm(k[16], (DEPTH, B_DK), 0.1),
        "lam_q2": nrm(k[17], (DEPTH, B_DK), 0.1),
        "lam_k2": nrm(k[18], (DEPTH, B_DK), 0.1),
        "diff_norm_g": 1.0 + nrm(k[19], (DEPTH, B_DV), 0.02),
        "conv_w": nrm(k[20], (DEPTH, CONV_W, C_QKV), CONV_W ** -0.5),
        "gdn_a_log": jnp.log(jax.random.uniform(k[21], (DEPTH, C_HEADS), f32, 1.0, 16.0)),
        "gdn_dt_bias": dt + jnp.log(-jnp.expm1(-dt)),
        "gdn_norm_g": 1.0 + nrm(k[23], (DEPTH, C_DV), 0.02),
        "w_out": nrm(k[24], (DEPTH, D_MIX, D_MODEL), DN_BETA * D_MIX ** -0.5),
        "ln1_g": 1.0 + nrm(k[25], (DEPTH, D_MODEL), 0.02),
        "ln1_b": nrm(k[26], (DEPTH, D_MODEL), 0.02),
        "peer_wq": nrm(k[27], (DEPTH, D_MODEL, PEER_HEADS * PEER_DQ), D_MODEL ** -0.5),
        "peer_keys": nrm(k[28], (DEPTH, PEER_HEADS, 2, PEER_NKEYS, PEER_DQ // 2), (PEER_DQ // 2) ** -0.5),
        "expert_u": nrm(k[29], (DEPTH, PEER_N, D_MODEL), D_MODEL ** -0.5),
        "expert_v": nrm(k[30], (DEPTH, PEER_N, D_MODEL), DN_BETA),
        "ln2_g": 1.0 + nrm(k[31], (DEPTH, D_MODEL), 0.02),
        "ln2_b": nrm(k[32], (DEPTH, D_MODEL), 0.02),
    }


def reference(x_prompt, x_sample, cache_k, cache_v, state_gdn, state_conv, c_prompt, c_sample,
              w_ada, b_ada, w_in, sgu_ln_g, sgu_ln_b, sgu_w, sgu_b,
              lam_q1, lam_k1, lam_q2, lam_k2, diff_norm_g,
              conv_w, gdn_a_log, gdn_dt_bias, gdn_norm_g,
              w_out, ln1_g, ln1_b, peer_wq, peer_keys, expert_u, expert_v, ln2_g, ln2_b):
    weights = (w_ada, b_ada, w_in, sgu_ln_g, sgu_ln_b, sgu_w, sgu_b,
               lam_q1, lam_k1, lam_q2, lam_k2, diff_norm_g,
               conv_w, gdn_a_log, gdn_dt_bias, gdn_norm_g,
               w_out, ln1_g, ln1_b, peer_wq, peer_keys, expert_u, expert_v, ln2_g, ln2_b)
    y_prompt, new_k_prompt, new_v_prompt, new_gdn_prompt, new_conv_prompt, _ = run_group(
        x_prompt, c_prompt, None, None, None, None, *weights)
    y_sample, new_k_sample, new_v_sample, new_gdn_sample, new_conv_sample, new_sgu_v_sample = run_group(
        x_sample, c_sample, cache_k, cache_v, state_gdn, state_conv, *weights)
    return (y_prompt, y_sample, new_k_prompt, new_v_prompt, new_gdn_prompt, new_conv_prompt,
            new_k_sample, new_v_sample, new_gdn_sample, new_conv_sample, new_sgu_v_sample)
```

```python
import math
import numpy as np
import concourse.bass as bass
import concourse.mybir as mybir
from concourse.bass_utils import run_bass_kernel_spmd

F32 = mybir.dt.float32
BF16 = mybir.dt.bfloat16
I32 = mybir.dt.int32
U32 = mybir.dt.uint32
AF = mybir.ActivationFunctionType
ALU = mybir.AluOpType
AX = mybir.AxisListType

NCORES = 8


class Cfg:
    def __init__(self, **kw):
        self.D = 1024
        self.BATCH = 32
        self.SEQ = 2048
        self.DEPTH = 4
        self.DEC_BATCH = 8
        self.DEC_SEQ = 16
        self.PAST = 4096
        for k, v in kw.items():
            setattr(self, k, v)
        self.ALPHA = (2 * self.DEPTH) ** 0.25


class Res:
    __slots__ = ("name", "lw", "rd")

    def __init__(self, name=""):
        self.name = name
        self.lw = None
        self.rd = {}


class T:
    def __init__(self, handle, name, nres=1):
        self.h = handle
        self.name = name
        self.res = [Res(f"{name}.{i}") for i in range(nres)]

    def __getitem__(self, idx):
        return self.h[idx]

    @property
    def r(self):
        return self.res[0]


COMPUTE = ("pe", "act", "dve", "pool")
NDMASEM = 24


class Prog:
    def __init__(self, nc, stack):
        self.nc = nc
        self.stack = stack
        self.ops = {e: [] for e in ("pe", "act", "dve", "pool", "sp")}
        self.sem = {}
        for e in COMPUTE:
            self.sem[e] = stack.enter_context(nc.semaphore(f"s_{e}"))
        self.cnt = {e: 0 for e in COMPUTE}
        self.dsem = {}
        self.dcnt = {}
        self.dnext = {}
        for q in ("sp", "pool"):
            self.dsem[q] = [stack.enter_context(nc.semaphore(f"d_{q}{i}")) for i in range(NDMASEM)]
            self.dcnt[q] = [0] * NDMASEM
            self.dnext[q] = 0
        self.known = {e: {} for e in self.ops}
        self.out_tokens = []
        self.n_ops = 0

    def _need(self, eng, tok, waits):
        if tok is None:
            return
        key, val = tok[0], tok[1]
        if self.known[eng].get(key, 0) >= val:
            return
        self.known[eng][key] = val
        waits.append((key, val))

    def barrier(self):
        toks = []
        for c in COMPUTE:
            if self.cnt[c] > 0:
                toks.append((("c", c), self.cnt[c], c, False))
        for q in ("sp", "pool"):
            for slot in range(NDMASEM):
                if self.dcnt[q][slot] > 0:
                    toks.append((("d", q, slot), self.dcnt[q][slot], q, True))
        for e in self.ops:
            waits = []
            for tk in toks:
                self._need(e, tk, waits)
            if waits:
                self.ops[e].append((waits, None, None))

    def op(self, eng, fn, reads=(), writes=(), dma=False, is_out=False):
        waits = []
        for r in reads:
            rr = r.r if isinstance(r, T) else r
            if rr.lw is not None:
                if rr.lw[2] == eng and eng == "pe" and not dma and rr.lw[3] is False:
                    pass
                else:
                    self._need(eng, rr.lw, waits)
        for w in writes:
            ww = w.r if isinstance(w, T) else w
            if ww.lw is not None:
                if not (ww.lw[2] == eng and not dma and ww.lw[3] is False):
                    self._need(eng, ww.lw, waits)
            for tk in ww.rd.values():
                if not (tk[2] == eng and not dma and tk[3] is False):
                    self._need(eng, tk, waits)
        if dma:
            q = eng
            slot = self.dnext[q]
            self.dnext[q] = (slot + 1) % NDMASEM
            key = ("d", q, slot)
            prev = self.dcnt[q][slot]
            if prev > 0:
                self._need(eng, (key, prev, q, True), waits)
            self.dcnt[q][slot] = prev + 16
            tok = (key, prev + 16, q, True)
            inc = (self.dsem[q][slot], 16)
        else:
            self.cnt[eng] += 1
            tok = (("c", eng), self.cnt[eng], eng, False)
            inc = (self.sem[eng], 1)
        self.ops[eng].append((waits, fn, inc))
        for r in reads:
            rr = r.r if isinstance(r, T) else r
            rr.rd[tok[0]] = tok
        for w in writes:
            ww = w.r if isinstance(w, T) else w
            ww.lw = tok
            ww.rd = {}
        if is_out:
            self.out_tokens.append(tok)
        self.n_ops += 1
        return tok

    def semobj(self, key):
        if key[0] == "c":
            return self.sem[key[1]]
        return self.dsem[key[1]][key[2]]

    def emit(self):
        nc = self.nc
        final_waits = []
        for tok in self.out_tokens:
            self._need("sp", tok, final_waits)
        for q in ("sp", "pool"):
            for slot in range(NDMASEM):
                if self.dcnt[q][slot] > 0:
                    self._need("sp", (("d", q, slot), self.dcnt[q][slot], q, True), final_waits)
        engmap = {"pe": "tensor", "act": "scalar", "dve": "vector", "pool": "gpsimd", "sp": "sync"}
        with nc.Block() as block:
            for e, attr in engmap.items():
                ops = self.ops[e]
                extra = final_waits if e == "sp" else []

                def body(eng, ops=ops, extra=extra):
                    for waits, fn, inc in ops:
                        for key, val in waits:
                            eng.wait_ge(self.semobj(key), val)
                        if fn is None:
                            continue
                        ins = fn(eng)
                        ins.then_inc(inc[0], inc[1])
                    for key, val in extra:
                        eng.wait_ge(self.semobj(key), val)

                getattr(block, attr)(body)


def build(cfg, stages=99):
    from contextlib import ExitStack
    D, L = cfg.D, cfg.DEPTH
    NP = cfg.BATCH // NCORES
    SEQ, TS, PAST = cfg.SEQ, cfg.DEC_SEQ, cfg.PAST
    NS = NP + 1
    P_IN = 3080
    ALPHA = cfg.ALPHA
    SLOPES = [2.0 ** (-(8.0 / 4) * (h + 1)) for h in range(4)]
    NEG = -30000.0
    nc = bass.Bass("TRN2", target_bir_lowering=False)

    def din(name, shape, dt=F32):
        return nc.dram_tensor(name, list(shape), dt, kind="ExternalInput")

    def dout(name, shape, dt=F32):
        return nc.dram_tensor(name, list(shape), dt, kind="ExternalOutput")

    xp = din("xp", [NP, SEQ, D]); xs = din("xs", [TS, D])
    cache_k = din("cache_k", [L, PAST, 512]); cache_v = din("cache_v", [L, PAST, 512])
    state_gdn = din("state_gdn", [L, 4, 64, 64]); state_conv = din("state_conv", [L, 3, 768])
    c_in = din("c_in", [NS, D])
    w_ada = din("w_ada", [L, D, 6 * D]); b_ada = din("b_ada", [L, 6 * D])
    w_in = din("w_in", [L, D, P_IN])
    sgu_ln_g = din("sgu_ln_g", [L, 256]); sgu_ln_b = din("sgu_ln_b", [L, 256])
    sgu_w = din("sgu_w", [L, 4, 128, 128]); sgu_b = din("sgu_b", [L, 4, 128])
    lam_q1 = din("lam_q1", [L, 64]); lam_k1 = din("lam_k1", [L, 64])
    lam_q2 = din("lam_q2", [L, 64]); lam_k2 = din("lam_k2", [L, 64])
    diff_norm_g = din("diff_norm_g", [L, 128])
    conv_w = din("conv_w", [L, 4, 768])
    gdn_a_log = din("gdn_a_log", [L, 4]); gdn_dt_bias = din("gdn_dt_bias", [L, 4])
    gdn_norm_g = din("gdn_norm_g", [L, 64])
    w_out = din("w_out", [L, D, D])
    ln1_g = din("ln1_g", [L, D]); ln1_b = din("ln1_b", [L, D])
    peer_wq = din("peer_wq", [L, D, 2048])
    peer_keys = din("peer_keys", [L, 16, 128, 128])
    expert_u = din("expert_u", [L * 16384, D]); expert_v = din("expert_v", [L * 16384, D])
    ln2_g = din("ln2_g", [L, D]); ln2_b = din("ln2_b", [L, D])

    y_p = dout("y_p", [NP, SEQ, D]); y_s = dout("y_s", [TS, D])
    nk_p = dout("nk_p", [L, NP, SEQ, 512]); nv_p = dout("nv_p", [L, NP, SEQ, 512])
    ngdn_p = dout("ngdn_p", [L, NP, 4, 64, 64]); nconv_p = dout("nconv_p", [L, NP, 3, 768])
    nk_s = dout("nk_s", [L, TS, 512]); nv_s = dout("nv_s", [L, TS, 512])
    ngdn_s = dout("ngdn_s", [L, 4, 64, 64]); nconv_s = dout("nconv_s", [L, 3, 768])
    nsgu_s = dout("nsgu_s", [L, TS, 256])

    with ExitStack() as st:
        P = Prog(nc, st)

        uid = [0]

        def sb(name, shape, dt=F32, stack=None):
            uid[0] += 1
            return T((stack or st).enter_context(nc.sbuf_tensor(f"{name}_{uid[0]}", list(shape), dt)), name)

        def ps(name, shape=(128, 512), dt=F32):
            return T(st.enter_context(nc.psum_tensor(name, list(shape), dt)), name)

        PS = [ps(f"ps{i}") for i in range(8)]

        def dma(out, in_, reads=(), writes=(), q="sp", is_out=False, **kw):
            P.op(q, lambda e: e.dma_start(out=out, in_=in_, **kw), reads, writes, dma=True, is_out=is_out)

        def mm(out, lhsT, rhs, reads, writes, start=True, stop=True, **kw):
            P.op("pe", lambda e: e.matmul(out, lhsT, rhs, start=start, stop=stop, **kw), reads, writes)

        def tr(out, in_, ident_ap, reads, writes):
            P.op("pe", lambda e: e.transpose(out, in_, ident_ap), reads, writes)

        def act(out, in_, func, reads, writes, **kw):
            P.op("act", lambda e: e.activation(out=out, in_=in_, func=func, **kw), reads, writes)

        def V(method, *args, reads=(), writes=(), eng="dve", **kw):
            P.op(eng, lambda e: getattr(e, method)(*args, **kw), reads, writes)

        def rsqrt(dst, src, eps, reads, dst_t, scale=1.0):
            V("tensor_scalar", dst, src, scale, eps, reads=reads, writes=[dst_t], op0=ALU.mult, op1=ALU.add)
            act(dst, dst, AF.Sqrt, [dst_t], [dst_t])
            V("reciprocal", dst, dst, reads=[dst_t], writes=[dst_t])

        def bcast_row(dram_row_ap, n):
            return dram_row_ap.partition_broadcast(128) if hasattr(dram_row_ap, "partition_broadcast") else dram_row_ap

        ident = sb("ident", [128, 128])
        identb = sb("identb", [128, 128], BF16)
        iota_p = sb("iota_p", [128, 1])
        iota_f = sb("iota_f", [128, 128])
        dif = sb("dif", [128, 128])
        triU = sb("triU", [128, 128])
        maskL = sb("maskL", [64, 64])
        maskLs = sb("maskLs", [64, 64])
        maskU = sb("maskU", [64, 64])
        ones64 = sb("ones64", [64, 64])
        ones_row = sb("ones_row", [1, 128])
        ones_rowb = sb("ones_rowb", [1, 128], BF16)
        kirow = sb("kirow", [1, 128], BF16)
        sloperow = sb("sloperow", [1, 4, 2, 128], BF16)
        nsqi = sb("nsqi", [1, 4, 2, 128], BF16)
        biasd = sb("biasd", [128, 4, 2, 128], BF16)
        tmpc = sb("tmpc", [128, 128])
        thr16 = sb("thr16", [128, 16])
        V("iota", iota_p[:], [[0, 1]], reads=[], writes=[iota_p], eng="pool", base=0, channel_multiplier=1,
          allow_small_or_imprecise_dtypes=True)
        V("iota", iota_f[:], [[1, 128]], reads=[], writes=[iota_f], eng="pool", base=0, channel_multiplier=0,
          allow_small_or_imprecise_dtypes=True)
        V("tensor_scalar", dif[:], iota_f[:], iota_p[:, 0:1], None, reads=[iota_f, iota_p], writes=[dif],
          op0=ALU.subtract)
        V("tensor_single_scalar", ident[:], dif[:], 0.0, reads=[dif], writes=[ident], op=ALU.is_equal)
        V("tensor_copy", identb[:], ident[:], reads=[ident], writes=[identb])
        V("tensor_single_scalar", triU[:], dif[:], 0.0, reads=[dif], writes=[triU], op=ALU.is_ge)
        V("tensor_single_scalar", maskU[:], dif[:64, :64], 0.0, reads=[dif], writes=[maskU], op=ALU.is_ge)
        V("tensor_single_scalar", maskL[:], dif[:64, :64], 0.0, reads=[dif], writes=[maskL], op=ALU.is_le)
        V("tensor_single_scalar", maskLs[:], dif[:64, :64], 0.0, reads=[dif], writes=[maskLs], op=ALU.is_lt)
        V("tensor_scalar", thr16[:], iota_f[:, 0:16], 1.0, 16.0, reads=[iota_f], writes=[thr16], op0=ALU.add, op1=ALU.mult)
        V("memset", ones64[:], 1.0, reads=[], writes=[ones64])
        V("memset", ones_row[:], 1.0, reads=[], writes=[ones_row])
        V("memset", ones_rowb[:], 1.0, reads=[], writes=[ones_rowb])
        V("tensor_copy", kirow[:], iota_f[0:1, :], reads=[iota_f], writes=[kirow])
        mk = sb("mk", [128, 128])
        V("tensor_single_scalar", mk[:], iota_f[:], 64.0, reads=[iota_f], writes=[mk], op=ALU.is_lt)
        V("tensor_single_scalar", tmpc[:], iota_p[:].to_broadcast([128, 128]), 64.0, reads=[iota_p], writes=[tmpc],
          op=ALU.is_ge)
        V("tensor_tensor", mk[:], mk[:], tmpc[:], reads=[mk, tmpc], writes=[mk], op=ALU.mult)
        V("tensor_single_scalar", mk[:], mk[:], NEG, reads=[mk], writes=[mk], op=ALU.mult)
        V("scalar_tensor_tensor", tmpc[:], dif[:], -1.0, dif[:], reads=[dif], writes=[tmpc], op0=ALU.mult, op1=ALU.max)
        for h in range(4):
            for m in range(2):
                V("memset", sloperow[0:1, h, m, :], SLOPES[h], reads=[], writes=[sloperow])
                V("tensor_single_scalar", nsqi[0:1, h, m, :], iota_f[0:1, :], -SLOPES[h], reads=[iota_f],
                  writes=[nsqi], op=ALU.mult)
                V("scalar_tensor_tensor", biasd[:, h, m, :], tmpc[:], -SLOPES[h], mk[:], reads=[tmpc, mk],
                  writes=[biasd], op0=ALU.mult, op1=ALU.add)

        modT = sb("modT", [128, L, 48, NS])
        with ExitStack() as s0:
            c_sb = sb("c_sb", [NS, D], stack=s0)
            cT = sb("cT", [128, 8, NS], stack=s0)
            wst = [sb(f"wst{i}", [128, 8, 512], stack=s0) for i in range(2)]
            brow = sb("brow", [1, 6 * D], stack=s0)
            dma(c_sb[:], c_in[:, :], [], [c_sb])
            act(c_sb[:], c_sb[:], AF.Silu, [c_sb], [c_sb])
            for kc in range(8):
                tr(PS[0][:, kc * NS:(kc + 1) * NS], c_sb[:NS, kc * 128:(kc + 1) * 128], ident[:NS, :NS],
                   [c_sb, ident], [PS[0]])
            V("tensor_copy", cT[:].rearrange("p a b -> p (a b)"), PS[0][:, 0:8 * NS], reads=[PS[0]], writes=[cT])
            for l in range(L):
                dma(brow[:], b_ada[l:l + 1, :], [], [brow])
                for cb in range(12):
                    w = wst[cb % 2]
                    dma(w[:], w_ada[l, :, cb * 512:(cb + 1) * 512].rearrange("(kc p) n -> p kc n", p=128), [], [w])
                    pt = PS[cb % 2]
                    for f4 in range(4):
                        fc = cb * 4 + f4
                        for kc in range(8):
                            mm(pt[:, f4 * NS:(f4 + 1) * NS], w[:, kc, f4 * 128:(f4 + 1) * 128], cT[:, kc, :],
                               [w, cT], [pt], start=(kc == 0), stop=False)
                        mm(pt[:, f4 * NS:(f4 + 1) * NS], brow[0:1, fc * 128:(fc + 1) * 128], ones_row[0:1, :NS],
                           [brow, ones_row], [pt], start=False, stop=True)
                    V("tensor_copy", modT[:, l, cb * 4:(cb + 1) * 4, :].rearrange("p a b -> p (a b)"),
                      pt[:, 0:4 * NS], reads=[pt], writes=[modT])
                for c0 in (8, 32):
                    V("tensor_scalar_add", modT[:, l, c0:c0 + 8, :], modT[:, l, c0:c0 + 8, :], 1.0,
                      reads=[modT], writes=[modT])
            P.barrier()

        wst = [sb(f"wstg{i}", [128, 8, 128]) for i in range(2)]
        x_t = sb("x_t", [128, D])
        xh = sb("xh", [128, D])
        stats = sb("stats", [128, 2, 6])
        mv = sb("mv", [128, 2])
        rstd = sb("rstd", [128, 1])
        lng = sb("lng", [128, D]); lnb = sb("lnb", [128, D])
        xres = {}

        def xr(j, t):
            return xres.setdefault((j, t), Res(f"x{j}.{t}"))

        kvres = {}

        def kvr(l, j, t):
            return kvres.setdefault((l, j, t), Res(f"kv{l}.{j}.{t}"))

        def ln_stats(src_ap, R, src_res):
            for i in range(2):
                V("bn_stats", stats[:R, i, :], src_ap[:, i * 512:(i + 1) * 512], reads=[src_res], writes=[stats])
            V("bn_aggr", mv[:R, :], stats[:R, :, :], reads=[stats], writes=[mv])
            rsqrt(rstd[:R, :], mv[:R, 1:2], 1e-5, [mv], rstd)

        def normalize(dst_t, src_t, R):
            ln_stats(src_t[:R, :], R, src_t)
            V("tensor_scalar", dst_t[:R, :], src_t[:R, :], mv[:R, 0:1], rstd[:R, 0:1], reads=[src_t, mv, rstd],
              writes=[dst_t], op0=ALU.subtract, op1=ALU.mult)

        def gate_tile(l, j, c0, Gt):
            for kc in range(8):
                pp = PS[kc // 4]
                V("tensor_copy", tmpc[:], modT[:, l, c0 + kc, j:j + 1].to_broadcast([128, 128]), reads=[modT], writes=[tmpc])
                mm(pp[:, (kc % 4) * 128:(kc % 4 + 1) * 128], tmpc[:], ident[:], [tmpc, ident], [pp])
            for hh in range(2):
                V("tensor_copy", Gt[:, hh * 512:(hh + 1) * 512], PS[hh][:, :], reads=[PS[hh]], writes=[Gt])

        def load_x(j, t, R, l, is_sample):
            if is_sample:
                src = xs[t * 128:t * 128 + R, :] if l == 0 else y_s[t * 128:t * 128 + R, :]
            else:
                src = xp[j, t * 128:t * 128 + R, :] if l == 0 else y_p[j, t * 128:t * 128 + R, :]
            dma(x_t[:R, :], src, [xr(j, t)], [x_t])

        def store_x(j, t, R, is_sample, src_t):
            dst = y_s[t * 128:t * 128 + R, :] if is_sample else y_p[j, t * 128:t * 128 + R, :]
            dma(dst, src_t[:R, :], [src_t], [xr(j, t)], is_out=True)

        def residual_ln(y_ps_list, R, dst_t):
            for hh in range(2):
                V("scalar_tensor_tensor", xh[:R, hh * 512:(hh + 1) * 512], x_t[:R, hh * 512:(hh + 1) * 512], ALPHA,
                  y_ps_list[hh], reads=[x_t] + [y_ps_list[2 + hh]], writes=[xh], op0=ALU.mult, op1=ALU.add)
            normalize(dst_t, xh, R)
            V("tensor_tensor", dst_t[:R, :], dst_t[:R, :], lng[:R, :], reads=[dst_t, lng], writes=[dst_t], op=ALU.mult)
            V("tensor_tensor", dst_t[:R, :], dst_t[:R, :], lnb[:R, :], reads=[dst_t, lnb], writes=[dst_t], op=ALU.add)

        def phase_a(l, j, T_len, is_sample):
            lam_init = 0.8 - 0.6 * math.exp(-0.3 * l)
            nt = (T_len + 127) // 128
            with ExitStack() as sa:
                def sba(name, shape, dt=F32):
                    return sb(name, shape, dt, stack=sa)
                w_in_sb = sba("w_in_sb", [128, 8, P_IN], BF16)
                w_out_sb = sba("w_out_sb", [128, 8, D], BF16)
                hT = sba("hT", [128, 8, 128], BF16)
                p_sb = sba("p_sb", [128, P_IN])
                mix = sba("mix", [128, D])
                mixT = sba("mixT", [128, 8, 128], BF16)
                wTs = sba("wTs", [128, 4, 128])
                bsT = sba("bsT", [128, 4])
                sg_g = sba("sg_g", [128, 256]); sg_b = sba("sg_b", [128, 256])
                uv = sba("uv", [128, 512]); gtmp = sba("gtmp", [128, 512])
                vst = sba("vst", [128, 4, 2]); vn = sba("vn", [128, 256]); vsq = sba("vsq", [128, 256])
                lamc = sba("lamc", [128, 4]); lamv = sba("lamv", [128, 4, 64]); nlam = sba("nlam", [128, 1])
                dng = sba("dng", [128, 128])
                Qblk = sba("Qblk", [128, 4, 2, 128], BF16)
                kst = [sba("kst0", [128, 512])] * 2
                vstg = [sba("vstg0", [128, 512])] * 2
                kTb = sba("kTb", [128, 4, 128], BF16)
                vb = sba("vb", [128, 512], BF16)
                onesc = sba("onesc", [128, 1], BF16)
                pT = sba("pT", [128, 2, 128], BF16)
                anum = sba("anum", [128, 8, 128]); aden = sba("aden", [128, 8])
                aout = sba("aout", [128, 4, 128]); asq = sba("asq", [128, 4, 128]); ass = sba("ass", [128, 4])
                cw = sba("cw", [64, 12, 4])
                negA = sba("negA", [64, 4]); dtb = sba("dtb", [64, 4]); gng = sba("gng", [64, 64])
                cbuf = sba("cbuf", [64, 12, 131]); cv = sba("cv", [64, 12, 128]); ctmp = sba("ctmp", [64, 12, 128])
                S_sb = sba("S_sb", [64, 4, 64])
                ktm = sba("ktm", [64, 4, 64]); vtm = sba("vtm", [64, 4, 64])
                gab = sba("gab", [64, 8]); gg = sba("gg", [64, 4]); beta = sba("beta", [64, 4])
                gB = sba("gB", [64, 4, 64]); gc_tm = sba("gc_tm", [64, 4]); Grow = sba("Grow", [64, 4, 64])
                D1 = sba("D1", [64, 4, 64]); E1 = sba("E1", [64, 4, 64]); E2 = sba("E2", [64, 4, 64])
                egc = sba("egc", [64, 4]); egrow = sba("egrow", [64, 4, 64]); kdf = sba("kdf", [64, 4])
                glc = sba("glc", [64, 4])
                Am = [sba(f"Am{i}", [64, 4, 64]) for i in range(2)]
                Bm = [sba(f"Bm{i}", [64, 4, 64]) for i in range(2)]
                XT = sba("XT", [64, 4, 64])
                Vb = gB; Kg = sba("Kg", [64, 4, 64]); kdec = sba("kdec", [64, 4, 64])
                u_sb = sba("u_sb", [64, 4, 64]); wT_sb = sba("wT_sb", [64, 4, 64])
                aqkT = sba("aqkT", [64, 4, 64]); qgT = sba("qgT", [64, 4, 64]); vnew = sba("vnew", [64, 4, 64])
                o_sb = E1; osq = D1; oss = sba("oss", [64, 4])
                z_sb = sba("z_sb", [64, 256])

                Gt = mix
                gate_tile(l, j, 16, Gt)
                for cb in range(25):
                    n0 = cb * 128
                    n1 = min(P_IN, n0 + 128)
                    w = wst[cb % 2]
                    dma(w[:, :, :n1 - n0], w_in[l, :, n0:n1].rearrange("(kc p) n -> p kc n", p=128), [], [w])
                    V("tensor_copy", w_in_sb[:, :, n0:n1], w[:, :, :n1 - n0], reads=[w], writes=[w_in_sb], eng="pool")
                for cb in range(8):
                    n0 = cb * 128
                    w = wst[(cb + 1) % 2]
                    dma(w[:], w_out[l, :, n0:n0 + 128].rearrange("(kc p) n -> p kc n", p=128), [], [w])
                    for kc in range(8):
                        V("tensor_tensor", w_out_sb[:, kc, n0:n0 + 128], w[:, kc, :], Gt[:, n0:n0 + 128],
                          reads=[w, Gt], writes=[w_out_sb], op=ALU.mult, eng="pool")
                dma(lng[:], ln1_g[l:l + 1, :].partition_broadcast(128), [], [lng])
                dma(lnb[:], ln1_b[l:l + 1, :].partition_broadcast(128), [], [lnb])
                wraw = p_sb
                dma(p_sb[:, 1536:2048].rearrange("p (g j) -> p g j", g=4), sgu_w[l].rearrange("g i j -> i g j"), [], [p_sb])
                for g in range(4):
                    tr(PS[2][:, g * 128:(g + 1) * 128], p_sb[:, 1536 + g * 128:1536 + (g + 1) * 128], ident[:], [p_sb, ident], [PS[2]])
                V("tensor_copy", wTs[:].rearrange("p a b -> p (a b)"), PS[2][:, :], reads=[PS[2]], writes=[wTs])
                V("memset", wTs[64:128, :, 0:64], 0.0, reads=[], writes=[wTs])
                dma(p_sb[:4, 2048:2176], sgu_b[l], [], [p_sb])
                tr(PS[3][:, 0:4], p_sb[:4, 2048:2176], ident[:4, :4], [p_sb, ident], [PS[3]])
                V("tensor_copy", bsT[:], PS[3][:, 0:4], reads=[PS[3]], writes=[bsT])
                dma(sg_g[:], sgu_ln_g[l:l + 1, :].partition_broadcast(128), [], [sg_g])
                dma(sg_b[:], sgu_ln_b[l:l + 1, :].partition_broadcast(128), [], [sg_b])
                for i, lq in enumerate((lam_q1, lam_k1, lam_q2, lam_k2)):
                    dma(lamv[:, i, :], lq[l:l + 1, :].partition_broadcast(128), [], [lamv])
                V("tensor_tensor", lamv[:, 0, :], lamv[:, 0, :], lamv[:, 1, :], reads=[lamv], writes=[lamv], op=ALU.mult)
                V("tensor_tensor", lamv[:, 2, :], lamv[:, 2, :], lamv[:, 3, :], reads=[lamv], writes=[lamv], op=ALU.mult)
                V("tensor_reduce", lamc[:, 0:1], lamv[:, 0, :], reads=[lamv], writes=[lamc], axis=AX.X, op=ALU.add)
                V("tensor_reduce", lamc[:, 1:2], lamv[:, 2, :], reads=[lamv], writes=[lamc], axis=AX.X, op=ALU.add)
                act(lamc[:, 0:2], lamc[:, 0:2], AF.Exp, [lamc], [lamc])
                V("tensor_tensor", nlam[:], lamc[:, 1:2], lamc[:, 0:1], reads=[lamc], writes=[nlam], op=ALU.subtract)
                V("tensor_scalar_add", nlam[:], nlam[:], -lam_init, reads=[nlam], writes=[nlam])
                dma(dng[:], diff_norm_g[l:l + 1, :].partition_broadcast(128), [], [dng])
                V("tensor_single_scalar", dng[:], dng[:], 1.0 - lam_init, reads=[dng], writes=[dng], op=ALU.mult)
                V("memset", Qblk[:], 0.0, reads=[], writes=[Qblk])
                V("memset", onesc[:], 1.0, reads=[], writes=[onesc])
                dma(p_sb[:4, 0:768], conv_w[l], [], [p_sb])
                for g in range(12):
                    tr(PS[3][:64, 16 + g * 4:16 + g * 4 + 4], p_sb[:4, g * 64:(g + 1) * 64], ident[:4, :4],
                       [p_sb, ident], [PS[3]])
                V("tensor_copy", cw[:].rearrange("p a b -> p (a b)"), PS[3][:64, 16:64], reads=[PS[3]], writes=[cw])
                dma(negA[:], gdn_a_log[l:l + 1, :].partition_broadcast(64), [], [negA])
                act(negA[:], negA[:], AF.Exp, [negA], [negA])
                V("tensor_single_scalar", negA[:], negA[:], -1.0, reads=[negA], writes=[negA], op=ALU.mult)
                dma(dtb[:], gdn_dt_bias[l:l + 1, :].partition_broadcast(64), [], [dtb])
                dma(gng[:], gdn_norm_g[l:l + 1, :].partition_broadcast(64), [], [gng])
                if is_sample:
                    dma(S_sb[:], state_gdn[l].rearrange("h k v -> k h v"), [], [S_sb])
                    dma(p_sb[:3, 768:1536], state_conv[l], [], [p_sb])
                    for g in range(12):
                        tr(PS[3][:64, 64 + g * 3:64 + g * 3 + 3], p_sb[:3, 768 + g * 64:768 + (g + 1) * 64], ident[:3, :3],
                           [p_sb, ident], [PS[3]])
                    V("tensor_copy", cbuf[:, :, 0:3], PS[3][:64, 64:100].rearrange("p (a b) -> p a b", b=3),
                      reads=[PS[3]], writes=[cbuf])
                else:
                    V("memset", S_sb[:], 0.0, reads=[], writes=[S_sb])
                    V("memset", cbuf[:, :, 0:3], 0.0, reads=[], writes=[cbuf])

                def attn_block(k_ap, v_ap, kv_reads, nk, R, diag, delta, first):
                    for h in range(4):
                        tr(PS[4][:, h * 128:h * 128 + nk], k_ap[:, h * 128:(h + 1) * 128], ident[:nk, :nk],
                           kv_reads + [ident], [PS[4]])
                    act(kTb[:, :, :nk], PS[4][:, :].rearrange("p (a b) -> p a b", b=128)[:, :, :nk], AF.Copy,
                        [PS[4]], [kTb])
                    V("tensor_copy", vb[:nk, :], v_ap, reads=kv_reads, writes=[vb])
                    for h in range(4):
                        sc = PS[5 + (h % 2)]
                        scv = sc[:nk, 0:256].rearrange("p (m q) -> p m q", m=2)[:, :, :R]
                        mm(scv, kTb[:, h, :nk], Qblk[:, h, :, :R], [kTb, Qblk], [sc], start=True, stop=False)
                        if diag:
                            mm(scv, identb[:nk, :nk], biasd[:nk, h, :, :R], [identb, biasd], [sc], start=False, stop=True)
                            bias_c = 0.0
                        else:
                            mm(scv, kirow[0:1, :nk], sloperow[0:1, h, :, :R], [kirow, sloperow], [sc],
                               start=False, stop=False)
                            mm(scv, ones_rowb[0:1, :nk], nsqi[0:1, h, :, :R], [ones_rowb, nsqi], [sc],
                               start=False, stop=True)
                            bias_c = -SLOPES[h] * delta
                        if bias_c != 0.0:
                            V("tensor_scalar_add", scv, scv, bias_c, reads=[sc], writes=[sc])
                        act(pT[:nk, :, :R], scv, AF.Exp, [sc], [pT])
                        pv = PS[7]
                        for m in range(2):
                            mm(pv[:R, m * 128:(m + 1) * 128], pT[:nk, m, :R], vb[:nk, h * 128:(h + 1) * 128],
                               [pT, vb], [pv])
                            mm(pv[:R, 256 + m:257 + m], pT[:nk, m, :R], onesc[:nk, 0:1], [pT, onesc], [pv])
                        if first:
                            V("tensor_copy", anum[:R, 2 * h:2 * h + 2, :], pv[:R, 0:256].rearrange("p (m d) -> p m d", m=2),
                              reads=[pv], writes=[anum])
                            V("tensor_copy", aden[:R, 2 * h:2 * h + 2], pv[:R, 256:258], reads=[pv], writes=[aden])
                        else:
                            V("tensor_tensor", anum[:R, 2 * h:2 * h + 2, :], anum[:R, 2 * h:2 * h + 2, :],
                              pv[:R, 0:256].rearrange("p (m d) -> p m d", m=2), reads=[pv, anum], writes=[anum], op=ALU.add)
                            V("tensor_tensor", aden[:R, 2 * h:2 * h + 2], aden[:R, 2 * h:2 * h + 2], pv[:R, 256:258],
                              reads=[pv, aden], writes=[aden], op=ALU.add)

                for t in range(nt):
                    R = min(128, T_len - t * 128)
                    load_x(j, t, R, l, is_sample)
                    normalize(xh, x_t, R)
                    for kc in range(8):
                        pp = PS[kc // 4]
                        tr(pp[:, (kc % 4) * 128:(kc % 4) * 128 + R], xh[:R, kc * 128:(kc + 1) * 128], ident[:R, :R],
                           [xh, ident], [pp])
                    for kc in range(8):
                        pp = PS[kc // 4]
                        act(hT[:, kc, :R], pp[:, (kc % 4) * 128:(kc % 4) * 128 + R], AF.Identity, [pp, modT], [hT],
                            scale=modT[:, l, 8 + kc, j:j + 1], bias=modT[:, l, 0 + kc, j:j + 1])
                    for cb in range(7):
                        n0 = cb * 512
                        n1 = min(P_IN, n0 + 512)
                        pb = PS[2 + cb % 2]
                        for kc in range(8):
                            mm(pb[:R, :n1 - n0], hT[:, kc, :R], w_in_sb[:, kc, n0:n1], [hT, w_in_sb], [pb],
                               start=(kc == 0), stop=(kc == 7))
                        if cb % 2 == 0:
                            act(p_sb[:R, n0:n1], pb[:R, :n1 - n0], AF.Copy, [pb], [p_sb])
                        else:
                            V("tensor_copy", p_sb[:R, n0:n1], pb[:R, :n1 - n0], reads=[pb], writes=[p_sb])
                    kdst = nk_s[l, t * 128:t * 128 + R, :] if is_sample else nk_p[l, j, t * 128:t * 128 + R, :]
                    vdst = nv_s[l, t * 128:t * 128 + R, :] if is_sample else nv_p[l, j, t * 128:t * 128 + R, :]
                    dma(kdst, p_sb[:R, 1024:1536], [p_sb], [kvr(l, j, t)], is_out=True)
                    dma(vdst, p_sb[:R, 1536:2048], [p_sb], [kvr(l, j, t)], is_out=True)
                    if t == nt - 1:
                        dst = nconv_s[l, :, :] if is_sample else nconv_p[l, j, :, :]
                        dma(dst, p_sb[R - 3:R, 2048:2816], [p_sb], [], is_out=True)

                    if stages >= 2:
                        V("tensor_tensor", gtmp[:R, :], p_sb[:R, 0:512], p_sb[:R, 0:512], reads=[p_sb], writes=[gtmp], op=ALU.mult)
                        V("tensor_scalar", gtmp[:R, :], gtmp[:R, :], 0.044715, 1.0, reads=[gtmp], writes=[gtmp],
                          op0=ALU.mult, op1=ALU.add)
                        V("tensor_tensor", gtmp[:R, :], gtmp[:R, :], p_sb[:R, 0:512], reads=[gtmp, p_sb], writes=[gtmp], op=ALU.mult)
                        act(gtmp[:R, :], gtmp[:R, :], AF.Sigmoid, [gtmp], [gtmp], scale=2.0 * math.sqrt(2.0 / math.pi))
                        V("tensor_tensor", uv[:R, :], gtmp[:R, :], p_sb[:R, 0:512], reads=[gtmp, p_sb], writes=[uv], op=ALU.mult)
                        v3 = uv[:R, 256:512].rearrange("p (g d) -> p g d", g=4)
                        V("tensor_reduce", vst[:R, :, 0], v3, reads=[uv], writes=[vst], axis=AX.X, op=ALU.add)
                        V("tensor_tensor", vsq[:R, :], uv[:R, 256:512], uv[:R, 256:512], reads=[uv], writes=[vsq], op=ALU.mult)
                        V("tensor_reduce", vst[:R, :, 1], vsq[:R, :].rearrange("p (g d) -> p g d", g=4), reads=[vsq],
                          writes=[vst], axis=AX.X, op=ALU.add)
                        V("tensor_single_scalar", vst[:R, :, :], vst[:R, :, :], 1.0 / 64, reads=[vst], writes=[vst], op=ALU.mult)
                        V("tensor_tensor", vsq[:R, 0:4], vst[:R, :, 0], vst[:R, :, 0], reads=[vst], writes=[vsq], op=ALU.mult)
                        V("tensor_tensor", vst[:R, :, 1], vst[:R, :, 1], vsq[:R, 0:4], reads=[vst, vsq], writes=[vst], op=ALU.subtract)
                        rsqrt(vst[:R, :, 1], vst[:R, :, 1], 1e-5, [vst], vst)
                        vn3 = vn[:R, :].rearrange("p (g d) -> p g d", g=4)
                        V("tensor_tensor", vn3, v3, vst[:R, :, 0:1].to_broadcast([R, 4, 64]), reads=[uv, vst], writes=[vn], op=ALU.subtract)
                        V("tensor_tensor", vn3, vn3, vst[:R, :, 1:2].to_broadcast([R, 4, 64]), reads=[vn, vst], writes=[vn], op=ALU.mult)
                        V("tensor_tensor", vn[:R, :], vn[:R, :], sg_g[:R, :], reads=[vn, sg_g], writes=[vn], op=ALU.mult)
                        V("tensor_tensor", vn[:R, :], vn[:R, :], sg_b[:R, :], reads=[vn, sg_b], writes=[vn], op=ALU.add)
                        if is_sample:
                            dma(nsgu_s[l, t * 128:t * 128 + R, :], vn[:R, :], [vn], [], is_out=True)
                        for g in range(4):
                            mm(PS[0][:R, g * 64:(g + 1) * 64], wTs[:R, g, :R], vn[:R, g * 64:(g + 1) * 64], [wTs, vn], [PS[0]])
                        for g in range(4):
                            V("scalar_tensor_tensor", mix[:R, g * 64:(g + 1) * 64], PS[0][:R, g * 64:(g + 1) * 64],
                              bsT[:R, g:g + 1], uv[:R, g * 64:(g + 1) * 64], reads=[PS[0], bsT, uv], writes=[mix],
                              op0=ALU.add, op1=ALU.mult)
                    else:
                        V("memset", mix[:R, 0:256], 0.0, reads=[], writes=[mix])

                    if stages >= 3:
                        for h in range(4):
                            pq = PS[4 + (h % 2)]
                            for kc in range(8):
                                mm(pq[:, :R], w_in_sb[:, kc, 512 + h * 128:512 + (h + 1) * 128], hT[:, kc, :R],
                                   [w_in_sb, hT], [pq], start=(kc == 0), stop=(kc == 7))
                            act(Qblk[0:64, h, 0, :R], pq[0:64, :R], AF.Copy, [pq], [Qblk], scale=0.125)
                            act(Qblk[64:128, h, 1, :R], pq[64:128, :R], AF.Copy, [pq], [Qblk], scale=0.125)
                        qpos0 = (PAST if is_sample else 0) + t * 128
                        nblk = 0
                        if is_sample:
                            for kb in range(PAST // 128):
                                ks, vs = kst[nblk % 2], vstg[nblk % 2]
                                dma(ks[:], cache_k[l, kb * 128:(kb + 1) * 128, :], [], [ks])
                                dma(vs[:], cache_v[l, kb * 128:(kb + 1) * 128, :], [], [vs])
                                attn_block(ks[:, :], vs[:, :], [ks, vs], 128, R, False, qpos0 - kb * 128, nblk == 0)
                                nblk += 1
                        for kb in range(t):
                            ks, vs = kst[nblk % 2], vstg[nblk % 2]
                            ksrc = nk_s[l, kb * 128:(kb + 1) * 128, :] if is_sample else nk_p[l, j, kb * 128:(kb + 1) * 128, :]
                            vsrc = nv_s[l, kb * 128:(kb + 1) * 128, :] if is_sample else nv_p[l, j, kb * 128:(kb + 1) * 128, :]
                            dma(ks[:], ksrc, [kvr(l, j, kb)], [ks])
                            dma(vs[:], vsrc, [kvr(l, j, kb)], [vs])
                            attn_block(ks[:, :], vs[:, :], [ks, vs], 128, R, False, (t - kb) * 128, nblk == 0)
                            nblk += 1
                        attn_block(p_sb[:R, 1024:1536], p_sb[:R, 1536:2048], [p_sb], R, R, True, 0, nblk == 0)
                        V("reciprocal", aden[:R, :], aden[:R, :], reads=[aden], writes=[aden])
                        V("tensor_tensor", anum[:R, :, :], anum[:R, :, :], aden[:R, :].unsqueeze(2).to_broadcast([R, 8, 128]),
                          reads=[anum, aden], writes=[anum], op=ALU.mult)
                        a4 = anum[:R, :, :].rearrange("p (h m) d -> p h m d", m=2)
                        V("scalar_tensor_tensor", aout[:R, :, :], a4[:, :, 1, :], nlam[:R, 0:1], a4[:, :, 0, :],
                          reads=[anum, nlam], writes=[aout], op0=ALU.mult, op1=ALU.add)
                        V("tensor_tensor", asq[:R, :, :], aout[:R, :, :], aout[:R, :, :], reads=[aout], writes=[asq], op=ALU.mult)
                        V("tensor_reduce", ass[:R, :], asq[:R, :, :], reads=[asq], writes=[ass], axis=AX.X, op=ALU.add)
                        rsqrt(ass[:R, :], ass[:R, :], 1e-5, [ass], ass, scale=1.0 / 128)
                        V("tensor_tensor", aout[:R, :, :], aout[:R, :, :], ass[:R, :].unsqueeze(2).to_broadcast([R, 4, 128]),
                          reads=[aout, ass], writes=[aout], op=ALU.mult)
                        V("tensor_tensor", mix[:R, 256:768].rearrange("p (h d) -> p h d", h=4), aout[:R, :, :],
                          dng[:R, :].unsqueeze(1).to_broadcast([R, 4, 128]), reads=[aout, dng], writes=[mix], op=ALU.mult)
                    else:
                        V("memset", mix[:R, 256:768], 0.0, reads=[], writes=[mix])

                    if stages >= 4:
                        for g in range(12):
                            pg = PS[4 + (g // 4)]
                            col = 2048 + g * 64
                            for kc in range(8):
                                mm(pg[:64, (g % 4) * 128:(g % 4) * 128 + R], w_in_sb[:, kc, col:col + 64], hT[:, kc, :R],
                                   [w_in_sb, hT], [pg], start=(kc == 0), stop=(kc == 7))
                        for q3 in range(3):
                            act(cbuf[:, q3 * 4:(q3 + 1) * 4, 3:3 + R],
                                PS[4 + q3][:64, :].rearrange("p (a b) -> p a b", b=128)[:, :, :R], AF.Copy,
                                [PS[4 + q3]], [cbuf])
                        for jt in range(4):
                            wj = cw[:, :, jt:jt + 1].to_broadcast([64, 12, R])
                            if jt == 0:
                                V("tensor_tensor", cv[:, :, :R], cbuf[:, :, jt:jt + R], wj, reads=[cbuf, cw], writes=[cv], op=ALU.mult)
                            else:
                                V("tensor_tensor", ctmp[:, :, :R], cbuf[:, :, jt:jt + R], wj, reads=[cbuf, cw], writes=[ctmp], op=ALU.mult)
                                V("tensor_tensor", cv[:, :, :R], cv[:, :, :R], ctmp[:, :, :R], reads=[cv, ctmp], writes=[cv], op=ALU.add)
                        V("tensor_copy", ctmp[:, :, 0:3], cbuf[:, :, R:R + 3], reads=[cbuf], writes=[ctmp])
                        V("tensor_copy", cbuf[:, :, 0:3], ctmp[:, :, 0:3], reads=[ctmp], writes=[cbuf])
                        act(cv[:, :, :R], cv[:, :, :R], AF.Silu, [cv], [cv])
                        V("tensor_tensor", ctmp[:, 0:8, :R], cv[:, 0:8, :R], cv[:, 0:8, :R], reads=[cv], writes=[ctmp], op=ALU.mult)
                        for q2 in range(2):
                            mm(PS[4 + q2][:64, :].rearrange("p (a b) -> p a b", b=128)[:, :, :R], ones64[:, :],
                               ctmp[:, q2 * 4:(q2 + 1) * 4, :R], [ones64, ctmp], [PS[4 + q2]])
                            V("tensor_scalar_add", ctmp[:, q2 * 4:(q2 + 1) * 4, :R],
                              PS[4 + q2][:64, :].rearrange("p (a b) -> p a b", b=128)[:, :, :R], 1e-6,
                              reads=[PS[4 + q2]], writes=[ctmp])
                        act(ctmp[:, 0:8, :R], ctmp[:, 0:8, :R], AF.Sqrt, [ctmp], [ctmp])
                        V("reciprocal", ctmp[:, 0:8, :R], ctmp[:, 0:8, :R], reads=[ctmp], writes=[ctmp])
                        V("tensor_tensor", cv[:, 0:8, :R], cv[:, 0:8, :R], ctmp[:, 0:8, :R], reads=[cv, ctmp], writes=[cv], op=ALU.mult)
                        V("tensor_single_scalar", cv[:, 0:4, :R], cv[:, 0:4, :R], 0.125, reads=[cv], writes=[cv], op=ALU.mult)
                        for c0 in range(0, R, 64):
                            c = min(64, R - c0)
                            nsteps = max(0, int(math.ceil(math.log2(c))) - 1)
                            for g in range(8):
                                tr(PS[6][:c, g * 64:(g + 1) * 64], cv[:, 4 + g, c0:c0 + c], ident[:64, :64], [cv, ident], [PS[6]])
                            V("tensor_copy", ktm[:c, :, :], PS[6][:c, 0:256].rearrange("p (a b) -> p a b", b=64), reads=[PS[6]], writes=[ktm])
                            V("tensor_copy", vtm[:c, :, :], PS[6][:c, 256:512].rearrange("p (a b) -> p a b", b=64), reads=[PS[6]], writes=[vtm])
                            for kc in range(8):
                                mm(PS[7][:c, 0:8], hT[:, kc, c0:c0 + c], w_in_sb[:, kc, 3072:3080], [hT, w_in_sb], [PS[7]],
                                   start=(kc == 0), stop=(kc == 7))
                            for kc in range(8):
                                mm(PS[7][:c, 64:320], hT[:, kc, c0:c0 + c], w_in_sb[:, kc, 2816:3072], [hT, w_in_sb], [PS[7]],
                                   start=(kc == 0), stop=(kc == 7))
                            V("tensor_tensor", gab[:c, 0:4], PS[7][:c, 0:4], dtb[:c, :], reads=[PS[7], dtb], writes=[gab], op=ALU.add)
                            act(gab[:c, 0:4], gab[:c, 0:4], AF.Exp, [gab], [gab])
                            V("tensor_scalar_add", gab[:c, 0:4], gab[:c, 0:4], 1.0, reads=[gab], writes=[gab])
                            act(gab[:c, 0:4], gab[:c, 0:4], AF.Ln, [gab], [gab])
                            V("tensor_tensor", gg[:c, :], gab[:c, 0:4], negA[:c, :], reads=[gab, negA], writes=[gg], op=ALU.mult)
                            act(beta[:c, :], PS[7][:c, 4:8], AF.Sigmoid, [PS[7]], [beta])
                            act(z_sb[:c, :], PS[7][:c, 64:320], AF.Silu, [PS[7]], [z_sb])
                            V("tensor_copy", gB[:c, :, :], gg[:c, :].unsqueeze(2).to_broadcast([c, 4, 64]), reads=[gg], writes=[gB])
                            mm(PS[6][:c, 0:4], triU[:c, :c], gg[:c, :], [triU, gg], [PS[6]])
                            for h in range(4):
                                mm(PS[6][:64, 64 + h * 64:64 + h * 64 + c], gB[:c, h, :], triU[:c, :c], [gB, triU], [PS[6]])
                            V("tensor_copy", gc_tm[:c, :], PS[6][:c, 0:4], reads=[PS[6]], writes=[gc_tm])
                            V("tensor_copy", Grow[:, :, :c], PS[6][:64, 64:320].rearrange("p (a b) -> p a b", b=64)[:, :, :c],
                              reads=[PS[6]], writes=[Grow])
                            V("tensor_tensor", D1[:c, :, :c], gc_tm[:c, :].unsqueeze(2).to_broadcast([c, 4, c]), Grow[:c, :, :c],
                              reads=[gc_tm, Grow], writes=[D1], op=ALU.subtract)
                            V("tensor_single_scalar", E1[:c, :, :c], D1[:c, :, :c], 0.0, reads=[D1], writes=[E1], op=ALU.min)
                            act(E1[:c, :, :c], E1[:c, :, :c], AF.Exp, [E1], [E1])
                            V("tensor_scalar", E2[:c, :, :c], D1[:c, :, :c], -1.0, 0.0, reads=[D1], writes=[E2], op0=ALU.mult, op1=ALU.min)
                            act(E2[:c, :, :c], E2[:c, :, :c], AF.Exp, [E2], [E2])
                            act(egc[:c, :], gc_tm[:c, :], AF.Exp, [gc_tm], [egc])
                            act(egrow[:, :, :c], Grow[:, :, :c], AF.Exp, [Grow], [egrow])
                            act(glc[:, :], Grow[:, :, c - 1], AF.Exp, [Grow], [glc])
                            V("tensor_tensor", kdf[:c, :], Grow[:c, :, c - 1], gc_tm[:c, :], reads=[Grow, gc_tm], writes=[kdf], op=ALU.subtract)
                            act(kdf[:c, :], kdf[:c, :], AF.Exp, [kdf], [kdf])
                            for h in range(4):
                                mm(PS[4][:c, h * 64:h * 64 + c], cv[:, 4 + h, c0:c0 + c], cv[:, 4 + h, c0:c0 + c], [cv], [PS[4]])
                                mm(PS[4][:c, 256 + h * 64:256 + h * 64 + c], cv[:, 4 + h, c0:c0 + c], cv[:, h, c0:c0 + c], [cv], [PS[4]])
                            V("tensor_tensor", D1[:c, :, :c], E1[:c, :, :c], maskLs[:c, :c].unsqueeze(1).to_broadcast([c, 4, c]),
                              reads=[E1, maskLs], writes=[D1], op=ALU.mult)
                            A0, B0 = Am[0], Bm[0]
                            V("tensor_tensor", A0[:c, :, :c], PS[4][:c, 0:256].rearrange("p (a b) -> p a b", b=64)[:, :, :c],
                              D1[:c, :, :c], reads=[PS[4], D1], writes=[A0], op=ALU.mult)
                            V("tensor_tensor", A0[:c, :, :c], A0[:c, :, :c], beta[:c, :].unsqueeze(2).to_broadcast([c, 4, c]),
                              reads=[A0, beta], writes=[A0], op=ALU.mult)
                            V("tensor_tensor", E2[:c, :, :c], E2[:c, :, :c], maskU[:c, :c].unsqueeze(1).to_broadcast([c, 4, c]),
                              reads=[E2, maskU], writes=[E2], op=ALU.mult)
                            V("tensor_tensor", aqkT[:c, :, :c], PS[4][:c, 256:512].rearrange("p (a b) -> p a b", b=64)[:, :, :c],
                              E2[:c, :, :c], reads=[PS[4], E2], writes=[aqkT], op=ALU.mult)
                            for h in range(4):
                                tr(PS[5][:c, h * 64:h * 64 + c], A0[:c, h, :c], ident[:c, :c], [A0, ident], [PS[5]])
                            V("tensor_copy", B0[:c, :, :c], PS[5][:c, 0:256].rearrange("p (a b) -> p a b", b=64)[:, :, :c],
                              reads=[PS[5]], writes=[B0])
                            V("tensor_tensor", XT[:c, :, :c], ident[:c, :c].unsqueeze(1).to_broadcast([c, 4, c]), B0[:c, :, :c],
                              reads=[ident, B0], writes=[XT], op=ALU.subtract)
                            cur = 0
                            for n in range(1, nsteps + 1):
                                Ac, Bc, An, Bn = Am[cur], Bm[cur], Am[1 - cur], Bm[1 - cur]
                                for h in range(4):
                                    mm(PS[4][:c, h * 64:h * 64 + c], Bc[:c, h, :c], Ac[:c, h, :c], [Ac, Bc], [PS[4]])
                                    if n < nsteps:
                                        mm(PS[4][:c, 256 + h * 64:256 + h * 64 + c], Ac[:c, h, :c], Bc[:c, h, :c], [Ac, Bc], [PS[4]])
                                V("tensor_copy", An[:c, :, :c], PS[4][:c, 0:256].rearrange("p (a b) -> p a b", b=64)[:, :, :c],
                                  reads=[PS[4]], writes=[An])
                                if n < nsteps:
                                    V("tensor_copy", Bn[:c, :, :c], PS[4][:c, 256:512].rearrange("p (a b) -> p a b", b=64)[:, :, :c],
                                      reads=[PS[4]], writes=[Bn])
                                for h in range(4):
                                    mm(PS[5][:c, h * 64:h * 64 + c], An[:c, h, :c], XT[:c, h, :c], [An, XT], [PS[5]])
                                V("tensor_tensor", XT[:c, :, :c], XT[:c, :, :c], PS[5][:c, 0:256].rearrange("p (a b) -> p a b", b=64)[:, :, :c],
                                  reads=[XT, PS[5]], writes=[XT], op=ALU.add)
                                cur = 1 - cur
                            V("tensor_tensor", Vb[:c, :, :], vtm[:c, :, :], beta[:c, :].unsqueeze(2).to_broadcast([c, 4, 64]),
                              reads=[vtm, beta], writes=[Vb], op=ALU.mult)
                            V("tensor_tensor", Kg[:c, :, :], ktm[:c, :, :], beta[:c, :].unsqueeze(2).to_broadcast([c, 4, 64]),
                              reads=[ktm, beta], writes=[Kg], op=ALU.mult)
                            V("tensor_tensor", Kg[:c, :, :], Kg[:c, :, :], egc[:c, :].unsqueeze(2).to_broadcast([c, 4, 64]),
                              reads=[Kg, egc], writes=[Kg], op=ALU.mult)
                            V("tensor_tensor", kdec[:c, :, :], ktm[:c, :, :], kdf[:c, :].unsqueeze(2).to_broadcast([c, 4, 64]),
                              reads=[ktm, kdf], writes=[kdec], op=ALU.mult)
                            V("tensor_tensor", qgT[:, :, :c], cv[:, 0:4, c0:c0 + c], egrow[:, :, :c], reads=[cv, egrow], writes=[qgT], op=ALU.mult)
                            for h in range(4):
                                mm(PS[6][:c, h * 64:(h + 1) * 64], XT[:c, h, :c], Vb[:c, h, :], [XT, Vb], [PS[6]])
                                mm(PS[6][:64, 256 + h * 64:256 + h * 64 + c], Kg[:c, h, :], XT[:c, h, :c], [Kg, XT], [PS[6]])
                            V("tensor_copy", u_sb[:c, :, :], PS[6][:c, 0:256].rearrange("p (a b) -> p a b", b=64), reads=[PS[6]], writes=[u_sb])
                            V("tensor_copy", wT_sb[:, :, :c], PS[6][:64, 256:512].rearrange("p (a b) -> p a b", b=64)[:, :, :c],
                              reads=[PS[6]], writes=[wT_sb])
                            for h in range(4):
                                mm(PS[5][:c, 256 + h * 64:256 + (h + 1) * 64], wT_sb[:, h, :c], S_sb[:, h, :], [wT_sb, S_sb], [PS[5]])
                            V("tensor_tensor", vnew[:c, :, :], u_sb[:c, :, :], PS[5][:c, 256:512].rearrange("p (a b) -> p a b", b=64),
                              reads=[u_sb, PS[5]], writes=[vnew], op=ALU.subtract)
                            for h in range(4):
                                mm(PS[6][:c, h * 64:(h + 1) * 64], qgT[:, h, :c], S_sb[:, h, :], [qgT, S_sb], [PS[6]], start=True, stop=False)
                                mm(PS[6][:c, h * 64:(h + 1) * 64], aqkT[:c, h, :c], vnew[:c, h, :], [aqkT, vnew], [PS[6]], start=False, stop=True)
                            for h in range(4):
                                mm(PS[5][:64, h * 64:(h + 1) * 64], kdec[:c, h, :], vnew[:c, h, :], [kdec, vnew], [PS[5]])
                            V("tensor_tensor", S_sb[:, :, :], S_sb[:, :, :], glc[:, :].unsqueeze(2).to_broadcast([64, 4, 64]),
                              reads=[S_sb, glc], writes=[S_sb], op=ALU.mult)
                            V("tensor_tensor", S_sb[:, :, :], S_sb[:, :, :], PS[5][:64, 0:256].rearrange("p (a b) -> p a b", b=64),
                              reads=[S_sb, PS[5]], writes=[S_sb], op=ALU.add)
                            V("tensor_copy", o_sb[:c, :, :], PS[6][:c, 0:256].rearrange("p (a b) -> p a b", b=64), reads=[PS[6]], writes=[o_sb])
                            V("tensor_tensor", osq[:c, :, :], o_sb[:c, :, :], o_sb[:c, :, :], reads=[o_sb], writes=[osq], op=ALU.mult)
                            V("tensor_reduce", oss[:c, :], osq[:c, :, :], reads=[osq], writes=[oss], axis=AX.X, op=ALU.add)
                            rsqrt(oss[:c, :], oss[:c, :], 1e-5, [oss], oss, scale=1.0 / 64)
                            V("tensor_tensor", o_sb[:c, :, :], o_sb[:c, :, :], oss[:c, :].unsqueeze(2).to_broadcast([c, 4, 64]),
                              reads=[o_sb, oss], writes=[o_sb], op=ALU.mult)
                            V("tensor_tensor", o_sb[:c, :, :], o_sb[:c, :, :], gng[:c, :].unsqueeze(1).to_broadcast([c, 4, 64]),
                              reads=[o_sb, gng], writes=[o_sb], op=ALU.mult)
                            V("tensor_tensor", o_sb[:c, :, :], o_sb[:c, :, :], z_sb[:c, :].rearrange("p (a b) -> p a b", b=64),
                              reads=[o_sb, z_sb], writes=[o_sb], op=ALU.mult)
                            dma(mix[c0:c0 + c, 768:1024], o_sb[:c, :, :].rearrange("p a b -> p (a b)"), [o_sb], [mix])
                        if t == nt - 1:
                            dst = ngdn_s[l].rearrange("h k v -> k h v") if is_sample else ngdn_p[l, j].rearrange("h k v -> k h v")
                            dma(dst, S_sb[:, :, :], [S_sb], [], is_out=True)
                    else:
                        V("memset", mix[:R, 768:1024], 0.0, reads=[], writes=[mix])

                    for kc in range(8):
                        pp = PS[kc // 4]
                        tr(pp[:, (kc % 4) * 128:(kc % 4) * 128 + R], mix[:R, kc * 128:(kc + 1) * 128], ident[:R, :R],
                           [mix, ident], [pp])
                    for hh in range(2):
                        V("tensor_copy", mixT[:, hh * 4:(hh + 1) * 4, :R],
                          PS[hh][:, :].rearrange("p (a b) -> p a b", b=128)[:, :, :R], reads=[PS[hh]], writes=[mixT])
                    for hh in range(2):
                        for kc in range(8):
                            mm(PS[2 + hh][:R, :], mixT[:, kc, :R], w_out_sb[:, kc, hh * 512:(hh + 1) * 512],
                               [mixT, w_out_sb], [PS[2 + hh]], start=(kc == 0), stop=(kc == 7))
                    residual_ln([PS[2][:R, :], PS[3][:R, :], PS[2], PS[3]], R, x_t)
                    store_x(j, t, R, is_sample, x_t)
                P.barrier()

        def phase_b(l, j, T_len, is_sample):
            nt = (T_len + 127) // 128
            with ExitStack() as sbk:
                def sbb(name, shape, dt=F32):
                    return sb(name, shape, dt, stack=sbk)
                wq_sb = sbb("wq_sb", [128, 8, 2048], BF16)
                keysT = sbb("keysT", [128, 16, 128])
                kraw = sbb("kraw", [128, 128])
                h2T = sbb("h2T", [128, 8, 128]); h2Tb = sbb("h2Tb", [128, 8, 128], BF16)
                h2 = sbb("h2", [128, D])
                qT = sbb("qT", [128, 16, 128])
                sc = sbb("sc", [128, 16, 128]); sc2 = sbb("sc2", [128, 16, 128])
                tops = sbb("tops", [128, 16, 16]); topi = sbb("topi", [128, 16, 16], U32); topf = sbb("topf", [128, 16, 16])
                cand = sbb("cand", [128, 8, 256]); cand2 = sbb("cand2", [128, 8, 256])
                bests = sbb("bests", [128, 8, 16]); besti = sbb("besti", [128, 8, 16], U32)
                af = sbb("af", [128, 8, 16]); bf = sbb("bf", [128, 8, 16])
                oh = sbb("oh", [128, 8, 16, 16])
                idf = sbb("idf", [128, 8, 16]); idf2 = sbb("idf2", [128, 8, 16]); ids = sbb("ids", [128, 128], I32)
                gmax = sbb("gmax", [128, 8]); gsum = sbb("gsum", [128, 8]); gates = sbb("gates", [128, 8, 16])
                dots = sbb("dots", [128, 128]); wts = sbb("wts", [128, 128]); gt2 = sbb("gt2", [128, 128])
                gbuf = [sbb(f"gbuf{i}", [128, D]) for i in range(4)]
                junk = sbb("junk", [128, D])
                facc = sbb("facc", [128, D])

                Gt = sbb("Gt", [128, D])
                gate_tile(l, j, 40, Gt)
                for cb in range(16):
                    n0 = cb * 128
                    w = wst[cb % 2]
                    dma(w[:], peer_wq[l, :, n0:n0 + 128].rearrange("(kc p) n -> p kc n", p=128), [], [w])
                    V("tensor_copy", wq_sb[:, :, n0:n0 + 128], w[:], reads=[w], writes=[wq_sb], eng="pool")
                for c16 in range(16):
                    dma(kraw[:], peer_keys[l, c16], [], [kraw])
                    tr(PS[0][:, 0:128], kraw[:], ident[:], [kraw, ident], [PS[0]])
                    V("tensor_copy", keysT[:, c16, :], PS[0][:, 0:128], reads=[PS[0]], writes=[keysT])
                dma(lng[:], ln2_g[l:l + 1, :].partition_broadcast(128), [], [lng])
                dma(lnb[:], ln2_b[l:l + 1, :].partition_broadcast(128), [], [lnb])

                for t in range(nt):
                    R = min(128, T_len - t * 128)
                    load_x(j, t, R, 1, is_sample)
                    normalize(xh, x_t, R)
                    for kc in range(8):
                        pp = PS[kc // 4]
                        tr(pp[:, (kc % 4) * 128:(kc % 4) * 128 + R], xh[:R, kc * 128:(kc + 1) * 128], ident[:R, :R],
                           [xh, ident], [pp])
                    for kc in range(8):
                        pp = PS[kc // 4]
                        act(h2T[:, kc, :R], pp[:, (kc % 4) * 128:(kc % 4) * 128 + R], AF.Identity, [pp, modT], [h2T],
                            scale=modT[:, l, 32 + kc, j:j + 1], bias=modT[:, l, 24 + kc, j:j + 1])
                    V("tensor_copy", h2Tb[:, :, :R], h2T[:, :, :R], reads=[h2T], writes=[h2Tb])
                    for kc in range(8):
                        pp = PS[2 + kc // 4]
                        tr(pp[:R, (kc % 4) * 128:(kc % 4 + 1) * 128], h2T[:, kc, :R], ident[:, :], [h2T, ident], [pp])
                    for hh in range(2):
                        V("tensor_copy", h2[:R, hh * 512:(hh + 1) * 512], PS[2 + hh][:R, :], reads=[PS[2 + hh]], writes=[h2])
                    for c16 in range(16):
                        pq = PS[4 + (c16 % 2)]
                        for kc in range(8):
                            mm(pq[:, :R], wq_sb[:, kc, c16 * 128:(c16 + 1) * 128], h2Tb[:, kc, :R], [wq_sb, h2Tb], [pq],
                               start=(kc == 0), stop=(kc == 7))
                        act(qT[:, c16, :R], pq[:, :R], AF.Copy, [pq], [qT])
                    for c16 in range(16):
                        pq = PS[6 + (c16 // 4) % 2]
                        mm(pq[:R, (c16 % 4) * 128:(c16 % 4 + 1) * 128], qT[:, c16, :R], keysT[:, c16, :], [qT, keysT], [pq])
                        if c16 % 4 == 3:
                            V("tensor_copy", sc[:R, c16 - 3:c16 + 1, :].rearrange("p a b -> p (a b)"), pq[:R, :], reads=[pq], writes=[sc])
                    for c16 in range(16):
                        V("max", tops[:R, c16, 0:8], sc[:R, c16, :], reads=[sc], writes=[tops])
                        V("max_index", topi[:R, c16, 0:8], tops[:R, c16, 0:8], sc[:R, c16, :], reads=[tops, sc], writes=[topi])
                        V("match_replace", sc2[:R, c16, :], tops[:R, c16, 0:8], sc[:R, c16, :], -1e30, reads=[tops, sc], writes=[sc2])
                        V("max", tops[:R, c16, 8:16], sc2[:R, c16, :], reads=[sc2], writes=[tops])
                        V("max_index", topi[:R, c16, 8:16], tops[:R, c16, 8:16], sc2[:R, c16, :], reads=[tops, sc2], writes=[topi])
                    V("tensor_copy", topf[:R, :, :], topi[:R, :, :], reads=[topi], writes=[topf])
                    t4 = tops[:R, :, :].rearrange("p (h s) k -> p h s k", s=2)
                    f4 = topf[:R, :, :].rearrange("p (h s) k -> p h s k", s=2)
                    c4 = cand[:R, :, :].rearrange("p h (a b) -> p h a b", b=16)
                    V("tensor_tensor", c4, t4[:, :, 0, :].unsqueeze(3).to_broadcast([R, 8, 16, 16]),
                      t4[:, :, 1, :].unsqueeze(2).to_broadcast([R, 8, 16, 16]), reads=[tops], writes=[cand], op=ALU.add)
                    for hd in range(8):
                        V("max", bests[:R, hd, 0:8], cand[:R, hd, :], reads=[cand], writes=[bests])
                        V("max_index", besti[:R, hd, 0:8], bests[:R, hd, 0:8], cand[:R, hd, :], reads=[bests, cand], writes=[besti])
                        V("match_replace", cand2[:R, hd, :], bests[:R, hd, 0:8], cand[:R, hd, :], -1e30, reads=[bests, cand], writes=[cand2])
                        V("max", bests[:R, hd, 8:16], cand2[:R, hd, :], reads=[cand2], writes=[bests])
                        V("max_index", besti[:R, hd, 8:16], bests[:R, hd, 8:16], cand2[:R, hd, :], reads=[bests, cand2], writes=[besti])
                    V("tensor_copy", af[:R, :, :], besti[:R, :, :], reads=[besti], writes=[af])
                    V("tensor_copy", bf[:R, :, :], af[:R, :, :], reads=[af], writes=[bf])
                    V("tensor_tensor", oh[:R], bf[:R, :, :].unsqueeze(3).to_broadcast([R, 8, 16, 16]),
                      thr16[:R, :].unsqueeze(1).unsqueeze(1).to_broadcast([R, 8, 16, 16]), reads=[bf, thr16], writes=[oh], op=ALU.is_ge)
                    V("tensor_reduce", af[:R, :, :], oh[:R], reads=[oh], writes=[af], axis=AX.X, op=ALU.add)
                    V("scalar_tensor_tensor", bf[:R, :, :], af[:R, :, :], -16.0, bf[:R, :, :], reads=[af, bf], writes=[bf],
                      op0=ALU.mult, op1=ALU.add)
                    io16 = iota_f[:R, 0:16].unsqueeze(1).unsqueeze(1).to_broadcast([R, 8, 16, 16])
                    for sel, half, dstf in ((af, 0, idf), (bf, 1, idf2)):
                        V("tensor_tensor", oh[:R], sel[:R, :, :].unsqueeze(3).to_broadcast([R, 8, 16, 16]), io16,
                          reads=[sel, iota_f], writes=[oh], op=ALU.is_equal)
                        V("tensor_tensor", oh[:R], oh[:R], f4[:, :, half, :].unsqueeze(2).to_broadcast([R, 8, 16, 16]),
                          reads=[oh, topf], writes=[oh], op=ALU.mult)
                        V("tensor_reduce", dstf[:R, :, :], oh[:R], reads=[oh], writes=[dstf], axis=AX.X, op=ALU.add)
                    V("scalar_tensor_tensor", idf[:R, :, :], idf[:R, :, :], 128.0, idf2[:R, :, :], reads=[idf, idf2], writes=[idf],
                      op0=ALU.mult, op1=ALU.add)
                    V("tensor_scalar_add", idf[:R, :, :], idf[:R, :, :], float(l * 16384), reads=[idf], writes=[idf])
                    V("tensor_copy", ids[:R, :], idf[:R, :, :].rearrange("p a b -> p (a b)"), reads=[idf], writes=[ids])
                    V("tensor_reduce", gmax[:R, :], bests[:R, :, :], reads=[bests], writes=[gmax], axis=AX.X, op=ALU.max)
                    V("tensor_tensor", gates[:R, :, :], bests[:R, :, :], gmax[:R, :].unsqueeze(2).to_broadcast([R, 8, 16]),
                      reads=[bests, gmax], writes=[gates], op=ALU.subtract)
                    act(gates[:R, :, :], gates[:R, :, :], AF.Exp, [gates], [gates])
                    V("tensor_reduce", gsum[:R, :], gates[:R, :, :], reads=[gates], writes=[gsum], axis=AX.X, op=ALU.add)
                    V("reciprocal", gsum[:R, :], gsum[:R, :], reads=[gsum], writes=[gsum])
                    V("tensor_tensor", gates[:R, :, :], gates[:R, :, :], gsum[:R, :].unsqueeze(2).to_broadcast([R, 8, 16]),
                      reads=[gates, gsum], writes=[gates], op=ALU.mult)
                    for k in range(128):
                        gb = gbuf[k % 4]
                        P.op("pool", lambda e, gb=gb, k=k, R=R: e.indirect_dma_start(
                            out=gb[:R, :], out_offset=None, in_=expert_u[:, :],
                            in_offset=bass.IndirectOffsetOnAxis(ap=ids[:R, k:k + 1], axis=0)),
                            [ids], [gb], dma=True)
                        V("tensor_tensor", junk[:R, :], gb[:R, :], h2[:R, :], reads=[gb, h2], writes=[junk], op=ALU.mult)
                        V("tensor_reduce", dots[:R, k:k + 1], junk[:R, :], reads=[junk], writes=[dots], axis=AX.X, op=ALU.add)
                    V("tensor_tensor", gt2[:R, :], dots[:R, :], dots[:R, :], reads=[dots], writes=[gt2], op=ALU.mult)
                    V("tensor_scalar", gt2[:R, :], gt2[:R, :], 0.044715, 1.0, reads=[gt2], writes=[gt2], op0=ALU.mult, op1=ALU.add)
                    V("tensor_tensor", gt2[:R, :], gt2[:R, :], dots[:R, :], reads=[gt2, dots], writes=[gt2], op=ALU.mult)
                    act(gt2[:R, :], gt2[:R, :], AF.Sigmoid, [gt2], [gt2], scale=2.0 * math.sqrt(2.0 / math.pi))
                    V("tensor_tensor", wts[:R, :], gt2[:R, :], dots[:R, :], reads=[gt2, dots], writes=[wts], op=ALU.mult)
                    V("tensor_tensor", wts[:R, :], wts[:R, :], gates[:R, :, :].rearrange("p a b -> p (a b)"), reads=[wts, gates],
                      writes=[wts], op=ALU.mult)
                    for k in range(128):
                        gb = gbuf[k % 4]
                        P.op("pool", lambda e, gb=gb, k=k, R=R: e.indirect_dma_start(
                            out=gb[:R, :], out_offset=None, in_=expert_v[:, :],
                            in_offset=bass.IndirectOffsetOnAxis(ap=ids[:R, k:k + 1], axis=0)),
                            [ids], [gb], dma=True)
                        if k == 0:
                            V("tensor_scalar", facc[:R, :], gb[:R, :], wts[:R, 0:1], None, reads=[gb, wts], writes=[facc], op0=ALU.mult)
                        else:
                            V("scalar_tensor_tensor", facc[:R, :], gb[:R, :], wts[:R, k:k + 1], facc[:R, :], reads=[gb, wts, facc],
                              writes=[facc], op0=ALU.mult, op1=ALU.add)
                    V("tensor_tensor", facc[:R, :], facc[:R, :], Gt[:R, :], reads=[facc, Gt], writes=[facc], op=ALU.mult)
                    residual_ln([facc[:R, 0:512], facc[:R, 512:1024], facc, facc], R, x_t)
                    store_x(j, t, R, is_sample, x_t)
                P.barrier()

        order = [NP] + list(range(NP))
        for j in order:
            is_sample = (j == NP)
            T_len = TS if is_sample else SEQ
            for l in range(L):
                phase_a(l, j, T_len, is_sample)
                if stages >= 5:
                    phase_b(l, j, T_len, is_sample)

        P.emit()
        print("n_ops", P.n_ops, {e: len(v) for e, v in P.ops.items()}, flush=True)
    return nc


WEIGHT_NAMES = ["w_ada", "b_ada", "w_in", "sgu_ln_g", "sgu_ln_b", "sgu_w", "sgu_b", "lam_q1", "lam_k1",
                "lam_q2", "lam_k2", "diff_norm_g", "conv_w", "gdn_a_log", "gdn_dt_bias", "gdn_norm_g",
                "w_out", "ln1_g", "ln1_b", "peer_wq", "peer_keys", "expert_u", "expert_v", "ln2_g", "ln2_b"]


def run(cfg, inputs, stages=99):
    L = cfg.DEPTH
    NP = cfg.BATCH // NCORES
    f = lambda a: np.ascontiguousarray(np.asarray(a, dtype=np.float32))
    W = {k: f(inputs[k]) for k in WEIGHT_NAMES}
    W["sgu_ln_g"] = W["sgu_ln_g"].reshape(L, 256)
    W["sgu_ln_b"] = W["sgu_ln_b"].reshape(L, 256)
    W["peer_keys"] = W["peer_keys"].reshape(L, 16, 128, 128)
    W["expert_u"] = W["expert_u"].reshape(L * 16384, cfg.D)
    W["expert_v"] = W["expert_v"].reshape(L * 16384, cfg.D)
    xp = f(inputs["x_prompt"]); xs = f(inputs["x_sample"])
    ck = f(inputs["cache_k"]); cv = f(inputs["cache_v"])
    sg = f(inputs["state_gdn"]); sc = f(inputs["state_conv"])
    cp = f(inputs["c_prompt"]); cs = f(inputs["c_sample"])
    in_maps = []
    for i in range(NCORES):
        m = dict(W)
        m["xp"] = xp[i * NP:(i + 1) * NP]
        m["xs"] = xs[i]
        m["cache_k"] = f(ck[:, i].reshape(L, cfg.PAST, 512))
        m["cache_v"] = f(cv[:, i].reshape(L, cfg.PAST, 512))
        m["state_gdn"] = f(sg[:, i])
        m["state_conv"] = f(sc[:, i])
        m["c_in"] = f(np.concatenate([cp[i * NP:(i + 1) * NP], cs[i:i + 1]], axis=0))
        in_maps.append(m)
    import time as _time
    _t0 = _time.time()
    nc = build(cfg, stages)
    print("build s", _time.time() - _t0, flush=True)
    _t0 = _time.time()
    res = run_bass_kernel_spmd(nc, in_maps, core_ids=list(range(NCORES)))
    print("run_spmd s", _time.time() - _t0, flush=True)
    R = res.results
    cat = lambda k, ax: np.concatenate([r[k] for r in R], axis=ax)
    stk = lambda k, ax: np.stack([r[k] for r in R], axis=ax)
    S = cfg.SEQ
    y_prompt = cat("y_p", 0)
    y_sample = stk("y_s", 0)
    nk_p = cat("nk_p", 1).reshape(L, cfg.BATCH, S, 4, 128)
    nv_p = cat("nv_p", 1).reshape(L, cfg.BATCH, S, 4, 128)
    ngdn_p = cat("ngdn_p", 1)
    nconv_p = cat("nconv_p", 1)
    nk_s = stk("nk_s", 1).reshape(L, cfg.DEC_BATCH, cfg.DEC_SEQ, 4, 128)
    nv_s = stk("nv_s", 1).reshape(L, cfg.DEC_BATCH, cfg.DEC_SEQ, 4, 128)
    ngdn_s = stk("ngdn_s", 1)
    nconv_s = stk("nconv_s", 1)
    nsgu_s = stk("nsgu_s", 1)
    return (y_prompt, y_sample, nk_p, nv_p, ngdn_p, nconv_p, nk_s, nv_s, ngdn_s, nconv_s, nsgu_s)


def kernel(**inputs):
    return run(Cfg(), inputs)
```

```python
import math
import numpy as np
import concourse.bass as bass
import concourse.mybir as mybir
from concourse.bass_utils import run_bass_kernel_spmd

F32 = mybir.dt.float32
BF16 = mybir.dt.bfloat16
I32 = mybir.dt.int32
U32 = mybir.dt.uint32
AF = mybir.ActivationFunctionType
ALU = mybir.AluOpType
AX = mybir.AxisListType

NCORES = 8


class Cfg:
    def __init__(self, **kw):
        self.D = 1024
        self.BATCH = 32
        self.SEQ = 2048
        self.DEPTH = 4
        self.DEC_BATCH = 8
        self.DEC_SEQ = 16
        self.PAST = 4096
        for k, v in kw.items():
            setattr(self, k, v)
        self.ALPHA = (2 * self.DEPTH) ** 0.25


class Res:
    __slots__ = ("name", "lw", "rd")

    def __init__(self, name=""):
        self.name = name
        self.lw = None
        self.rd = {}


class T:
    def __init__(self, handle, name, nres=1):
        self.h = handle
        self.name = name
        self.res = [Res(f"{name}.{i}") for i in range(nres)]

    def __getitem__(self, idx):
        return self.h[idx]

    @property
    def r(self):
        return self.res[0]


COMPUTE = ("pe", "act", "dve", "pool")
NDMASEM = 24


class Prog:
    def __init__(self, nc, stack):
        self.nc = nc
        self.stack = stack
        self.ops = {e: [] for e in ("pe", "act", "dve", "pool", "sp")}
        self.sem = {}
        for e in COMPUTE:
            self.sem[e] = stack.enter_context(nc.semaphore(f"s_{e}"))
        self.cnt = {e: 0 for e in COMPUTE}
        self.dsem = {}
        self.dcnt = {}
        self.dnext = {}
        for q in ("sp", "pool"):
            self.dsem[q] = [stack.enter_context(nc.semaphore(f"d_{q}{i}")) for i in range(NDMASEM)]
            self.dcnt[q] = [0] * NDMASEM
            self.dnext[q] = 0
        self.known = {e: {} for e in self.ops}
        self.out_tokens = []
        self.n_ops = 0

    def _need(self, eng, tok, waits):
        if tok is None:
            return
        key, val = tok[0], tok[1]
        if self.known[eng].get(key, 0) >= val:
            return
        self.known[eng][key] = val
        waits.append((key, val))

    def barrier(self):
        toks = []
        for c in COMPUTE:
            if self.cnt[c] > 0:
                toks.append((("c", c), self.cnt[c], c, False))
        for q in ("sp", "pool"):
            for slot in range(NDMASEM):
                if self.dcnt[q][slot] > 0:
                    toks.append((("d", q, slot), self.dcnt[q][slot], q, True))
        for e in self.ops:
            waits = []
            for tk in toks:
                self._need(e, tk, waits)
            if waits:
                self.ops[e].append((waits, None, None))

    def op(self, eng, fn, reads=(), writes=(), dma=False, is_out=False):
        waits = []
        for r in reads:
            rr = r.r if isinstance(r, T) else r
            if rr.lw is not None:
                if rr.lw[2] == eng and eng == "pe" and not dma and rr.lw[3] is False:
                    pass
                else:
                    self._need(eng, rr.lw, waits)
        for w in writes:
            ww = w.r if isinstance(w, T) else w
            if ww.lw is not None:
                if not (ww.lw[2] == eng and not dma and ww.lw[3] is False):
                    self._need(eng, ww.lw, waits)
            for tk in ww.rd.values():
                if not (tk[2] == eng and not dma and tk[3] is False):
                    self._need(eng, tk, waits)
        if dma:
            q = eng
            slot = self.dnext[q]
            self.dnext[q] = (slot + 1) % NDMASEM
            key = ("d", q, slot)
            prev = self.dcnt[q][slot]
            if prev > 0:
                self._need(eng, (key, prev, q, True), waits)
            self.dcnt[q][slot] = prev + 16
            tok = (key, prev + 16, q, True)
            inc = (self.dsem[q][slot], 16)
        else:
            self.cnt[eng] += 1
            tok = (("c", eng), self.cnt[eng], eng, False)
            inc = (self.sem[eng], 1)
        self.ops[eng].append((waits, fn, inc))
        for r in reads:
            rr = r.r if isinstance(r, T) else r
            rr.rd[tok[0]] = tok
        for w in writes:
            ww = w.r if isinstance(w, T) else w
            ww.lw = tok
            ww.rd = {}
        if is_out:
            self.out_tokens.append(tok)
        self.n_ops += 1
        return tok

    def semobj(self, key):
        if key[0] == "c":
            return self.sem[key[1]]
        return self.dsem[key[1]][key[2]]

    def emit(self):
        nc = self.nc
        final_waits = []
        for tok in self.out_tokens:
            self._need("sp", tok, final_waits)
        for q in ("sp", "pool"):
            for slot in range(NDMASEM):
                if self.dcnt[q][slot] > 0:
                    self._need("sp", (("d", q, slot), self.dcnt[q][slot], q, True), final_waits)
        engmap = {"pe": "tensor", "act": "scalar", "dve": "vector", "pool": "gpsimd", "sp": "sync"}
        with nc.Block() as block:
            for e, attr in engmap.items():
                ops = self.ops[e]
                extra = final_waits if e == "sp" else []

                def body(eng, ops=ops, extra=extra):
                    for waits, fn, inc in ops:
                        for key, val in waits:
                            eng.wait_ge(self.semobj(key), val)
                        if fn is None:
                            continue
                        ins = fn(eng)
                        ins.then_inc(inc[0], inc[1])
                    for key, val in extra:
                        eng.wait_ge(self.semobj(key), val)

                getattr(block, attr)(body)


def build(cfg, stages=99):
    from contextlib import ExitStack
    D, L = cfg.D, cfg.DEPTH
    NP = cfg.BATCH // NCORES
    SEQ, TS, PAST = cfg.SEQ, cfg.DEC_SEQ, cfg.PAST
    NS = NP + 1
    P_IN = 3080
    ALPHA = cfg.ALPHA
    SLOPES = [2.0 ** (-(8.0 / 4) * (h + 1)) for h in range(4)]
    NEG = -30000.0
    nc = bass.Bass("TRN2", target_bir_lowering=False)

    def din(name, shape, dt=F32):
        return nc.dram_tensor(name, list(shape), dt, kind="ExternalInput")

    def dout(name, shape, dt=F32):
        return nc.dram_tensor(name, list(shape), dt, kind="ExternalOutput")

    xp = din("xp", [NP, SEQ, D]); xs = din("xs", [TS, D])
    cache_k = din("cache_k", [L, PAST, 512]); cache_v = din("cache_v", [L, PAST, 512])
    state_gdn = din("state_gdn", [L, 4, 64, 64]); state_conv = din("state_conv", [L, 3, 768])
    c_in = din("c_in", [NS, D])
    w_ada = din("w_ada", [L, D, 6 * D]); b_ada = din("b_ada", [L, 6 * D])
    w_in = din("w_in", [L, D, P_IN])
    sgu_ln_g = din("sgu_ln_g", [L, 256]); sgu_ln_b = din("sgu_ln_b", [L, 256])
    sgu_w = din("sgu_w", [L, 4, 128, 128]); sgu_b = din("sgu_b", [L, 4, 128])
    lam_q1 = din("lam_q1", [L, 64]); lam_k1 = din("lam_k1", [L, 64])
    lam_q2 = din("lam_q2", [L, 64]); lam_k2 = din("lam_k2", [L, 64])
    diff_norm_g = din("diff_norm_g", [L, 128])
    conv_w = din("conv_w", [L, 4, 768])
    gdn_a_log = din("gdn_a_log", [L, 4]); gdn_dt_bias = din("gdn_dt_bias", [L, 4])
    gdn_norm_g = din("gdn_norm_g", [L, 64])
    w_out = din("w_out", [L, D, D])
    ln1_g = din("ln1_g", [L, D]); ln1_b = din("ln1_b", [L, D])
    peer_wq = din("peer_wq", [L, D, 2048])
    peer_keys = din("peer_keys", [L, 16, 128, 128])
    expert_u = din("expert_u", [L * 16384, D]); expert_v = din("expert_v", [L * 16384, D])
    ln2_g = din("ln2_g", [L, D]); ln2_b = din("ln2_b", [L, D])

    eu_bf = nc.dram_tensor("eu_bf", [L * 16384, D], BF16, kind="Internal")
    ev_bf = nc.dram_tensor("ev_bf", [L * 16384, D], BF16, kind="Internal")
    y_p = dout("y_p", [NP, SEQ, D]); y_s = dout("y_s", [TS, D])
    nk_p = dout("nk_p", [L, NP, SEQ, 512]); nv_p = dout("nv_p", [L, NP, SEQ, 512])
    ngdn_p = dout("ngdn_p", [L, NP, 4, 64, 64]); nconv_p = dout("nconv_p", [L, NP, 3, 768])
    nk_s = dout("nk_s", [L, TS, 512]); nv_s = dout("nv_s", [L, TS, 512])
    ngdn_s = dout("ngdn_s", [L, 4, 64, 64]); nconv_s = dout("nconv_s", [L, 3, 768])
    nsgu_s = dout("nsgu_s", [L, TS, 256])

    with ExitStack() as st:
        P = Prog(nc, st)

        uid = [0]

        def sb(name, shape, dt=F32, stack=None):
            uid[0] += 1
            return T((stack or st).enter_context(nc.sbuf_tensor(f"{name}_{uid[0]}", list(shape), dt)), name)

        def ps(name, shape=(128, 512), dt=F32):
            return T(st.enter_context(nc.psum_tensor(name, list(shape), dt)), name)

        PS = [ps(f"ps{i}") for i in range(8)]

        def dma(out, in_, reads=(), writes=(), q="sp", is_out=False, **kw):
            P.op(q, lambda e: e.dma_start(out=out, in_=in_, **kw), reads, writes, dma=True, is_out=is_out)

        def mm(out, lhsT, rhs, reads, writes, start=True, stop=True, **kw):
            P.op("pe", lambda e: e.matmul(out, lhsT, rhs, start=start, stop=stop, **kw), reads, writes)

        def tr(out, in_, ident_ap, reads, writes):
            P.op("pe", lambda e: e.transpose(out, in_, ident_ap), reads, writes)

        def act(out, in_, func, reads, writes, **kw):
            P.op("act", lambda e: e.activation(out=out, in_=in_, func=func, **kw), reads, writes)

        def V(method, *args, reads=(), writes=(), eng="dve", **kw):
            P.op(eng, lambda e: getattr(e, method)(*args, **kw), reads, writes)

        def rsqrt(dst, src, eps, reads, dst_t, scale=1.0):
            V("tensor_scalar", dst, src, scale, eps, reads=reads, writes=[dst_t], op0=ALU.mult, op1=ALU.add)
            act(dst, dst, AF.Sqrt, [dst_t], [dst_t])
            V("reciprocal", dst, dst, reads=[dst_t], writes=[dst_t])

        def bcast_row(dram_row_ap, n):
            return dram_row_ap.partition_broadcast(128) if hasattr(dram_row_ap, "partition_broadcast") else dram_row_ap

        ident = sb("ident", [128, 128])
        identb = sb("identb", [128, 128], BF16)
        iota_p = sb("iota_p", [128, 1])
        iota_f = sb("iota_f", [128, 128])
        dif = sb("dif", [128, 128])
        triU = sb("triU", [128, 128])
        maskL = sb("maskL", [64, 64])
        maskLs = sb("maskLs", [64, 64])
        maskU = sb("maskU", [64, 64])
        ones64 = sb("ones64", [64, 64])
        ones_row = sb("ones_row", [1, 128])
        ones_rowb = sb("ones_rowb", [1, 128], BF16)
        kirow = sb("kirow", [1, 128], BF16)
        sloperow = sb("sloperow", [1, 4, 2, 128], BF16)
        nsqi = sb("nsqi", [1, 4, 2, 128], BF16)
        biasd = sb("biasd", [128, 4, 2, 128], BF16)
        tmpc = sb("tmpc", [128, 128])
        thr16 = sb("thr16", [128, 16])
        V("iota", iota_p[:], [[0, 1]], reads=[], writes=[iota_p], eng="pool", base=0, channel_multiplier=1,
          allow_small_or_imprecise_dtypes=True)
        V("iota", iota_f[:], [[1, 128]], reads=[], writes=[iota_f], eng="pool", base=0, channel_multiplier=0,
          allow_small_or_imprecise_dtypes=True)
        V("tensor_scalar", dif[:], iota_f[:], iota_p[:, 0:1], None, reads=[iota_f, iota_p], writes=[dif],
          op0=ALU.subtract)
        V("tensor_single_scalar", ident[:], dif[:], 0.0, reads=[dif], writes=[ident], op=ALU.is_equal)
        V("tensor_copy", identb[:], ident[:], reads=[ident], writes=[identb])
        V("tensor_single_scalar", triU[:], dif[:], 0.0, reads=[dif], writes=[triU], op=ALU.is_ge)
        V("tensor_single_scalar", maskU[:], dif[:64, :64], 0.0, reads=[dif], writes=[maskU], op=ALU.is_ge)
        V("tensor_single_scalar", maskL[:], dif[:64, :64], 0.0, reads=[dif], writes=[maskL], op=ALU.is_le)
        V("tensor_single_scalar", maskLs[:], dif[:64, :64], 0.0, reads=[dif], writes=[maskLs], op=ALU.is_lt)
        V("tensor_scalar", thr16[:], iota_f[:, 0:16], 1.0, 16.0, reads=[iota_f], writes=[thr16], op0=ALU.add, op1=ALU.mult)
        V("memset", ones64[:], 1.0, reads=[], writes=[ones64])
        V("memset", ones_row[:], 1.0, reads=[], writes=[ones_row])
        V("memset", ones_rowb[:], 1.0, reads=[], writes=[ones_rowb])
        V("tensor_copy", kirow[:], iota_f[0:1, :], reads=[iota_f], writes=[kirow])
        mk = sb("mk", [128, 128])
        V("tensor_single_scalar", mk[:], iota_f[:], 64.0, reads=[iota_f], writes=[mk], op=ALU.is_lt)
        V("tensor_single_scalar", tmpc[:], iota_p[:].to_broadcast([128, 128]), 64.0, reads=[iota_p], writes=[tmpc],
          op=ALU.is_ge)
        V("tensor_tensor", mk[:], mk[:], tmpc[:], reads=[mk, tmpc], writes=[mk], op=ALU.mult)
        V("tensor_single_scalar", mk[:], mk[:], NEG, reads=[mk], writes=[mk], op=ALU.mult)
        V("scalar_tensor_tensor", tmpc[:], dif[:], -1.0, dif[:], reads=[dif], writes=[tmpc], op0=ALU.mult, op1=ALU.max)
        for h in range(4):
            for m in range(2):
                V("memset", sloperow[0:1, h, m, :], SLOPES[h], reads=[], writes=[sloperow])
                V("tensor_single_scalar", nsqi[0:1, h, m, :], iota_f[0:1, :], -SLOPES[h], reads=[iota_f],
                  writes=[nsqi], op=ALU.mult)
                V("scalar_tensor_tensor", biasd[:, h, m, :], tmpc[:], -SLOPES[h], mk[:], reads=[tmpc, mk],
                  writes=[biasd], op0=ALU.mult, op1=ALU.add)

        modT = sb("modT", [128, L, 48, NS])
        with ExitStack() as s0:
            c_sb = sb("c_sb", [NS, D], stack=s0)
            cT = sb("cT", [128, 8, NS], stack=s0)
            wst = [sb(f"wst{i}", [128, 8, 512], stack=s0) for i in range(2)]
            brow = sb("brow", [1, 6 * D], stack=s0)
            dma(c_sb[:], c_in[:, :], [], [c_sb])
            act(c_sb[:], c_sb[:], AF.Silu, [c_sb], [c_sb])
            for kc in range(8):
                tr(PS[0][:, kc * NS:(kc + 1) * NS], c_sb[:NS, kc * 128:(kc + 1) * 128], ident[:NS, :NS],
                   [c_sb, ident], [PS[0]])
            V("tensor_copy", cT[:].rearrange("p a b -> p (a b)"), PS[0][:, 0:8 * NS], reads=[PS[0]], writes=[cT])
            for l in range(L):
                dma(brow[:], b_ada[l:l + 1, :], [], [brow])
                for cb in range(12):
                    w = wst[cb % 2]
                    dma(w[:], w_ada[l, :, cb * 512:(cb + 1) * 512].rearrange("(kc p) n -> p kc n", p=128), [], [w])
                    pt = PS[cb % 2]
                    for f4 in range(4):
                        fc = cb * 4 + f4
                        for kc in range(8):
                            mm(pt[:, f4 * NS:(f4 + 1) * NS], w[:, kc, f4 * 128:(f4 + 1) * 128], cT[:, kc, :],
                               [w, cT], [pt], start=(kc == 0), stop=False)
                        mm(pt[:, f4 * NS:(f4 + 1) * NS], brow[0:1, fc * 128:(fc + 1) * 128], ones_row[0:1, :NS],
                           [brow, ones_row], [pt], start=False, stop=True)
                    V("tensor_copy", modT[:, l, cb * 4:(cb + 1) * 4, :].rearrange("p a b -> p (a b)"),
                      pt[:, 0:4 * NS], reads=[pt], writes=[modT])
                for c0 in (8, 32):
                    V("tensor_scalar_add", modT[:, l, c0:c0 + 8, :], modT[:, l, c0:c0 + 8, :], 1.0,
                      reads=[modT], writes=[modT])
            P.barrier()

        with ExitStack() as sc:
            cin = [sb(f"cin{i}", [128, 4096], stack=sc) for i in range(2)]
            cout = [sb(f"cout{i}", [128, 4096], BF16, stack=sc) for i in range(2)]
            nchunk = L * 16384 // 512
            ci = 0
            for (src, dst) in ((expert_u, eu_bf), (expert_v, ev_bf)):
                sv = src[:, :].rearrange("(c p r) d -> c p (r d)", p=128, r=4)
                dv = dst[:, :].rearrange("(c p r) d -> c p (r d)", p=128, r=4)
                for c in range(nchunk):
                    a, b = cin[ci % 2], cout[ci % 2]
                    dma(a[:], sv[c], [], [a])
                    eng = ("act", "dve", "pool")[ci % 3]
                    if eng == "act":
                        act(b[:], a[:], AF.Copy, [a], [b])
                    else:
                        V("tensor_copy", b[:], a[:], reads=[a], writes=[b], eng=eng)
                    dma(dv[c], b[:], [b], [])
                    ci += 1
            P.barrier()

        wst = [sb(f"wstg{i}", [128, 8, 128]) for i in range(2)]
        x_t = sb("x_t", [128, D])
        xh = sb("xh", [128, D])
        stats = sb("stats", [128, 2, 6])
        mv = sb("mv", [128, 2])
        rstd = sb("rstd", [128, 1])
        lng = sb("lng", [128, D]); lnb = sb("lnb", [128, D])
        xres = {}

        def xr(j, t):
            return xres.setdefault((j, t), Res(f"x{j}.{t}"))

        kvres = {}

        def kvr(l, j, t):
            return kvres.setdefault((l, j, t), Res(f"kv{l}.{j}.{t}"))

        def ln_stats(src_ap, R, src_res):
            for i in range(2):
                V("bn_stats", stats[:R, i, :], src_ap[:, i * 512:(i + 1) * 512], reads=[src_res], writes=[stats])
            V("bn_aggr", mv[:R, :], stats[:R, :, :], reads=[stats], writes=[mv])
            rsqrt(rstd[:R, :], mv[:R, 1:2], 1e-5, [mv], rstd)

        def normalize(dst_t, src_t, R):
            ln_stats(src_t[:R, :], R, src_t)
            V("tensor_scalar", dst_t[:R, :], src_t[:R, :], mv[:R, 0:1], rstd[:R, 0:1], reads=[src_t, mv, rstd],
              writes=[dst_t], op0=ALU.subtract, op1=ALU.mult)

        def gate_tile(l, j, c0, Gt):
            for kc in range(8):
                pp = PS[kc // 4]
                V("tensor_copy", tmpc[:], modT[:, l, c0 + kc, j:j + 1].to_broadcast([128, 128]), reads=[modT], writes=[tmpc])
                mm(pp[:, (kc % 4) * 128:(kc % 4 + 1) * 128], tmpc[:], ident[:], [tmpc, ident], [pp])
            for hh in range(2):
                V("tensor_copy", Gt[:, hh * 512:(hh + 1) * 512], PS[hh][:, :], reads=[PS[hh]], writes=[Gt])

        def load_x(j, t, R, l, is_sample):
            if is_sample:
                src = xs[t * 128:t * 128 + R, :] if l == 0 else y_s[t * 128:t * 128 + R, :]
            else:
                src = xp[j, t * 128:t * 128 + R, :] if l == 0 else y_p[j, t * 128:t * 128 + R, :]
            dma(x_t[:R, :], src, [xr(j, t)], [x_t])

        def store_x(j, t, R, is_sample, src_t):
            dst = y_s[t * 128:t * 128 + R, :] if is_sample else y_p[j, t * 128:t * 128 + R, :]
            dma(dst, src_t[:R, :], [src_t], [xr(j, t)], is_out=True)

        def residual_ln(y_ps_list, R, dst_t):
            for hh in range(2):
                V("scalar_tensor_tensor", xh[:R, hh * 512:(hh + 1) * 512], x_t[:R, hh * 512:(hh + 1) * 512], ALPHA,
                  y_ps_list[hh], reads=[x_t] + [y_ps_list[2 + hh]], writes=[xh], op0=ALU.mult, op1=ALU.add)
            normalize(dst_t, xh, R)
            V("tensor_tensor", dst_t[:R, :], dst_t[:R, :], lng[:R, :], reads=[dst_t, lng], writes=[dst_t], op=ALU.mult)
            V("tensor_tensor", dst_t[:R, :], dst_t[:R, :], lnb[:R, :], reads=[dst_t, lnb], writes=[dst_t], op=ALU.add)

        def phase_a(l, j, T_len, is_sample):
            lam_init = 0.8 - 0.6 * math.exp(-0.3 * l)
            nt = (T_len + 127) // 128
            with ExitStack() as sa:
                def sba(name, shape, dt=F32):
                    return sb(name, shape, dt, stack=sa)
                w_in_sb = sba("w_in_sb", [128, 8, P_IN], BF16)
                w_out_sb = sba("w_out_sb", [128, 8, D], BF16)
                hT = sba("hT", [128, 8, 128], BF16)
                p_sb = sba("p_sb", [128, P_IN])
                mix = sba("mix", [128, D])
                mixT = sba("mixT", [128, 8, 128], BF16)
                wTs = sba("wTs", [128, 4, 128])
                bsT = sba("bsT", [128, 4])
                sg_g = sba("sg_g", [128, 256]); sg_b = sba("sg_b", [128, 256])
                uv = sba("uv", [128, 512]); gtmp = sba("gtmp", [128, 512])
                vst = sba("vst", [128, 4, 2]); vn = sba("vn", [128, 256]); vsq = sba("vsq", [128, 256])
                lamc = sba("lamc", [128, 4]); lamv = sba("lamv", [128, 4, 64]); nlam = sba("nlam", [128, 1])
                dng = sba("dng", [128, 128])
                Qblk = sba("Qblk", [128, 4, 2, 128], BF16)
                kst = [sba("kst0", [128, 512])] * 2
                vstg = [sba("vstg0", [128, 512])] * 2
                kTb = sba("kTb", [128, 4, 128], BF16)
                vb = sba("vb", [128, 512], BF16)
                onesc = sba("onesc", [128, 1], BF16)
                pT = sba("pT", [128, 2, 128], BF16)
                anum = sba("anum", [128, 8, 128]); aden = sba("aden", [128, 8])
                aout = sba("aout", [128, 4, 128]); asq = sba("asq", [128, 4, 128]); ass = sba("ass", [128, 4])
                cw = sba("cw", [64, 12, 4])
                negA = sba("negA", [64, 4]); dtb = sba("dtb", [64, 4]); gng = sba("gng", [64, 64])
                cbuf = sba("cbuf", [64, 12, 131]); cv = sba("cv", [64, 12, 128]); ctmp = sba("ctmp", [64, 12, 128])
                S_sb = sba("S_sb", [64, 4, 64])
                ktm = sba("ktm", [64, 4, 64]); vtm = sba("vtm", [64, 4, 64])
                gab = sba("gab", [64, 8]); gg = sba("gg", [64, 4]); beta = sba("beta", [64, 4])
                gB = sba("gB", [64, 4, 64]); gc_tm = sba("gc_tm", [64, 4]); Grow = sba("Grow", [64, 4, 64])
                D1 = sba("D1", [64, 4, 64]); E1 = sba("E1", [64, 4, 64]); E2 = sba("E2", [64, 4, 64])
                egc = sba("egc", [64, 4]); egrow = sba("egrow", [64, 4, 64]); kdf = sba("kdf", [64, 4])
                glc = sba("glc", [64, 4])
                Am = [sba(f"Am{i}", [64, 4, 64]) for i in range(2)]
                Bm = [sba(f"Bm{i}", [64, 4, 64]) for i in range(2)]
                XT = sba("XT", [64, 4, 64])
                Vb = gB; Kg = sba("Kg", [64, 4, 64]); kdec = sba("kdec", [64, 4, 64])
                u_sb = sba("u_sb", [64, 4, 64]); wT_sb = sba("wT_sb", [64, 4, 64])
                aqkT = sba("aqkT", [64, 4, 64]); qgT = sba("qgT", [64, 4, 64]); vnew = sba("vnew", [64, 4, 64])
                o_sb = E1; osq = D1; oss = sba("oss", [64, 4])
                z_sb = sba("z_sb", [64, 256])

                Gt = mix
                gate_tile(l, j, 16, Gt)
                for cb in range(25):
                    n0 = cb * 128
                    n1 = min(P_IN, n0 + 128)
                    w = wst[cb % 2]
                    dma(w[:, :, :n1 - n0], w_in[l, :, n0:n1].rearrange("(kc p) n -> p kc n", p=128), [], [w])
                    V("tensor_copy", w_in_sb[:, :, n0:n1], w[:, :, :n1 - n0], reads=[w], writes=[w_in_sb], eng="pool")
                for cb in range(8):
                    n0 = cb * 128
                    w = wst[(cb + 1) % 2]
                    dma(w[:], w_out[l, :, n0:n0 + 128].rearrange("(kc p) n -> p kc n", p=128), [], [w])
                    for kc in range(8):
                        V("tensor_tensor", w_out_sb[:, kc, n0:n0 + 128], w[:, kc, :], Gt[:, n0:n0 + 128],
                          reads=[w, Gt], writes=[w_out_sb], op=ALU.mult, eng="pool")
                dma(lng[:], ln1_g[l:l + 1, :].partition_broadcast(128), [], [lng])
                dma(lnb[:], ln1_b[l:l + 1, :].partition_broadcast(128), [], [lnb])
                wraw = p_sb
                dma(p_sb[:, 1536:2048].rearrange("p (g j) -> p g j", g=4), sgu_w[l].rearrange("g i j -> i g j"), [], [p_sb])
                for g in range(4):
                    tr(PS[2][:, g * 128:(g + 1) * 128], p_sb[:, 1536 + g * 128:1536 + (g + 1) * 128], ident[:], [p_sb, ident], [PS[2]])
                V("tensor_copy", wTs[:].rearrange("p a b -> p (a b)"), PS[2][:, :], reads=[PS[2]], writes=[wTs])
                V("memset", wTs[64:128, :, 0:64], 0.0, reads=[], writes=[wTs])
                dma(p_sb[:4, 2048:2176], sgu_b[l], [], [p_sb])
                tr(PS[3][:, 0:4], p_sb[:4, 2048:2176], ident[:4, :4], [p_sb, ident], [PS[3]])
                V("tensor_copy", bsT[:], PS[3][:, 0:4], reads=[PS[3]], writes=[bsT])
                dma(sg_g[:], sgu_ln_g[l:l + 1, :].partition_broadcast(128), [], [sg_g])
                dma(sg_b[:], sgu_ln_b[l:l + 1, :].partition_broadcast(128), [], [sg_b])
                for i, lq in enumerate((lam_q1, lam_k1, lam_q2, lam_k2)):
                    dma(lamv[:, i, :], lq[l:l + 1, :].partition_broadcast(128), [], [lamv])
                V("tensor_tensor", lamv[:, 0, :], lamv[:, 0, :], lamv[:, 1, :], reads=[lamv], writes=[lamv], op=ALU.mult)
                V("tensor_tensor", lamv[:, 2, :], lamv[:, 2, :], lamv[:, 3, :], reads=[lamv], writes=[lamv], op=ALU.mult)
                V("tensor_reduce", lamc[:, 0:1], lamv[:, 0, :], reads=[lamv], writes=[lamc], axis=AX.X, op=ALU.add)
                V("tensor_reduce", lamc[:, 1:2], lamv[:, 2, :], reads=[lamv], writes=[lamc], axis=AX.X, op=ALU.add)
                act(lamc[:, 0:2], lamc[:, 0:2], AF.Exp, [lamc], [lamc])
                V("tensor_tensor", nlam[:], lamc[:, 1:2], lamc[:, 0:1], reads=[lamc], writes=[nlam], op=ALU.subtract)
                V("tensor_scalar_add", nlam[:], nlam[:], -lam_init, reads=[nlam], writes=[nlam])
                dma(dng[:], diff_norm_g[l:l + 1, :].partition_broadcast(128), [], [dng])
                V("tensor_single_scalar", dng[:], dng[:], 1.0 - lam_init, reads=[dng], writes=[dng], op=ALU.mult)
                V("memset", Qblk[:], 0.0, reads=[], writes=[Qblk])
                V("memset", onesc[:], 1.0, reads=[], writes=[onesc])
                dma(p_sb[:4, 0:768], conv_w[l], [], [p_sb])
                for g in range(12):
                    tr(PS[3][:64, 16 + g * 4:16 + g * 4 + 4], p_sb[:4, g * 64:(g + 1) * 64], ident[:4, :4],
                       [p_sb, ident], [PS[3]])
                V("tensor_copy", cw[:].rearrange("p a b -> p (a b)"), PS[3][:64, 16:64], reads=[PS[3]], writes=[cw])
                dma(negA[:], gdn_a_log[l:l + 1, :].partition_broadcast(64), [], [negA])
                act(negA[:], negA[:], AF.Exp, [negA], [negA])
                V("tensor_single_scalar", negA[:], negA[:], -1.0, reads=[negA], writes=[negA], op=ALU.mult)
                dma(dtb[:], gdn_dt_bias[l:l + 1, :].partition_broadcast(64), [], [dtb])
                dma(gng[:], gdn_norm_g[l:l + 1, :].partition_broadcast(64), [], [gng])
                if is_sample:
                    dma(S_sb[:], state_gdn[l].rearrange("h k v -> k h v"), [], [S_sb])
                    dma(p_sb[:3, 768:1536], state_conv[l], [], [p_sb])
                    for g in range(12):
                        tr(PS[3][:64, 64 + g * 3:64 + g * 3 + 3], p_sb[:3, 768 + g * 64:768 + (g + 1) * 64], ident[:3, :3],
                           [p_sb, ident], [PS[3]])
                    V("tensor_copy", cbuf[:, :, 0:3], PS[3][:64, 64:100].rearrange("p (a b) -> p a b", b=3),
                      reads=[PS[3]], writes=[cbuf])
                else:
                    V("memset", S_sb[:], 0.0, reads=[], writes=[S_sb])
                    V("memset", cbuf[:, :, 0:3], 0.0, reads=[], writes=[cbuf])

                def attn_block(k_ap, v_ap, kv_reads, nk, R, diag, delta, first):
                    for h in range(4):
                        tr(PS[4][:, h * 128:h * 128 + nk], k_ap[:, h * 128:(h + 1) * 128], ident[:nk, :nk],
                           kv_reads + [ident], [PS[4]])
                    act(kTb[:, :, :nk], PS[4][:, :].rearrange("p (a b) -> p a b", b=128)[:, :, :nk], AF.Copy,
                        [PS[4]], [kTb])
                    V("tensor_copy", vb[:nk, :], v_ap, reads=kv_reads, writes=[vb])
                    for h in range(4):
                        sc = PS[5 + (h % 2)]
                        scv = sc[:nk, 0:256].rearrange("p (m q) -> p m q", m=2)[:, :, :R]
                        mm(scv, kTb[:, h, :nk], Qblk[:, h, :, :R], [kTb, Qblk], [sc], start=True, stop=False)
                        if diag:
                            mm(scv, identb[:nk, :nk], biasd[:nk, h, :, :R], [identb, biasd], [sc], start=False, stop=True)
                            bias_c = 0.0
                        else:
                            mm(scv, kirow[0:1, :nk], sloperow[0:1, h, :, :R], [kirow, sloperow], [sc],
                               start=False, stop=False)
                            mm(scv, ones_rowb[0:1, :nk], nsqi[0:1, h, :, :R], [ones_rowb, nsqi], [sc],
                               start=False, stop=True)
                            bias_c = -SLOPES[h] * delta
                        if bias_c != 0.0:
                            V("tensor_scalar_add", scv, scv, bias_c, reads=[sc], writes=[sc])
                        act(pT[:nk, :, :R], scv, AF.Exp, [sc], [pT])
                        pv = PS[7]
                        for m in range(2):
                            mm(pv[:R, m * 128:(m + 1) * 128], pT[:nk, m, :R], vb[:nk, h * 128:(h + 1) * 128],
                               [pT, vb], [pv])
                            mm(pv[:R, 256 + m:257 + m], pT[:nk, m, :R], onesc[:nk, 0:1], [pT, onesc], [pv])
                        if first:
                            V("tensor_copy", anum[:R, 2 * h:2 * h + 2, :], pv[:R, 0:256].rearrange("p (m d) -> p m d", m=2),
                              reads=[pv], writes=[anum])
                            V("tensor_copy", aden[:R, 2 * h:2 * h + 2], pv[:R, 256:258], reads=[pv], writes=[aden])
                        else:
                            V("tensor_tensor", anum[:R, 2 * h:2 * h + 2, :], anum[:R, 2 * h:2 * h + 2, :],
                              pv[:R, 0:256].rearrange("p (m d) -> p m d", m=2), reads=[pv, anum], writes=[anum], op=ALU.add)
                            V("tensor_tensor", aden[:R, 2 * h:2 * h + 2], aden[:R, 2 * h:2 * h + 2], pv[:R, 256:258],
                              reads=[pv, aden], writes=[aden], op=ALU.add)

                for t in range(nt):
                    R = min(128, T_len - t * 128)
                    load_x(j, t, R, l, is_sample)
                    normalize(xh, x_t, R)
                    for kc in range(8):
                        pp = PS[kc // 4]
                        tr(pp[:, (kc % 4) * 128:(kc % 4) * 128 + R], xh[:R, kc * 128:(kc + 1) * 128], ident[:R, :R],
                           [xh, ident], [pp])
                    for kc in range(8):
                        pp = PS[kc // 4]
                        act(hT[:, kc, :R], pp[:, (kc % 4) * 128:(kc % 4) * 128 + R], AF.Identity, [pp, modT], [hT],
                            scale=modT[:, l, 8 + kc, j:j + 1], bias=modT[:, l, 0 + kc, j:j + 1])
                    for cb in range(7):
                        n0 = cb * 512
                        n1 = min(P_IN, n0 + 512)
                        pb = PS[2 + cb % 2]
                        for kc in range(8):
                            mm(pb[:R, :n1 - n0], hT[:, kc, :R], w_in_sb[:, kc, n0:n1], [hT, w_in_sb], [pb],
                               start=(kc == 0), stop=(kc == 7))
                        if cb % 2 == 0:
                            act(p_sb[:R, n0:n1], pb[:R, :n1 - n0], AF.Copy, [pb], [p_sb])
                        else:
                            V("tensor_copy", p_sb[:R, n0:n1], pb[:R, :n1 - n0], reads=[pb], writes=[p_sb])
                    kdst = nk_s[l, t * 128:t * 128 + R, :] if is_sample else nk_p[l, j, t * 128:t * 128 + R, :]
                    vdst = nv_s[l, t * 128:t * 128 + R, :] if is_sample else nv_p[l, j, t * 128:t * 128 + R, :]
                    dma(kdst, p_sb[:R, 1024:1536], [p_sb], [kvr(l, j, t)], is_out=True)
                    dma(vdst, p_sb[:R, 1536:2048], [p_sb], [kvr(l, j, t)], is_out=True)
                    if t == nt - 1:
                        dst = nconv_s[l, :, :] if is_sample else nconv_p[l, j, :, :]
                        dma(dst, p_sb[R - 3:R, 2048:2816], [p_sb], [], is_out=True)

                    if stages >= 2:
                        V("tensor_tensor", gtmp[:R, :], p_sb[:R, 0:512], p_sb[:R, 0:512], reads=[p_sb], writes=[gtmp], op=ALU.mult)
                        V("tensor_scalar", gtmp[:R, :], gtmp[:R, :], 0.044715, 1.0, reads=[gtmp], writes=[gtmp],
                          op0=ALU.mult, op1=ALU.add)
                        V("tensor_tensor", gtmp[:R, :], gtmp[:R, :], p_sb[:R, 0:512], reads=[gtmp, p_sb], writes=[gtmp], op=ALU.mult)
                        act(gtmp[:R, :], gtmp[:R, :], AF.Sigmoid, [gtmp], [gtmp], scale=2.0 * math.sqrt(2.0 / math.pi))
                        V("tensor_tensor", uv[:R, :], gtmp[:R, :], p_sb[:R, 0:512], reads=[gtmp, p_sb], writes=[uv], op=ALU.mult)
                        v3 = uv[:R, 256:512].rearrange("p (g d) -> p g d", g=4)
                        V("tensor_reduce", vst[:R, :, 0], v3, reads=[uv], writes=[vst], axis=AX.X, op=ALU.add)
                        V("tensor_tensor", vsq[:R, :], uv[:R, 256:512], uv[:R, 256:512], reads=[uv], writes=[vsq], op=ALU.mult)
                        V("tensor_reduce", vst[:R, :, 1], vsq[:R, :].rearrange("p (g d) -> p g d", g=4), reads=[vsq],
                          writes=[vst], axis=AX.X, op=ALU.add)
                        V("tensor_single_scalar", vst[:R, :, :], vst[:R, :, :], 1.0 / 64, reads=[vst], writes=[vst], op=ALU.mult)
                        V("tensor_tensor", vsq[:R, 0:4], vst[:R, :, 0], vst[:R, :, 0], reads=[vst], writes=[vsq], op=ALU.mult)
                        V("tensor_tensor", vst[:R, :, 1], vst[:R, :, 1], vsq[:R, 0:4], reads=[vst, vsq], writes=[vst], op=ALU.subtract)
                        rsqrt(vst[:R, :, 1], vst[:R, :, 1], 1e-5, [vst], vst)
                        vn3 = vn[:R, :].rearrange("p (g d) -> p g d", g=4)
                        V("tensor_tensor", vn3, v3, vst[:R, :, 0:1].to_broadcast([R, 4, 64]), reads=[uv, vst], writes=[vn], op=ALU.subtract)
                        V("tensor_tensor", vn3, vn3, vst[:R, :, 1:2].to_broadcast([R, 4, 64]), reads=[vn, vst], writes=[vn], op=ALU.mult)
                        V("tensor_tensor", vn[:R, :], vn[:R, :], sg_g[:R, :], reads=[vn, sg_g], writes=[vn], op=ALU.mult)
                        V("tensor_tensor", vn[:R, :], vn[:R, :], sg_b[:R, :], reads=[vn, sg_b], writes=[vn], op=ALU.add)
                        if is_sample:
                            dma(nsgu_s[l, t * 128:t * 128 + R, :], vn[:R, :], [vn], [], is_out=True)
                        for g in range(4):
                            mm(PS[0][:R, g * 64:(g + 1) * 64], wTs[:R, g, :R], vn[:R, g * 64:(g + 1) * 64], [wTs, vn], [PS[0]])
                        for g in range(4):
                            V("scalar_tensor_tensor", mix[:R, g * 64:(g + 1) * 64], PS[0][:R, g * 64:(g + 1) * 64],
                              bsT[:R, g:g + 1], uv[:R, g * 64:(g + 1) * 64], reads=[PS[0], bsT, uv], writes=[mix],
                              op0=ALU.add, op1=ALU.mult)
                    else:
                        V("memset", mix[:R, 0:256], 0.0, reads=[], writes=[mix])

                    if stages >= 3:
                        for h in range(4):
                            pq = PS[4 + (h % 2)]
                            for kc in range(8):
                                mm(pq[:, :R], w_in_sb[:, kc, 512 + h * 128:512 + (h + 1) * 128], hT[:, kc, :R],
                                   [w_in_sb, hT], [pq], start=(kc == 0), stop=(kc == 7))
                            act(Qblk[0:64, h, 0, :R], pq[0:64, :R], AF.Copy, [pq], [Qblk], scale=0.125)
                            act(Qblk[64:128, h, 1, :R], pq[64:128, :R], AF.Copy, [pq], [Qblk], scale=0.125)
                        qpos0 = (PAST if is_sample else 0) + t * 128
                        nblk = 0
                        if is_sample:
                            for kb in range(PAST // 128):
                                ks, vs = kst[nblk % 2], vstg[nblk % 2]
                                dma(ks[:], cache_k[l, kb * 128:(kb + 1) * 128, :], [], [ks])
                                dma(vs[:], cache_v[l, kb * 128:(kb + 1) * 128, :], [], [vs])
                                attn_block(ks[:, :], vs[:, :], [ks, vs], 128, R, False, qpos0 - kb * 128, nblk == 0)
                                nblk += 1
                        for kb in range(t):
                            ks, vs = kst[nblk % 2], vstg[nblk % 2]
                            ksrc = nk_s[l, kb * 128:(kb + 1) * 128, :] if is_sample else nk_p[l, j, kb * 128:(kb + 1) * 128, :]
                            vsrc = nv_s[l, kb * 128:(kb + 1) * 128, :] if is_sample else nv_p[l, j, kb * 128:(kb + 1) * 128, :]
                            dma(ks[:], ksrc, [kvr(l, j, kb)], [ks])
                            dma(vs[:], vsrc, [kvr(l, j, kb)], [vs])
                            attn_block(ks[:, :], vs[:, :], [ks, vs], 128, R, False, (t - kb) * 128, nblk == 0)
                            nblk += 1
                        attn_block(p_sb[:R, 1024:1536], p_sb[:R, 1536:2048], [p_sb], R, R, True, 0, nblk == 0)
                        V("reciprocal", aden[:R, :], aden[:R, :], reads=[aden], writes=[aden])
                        V("tensor_tensor", anum[:R, :, :], anum[:R, :, :], aden[:R, :].unsqueeze(2).to_broadcast([R, 8, 128]),
                          reads=[anum, aden], writes=[anum], op=ALU.mult)
                        a4 = anum[:R, :, :].rearrange("p (h m) d -> p h m d", m=2)
                        V("scalar_tensor_tensor", aout[:R, :, :], a4[:, :, 1, :], nlam[:R, 0:1], a4[:, :, 0, :],
                          reads=[anum, nlam], writes=[aout], op0=ALU.mult, op1=ALU.add)
                        V("tensor_tensor", asq[:R, :, :], aout[:R, :, :], aout[:R, :, :], reads=[aout], writes=[asq], op=ALU.mult)
                        V("tensor_reduce", ass[:R, :], asq[:R, :, :], reads=[asq], writes=[ass], axis=AX.X, op=ALU.add)
                        rsqrt(ass[:R, :], ass[:R, :], 1e-5, [ass], ass, scale=1.0 / 128)
                        V("tensor_tensor", aout[:R, :, :], aout[:R, :, :], ass[:R, :].unsqueeze(2).to_broadcast([R, 4, 128]),
                          reads=[aout, ass], writes=[aout], op=ALU.mult)
                        V("tensor_tensor", mix[:R, 256:768].rearrange("p (h d) -> p h d", h=4), aout[:R, :, :],
                          dng[:R, :].unsqueeze(1).to_broadcast([R, 4, 128]), reads=[aout, dng], writes=[mix], op=ALU.mult)
                    else:
                        V("memset", mix[:R, 256:768], 0.0, reads=[], writes=[mix])

                    if stages >= 4:
                        for g in range(12):
                            pg = PS[4 + (g // 4)]
                            col = 2048 + g * 64
                            for kc in range(8):
                                mm(pg[:64, (g % 4) * 128:(g % 4) * 128 + R], w_in_sb[:, kc, col:col + 64], hT[:, kc, :R],
                                   [w_in_sb, hT], [pg], start=(kc == 0), stop=(kc == 7))
                        for q3 in range(3):
                            act(cbuf[:, q3 * 4:(q3 + 1) * 4, 3:3 + R],
                                PS[4 + q3][:64, :].rearrange("p (a b) -> p a b", b=128)[:, :, :R], AF.Copy,
                                [PS[4 + q3]], [cbuf])
                        for jt in range(4):
                            wj = cw[:, :, jt:jt + 1].to_broadcast([64, 12, R])
                            if jt == 0:
                                V("tensor_tensor", cv[:, :, :R], cbuf[:, :, jt:jt + R], wj, reads=[cbuf, cw], writes=[cv], op=ALU.mult)
                            else:
                                V("tensor_tensor", ctmp[:, :, :R], cbuf[:, :, jt:jt + R], wj, reads=[cbuf, cw], writes=[ctmp], op=ALU.mult)
                                V("tensor_tensor", cv[:, :, :R], cv[:, :, :R], ctmp[:, :, :R], reads=[cv, ctmp], writes=[cv], op=ALU.add)
                        V("tensor_copy", ctmp[:, :, 0:3], cbuf[:, :, R:R + 3], reads=[cbuf], writes=[ctmp])
                        V("tensor_copy", cbuf[:, :, 0:3], ctmp[:, :, 0:3], reads=[ctmp], writes=[cbuf])
                        act(cv[:, :, :R], cv[:, :, :R], AF.Silu, [cv], [cv])
                        V("tensor_tensor", ctmp[:, 0:8, :R], cv[:, 0:8, :R], cv[:, 0:8, :R], reads=[cv], writes=[ctmp], op=ALU.mult)
                        for q2 in range(2):
                            mm(PS[4 + q2][:64, :].rearrange("p (a b) -> p a b", b=128)[:, :, :R], ones64[:, :],
                               ctmp[:, q2 * 4:(q2 + 1) * 4, :R], [ones64, ctmp], [PS[4 + q2]])
                            V("tensor_scalar_add", ctmp[:, q2 * 4:(q2 + 1) * 4, :R],
                              PS[4 + q2][:64, :].rearrange("p (a b) -> p a b", b=128)[:, :, :R], 1e-6,
                              reads=[PS[4 + q2]], writes=[ctmp])
                        act(ctmp[:, 0:8, :R], ctmp[:, 0:8, :R], AF.Sqrt, [ctmp], [ctmp])
                        V("reciprocal", ctmp[:, 0:8, :R], ctmp[:, 0:8, :R], reads=[ctmp], writes=[ctmp])
                        V("tensor_tensor", cv[:, 0:8, :R], cv[:, 0:8, :R], ctmp[:, 0:8, :R], reads=[cv, ctmp], writes=[cv], op=ALU.mult)
                        V("tensor_single_scalar", cv[:, 0:4, :R], cv[:, 0:4, :R], 0.125, reads=[cv], writes=[cv], op=ALU.mult)
                        for c0 in range(0, R, 64):
                            c = min(64, R - c0)
                            nsteps = max(0, int(math.ceil(math.log2(c))) - 1)
                            for g in range(8):
                                tr(PS[6][:c, g * 64:(g + 1) * 64], cv[:, 4 + g, c0:c0 + c], ident[:64, :64], [cv, ident], [PS[6]])
                            V("tensor_copy", ktm[:c, :, :], PS[6][:c, 0:256].rearrange("p (a b) -> p a b", b=64), reads=[PS[6]], writes=[ktm])
                            V("tensor_copy", vtm[:c, :, :], PS[6][:c, 256:512].rearrange("p (a b) -> p a b", b=64), reads=[PS[6]], writes=[vtm])
                            for kc in range(8):
                                mm(PS[7][:c, 0:8], hT[:, kc, c0:c0 + c], w_in_sb[:, kc, 3072:3080], [hT, w_in_sb], [PS[7]],
                                   start=(kc == 0), stop=(kc == 7))
                            for kc in range(8):
                                mm(PS[7][:c, 64:320], hT[:, kc, c0:c0 + c], w_in_sb[:, kc, 2816:3072], [hT, w_in_sb], [PS[7]],
                                   start=(kc == 0), stop=(kc == 7))
                            V("tensor_tensor", gab[:c, 0:4], PS[7][:c, 0:4], dtb[:c, :], reads=[PS[7], dtb], writes=[gab], op=ALU.add)
                            act(gab[:c, 0:4], gab[:c, 0:4], AF.Exp, [gab], [gab])
                            V("tensor_scalar_add", gab[:c, 0:4], gab[:c, 0:4], 1.0, reads=[gab], writes=[gab])
                            act(gab[:c, 0:4], gab[:c, 0:4], AF.Ln, [gab], [gab])
                            V("tensor_tensor", gg[:c, :], gab[:c, 0:4], negA[:c, :], reads=[gab, negA], writes=[gg], op=ALU.mult)
                            act(beta[:c, :], PS[7][:c, 4:8], AF.Sigmoid, [PS[7]], [beta])
                            act(z_sb[:c, :], PS[7][:c, 64:320], AF.Silu, [PS[7]], [z_sb])
                            V("tensor_copy", gB[:c, :, :], gg[:c, :].unsqueeze(2).to_broadcast([c, 4, 64]), reads=[gg], writes=[gB])
                            mm(PS[6][:c, 0:4], triU[:c, :c], gg[:c, :], [triU, gg], [PS[6]])
                            for h in range(4):
                                mm(PS[6][:64, 64 + h * 64:64 + h * 64 + c], gB[:c, h, :], triU[:c, :c], [gB, triU], [PS[6]])
                            V("tensor_copy", gc_tm[:c, :], PS[6][:c, 0:4], reads=[PS[6]], writes=[gc_tm])
                            V("tensor_copy", Grow[:, :, :c], PS[6][:64, 64:320].rearrange("p (a b) -> p a b", b=64)[:, :, :c],
                              reads=[PS[6]], writes=[Grow])
                            V("tensor_tensor", D1[:c, :, :c], gc_tm[:c, :].unsqueeze(2).to_broadcast([c, 4, c]), Grow[:c, :, :c],
                              reads=[gc_tm, Grow], writes=[D1], op=ALU.subtract)
                            V("tensor_single_scalar", E1[:c, :, :c], D1[:c, :, :c], 0.0, reads=[D1], writes=[E1], op=ALU.min)
                            act(E1[:c, :, :c], E1[:c, :, :c], AF.Exp, [E1], [E1])
                            V("tensor_scalar", E2[:c, :, :c], D1[:c, :, :c], -1.0, 0.0, reads=[D1], writes=[E2], op0=ALU.mult, op1=ALU.min)
                            act(E2[:c, :, :c], E2[:c, :, :c], AF.Exp, [E2], [E2])
                            act(egc[:c, :], gc_tm[:c, :], AF.Exp, [gc_tm], [egc])
                            act(egrow[:, :, :c], Grow[:, :, :c], AF.Exp, [Grow], [egrow])
                            act(glc[:, :], Grow[:, :, c - 1], AF.Exp, [Grow], [glc])
                            V("tensor_tensor", kdf[:c, :], Grow[:c, :, c - 1], gc_tm[:c, :], reads=[Grow, gc_tm], writes=[kdf], op=ALU.subtract)
                            act(kdf[:c, :], kdf[:c, :], AF.Exp, [kdf], [kdf])
                            for h in range(4):
                                mm(PS[4][:c, h * 64:h * 64 + c], cv[:, 4 + h, c0:c0 + c], cv[:, 4 + h, c0:c0 + c], [cv], [PS[4]])
                                mm(PS[4][:c, 256 + h * 64:256 + h * 64 + c], cv[:, 4 + h, c0:c0 + c], cv[:, h, c0:c0 + c], [cv], [PS[4]])
                            V("tensor_tensor", D1[:c, :, :c], E1[:c, :, :c], maskLs[:c, :c].unsqueeze(1).to_broadcast([c, 4, c]),
                              reads=[E1, maskLs], writes=[D1], op=ALU.mult)
                            A0, B0 = Am[0], Bm[0]
                            V("tensor_tensor", A0[:c, :, :c], PS[4][:c, 0:256].rearrange("p (a b) -> p a b", b=64)[:, :, :c],
                              D1[:c, :, :c], reads=[PS[4], D1], writes=[A0], op=ALU.mult)
                            V("tensor_tensor", A0[:c, :, :c], A0[:c, :, :c], beta[:c, :].unsqueeze(2).to_broadcast([c, 4, c]),
                              reads=[A0, beta], writes=[A0], op=ALU.mult)
                            V("tensor_tensor", E2[:c, :, :c], E2[:c, :, :c], maskU[:c, :c].unsqueeze(1).to_broadcast([c, 4, c]),
                              reads=[E2, maskU], writes=[E2], op=ALU.mult)
                            V("tensor_tensor", aqkT[:c, :, :c], PS[4][:c, 256:512].rearrange("p (a b) -> p a b", b=64)[:, :, :c],
                              E2[:c, :, :c], reads=[PS[4], E2], writes=[aqkT], op=ALU.mult)
                            for h in range(4):
                                tr(PS[5][:c, h * 64:h * 64 + c], A0[:c, h, :c], ident[:c, :c], [A0, ident], [PS[5]])
                            V("tensor_copy", B0[:c, :, :c], PS[5][:c, 0:256].rearrange("p (a b) -> p a b", b=64)[:, :, :c],
                              reads=[PS[5]], writes=[B0])
                            V("tensor_tensor", XT[:c, :, :c], ident[:c, :c].unsqueeze(1).to_broadcast([c, 4, c]), B0[:c, :, :c],
                              reads=[ident, B0], writes=[XT], op=ALU.subtract)
                            cur = 0
                            for n in range(1, nsteps + 1):
                                Ac, Bc, An, Bn = Am[cur], Bm[cur], Am[1 - cur], Bm[1 - cur]
                                for h in range(4):
                                    mm(PS[4][:c, h * 64:h * 64 + c], Bc[:c, h, :c], Ac[:c, h, :c], [Ac, Bc], [PS[4]])
                                    if n < nsteps:
                                        mm(PS[4][:c, 256 + h * 64:256 + h * 64 + c], Ac[:c, h, :c], Bc[:c, h, :c], [Ac, Bc], [PS[4]])
                                V("tensor_copy", An[:c, :, :c], PS[4][:c, 0:256].rearrange("p (a b) -> p a b", b=64)[:, :, :c],
                                  reads=[PS[4]], writes=[An])
                                if n < nsteps:
                                    V("tensor_copy", Bn[:c, :, :c], PS[4][:c, 256:512].rearrange("p (a b) -> p a b", b=64)[:, :, :c],
                                      reads=[PS[4]], writes=[Bn])
                                for h in range(4):
                                    mm(PS[5][:c, h * 64:h * 64 + c], An[:c, h, :c], XT[:c, h, :c], [An, XT], [PS[5]])
                                V("tensor_tensor", XT[:c, :, :c], XT[:c, :, :c], PS[5][:c, 0:256].rearrange("p (a b) -> p a b", b=64)[:, :, :c],
                                  reads=[XT, PS[5]], writes=[XT], op=ALU.add)
                                cur = 1 - cur
                            V("tensor_tensor", Vb[:c, :, :], vtm[:c, :, :], beta[:c, :].unsqueeze(2).to_broadcast([c, 4, 64]),
                              reads=[vtm, beta], writes=[Vb], op=ALU.mult)
                            V("tensor_tensor", Kg[:c, :, :], ktm[:c, :, :], beta[:c, :].unsqueeze(2).to_broadcast([c, 4, 64]),
                              reads=[ktm, beta], writes=[Kg], op=ALU.mult)
                            V("tensor_tensor", Kg[:c, :, :], Kg[:c, :, :], egc[:c, :].unsqueeze(2).to_broadcast([c, 4, 64]),
                              reads=[Kg, egc], writes=[Kg], op=ALU.mult)
                            V("tensor_tensor", kdec[:c, :, :], ktm[:c, :, :], kdf[:c, :].unsqueeze(2).to_broadcast([c, 4, 64]),
                              reads=[ktm, kdf], writes=[kdec], op=ALU.mult)
                            V("tensor_tensor", qgT[:, :, :c], cv[:, 0:4, c0:c0 + c], egrow[:, :, :c], reads=[cv, egrow], writes=[qgT], op=ALU.mult)
                            for h in range(4):
                                mm(PS[6][:c, h * 64:(h + 1) * 64], XT[:c, h, :c], Vb[:c, h, :], [XT, Vb], [PS[6]])
                                mm(PS[6][:64, 256 + h * 64:256 + h * 64 + c], Kg[:c, h, :], XT[:c, h, :c], [Kg, XT], [PS[6]])
                            V("tensor_copy", u_sb[:c, :, :], PS[6][:c, 0:256].rearrange("p (a b) -> p a b", b=64), reads=[PS[6]], writes=[u_sb])
                            V("tensor_copy", wT_sb[:, :, :c], PS[6][:64, 256:512].rearrange("p (a b) -> p a b", b=64)[:, :, :c],
                              reads=[PS[6]], writes=[wT_sb])
                            for h in range(4):
                                mm(PS[5][:c, 256 + h * 64:256 + (h + 1) * 64], wT_sb[:, h, :c], S_sb[:, h, :], [wT_sb, S_sb], [PS[5]])
                            V("tensor_tensor", vnew[:c, :, :], u_sb[:c, :, :], PS[5][:c, 256:512].rearrange("p (a b) -> p a b", b=64),
                              reads=[u_sb, PS[5]], writes=[vnew], op=ALU.subtract)
                            for h in range(4):
                                mm(PS[6][:c, h * 64:(h + 1) * 64], qgT[:, h, :c], S_sb[:, h, :], [qgT, S_sb], [PS[6]], start=True, stop=False)
                                mm(PS[6][:c, h * 64:(h + 1) * 64], aqkT[:c, h, :c], vnew[:c, h, :], [aqkT, vnew], [PS[6]], start=False, stop=True)
                            for h in range(4):
                                mm(PS[5][:64, h * 64:(h + 1) * 64], kdec[:c, h, :], vnew[:c, h, :], [kdec, vnew], [PS[5]])
                            V("tensor_tensor", S_sb[:, :, :], S_sb[:, :, :], glc[:, :].unsqueeze(2).to_broadcast([64, 4, 64]),
                              reads=[S_sb, glc], writes=[S_sb], op=ALU.mult)
                            V("tensor_tensor", S_sb[:, :, :], S_sb[:, :, :], PS[5][:64, 0:256].rearrange("p (a b) -> p a b", b=64),
                              reads=[S_sb, PS[5]], writes=[S_sb], op=ALU.add)
                            V("tensor_copy", o_sb[:c, :, :], PS[6][:c, 0:256].rearrange("p (a b) -> p a b", b=64), reads=[PS[6]], writes=[o_sb])
                            V("tensor_tensor", osq[:c, :, :], o_sb[:c, :, :], o_sb[:c, :, :], reads=[o_sb], writes=[osq], op=ALU.mult)
                            V("tensor_reduce", oss[:c, :], osq[:c, :, :], reads=[osq], writes=[oss], axis=AX.X, op=ALU.add)
                            rsqrt(oss[:c, :], oss[:c, :], 1e-5, [oss], oss, scale=1.0 / 64)
                            V("tensor_tensor", o_sb[:c, :, :], o_sb[:c, :, :], oss[:c, :].unsqueeze(2).to_broadcast([c, 4, 64]),
                              reads=[o_sb, oss], writes=[o_sb], op=ALU.mult)
                            V("tensor_tensor", o_sb[:c, :, :], o_sb[:c, :, :], gng[:c, :].unsqueeze(1).to_broadcast([c, 4, 64]),
                              reads=[o_sb, gng], writes=[o_sb], op=ALU.mult)
                            V("tensor_tensor", o_sb[:c, :, :], o_sb[:c, :, :], z_sb[:c, :].rearrange("p (a b) -> p a b", b=64),
                              reads=[o_sb, z_sb], writes=[o_sb], op=ALU.mult)
                            dma(mix[c0:c0 + c, 768:1024], o_sb[:c, :, :].rearrange("p a b -> p (a b)"), [o_sb], [mix])
                        if t == nt - 1:
                            dst = ngdn_s[l].rearrange("h k v -> k h v") if is_sample else ngdn_p[l, j].rearrange("h k v -> k h v")
                            dma(dst, S_sb[:, :, :], [S_sb], [], is_out=True)
                    else:
                        V("memset", mix[:R, 768:1024], 0.0, reads=[], writes=[mix])

                    for kc in range(8):
                        pp = PS[kc // 4]
                        tr(pp[:, (kc % 4) * 128:(kc % 4) * 128 + R], mix[:R, kc * 128:(kc + 1) * 128], ident[:R, :R],
                           [mix, ident], [pp])
                    for hh in range(2):
                        V("tensor_copy", mixT[:, hh * 4:(hh + 1) * 4, :R],
                          PS[hh][:, :].rearrange("p (a b) -> p a b", b=128)[:, :, :R], reads=[PS[hh]], writes=[mixT])
                    for hh in range(2):
                        for kc in range(8):
                            mm(PS[2 + hh][:R, :], mixT[:, kc, :R], w_out_sb[:, kc, hh * 512:(hh + 1) * 512],
                               [mixT, w_out_sb], [PS[2 + hh]], start=(kc == 0), stop=(kc == 7))
                    residual_ln([PS[2][:R, :], PS[3][:R, :], PS[2], PS[3]], R, x_t)
                    store_x(j, t, R, is_sample, x_t)
                P.barrier()

        def phase_b(l, j, T_len, is_sample):
            nt = (T_len + 127) // 128
            with ExitStack() as sbk:
                def sbb(name, shape, dt=F32):
                    return sb(name, shape, dt, stack=sbk)
                wq_sb = sbb("wq_sb", [128, 8, 2048], BF16)
                keysT = sbb("keysT", [128, 16, 128])
                kraw = sbb("kraw", [128, 128])
                h2T = sbb("h2T", [128, 8, 128]); h2Tb = sbb("h2Tb", [128, 8, 128], BF16)
                h2 = sbb("h2", [128, D])
                qT = sbb("qT", [128, 16, 128])
                sc = sbb("sc", [128, 16, 128]); sc2 = sbb("sc2", [128, 16, 128])
                tops = sbb("tops", [128, 16, 16]); topi = sbb("topi", [128, 16, 16], U32); topf = sbb("topf", [128, 16, 16])
                cand = sbb("cand", [128, 8, 256]); cand2 = sbb("cand2", [128, 8, 256])
                bests = sbb("bests", [128, 8, 16]); besti = sbb("besti", [128, 8, 16], U32)
                af = sbb("af", [128, 8, 16]); bf = sbb("bf", [128, 8, 16])
                oh = sbb("oh", [128, 8, 16, 16])
                idf = sbb("idf", [128, 8, 16]); idf2 = sbb("idf2", [128, 8, 16]); ids = sbb("ids", [128, 128], I32)
                gmax = sbb("gmax", [128, 8]); gsum = sbb("gsum", [128, 8]); gates = sbb("gates", [128, 8, 16])
                dots = sbb("dots", [128, 128]); wts = sbb("wts", [128, 128]); gt2 = sbb("gt2", [128, 128])
                gbuf = [sbb(f"gbuf{i}", [128, D], BF16) for i in range(8)]
                junk = sbb("junk", [128, D], BF16)
                facc = sbb("facc", [128, D])
                dg = [sbb(f"dg{i}", [128, 128], BF16) for i in range(2)]

                Gt = sbb("Gt", [128, D])
                gate_tile(l, j, 40, Gt)
                for cb in range(16):
                    n0 = cb * 128
                    w = wst[cb % 2]
                    dma(w[:], peer_wq[l, :, n0:n0 + 128].rearrange("(kc p) n -> p kc n", p=128), [], [w])
                    V("tensor_copy", wq_sb[:, :, n0:n0 + 128], w[:], reads=[w], writes=[wq_sb], eng="pool")
                for c16 in range(16):
                    dma(kraw[:], peer_keys[l, c16], [], [kraw])
                    tr(PS[0][:, 0:128], kraw[:], ident[:], [kraw, ident], [PS[0]])
                    V("tensor_copy", keysT[:, c16, :], PS[0][:, 0:128], reads=[PS[0]], writes=[keysT])
                dma(lng[:], ln2_g[l:l + 1, :].partition_broadcast(128), [], [lng])
                dma(lnb[:], ln2_b[l:l + 1, :].partition_broadcast(128), [], [lnb])

                for t in range(nt):
                    R = min(128, T_len - t * 128)
                    load_x(j, t, R, 1, is_sample)
                    normalize(xh, x_t, R)
                    for kc in range(8):
                        pp = PS[kc // 4]
                        tr(pp[:, (kc % 4) * 128:(kc % 4) * 128 + R], xh[:R, kc * 128:(kc + 1) * 128], ident[:R, :R],
                           [xh, ident], [pp])
                    for kc in range(8):
                        pp = PS[kc // 4]
                        act(h2T[:, kc, :R], pp[:, (kc % 4) * 128:(kc % 4) * 128 + R], AF.Identity, [pp, modT], [h2T],
                            scale=modT[:, l, 32 + kc, j:j + 1], bias=modT[:, l, 24 + kc, j:j + 1])
                    V("tensor_copy", h2Tb[:, :, :R], h2T[:, :, :R], reads=[h2T], writes=[h2Tb])
                    for kc in range(8):
                        pp = PS[2 + kc // 4]
                        tr(pp[:R, (kc % 4) * 128:(kc % 4 + 1) * 128], h2T[:, kc, :R], ident[:, :], [h2T, ident], [pp])
                    for hh in range(2):
                        V("tensor_copy", h2[:R, hh * 512:(hh + 1) * 512], PS[2 + hh][:R, :], reads=[PS[2 + hh]], writes=[h2])
                    for c16 in range(16):
                        pq = PS[4 + (c16 % 2)]
                        for kc in range(8):
                            mm(pq[:, :R], wq_sb[:, kc, c16 * 128:(c16 + 1) * 128], h2Tb[:, kc, :R], [wq_sb, h2Tb], [pq],
                               start=(kc == 0), stop=(kc == 7))
                        act(qT[:, c16, :R], pq[:, :R], AF.Copy, [pq], [qT])
                    for c16 in range(16):
                        pq = PS[6 + (c16 // 4) % 2]
                        mm(pq[:R, (c16 % 4) * 128:(c16 % 4 + 1) * 128], qT[:, c16, :R], keysT[:, c16, :], [qT, keysT], [pq])
                        if c16 % 4 == 3:
                            V("tensor_copy", sc[:R, c16 - 3:c16 + 1, :].rearrange("p a b -> p (a b)"), pq[:R, :], reads=[pq], writes=[sc])
                    for c16 in range(16):
                        V("max", tops[:R, c16, 0:8], sc[:R, c16, :], reads=[sc], writes=[tops])
                        V("max_index", topi[:R, c16, 0:8], tops[:R, c16, 0:8], sc[:R, c16, :], reads=[tops, sc], writes=[topi])
                        V("match_replace", sc2[:R, c16, :], tops[:R, c16, 0:8], sc[:R, c16, :], -1e30, reads=[tops, sc], writes=[sc2])
                        V("max", tops[:R, c16, 8:16], sc2[:R, c16, :], reads=[sc2], writes=[tops])
                        V("max_index", topi[:R, c16, 8:16], tops[:R, c16, 8:16], sc2[:R, c16, :], reads=[tops, sc2], writes=[topi])
                    V("tensor_copy", topf[:R, :, :], topi[:R, :, :], reads=[topi], writes=[topf])
                    t4 = tops[:R, :, :].rearrange("p (h s) k -> p h s k", s=2)
                    f4 = topf[:R, :, :].rearrange("p (h s) k -> p h s k", s=2)
                    c4 = cand[:R, :, :].rearrange("p h (a b) -> p h a b", b=16)
                    V("tensor_tensor", c4, t4[:, :, 0, :].unsqueeze(3).to_broadcast([R, 8, 16, 16]),
                      t4[:, :, 1, :].unsqueeze(2).to_broadcast([R, 8, 16, 16]), reads=[tops], writes=[cand], op=ALU.add)
                    for hd in range(8):
                        V("max", bests[:R, hd, 0:8], cand[:R, hd, :], reads=[cand], writes=[bests])
                        V("max_index", besti[:R, hd, 0:8], bests[:R, hd, 0:8], cand[:R, hd, :], reads=[bests, cand], writes=[besti])
                        V("match_replace", cand2[:R, hd, :], bests[:R, hd, 0:8], cand[:R, hd, :], -1e30, reads=[bests, cand], writes=[cand2])
                        V("max", bests[:R, hd, 8:16], cand2[:R, hd, :], reads=[cand2], writes=[bests])
                        V("max_index", besti[:R, hd, 8:16], bests[:R, hd, 8:16], cand2[:R, hd, :], reads=[bests, cand2], writes=[besti])
                    V("tensor_copy", af[:R, :, :], besti[:R, :, :], reads=[besti], writes=[af])
                    V("tensor_copy", bf[:R, :, :], af[:R, :, :], reads=[af], writes=[bf])
                    V("tensor_tensor", oh[:R], bf[:R, :, :].unsqueeze(3).to_broadcast([R, 8, 16, 16]),
                      thr16[:R, :].unsqueeze(1).unsqueeze(1).to_broadcast([R, 8, 16, 16]), reads=[bf, thr16], writes=[oh], op=ALU.is_ge)
                    V("tensor_reduce", af[:R, :, :], oh[:R], reads=[oh], writes=[af], axis=AX.X, op=ALU.add)
                    V("scalar_tensor_tensor", bf[:R, :, :], af[:R, :, :], -16.0, bf[:R, :, :], reads=[af, bf], writes=[bf],
                      op0=ALU.mult, op1=ALU.add)
                    io16 = iota_f[:R, 0:16].unsqueeze(1).unsqueeze(1).to_broadcast([R, 8, 16, 16])
                    for sel, half, dstf in ((af, 0, idf), (bf, 1, idf2)):
                        V("tensor_tensor", oh[:R], sel[:R, :, :].unsqueeze(3).to_broadcast([R, 8, 16, 16]), io16,
                          reads=[sel, iota_f], writes=[oh], op=ALU.is_equal)
                        V("tensor_tensor", oh[:R], oh[:R], f4[:, :, half, :].unsqueeze(2).to_broadcast([R, 8, 16, 16]),
                          reads=[oh, topf], writes=[oh], op=ALU.mult)
                        V("tensor_reduce", dstf[:R, :, :], oh[:R], reads=[oh], writes=[dstf], axis=AX.X, op=ALU.add)
                    V("scalar_tensor_tensor", idf[:R, :, :], idf[:R, :, :], 128.0, idf2[:R, :, :], reads=[idf, idf2], writes=[idf],
                      op0=ALU.mult, op1=ALU.add)
                    V("tensor_scalar_add", idf[:R, :, :], idf[:R, :, :], float(l * 16384), reads=[idf], writes=[idf])
                    V("tensor_copy", ids[:R, :], idf[:R, :, :].rearrange("p a b -> p (a b)"), reads=[idf], writes=[ids])
                    V("tensor_reduce", gmax[:R, :], bests[:R, :, :], reads=[bests], writes=[gmax], axis=AX.X, op=ALU.max)
                    V("tensor_tensor", gates[:R, :, :], bests[:R, :, :], gmax[:R, :].unsqueeze(2).to_broadcast([R, 8, 16]),
                      reads=[bests, gmax], writes=[gates], op=ALU.subtract)
                    act(gates[:R, :, :], gates[:R, :, :], AF.Exp, [gates], [gates])
                    V("tensor_reduce", gsum[:R, :], gates[:R, :, :], reads=[gates], writes=[gsum], axis=AX.X, op=ALU.add)
                    V("reciprocal", gsum[:R, :], gsum[:R, :], reads=[gsum], writes=[gsum])
                    V("tensor_tensor", gates[:R, :, :], gates[:R, :, :], gsum[:R, :].unsqueeze(2).to_broadcast([R, 8, 16]),
                      reads=[gates, gsum], writes=[gates], op=ALU.mult)
                    for k in range(128):
                        gb = gbuf[k % 8]
                        P.op("pool", lambda e, gb=gb, k=k, R=R: e.indirect_dma_start(
                            out=gb[:R, :], out_offset=None, in_=eu_bf[:, :],
                            in_offset=bass.IndirectOffsetOnAxis(ap=ids[:R, k:k + 1], axis=0)),
                            [ids], [gb], dma=True)
                        V("scalar_tensor_tensor", junk[:R, :], gb[:R, :], 1.0, h2[:R, :], reads=[gb, h2], writes=[junk, dots],
                          op0=ALU.mult, op1=ALU.mult, accum_out=dots[:R, k:k + 1])
                    V("tensor_tensor", gt2[:R, :], dots[:R, :], dots[:R, :], reads=[dots], writes=[gt2], op=ALU.mult)
                    V("tensor_scalar", gt2[:R, :], gt2[:R, :], 0.044715, 1.0, reads=[gt2], writes=[gt2], op0=ALU.mult, op1=ALU.add)
                    V("tensor_tensor", gt2[:R, :], gt2[:R, :], dots[:R, :], reads=[gt2, dots], writes=[gt2], op=ALU.mult)
                    act(gt2[:R, :], gt2[:R, :], AF.Sigmoid, [gt2], [gt2], scale=2.0 * math.sqrt(2.0 / math.pi))
                    V("tensor_tensor", wts[:R, :], gt2[:R, :], dots[:R, :], reads=[gt2, dots], writes=[wts], op=ALU.mult)
                    V("tensor_tensor", wts[:R, :], wts[:R, :], gates[:R, :, :].rearrange("p a b -> p (a b)"), reads=[wts, gates],
                      writes=[wts], op=ALU.mult)
                    for k in range(128):
                        gb = gbuf[k % 8]
                        P.op("pool", lambda e, gb=gb, k=k, R=R: e.indirect_dma_start(
                            out=gb[:R, :], out_offset=None, in_=ev_bf[:, :],
                            in_offset=bass.IndirectOffsetOnAxis(ap=ids[:R, k:k + 1], axis=0)),
                            [ids], [gb], dma=True)
                        d = dg[k % 2]
                        V("tensor_scalar", d[:R, :R], identb[:R, :R], wts[:R, k:k + 1], None, reads=[identb, wts], writes=[d], op0=ALU.mult)
                        for hh in range(2):
                            mm(PS[6 + hh][:R, :], d[:R, :R], gb[:R, hh * 512:(hh + 1) * 512], [d, gb], [PS[6 + hh]],
                               start=(k == 0), stop=(k == 127))
                    for hh in range(2):
                        V("tensor_tensor", facc[:R, hh * 512:(hh + 1) * 512], PS[6 + hh][:R, :], Gt[:R, hh * 512:(hh + 1) * 512],
                          reads=[PS[6 + hh], Gt], writes=[facc], op=ALU.mult)
                    residual_ln([facc[:R, 0:512], facc[:R, 512:1024], facc, facc], R, x_t)
                    store_x(j, t, R, is_sample, x_t)
                P.barrier()

        order = [NP] + list(range(NP))
        for j in order:
            is_sample = (j == NP)
            T_len = TS if is_sample else SEQ
            for l in range(L):
                phase_a(l, j, T_len, is_sample)
                if stages >= 5:
                    phase_b(l, j, T_len, is_sample)

        P.emit()
        print("n_ops", P.n_ops, {e: len(v) for e, v in P.ops.items()}, flush=True)
    return nc


WEIGHT_NAMES = ["w_ada", "b_ada", "w_in", "sgu_ln_g", "sgu_ln_b", "sgu_w", "sgu_b", "lam_q1", "lam_k1",
                "lam_q2", "lam_k2", "diff_norm_g", "conv_w", "gdn_a_log", "gdn_dt_bias", "gdn_norm_g",
                "w_out", "ln1_g", "ln1_b", "peer_wq", "peer_keys", "expert_u", "expert_v", "ln2_g", "ln2_b"]


def run(cfg, inputs, stages=99):
    L = cfg.DEPTH
    NP = cfg.BATCH // NCORES
    f = lambda a: np.ascontiguousarray(np.asarray(a, dtype=np.float32))
    W = {k: f(inputs[k]) for k in WEIGHT_NAMES}
    W["sgu_ln_g"] = W["sgu_ln_g"].reshape(L, 256)
    W["sgu_ln_b"] = W["sgu_ln_b"].reshape(L, 256)
    W["peer_keys"] = W["peer_keys"].reshape(L, 16, 128, 128)
    W["expert_u"] = W["expert_u"].reshape(L * 16384, cfg.D)
    W["expert_v"] = W["expert_v"].reshape(L * 16384, cfg.D)
    xp = f(inputs["x_prompt"]); xs = f(inputs["x_sample"])
    ck = f(inputs["cache_k"]); cv = f(inputs["cache_v"])
    sg = f(inputs["state_gdn"]); sc = f(inputs["state_conv"])
    cp = f(inputs["c_prompt"]); cs = f(inputs["c_sample"])
    in_maps = []
    for i in range(NCORES):
        m = dict(W)
        m["xp"] = xp[i * NP:(i + 1) * NP]
        m["xs"] = xs[i]
        m["cache_k"] = f(ck[:, i].reshape(L, cfg.PAST, 512))
        m["cache_v"] = f(cv[:, i].reshape(L, cfg.PAST, 512))
        m["state_gdn"] = f(sg[:, i])
        m["state_conv"] = f(sc[:, i])
        m["c_in"] = f(np.concatenate([cp[i * NP:(i + 1) * NP], cs[i:i + 1]], axis=0))
        in_maps.append(m)
    import time as _time
    _t0 = _time.time()
    nc = build(cfg, stages)
    print("build s", _time.time() - _t0, flush=True)
    _t0 = _time.time()
    res = run_bass_kernel_spmd(nc, in_maps, core_ids=list(range(NCORES)))
    print("run_spmd s", _time.time() - _t0, flush=True)
    R = res.results
    cat = lambda k, ax: np.concatenate([r[k] for r in R], axis=ax)
    stk = lambda k, ax: np.stack([r[k] for r in R], axis=ax)
    S = cfg.SEQ
    y_prompt = cat("y_p", 0)
    y_sample = stk("y_s", 0)
    nk_p = cat("nk_p", 1).reshape(L, cfg.BATCH, S, 4, 128)
    nv_p = cat("nv_p", 1).reshape(L, cfg.BATCH, S, 4, 128)
    ngdn_p = cat("ngdn_p", 1)
    nconv_p = cat("nconv_p", 1)
    nk_s = stk("nk_s", 1).reshape(L, cfg.DEC_BATCH, cfg.DEC_SEQ, 4, 128)
    nv_s = stk("nv_s", 1).reshape(L, cfg.DEC_BATCH, cfg.DEC_SEQ, 4, 128)
    ngdn_s = stk("ngdn_s", 1)
    nconv_s = stk("nconv_s", 1)
    nsgu_s = stk("nsgu_s", 1)
    return (y_prompt, y_sample, nk_p, nv_p, ngdn_p, nconv_p, nk_s, nv_s, ngdn_s, nconv_s, nsgu_s)


def kernel(**inputs):
    return run(Cfg(), inputs)
```

```python
import math
import numpy as np
import concourse.bass as bass
import concourse.mybir as mybir
from concourse.bass_utils import run_bass_kernel_spmd

F32 = mybir.dt.float32
BF16 = mybir.dt.bfloat16
I32 = mybir.dt.int32
U32 = mybir.dt.uint32
AF = mybir.ActivationFunctionType
ALU = mybir.AluOpType
AX = mybir.AxisListType

NCORES = 8


class Cfg:
    def __init__(self, **kw):
        self.D = 1024
        self.BATCH = 32
        self.SEQ = 2048
        self.DEPTH = 4
        self.DEC_BATCH = 8
        self.DEC_SEQ = 16
        self.PAST = 4096
        for k, v in kw.items():
            setattr(self, k, v)
        self.ALPHA = (2 * self.DEPTH) ** 0.25


class Res:
    __slots__ = ("name", "lw", "rd")

    def __init__(self, name=""):
        self.name = name
        self.lw = None
        self.rd = {}


class T:
    def __init__(self, handle, name, nres=1):
        self.h = handle
        self.name = name
        self.res = [Res(f"{name}.{i}") for i in range(nres)]

    def __getitem__(self, idx):
        return self.h[idx]

    @property
    def r(self):
        return self.res[0]


COMPUTE = ("pe", "act", "dve", "pool")
NDMASEM = 24


class Prog:
    def __init__(self, nc, stack):
        self.nc = nc
        self.stack = stack
        self.ops = {e: [] for e in ("pe", "act", "dve", "pool", "sp")}
        self.sem = {}
        for e in COMPUTE:
            self.sem[e] = stack.enter_context(nc.semaphore(f"s_{e}"))
        self.cnt = {e: 0 for e in COMPUTE}
        self.dsem = {}
        self.dcnt = {}
        self.dnext = {}
        for q in ("sp", "pool"):
            self.dsem[q] = [stack.enter_context(nc.semaphore(f"d_{q}{i}")) for i in range(NDMASEM)]
            self.dcnt[q] = [0] * NDMASEM
            self.dnext[q] = 0
        self.known = {e: {} for e in self.ops}
        self.out_tokens = []
        self.n_ops = 0

    def _need(self, eng, tok, waits):
        if tok is None:
            return
        key, val = tok[0], tok[1]
        if self.known[eng].get(key, 0) >= val:
            return
        self.known[eng][key] = val
        waits.append((key, val))

    def barrier(self):
        toks = []
        for c in COMPUTE:
            if self.cnt[c] > 0:
                toks.append((("c", c), self.cnt[c], c, False))
        for q in ("sp", "pool"):
            for slot in range(NDMASEM):
                if self.dcnt[q][slot] > 0:
                    toks.append((("d", q, slot), self.dcnt[q][slot], q, True))
        for e in self.ops:
            waits = []
            for tk in toks:
                self._need(e, tk, waits)
            if waits:
                self.ops[e].append((waits, None, None))

    def op(self, eng, fn, reads=(), writes=(), dma=False, is_out=False):
        waits = []
        for r in reads:
            rr = r.r if isinstance(r, T) else r
            if rr.lw is not None:
                if rr.lw[2] == eng and eng == "pe" and not dma and rr.lw[3] is False:
                    pass
                else:
                    self._need(eng, rr.lw, waits)
        for w in writes:
            ww = w.r if isinstance(w, T) else w
            if ww.lw is not None:
                if not (ww.lw[2] == eng and not dma and ww.lw[3] is False):
                    self._need(eng, ww.lw, waits)
            for tk in ww.rd.values():
                if not (tk[2] == eng and not dma and tk[3] is False):
                    self._need(eng, tk, waits)
        if dma:
            q = eng
            slot = self.dnext[q]
            self.dnext[q] = (slot + 1) % NDMASEM
            key = ("d", q, slot)
            prev = self.dcnt[q][slot]
            if prev > 0:
                self._need(eng, (key, prev, q, True), waits)
            self.dcnt[q][slot] = prev + 16
            tok = (key, prev + 16, q, True)
            inc = (self.dsem[q][slot], 16)
        else:
            self.cnt[eng] += 1
            tok = (("c", eng), self.cnt[eng], eng, False)
            inc = (self.sem[eng], 1)
        self.ops[eng].append((waits, fn, inc))
        for r in reads:
            rr = r.r if isinstance(r, T) else r
            rr.rd[tok[0]] = tok
        for w in writes:
            ww = w.r if isinstance(w, T) else w
            ww.lw = tok
            ww.rd = {}
        if is_out:
            self.out_tokens.append(tok)
        self.n_ops += 1
        return tok

    def semobj(self, key):
        if key[0] == "c":
            return self.sem[key[1]]
        return self.dsem[key[1]][key[2]]

    def emit(self):
        nc = self.nc
        final_waits = []
        for tok in self.out_tokens:
            self._need("sp", tok, final_waits)
        for q in ("sp", "pool"):
            for slot in range(NDMASEM):
                if self.dcnt[q][slot] > 0:
                    self._need("sp", (("d", q, slot), self.dcnt[q][slot], q, True), final_waits)
        engmap = {"pe": "tensor", "act": "scalar", "dve": "vector", "pool": "gpsimd", "sp": "sync"}
        with nc.Block() as block:
            for e, attr in engmap.items():
                ops = self.ops[e]
                extra = final_waits if e == "sp" else []

                def body(eng, ops=ops, extra=extra):
                    for waits, fn, inc in ops:
                        for key, val in waits:
                            eng.wait_ge(self.semobj(key), val)
                        if fn is None:
                            continue
                        ins = fn(eng)
                        ins.then_inc(inc[0], inc[1])
                    for key, val in extra:
                        eng.wait_ge(self.semobj(key), val)

                getattr(block, attr)(body)


def build(cfg, stages=99):
    from contextlib import ExitStack
    D, L = cfg.D, cfg.DEPTH
    NP = cfg.BATCH // NCORES
    SEQ, TS, PAST = cfg.SEQ, cfg.DEC_SEQ, cfg.PAST
    NS = NP + 1
    P_IN = 3080
    ALPHA = cfg.ALPHA
    SLOPES = [2.0 ** (-(8.0 / 4) * (h + 1)) for h in range(4)]
    NEG = -30000.0
    nc = bass.Bass("TRN2", target_bir_lowering=False)

    def din(name, shape, dt=F32):
        return nc.dram_tensor(name, list(shape), dt, kind="ExternalInput")

    def dout(name, shape, dt=F32):
        return nc.dram_tensor(name, list(shape), dt, kind="ExternalOutput")

    xp = din("xp", [NP, SEQ, D]); xs = din("xs", [TS, D])
    cache_k = din("cache_k", [L, PAST, 512]); cache_v = din("cache_v", [L, PAST, 512])
    state_gdn = din("state_gdn", [L, 4, 64, 64]); state_conv = din("state_conv", [L, 3, 768])
    c_in = din("c_in", [NS, D])
    w_ada = din("w_ada", [L, D, 6 * D]); b_ada = din("b_ada", [L, 6 * D])
    w_in = din("w_in", [L, D, P_IN])
    sgu_ln_g = din("sgu_ln_g", [L, 256]); sgu_ln_b = din("sgu_ln_b", [L, 256])
    sgu_w = din("sgu_w", [L, 4, 128, 128]); sgu_b = din("sgu_b", [L, 4, 128])
    lam_q1 = din("lam_q1", [L, 64]); lam_k1 = din("lam_k1", [L, 64])
    lam_q2 = din("lam_q2", [L, 64]); lam_k2 = din("lam_k2", [L, 64])
    diff_norm_g = din("diff_norm_g", [L, 128])
    conv_w = din("conv_w", [L, 4, 768])
    gdn_a_log = din("gdn_a_log", [L, 4]); gdn_dt_bias = din("gdn_dt_bias", [L, 4])
    gdn_norm_g = din("gdn_norm_g", [L, 64])
    w_out = din("w_out", [L, D, D])
    ln1_g = din("ln1_g", [L, D]); ln1_b = din("ln1_b", [L, D])
    peer_wq = din("peer_wq", [L, D, 2048])
    peer_keys = din("peer_keys", [L, 16, 128, 128])
    expert_u = din("expert_u", [L * 16384, D]); expert_v = din("expert_v", [L * 16384, D])
    ln2_g = din("ln2_g", [L, D]); ln2_b = din("ln2_b", [L, D])

    euv = nc.dram_tensor("euv_bf", [L * 16384, 2 * D], BF16, kind="Internal")
    y_p = dout("y_p", [NP, SEQ, D]); y_s = dout("y_s", [TS, D])
    nk_p = dout("nk_p", [L, NP, SEQ, 512]); nv_p = dout("nv_p", [L, NP, SEQ, 512])
    ngdn_p = dout("ngdn_p", [L, NP, 4, 64, 64]); nconv_p = dout("nconv_p", [L, NP, 3, 768])
    nk_s = dout("nk_s", [L, TS, 512]); nv_s = dout("nv_s", [L, TS, 512])
    ngdn_s = dout("ngdn_s", [L, 4, 64, 64]); nconv_s = dout("nconv_s", [L, 3, 768])
    nsgu_s = dout("nsgu_s", [L, TS, 256])

    with ExitStack() as st:
        P = Prog(nc, st)

        uid = [0]

        def sb(name, shape, dt=F32, stack=None):
            uid[0] += 1
            return T((stack or st).enter_context(nc.sbuf_tensor(f"{name}_{uid[0]}", list(shape), dt)), name)

        def ps(name, shape=(128, 512), dt=F32):
            return T(st.enter_context(nc.psum_tensor(name, list(shape), dt)), name)

        PS = [ps(f"ps{i}") for i in range(8)]

        def dma(out, in_, reads=(), writes=(), q="sp", is_out=False, **kw):
            P.op(q, lambda e: e.dma_start(out=out, in_=in_, **kw), reads, writes, dma=True, is_out=is_out)

        def mm(out, lhsT, rhs, reads, writes, start=True, stop=True, **kw):
            P.op("pe", lambda e: e.matmul(out, lhsT, rhs, start=start, stop=stop, **kw), reads, writes)

        def tr(out, in_, ident_ap, reads, writes):
            P.op("pe", lambda e: e.transpose(out, in_, ident_ap), reads, writes)

        def act(out, in_, func, reads, writes, **kw):
            P.op("act", lambda e: e.activation(out=out, in_=in_, func=func, **kw), reads, writes)

        def V(method, *args, reads=(), writes=(), eng="dve", **kw):
            P.op(eng, lambda e: getattr(e, method)(*args, **kw), reads, writes)

        def rsqrt(dst, src, eps, reads, dst_t, scale=1.0):
            V("tensor_scalar", dst, src, scale, eps, reads=reads, writes=[dst_t], op0=ALU.mult, op1=ALU.add)
            act(dst, dst, AF.Sqrt, [dst_t], [dst_t])
            V("reciprocal", dst, dst, reads=[dst_t], writes=[dst_t])

        def bcast_row(dram_row_ap, n):
            return dram_row_ap.partition_broadcast(128) if hasattr(dram_row_ap, "partition_broadcast") else dram_row_ap

        ident = sb("ident", [128, 128])
        identb = sb("identb", [128, 128], BF16)
        iota_p = sb("iota_p", [128, 1])
        iota_f = sb("iota_f", [128, 128])
        dif = sb("dif", [128, 128])
        triU = sb("triU", [128, 128])
        maskL = sb("maskL", [64, 64])
        maskLs = sb("maskLs", [64, 64])
        maskU = sb("maskU", [64, 64])
        ones64 = sb("ones64", [64, 64])
        ones_row = sb("ones_row", [1, 128])
        ones_rowb = sb("ones_rowb", [1, 128], BF16)
        kirow = sb("kirow", [1, 128], BF16)
        sloperow = sb("sloperow", [1, 4, 2, 128], BF16)
        nsqi = sb("nsqi", [1, 4, 2, 128], BF16)
        biasd = sb("biasd", [128, 4, 2, 128], BF16)
        tmpc = sb("tmpc", [128, 128])
        thr16 = sb("thr16", [128, 16])
        V("iota", iota_p[:], [[0, 1]], reads=[], writes=[iota_p], eng="pool", base=0, channel_multiplier=1,
          allow_small_or_imprecise_dtypes=True)
        V("iota", iota_f[:], [[1, 128]], reads=[], writes=[iota_f], eng="pool", base=0, channel_multiplier=0,
          allow_small_or_imprecise_dtypes=True)
        V("tensor_scalar", dif[:], iota_f[:], iota_p[:, 0:1], None, reads=[iota_f, iota_p], writes=[dif],
          op0=ALU.subtract)
        V("tensor_single_scalar", ident[:], dif[:], 0.0, reads=[dif], writes=[ident], op=ALU.is_equal)
        V("tensor_copy", identb[:], ident[:], reads=[ident], writes=[identb])
        V("tensor_single_scalar", triU[:], dif[:], 0.0, reads=[dif], writes=[triU], op=ALU.is_ge)
        V("tensor_single_scalar", maskU[:], dif[:64, :64], 0.0, reads=[dif], writes=[maskU], op=ALU.is_ge)
        V("tensor_single_scalar", maskL[:], dif[:64, :64], 0.0, reads=[dif], writes=[maskL], op=ALU.is_le)
        V("tensor_single_scalar", maskLs[:], dif[:64, :64], 0.0, reads=[dif], writes=[maskLs], op=ALU.is_lt)
        V("tensor_scalar", thr16[:], iota_f[:, 0:16], 1.0, 16.0, reads=[iota_f], writes=[thr16], op0=ALU.add, op1=ALU.mult)
        V("memset", ones64[:], 1.0, reads=[], writes=[ones64])
        V("memset", ones_row[:], 1.0, reads=[], writes=[ones_row])
        V("memset", ones_rowb[:], 1.0, reads=[], writes=[ones_rowb])
        V("tensor_copy", kirow[:], iota_f[0:1, :], reads=[iota_f], writes=[kirow])
        mk = sb("mk", [128, 128])
        V("tensor_single_scalar", mk[:], iota_f[:], 64.0, reads=[iota_f], writes=[mk], op=ALU.is_lt)
        V("tensor_single_scalar", tmpc[:], iota_p[:].to_broadcast([128, 128]), 64.0, reads=[iota_p], writes=[tmpc],
          op=ALU.is_ge)
        V("tensor_tensor", mk[:], mk[:], tmpc[:], reads=[mk, tmpc], writes=[mk], op=ALU.mult)
        V("tensor_single_scalar", mk[:], mk[:], NEG, reads=[mk], writes=[mk], op=ALU.mult)
        V("scalar_tensor_tensor", tmpc[:], dif[:], -1.0, dif[:], reads=[dif], writes=[tmpc], op0=ALU.mult, op1=ALU.max)
        for h in range(4):
            for m in range(2):
                V("memset", sloperow[0:1, h, m, :], SLOPES[h], reads=[], writes=[sloperow])
                V("tensor_single_scalar", nsqi[0:1, h, m, :], iota_f[0:1, :], -SLOPES[h], reads=[iota_f],
                  writes=[nsqi], op=ALU.mult)
                V("scalar_tensor_tensor", biasd[:, h, m, :], tmpc[:], -SLOPES[h], mk[:], reads=[tmpc, mk],
                  writes=[biasd], op0=ALU.mult, op1=ALU.add)

        modT = sb("modT", [128, L, 48, NS])
        with ExitStack() as s0:
            c_sb = sb("c_sb", [NS, D], stack=s0)
            cT = sb("cT", [128, 8, NS], stack=s0)
            wst = [sb(f"wst{i}", [128, 8, 512], stack=s0) for i in range(2)]
            brow = sb("brow", [1, 6 * D], stack=s0)
            dma(c_sb[:], c_in[:, :], [], [c_sb])
            act(c_sb[:], c_sb[:], AF.Silu, [c_sb], [c_sb])
            for kc in range(8):
                tr(PS[0][:, kc * NS:(kc + 1) * NS], c_sb[:NS, kc * 128:(kc + 1) * 128], ident[:NS, :NS],
                   [c_sb, ident], [PS[0]])
            V("tensor_copy", cT[:].rearrange("p a b -> p (a b)"), PS[0][:, 0:8 * NS], reads=[PS[0]], writes=[cT])
            for l in range(L):
                dma(brow[:], b_ada[l:l + 1, :], [], [brow])
                for cb in range(12):
                    w = wst[cb % 2]
                    dma(w[:], w_ada[l, :, cb * 512:(cb + 1) * 512].rearrange("(kc p) n -> p kc n", p=128), [], [w])
                    pt = PS[cb % 2]
                    for f4 in range(4):
                        fc = cb * 4 + f4
                        for kc in range(8):
                            mm(pt[:, f4 * NS:(f4 + 1) * NS], w[:, kc, f4 * 128:(f4 + 1) * 128], cT[:, kc, :],
                               [w, cT], [pt], start=(kc == 0), stop=False)
                        mm(pt[:, f4 * NS:(f4 + 1) * NS], brow[0:1, fc * 128:(fc + 1) * 128], ones_row[0:1, :NS],
                           [brow, ones_row], [pt], start=False, stop=True)
                    V("tensor_copy", modT[:, l, cb * 4:(cb + 1) * 4, :].rearrange("p a b -> p (a b)"),
                      pt[:, 0:4 * NS], reads=[pt], writes=[modT])
                for c0 in (8, 32):
                    V("tensor_scalar_add", modT[:, l, c0:c0 + 8, :], modT[:, l, c0:c0 + 8, :], 1.0,
                      reads=[modT], writes=[modT])
            P.barrier()

        with ExitStack() as sc:
            cin = [sb(f"cin{i}", [128, 4096], stack=sc) for i in range(2)]
            cout = [sb(f"cout{i}", [128, 4096], BF16, stack=sc) for i in range(2)]
            nchunk = L * 16384 // 512
            ci = 0
            for (src, half) in ((expert_u, 0), (expert_v, 1)):
                sv = src[:, :].rearrange("(c p r) d -> c p (r d)", p=128, r=4)
                dv = euv[:, half * D:(half + 1) * D].rearrange("(c p r) d -> c p r d", p=128, r=4)
                for c in range(nchunk):
                    a, b = cin[ci % 2], cout[ci % 2]
                    dma(a[:], sv[c], [], [a])
                    eng = ("act", "dve", "pool")[ci % 3]
                    if eng == "act":
                        act(b[:], a[:], AF.Copy, [a], [b])
                    else:
                        V("tensor_copy", b[:], a[:], reads=[a], writes=[b], eng=eng)
                    dma(dv[c], b[:].rearrange("p (r d) -> p r d", r=4), [b], [])
                    ci += 1
            P.barrier()

        wst = [sb(f"wstg{i}", [128, 8, 128]) for i in range(2)]
        x_t = sb("x_t", [128, D])
        xh = sb("xh", [128, D])
        stats = sb("stats", [128, 2, 6])
        mv = sb("mv", [128, 2])
        rstd = sb("rstd", [128, 1])
        lng = sb("lng", [128, D]); lnb = sb("lnb", [128, D])
        xres = {}

        def xr(j, t):
            return xres.setdefault((j, t), Res(f"x{j}.{t}"))

        kvres = {}

        def kvr(l, j, t):
            return kvres.setdefault((l, j, t), Res(f"kv{l}.{j}.{t}"))

        def ln_stats(src_ap, R, src_res):
            for i in range(2):
                V("bn_stats", stats[:R, i, :], src_ap[:, i * 512:(i + 1) * 512], reads=[src_res], writes=[stats])
            V("bn_aggr", mv[:R, :], stats[:R, :, :], reads=[stats], writes=[mv])
            rsqrt(rstd[:R, :], mv[:R, 1:2], 1e-5, [mv], rstd)

        def normalize(dst_t, src_t, R):
            ln_stats(src_t[:R, :], R, src_t)
            V("tensor_scalar", dst_t[:R, :], src_t[:R, :], mv[:R, 0:1], rstd[:R, 0:1], reads=[src_t, mv, rstd],
              writes=[dst_t], op0=ALU.subtract, op1=ALU.mult)

        def gate_tile(l, j, c0, Gt):
            for kc in range(8):
                pp = PS[kc // 4]
                V("tensor_copy", tmpc[:], modT[:, l, c0 + kc, j:j + 1].to_broadcast([128, 128]), reads=[modT], writes=[tmpc])
                mm(pp[:, (kc % 4) * 128:(kc % 4 + 1) * 128], tmpc[:], ident[:], [tmpc, ident], [pp])
            for hh in range(2):
                V("tensor_copy", Gt[:, hh * 512:(hh + 1) * 512], PS[hh][:, :], reads=[PS[hh]], writes=[Gt])

        def load_x(j, t, R, l, is_sample):
            if is_sample:
                src = xs[t * 128:t * 128 + R, :] if l == 0 else y_s[t * 128:t * 128 + R, :]
            else:
                src = xp[j, t * 128:t * 128 + R, :] if l == 0 else y_p[j, t * 128:t * 128 + R, :]
            dma(x_t[:R, :], src, [xr(j, t)], [x_t])

        def store_x(j, t, R, is_sample, src_t):
            dst = y_s[t * 128:t * 128 + R, :] if is_sample else y_p[j, t * 128:t * 128 + R, :]
            dma(dst, src_t[:R, :], [src_t], [xr(j, t)], is_out=True)

        def residual_ln(y_ps_list, R, dst_t):
            for hh in range(2):
                V("scalar_tensor_tensor", xh[:R, hh * 512:(hh + 1) * 512], x_t[:R, hh * 512:(hh + 1) * 512], ALPHA,
                  y_ps_list[hh], reads=[x_t] + [y_ps_list[2 + hh]], writes=[xh], op0=ALU.mult, op1=ALU.add)
            normalize(dst_t, xh, R)
            V("tensor_tensor", dst_t[:R, :], dst_t[:R, :], lng[:R, :], reads=[dst_t, lng], writes=[dst_t], op=ALU.mult)
            V("tensor_tensor", dst_t[:R, :], dst_t[:R, :], lnb[:R, :], reads=[dst_t, lnb], writes=[dst_t], op=ALU.add)

        def phase_a(l, j, T_len, is_sample):
            lam_init = 0.8 - 0.6 * math.exp(-0.3 * l)
            nt = (T_len + 127) // 128
            with ExitStack() as sa:
                def sba(name, shape, dt=F32):
                    return sb(name, shape, dt, stack=sa)
                w_in_sb = sba("w_in_sb", [128, 8, P_IN], BF16)
                w_out_sb = sba("w_out_sb", [128, 8, D], BF16)
                hT = sba("hT", [128, 8, 128], BF16)
                p_sb = sba("p_sb", [128, P_IN])
                mix = sba("mix", [128, D])
                mixT = sba("mixT", [128, 8, 128], BF16)
                wTs = sba("wTs", [128, 4, 128])
                bsT = sba("bsT", [128, 4])
                sg_g = sba("sg_g", [128, 256]); sg_b = sba("sg_b", [128, 256])
                uv = sba("uv", [128, 512]); gtmp = sba("gtmp", [128, 512])
                vst = sba("vst", [128, 4, 2]); vn = sba("vn", [128, 256]); vsq = sba("vsq", [128, 256])
                lamc = sba("lamc", [128, 4]); lamv = sba("lamv", [128, 4, 64]); nlam = sba("nlam", [128, 1])
                dng = sba("dng", [128, 128])
                Qblk = sba("Qblk", [128, 4, 2, 128], BF16)
                kst = [sba("kst0", [128, 512])] * 2
                vstg = [sba("vstg0", [128, 512])] * 2
                kTb = sba("kTb", [128, 4, 128], BF16)
                vb = sba("vb", [128, 512], BF16)
                onesc = sba("onesc", [128, 1], BF16)
                pT = sba("pT", [128, 2, 128], BF16)
                anum = sba("anum", [128, 8, 128]); aden = sba("aden", [128, 8])
                aout = sba("aout", [128, 4, 128]); asq = sba("asq", [128, 4, 128]); ass = sba("ass", [128, 4])
                cw = sba("cw", [64, 12, 4])
                negA = sba("negA", [64, 4]); dtb = sba("dtb", [64, 4]); gng = sba("gng", [64, 64])
                cbuf = sba("cbuf", [64, 12, 131]); cv = sba("cv", [64, 12, 128]); ctmp = sba("ctmp", [64, 12, 128])
                S_sb = sba("S_sb", [64, 4, 64])
                ktm = sba("ktm", [64, 4, 64]); vtm = sba("vtm", [64, 4, 64])
                gab = sba("gab", [64, 8]); gg = sba("gg", [64, 4]); beta = sba("beta", [64, 4])
                gB = sba("gB", [64, 4, 64]); gc_tm = sba("gc_tm", [64, 4]); Grow = sba("Grow", [64, 4, 64])
                D1 = sba("D1", [64, 4, 64]); E1 = sba("E1", [64, 4, 64]); E2 = sba("E2", [64, 4, 64])
                egc = sba("egc", [64, 4]); egrow = sba("egrow", [64, 4, 64]); kdf = sba("kdf", [64, 4])
                glc = sba("glc", [64, 4])
                Am = [sba(f"Am{i}", [64, 4, 64]) for i in range(2)]
                Bm = [sba(f"Bm{i}", [64, 4, 64]) for i in range(2)]
                XT = sba("XT", [64, 4, 64])
                Vb = gB; Kg = sba("Kg", [64, 4, 64]); kdec = sba("kdec", [64, 4, 64])
                u_sb = sba("u_sb", [64, 4, 64]); wT_sb = sba("wT_sb", [64, 4, 64])
                aqkT = sba("aqkT", [64, 4, 64]); qgT = sba("qgT", [64, 4, 64]); vnew = sba("vnew", [64, 4, 64])
                o_sb = E1; osq = D1; oss = sba("oss", [64, 4])
                z_sb = sba("z_sb", [64, 256])

                Gt = mix
                gate_tile(l, j, 16, Gt)
                for cb in range(25):
                    n0 = cb * 128
                    n1 = min(P_IN, n0 + 128)
                    w = wst[cb % 2]
                    dma(w[:, :, :n1 - n0], w_in[l, :, n0:n1].rearrange("(kc p) n -> p kc n", p=128), [], [w])
                    V("tensor_copy", w_in_sb[:, :, n0:n1], w[:, :, :n1 - n0], reads=[w], writes=[w_in_sb], eng="pool")
                for cb in range(8):
                    n0 = cb * 128
                    w = wst[(cb + 1) % 2]
                    dma(w[:], w_out[l, :, n0:n0 + 128].rearrange("(kc p) n -> p kc n", p=128), [], [w])
                    for kc in range(8):
                        V("tensor_tensor", w_out_sb[:, kc, n0:n0 + 128], w[:, kc, :], Gt[:, n0:n0 + 128],
                          reads=[w, Gt], writes=[w_out_sb], op=ALU.mult, eng="pool")
                dma(lng[:], ln1_g[l:l + 1, :].partition_broadcast(128), [], [lng])
                dma(lnb[:], ln1_b[l:l + 1, :].partition_broadcast(128), [], [lnb])
                wraw = p_sb
                dma(p_sb[:, 1536:2048].rearrange("p (g j) -> p g j", g=4), sgu_w[l].rearrange("g i j -> i g j"), [], [p_sb])
                for g in range(4):
                    tr(PS[2][:, g * 128:(g + 1) * 128], p_sb[:, 1536 + g * 128:1536 + (g + 1) * 128], ident[:], [p_sb, ident], [PS[2]])
                V("tensor_copy", wTs[:].rearrange("p a b -> p (a b)"), PS[2][:, :], reads=[PS[2]], writes=[wTs])
                V("memset", wTs[64:128, :, 0:64], 0.0, reads=[], writes=[wTs])
                dma(p_sb[:4, 2048:2176], sgu_b[l], [], [p_sb])
                tr(PS[3][:, 0:4], p_sb[:4, 2048:2176], ident[:4, :4], [p_sb, ident], [PS[3]])
                V("tensor_copy", bsT[:], PS[3][:, 0:4], reads=[PS[3]], writes=[bsT])
                dma(sg_g[:], sgu_ln_g[l:l + 1, :].partition_broadcast(128), [], [sg_g])
                dma(sg_b[:], sgu_ln_b[l:l + 1, :].partition_broadcast(128), [], [sg_b])
                for i, lq in enumerate((lam_q1, lam_k1, lam_q2, lam_k2)):
                    dma(lamv[:, i, :], lq[l:l + 1, :].partition_broadcast(128), [], [lamv])
                V("tensor_tensor", lamv[:, 0, :], lamv[:, 0, :], lamv[:, 1, :], reads=[lamv], writes=[lamv], op=ALU.mult)
                V("tensor_tensor", lamv[:, 2, :], lamv[:, 2, :], lamv[:, 3, :], reads=[lamv], writes=[lamv], op=ALU.mult)
                V("tensor_reduce", lamc[:, 0:1], lamv[:, 0, :], reads=[lamv], writes=[lamc], axis=AX.X, op=ALU.add)
                V("tensor_reduce", lamc[:, 1:2], lamv[:, 2, :], reads=[lamv], writes=[lamc], axis=AX.X, op=ALU.add)
                act(lamc[:, 0:2], lamc[:, 0:2], AF.Exp, [lamc], [lamc])
                V("tensor_tensor", nlam[:], lamc[:, 1:2], lamc[:, 0:1], reads=[lamc], writes=[nlam], op=ALU.subtract)
                V("tensor_scalar_add", nlam[:], nlam[:], -lam_init, reads=[nlam], writes=[nlam])
                dma(dng[:], diff_norm_g[l:l + 1, :].partition_broadcast(128), [], [dng])
                V("tensor_single_scalar", dng[:], dng[:], 1.0 - lam_init, reads=[dng], writes=[dng], op=ALU.mult)
                V("memset", Qblk[:], 0.0, reads=[], writes=[Qblk])
                V("memset", onesc[:], 1.0, reads=[], writes=[onesc])
                dma(p_sb[:4, 0:768], conv_w[l], [], [p_sb])
                for g in range(12):
                    tr(PS[3][:64, 16 + g * 4:16 + g * 4 + 4], p_sb[:4, g * 64:(g + 1) * 64], ident[:4, :4],
                       [p_sb, ident], [PS[3]])
                V("tensor_copy", cw[:].rearrange("p a b -> p (a b)"), PS[3][:64, 16:64], reads=[PS[3]], writes=[cw])
                dma(negA[:], gdn_a_log[l:l + 1, :].partition_broadcast(64), [], [negA])
                act(negA[:], negA[:], AF.Exp, [negA], [negA])
                V("tensor_single_scalar", negA[:], negA[:], -1.0, reads=[negA], writes=[negA], op=ALU.mult)
                dma(dtb[:], gdn_dt_bias[l:l + 1, :].partition_broadcast(64), [], [dtb])
                dma(gng[:], gdn_norm_g[l:l + 1, :].partition_broadcast(64), [], [gng])
                if is_sample:
                    dma(S_sb[:], state_gdn[l].rearrange("h k v -> k h v"), [], [S_sb])
                    dma(p_sb[:3, 768:1536], state_conv[l], [], [p_sb])
                    for g in range(12):
                        tr(PS[3][:64, 64 + g * 3:64 + g * 3 + 3], p_sb[:3, 768 + g * 64:768 + (g + 1) * 64], ident[:3, :3],
                           [p_sb, ident], [PS[3]])
                    V("tensor_copy", cbuf[:, :, 0:3], PS[3][:64, 64:100].rearrange("p (a b) -> p a b", b=3),
                      reads=[PS[3]], writes=[cbuf])
                else:
                    V("memset", S_sb[:], 0.0, reads=[], writes=[S_sb])
                    V("memset", cbuf[:, :, 0:3], 0.0, reads=[], writes=[cbuf])

                def attn_block(k_ap, v_ap, kv_reads, nk, R, diag, delta, first):
                    for h in range(4):
                        tr(PS[4][:, h * 128:h * 128 + nk], k_ap[:, h * 128:(h + 1) * 128], ident[:nk, :nk],
                           kv_reads + [ident], [PS[4]])
                    act(kTb[:, :, :nk], PS[4][:, :].rearrange("p (a b) -> p a b", b=128)[:, :, :nk], AF.Copy,
                        [PS[4]], [kTb])
                    V("tensor_copy", vb[:nk, :], v_ap, reads=kv_reads, writes=[vb])
                    for h in range(4):
                        sc = PS[5 + (h % 2)]
                        scv = sc[:nk, 0:256].rearrange("p (m q) -> p m q", m=2)[:, :, :R]
                        mm(scv, kTb[:, h, :nk], Qblk[:, h, :, :R], [kTb, Qblk], [sc], start=True, stop=False)
                        if diag:
                            mm(scv, identb[:nk, :nk], biasd[:nk, h, :, :R], [identb, biasd], [sc], start=False, stop=True)
                            bias_c = 0.0
                        else:
                            mm(scv, kirow[0:1, :nk], sloperow[0:1, h, :, :R], [kirow, sloperow], [sc],
                               start=False, stop=False)
                            mm(scv, ones_rowb[0:1, :nk], nsqi[0:1, h, :, :R], [ones_rowb, nsqi], [sc],
                               start=False, stop=True)
                            bias_c = -SLOPES[h] * delta
                        if bias_c != 0.0:
                            V("tensor_scalar_add", scv, scv, bias_c, reads=[sc], writes=[sc])
                        act(pT[:nk, :, :R], scv, AF.Exp, [sc], [pT])
                        pv = PS[7]
                        for m in range(2):
                            mm(pv[:R, m * 128:(m + 1) * 128], pT[:nk, m, :R], vb[:nk, h * 128:(h + 1) * 128],
                               [pT, vb], [pv])
                            mm(pv[:R, 256 + m:257 + m], pT[:nk, m, :R], onesc[:nk, 0:1], [pT, onesc], [pv])
                        if first:
                            V("tensor_copy", anum[:R, 2 * h:2 * h + 2, :], pv[:R, 0:256].rearrange("p (m d) -> p m d", m=2),
                              reads=[pv], writes=[anum])
                            V("tensor_copy", aden[:R, 2 * h:2 * h + 2], pv[:R, 256:258], reads=[pv], writes=[aden])
                        else:
                            V("tensor_tensor", anum[:R, 2 * h:2 * h + 2, :], anum[:R, 2 * h:2 * h + 2, :],
                              pv[:R, 0:256].rearrange("p (m d) -> p m d", m=2), reads=[pv, anum], writes=[anum], op=ALU.add)
                            V("tensor_tensor", aden[:R, 2 * h:2 * h + 2], aden[:R, 2 * h:2 * h + 2], pv[:R, 256:258],
                              reads=[pv, aden], writes=[aden], op=ALU.add)

                for t in range(nt):
                    R = min(128, T_len - t * 128)
                    load_x(j, t, R, l, is_sample)
                    normalize(xh, x_t, R)
                    for kc in range(8):
                        pp = PS[kc // 4]
                        tr(pp[:, (kc % 4) * 128:(kc % 4) * 128 + R], xh[:R, kc * 128:(kc + 1) * 128], ident[:R, :R],
                           [xh, ident], [pp])
                    for kc in range(8):
                        pp = PS[kc // 4]
                        act(hT[:, kc, :R], pp[:, (kc % 4) * 128:(kc % 4) * 128 + R], AF.Identity, [pp, modT], [hT],
                            scale=modT[:, l, 8 + kc, j:j + 1], bias=modT[:, l, 0 + kc, j:j + 1])
                    for cb in range(7):
                        n0 = cb * 512
                        n1 = min(P_IN, n0 + 512)
                        pb = PS[2 + cb % 2]
                        for kc in range(8):
                            mm(pb[:R, :n1 - n0], hT[:, kc, :R], w_in_sb[:, kc, n0:n1], [hT, w_in_sb], [pb],
                               start=(kc == 0), stop=(kc == 7))
                        if cb % 2 == 0:
                            act(p_sb[:R, n0:n1], pb[:R, :n1 - n0], AF.Copy, [pb], [p_sb])
                        else:
                            V("tensor_copy", p_sb[:R, n0:n1], pb[:R, :n1 - n0], reads=[pb], writes=[p_sb])
                    kdst = nk_s[l, t * 128:t * 128 + R, :] if is_sample else nk_p[l, j, t * 128:t * 128 + R, :]
                    vdst = nv_s[l, t * 128:t * 128 + R, :] if is_sample else nv_p[l, j, t * 128:t * 128 + R, :]
                    dma(kdst, p_sb[:R, 1024:1536], [p_sb], [kvr(l, j, t)], is_out=True)
                    dma(vdst, p_sb[:R, 1536:2048], [p_sb], [kvr(l, j, t)], is_out=True)
                    if t == nt - 1:
                        dst = nconv_s[l, :, :] if is_sample else nconv_p[l, j, :, :]
                        dma(dst, p_sb[R - 3:R, 2048:2816], [p_sb], [], is_out=True)

                    if stages >= 2:
                        V("tensor_tensor", gtmp[:R, :], p_sb[:R, 0:512], p_sb[:R, 0:512], reads=[p_sb], writes=[gtmp], op=ALU.mult)
                        V("tensor_scalar", gtmp[:R, :], gtmp[:R, :], 0.044715, 1.0, reads=[gtmp], writes=[gtmp],
                          op0=ALU.mult, op1=ALU.add)
                        V("tensor_tensor", gtmp[:R, :], gtmp[:R, :], p_sb[:R, 0:512], reads=[gtmp, p_sb], writes=[gtmp], op=ALU.mult)
                        act(gtmp[:R, :], gtmp[:R, :], AF.Sigmoid, [gtmp], [gtmp], scale=2.0 * math.sqrt(2.0 / math.pi))
                        V("tensor_tensor", uv[:R, :], gtmp[:R, :], p_sb[:R, 0:512], reads=[gtmp, p_sb], writes=[uv], op=ALU.mult)
                        v3 = uv[:R, 256:512].rearrange("p (g d) -> p g d", g=4)
                        V("tensor_reduce", vst[:R, :, 0], v3, reads=[uv], writes=[vst], axis=AX.X, op=ALU.add)
                        V("tensor_tensor", vsq[:R, :], uv[:R, 256:512], uv[:R, 256:512], reads=[uv], writes=[vsq], op=ALU.mult)
                        V("tensor_reduce", vst[:R, :, 1], vsq[:R, :].rearrange("p (g d) -> p g d", g=4), reads=[vsq],
                          writes=[vst], axis=AX.X, op=ALU.add)
                        V("tensor_single_scalar", vst[:R, :, :], vst[:R, :, :], 1.0 / 64, reads=[vst], writes=[vst], op=ALU.mult)
                        V("tensor_tensor", vsq[:R, 0:4], vst[:R, :, 0], vst[:R, :, 0], reads=[vst], writes=[vsq], op=ALU.mult)
                        V("tensor_tensor", vst[:R, :, 1], vst[:R, :, 1], vsq[:R, 0:4], reads=[vst, vsq], writes=[vst], op=ALU.subtract)
                        rsqrt(vst[:R, :, 1], vst[:R, :, 1], 1e-5, [vst], vst)
                        vn3 = vn[:R, :].rearrange("p (g d) -> p g d", g=4)
                        V("tensor_tensor", vn3, v3, vst[:R, :, 0:1].to_broadcast([R, 4, 64]), reads=[uv, vst], writes=[vn], op=ALU.subtract)
                        V("tensor_tensor", vn3, vn3, vst[:R, :, 1:2].to_broadcast([R, 4, 64]), reads=[vn, vst], writes=[vn], op=ALU.mult)
                        V("tensor_tensor", vn[:R, :], vn[:R, :], sg_g[:R, :], reads=[vn, sg_g], writes=[vn], op=ALU.mult)
                        V("tensor_tensor", vn[:R, :], vn[:R, :], sg_b[:R, :], reads=[vn, sg_b], writes=[vn], op=ALU.add)
                        if is_sample:
                            dma(nsgu_s[l, t * 128:t * 128 + R, :], vn[:R, :], [vn], [], is_out=True)
                        for g in range(4):
                            mm(PS[0][:R, g * 64:(g + 1) * 64], wTs[:R, g, :R], vn[:R, g * 64:(g + 1) * 64], [wTs, vn], [PS[0]])
                        for g in range(4):
                            V("scalar_tensor_tensor", mix[:R, g * 64:(g + 1) * 64], PS[0][:R, g * 64:(g + 1) * 64],
                              bsT[:R, g:g + 1], uv[:R, g * 64:(g + 1) * 64], reads=[PS[0], bsT, uv], writes=[mix],
                              op0=ALU.add, op1=ALU.mult)
                    else:
                        V("memset", mix[:R, 0:256], 0.0, reads=[], writes=[mix])

                    if stages >= 3:
                        for h in range(4):
                            pq = PS[4 + (h % 2)]
                            for kc in range(8):
                                mm(pq[:, :R], w_in_sb[:, kc, 512 + h * 128:512 + (h + 1) * 128], hT[:, kc, :R],
                                   [w_in_sb, hT], [pq], start=(kc == 0), stop=(kc == 7))
                            act(Qblk[0:64, h, 0, :R], pq[0:64, :R], AF.Copy, [pq], [Qblk], scale=0.125)
                            act(Qblk[64:128, h, 1, :R], pq[64:128, :R], AF.Copy, [pq], [Qblk], scale=0.125)
                        qpos0 = (PAST if is_sample else 0) + t * 128
                        nblk = 0
                        if is_sample:
                            for kb in range(PAST // 128):
                                ks, vs = kst[nblk % 2], vstg[nblk % 2]
                                dma(ks[:], cache_k[l, kb * 128:(kb + 1) * 128, :], [], [ks])
                                dma(vs[:], cache_v[l, kb * 128:(kb + 1) * 128, :], [], [vs])
                                attn_block(ks[:, :], vs[:, :], [ks, vs], 128, R, False, qpos0 - kb * 128, nblk == 0)
                                nblk += 1
                        for kb in range(t):
                            ks, vs = kst[nblk % 2], vstg[nblk % 2]
                            ksrc = nk_s[l, kb * 128:(kb + 1) * 128, :] if is_sample else nk_p[l, j, kb * 128:(kb + 1) * 128, :]
                            vsrc = nv_s[l, kb * 128:(kb + 1) * 128, :] if is_sample else nv_p[l, j, kb * 128:(kb + 1) * 128, :]
                            dma(ks[:], ksrc, [kvr(l, j, kb)], [ks])
                            dma(vs[:], vsrc, [kvr(l, j, kb)], [vs])
                            attn_block(ks[:, :], vs[:, :], [ks, vs], 128, R, False, (t - kb) * 128, nblk == 0)
                            nblk += 1
                        attn_block(p_sb[:R, 1024:1536], p_sb[:R, 1536:2048], [p_sb], R, R, True, 0, nblk == 0)
                        V("reciprocal", aden[:R, :], aden[:R, :], reads=[aden], writes=[aden])
                        V("tensor_tensor", anum[:R, :, :], anum[:R, :, :], aden[:R, :].unsqueeze(2).to_broadcast([R, 8, 128]),
                          reads=[anum, aden], writes=[anum], op=ALU.mult)
                        a4 = anum[:R, :, :].rearrange("p (h m) d -> p h m d", m=2)
                        V("scalar_tensor_tensor", aout[:R, :, :], a4[:, :, 1, :], nlam[:R, 0:1], a4[:, :, 0, :],
                          reads=[anum, nlam], writes=[aout], op0=ALU.mult, op1=ALU.add)
                        V("tensor_tensor", asq[:R, :, :], aout[:R, :, :], aout[:R, :, :], reads=[aout], writes=[asq], op=ALU.mult)
                        V("tensor_reduce", ass[:R, :], asq[:R, :, :], reads=[asq], writes=[ass], axis=AX.X, op=ALU.add)
                        rsqrt(ass[:R, :], ass[:R, :], 1e-5, [ass], ass, scale=1.0 / 128)
                        V("tensor_tensor", aout[:R, :, :], aout[:R, :, :], ass[:R, :].unsqueeze(2).to_broadcast([R, 4, 128]),
                          reads=[aout, ass], writes=[aout], op=ALU.mult)
                        V("tensor_tensor", mix[:R, 256:768].rearrange("p (h d) -> p h d", h=4), aout[:R, :, :],
                          dng[:R, :].unsqueeze(1).to_broadcast([R, 4, 128]), reads=[aout, dng], writes=[mix], op=ALU.mult)
                    else:
                        V("memset", mix[:R, 256:768], 0.0, reads=[], writes=[mix])

                    if stages >= 4:
                        for g in range(12):
                            pg = PS[4 + (g // 4)]
                            col = 2048 + g * 64
                            for kc in range(8):
                                mm(pg[:64, (g % 4) * 128:(g % 4) * 128 + R], w_in_sb[:, kc, col:col + 64], hT[:, kc, :R],
                                   [w_in_sb, hT], [pg], start=(kc == 0), stop=(kc == 7))
                        for q3 in range(3):
                            act(cbuf[:, q3 * 4:(q3 + 1) * 4, 3:3 + R],
                                PS[4 + q3][:64, :].rearrange("p (a b) -> p a b", b=128)[:, :, :R], AF.Copy,
                                [PS[4 + q3]], [cbuf])
                        for jt in range(4):
                            wj = cw[:, :, jt:jt + 1].to_broadcast([64, 12, R])
                            if jt == 0:
                                V("tensor_tensor", cv[:, :, :R], cbuf[:, :, jt:jt + R], wj, reads=[cbuf, cw], writes=[cv], op=ALU.mult)
                            else:
                                V("tensor_tensor", ctmp[:, :, :R], cbuf[:, :, jt:jt + R], wj, reads=[cbuf, cw], writes=[ctmp], op=ALU.mult)
                                V("tensor_tensor", cv[:, :, :R], cv[:, :, :R], ctmp[:, :, :R], reads=[cv, ctmp], writes=[cv], op=ALU.add)
                        V("tensor_copy", ctmp[:, :, 0:3], cbuf[:, :, R:R + 3], reads=[cbuf], writes=[ctmp])
                        V("tensor_copy", cbuf[:, :, 0:3], ctmp[:, :, 0:3], reads=[ctmp], writes=[cbuf])
                        act(cv[:, :, :R], cv[:, :, :R], AF.Silu, [cv], [cv])
                        V("tensor_tensor", ctmp[:, 0:8, :R], cv[:, 0:8, :R], cv[:, 0:8, :R], reads=[cv], writes=[ctmp], op=ALU.mult)
                        for q2 in range(2):
                            mm(PS[4 + q2][:64, :].rearrange("p (a b) -> p a b", b=128)[:, :, :R], ones64[:, :],
                               ctmp[:, q2 * 4:(q2 + 1) * 4, :R], [ones64, ctmp], [PS[4 + q2]])
                            V("tensor_scalar_add", ctmp[:, q2 * 4:(q2 + 1) * 4, :R],
                              PS[4 + q2][:64, :].rearrange("p (a b) -> p a b", b=128)[:, :, :R], 1e-6,
                              reads=[PS[4 + q2]], writes=[ctmp])
                        act(ctmp[:, 0:8, :R], ctmp[:, 0:8, :R], AF.Sqrt, [ctmp], [ctmp])
                        V("reciprocal", ctmp[:, 0:8, :R], ctmp[:, 0:8, :R], reads=[ctmp], writes=[ctmp])
                        V("tensor_tensor", cv[:, 0:8, :R], cv[:, 0:8, :R], ctmp[:, 0:8, :R], reads=[cv, ctmp], writes=[cv], op=ALU.mult)
                        V("tensor_single_scalar", cv[:, 0:4, :R], cv[:, 0:4, :R], 0.125, reads=[cv], writes=[cv], op=ALU.mult)
                        for c0 in range(0, R, 64):
                            c = min(64, R - c0)
                            nsteps = max(0, int(math.ceil(math.log2(c))) - 1)
                            for g in range(8):
                                tr(PS[6][:c, g * 64:(g + 1) * 64], cv[:, 4 + g, c0:c0 + c], ident[:64, :64], [cv, ident], [PS[6]])
                            V("tensor_copy", ktm[:c, :, :], PS[6][:c, 0:256].rearrange("p (a b) -> p a b", b=64), reads=[PS[6]], writes=[ktm])
                            V("tensor_copy", vtm[:c, :, :], PS[6][:c, 256:512].rearrange("p (a b) -> p a b", b=64), reads=[PS[6]], writes=[vtm])
                            for kc in range(8):
                                mm(PS[7][:c, 0:8], hT[:, kc, c0:c0 + c], w_in_sb[:, kc, 3072:3080], [hT, w_in_sb], [PS[7]],
                                   start=(kc == 0), stop=(kc == 7))
                            for kc in range(8):
                                mm(PS[7][:c, 64:320], hT[:, kc, c0:c0 + c], w_in_sb[:, kc, 2816:3072], [hT, w_in_sb], [PS[7]],
                                   start=(kc == 0), stop=(kc == 7))
                            V("tensor_tensor", gab[:c, 0:4], PS[7][:c, 0:4], dtb[:c, :], reads=[PS[7], dtb], writes=[gab], op=ALU.add)
                            act(gab[:c, 0:4], gab[:c, 0:4], AF.Exp, [gab], [gab])
                            V("tensor_scalar_add", gab[:c, 0:4], gab[:c, 0:4], 1.0, reads=[gab], writes=[gab])
                            act(gab[:c, 0:4], gab[:c, 0:4], AF.Ln, [gab], [gab])
                            V("tensor_tensor", gg[:c, :], gab[:c, 0:4], negA[:c, :], reads=[gab, negA], writes=[gg], op=ALU.mult)
                            act(beta[:c, :], PS[7][:c, 4:8], AF.Sigmoid, [PS[7]], [beta])
                            act(z_sb[:c, :], PS[7][:c, 64:320], AF.Silu, [PS[7]], [z_sb])
                            V("tensor_copy", gB[:c, :, :], gg[:c, :].unsqueeze(2).to_broadcast([c, 4, 64]), reads=[gg], writes=[gB])
                            mm(PS[6][:c, 0:4], triU[:c, :c], gg[:c, :], [triU, gg], [PS[6]])
                            for h in range(4):
                                mm(PS[6][:64, 64 + h * 64:64 + h * 64 + c], gB[:c, h, :], triU[:c, :c], [gB, triU], [PS[6]])
                            V("tensor_copy", gc_tm[:c, :], PS[6][:c, 0:4], reads=[PS[6]], writes=[gc_tm])
                            V("tensor_copy", Grow[:, :, :c], PS[6][:64, 64:320].rearrange("p (a b) -> p a b", b=64)[:, :, :c],
                              reads=[PS[6]], writes=[Grow])
                            V("tensor_tensor", D1[:c, :, :c], gc_tm[:c, :].unsqueeze(2).to_broadcast([c, 4, c]), Grow[:c, :, :c],
                              reads=[gc_tm, Grow], writes=[D1], op=ALU.subtract)
                            V("tensor_single_scalar", E1[:c, :, :c], D1[:c, :, :c], 0.0, reads=[D1], writes=[E1], op=ALU.min)
                            act(E1[:c, :, :c], E1[:c, :, :c], AF.Exp, [E1], [E1])
                            V("tensor_scalar", E2[:c, :, :c], D1[:c, :, :c], -1.0, 0.0, reads=[D1], writes=[E2], op0=ALU.mult, op1=ALU.min)
                            act(E2[:c, :, :c], E2[:c, :, :c], AF.Exp, [E2], [E2])
                            act(egc[:c, :], gc_tm[:c, :], AF.Exp, [gc_tm], [egc])
                            act(egrow[:, :, :c], Grow[:, :, :c], AF.Exp, [Grow], [egrow])
                            act(glc[:, :], Grow[:, :, c - 1], AF.Exp, [Grow], [glc])
                            V("tensor_tensor", kdf[:c, :], Grow[:c, :, c - 1], gc_tm[:c, :], reads=[Grow, gc_tm], writes=[kdf], op=ALU.subtract)
                            act(kdf[:c, :], kdf[:c, :], AF.Exp, [kdf], [kdf])
                            for h in range(4):
                                mm(PS[4][:c, h * 64:h * 64 + c], cv[:, 4 + h, c0:c0 + c], cv[:, 4 + h, c0:c0 + c], [cv], [PS[4]])
                                mm(PS[4][:c, 256 + h * 64:256 + h * 64 + c], cv[:, 4 + h, c0:c0 + c], cv[:, h, c0:c0 + c], [cv], [PS[4]])
                            V("tensor_tensor", D1[:c, :, :c], E1[:c, :, :c], maskLs[:c, :c].unsqueeze(1).to_broadcast([c, 4, c]),
                              reads=[E1, maskLs], writes=[D1], op=ALU.mult)
                            A0, B0 = Am[0], Bm[0]
                            V("tensor_tensor", A0[:c, :, :c], PS[4][:c, 0:256].rearrange("p (a b) -> p a b", b=64)[:, :, :c],
                              D1[:c, :, :c], reads=[PS[4], D1], writes=[A0], op=ALU.mult)
                            V("tensor_tensor", A0[:c, :, :c], A0[:c, :, :c], beta[:c, :].unsqueeze(2).to_broadcast([c, 4, c]),
                              reads=[A0, beta], writes=[A0], op=ALU.mult)
                            V("tensor_tensor", E2[:c, :, :c], E2[:c, :, :c], maskU[:c, :c].unsqueeze(1).to_broadcast([c, 4, c]),
                              reads=[E2, maskU], writes=[E2], op=ALU.mult)
                            V("tensor_tensor", aqkT[:c, :, :c], PS[4][:c, 256:512].rearrange("p (a b) -> p a b", b=64)[:, :, :c],
                              E2[:c, :, :c], reads=[PS[4], E2], writes=[aqkT], op=ALU.mult)
                            for h in range(4):
                                tr(PS[5][:c, h * 64:h * 64 + c], A0[:c, h, :c], ident[:c, :c], [A0, ident], [PS[5]])
                            V("tensor_copy", B0[:c, :, :c], PS[5][:c, 0:256].rearrange("p (a b) -> p a b", b=64)[:, :, :c],
                              reads=[PS[5]], writes=[B0])
                            V("tensor_tensor", XT[:c, :, :c], ident[:c, :c].unsqueeze(1).to_broadcast([c, 4, c]), B0[:c, :, :c],
                              reads=[ident, B0], writes=[XT], op=ALU.subtract)
                            cur = 0
                            for n in range(1, nsteps + 1):
                                Ac, Bc, An, Bn = Am[cur], Bm[cur], Am[1 - cur], Bm[1 - cur]
                                for h in range(4):
                                    mm(PS[4][:c, h * 64:h * 64 + c], Bc[:c, h, :c], Ac[:c, h, :c], [Ac, Bc], [PS[4]])
                                    if n < nsteps:
                                        mm(PS[4][:c, 256 + h * 64:256 + h * 64 + c], Ac[:c, h, :c], Bc[:c, h, :c], [Ac, Bc], [PS[4]])
                                V("tensor_copy", An[:c, :, :c], PS[4][:c, 0:256].rearrange("p (a b) -> p a b", b=64)[:, :, :c],
                                  reads=[PS[4]], writes=[An])
                                if n < nsteps:
                                    V("tensor_copy", Bn[:c, :, :c], PS[4][:c, 256:512].rearrange("p (a b) -> p a b", b=64)[:, :, :c],
                                      reads=[PS[4]], writes=[Bn])
                                for h in range(4):
                                    mm(PS[5][:c, h * 64:h * 64 + c], An[:c, h, :c], XT[:c, h, :c], [An, XT], [PS[5]])
                                V("tensor_tensor", XT[:c, :, :c], XT[:c, :, :c], PS[5][:c, 0:256].rearrange("p (a b) -> p a b", b=64)[:, :, :c],
                                  reads=[XT, PS[5]], writes=[XT], op=ALU.add)
                                cur = 1 - cur
                            V("tensor_tensor", Vb[:c, :, :], vtm[:c, :, :], beta[:c, :].unsqueeze(2).to_broadcast([c, 4, 64]),
                              reads=[vtm, beta], writes=[Vb], op=ALU.mult)
                            V("tensor_tensor", Kg[:c, :, :], ktm[:c, :, :], beta[:c, :].unsqueeze(2).to_broadcast([c, 4, 64]),
                              reads=[ktm, beta], writes=[Kg], op=ALU.mult)
                            V("tensor_tensor", Kg[:c, :, :], Kg[:c, :, :], egc[:c, :].unsqueeze(2).to_broadcast([c, 4, 64]),
                              reads=[Kg, egc], writes=[Kg], op=ALU.mult)
                            V("tensor_tensor", kdec[:c, :, :], ktm[:c, :, :], kdf[:c, :].unsqueeze(2).to_broadcast([c, 4, 64]),
                              reads=[ktm, kdf], writes=[kdec], op=ALU.mult)
                            V("tensor_tensor", qgT[:, :, :c], cv[:, 0:4, c0:c0 + c], egrow[:, :, :c], reads=[cv, egrow], writes=[qgT], op=ALU.mult)
                            for h in range(4):
                                mm(PS[6][:c, h * 64:(h + 1) * 64], XT[:c, h, :c], Vb[:c, h, :], [XT, Vb], [PS[6]])
                                mm(PS[6][:64, 256 + h * 64:256 + h * 64 + c], Kg[:c, h, :], XT[:c, h, :c], [Kg, XT], [PS[6]])
                            V("tensor_copy", u_sb[:c, :, :], PS[6][:c, 0:256].rearrange("p (a b) -> p a b", b=64), reads=[PS[6]], writes=[u_sb])
                            V("tensor_copy", wT_sb[:, :, :c], PS[6][:64, 256:512].rearrange("p (a b) -> p a b", b=64)[:, :, :c],
                              reads=[PS[6]], writes=[wT_sb])
                            for h in range(4):
                                mm(PS[5][:c, 256 + h * 64:256 + (h + 1) * 64], wT_sb[:, h, :c], S_sb[:, h, :], [wT_sb, S_sb], [PS[5]])
                            V("tensor_tensor", vnew[:c, :, :], u_sb[:c, :, :], PS[5][:c, 256:512].rearrange("p (a b) -> p a b", b=64),
                              reads=[u_sb, PS[5]], writes=[vnew], op=ALU.subtract)
                            for h in range(4):
                                mm(PS[6][:c, h * 64:(h + 1) * 64], qgT[:, h, :c], S_sb[:, h, :], [qgT, S_sb], [PS[6]], start=True, stop=False)
                                mm(PS[6][:c, h * 64:(h + 1) * 64], aqkT[:c, h, :c], vnew[:c, h, :], [aqkT, vnew], [PS[6]], start=False, stop=True)
                            for h in range(4):
                                mm(PS[5][:64, h * 64:(h + 1) * 64], kdec[:c, h, :], vnew[:c, h, :], [kdec, vnew], [PS[5]])
                            V("tensor_tensor", S_sb[:, :, :], S_sb[:, :, :], glc[:, :].unsqueeze(2).to_broadcast([64, 4, 64]),
                              reads=[S_sb, glc], writes=[S_sb], op=ALU.mult)
                            V("tensor_tensor", S_sb[:, :, :], S_sb[:, :, :], PS[5][:64, 0:256].rearrange("p (a b) -> p a b", b=64),
                              reads=[S_sb, PS[5]], writes=[S_sb], op=ALU.add)
                            V("tensor_copy", o_sb[:c, :, :], PS[6][:c, 0:256].rearrange("p (a b) -> p a b", b=64), reads=[PS[6]], writes=[o_sb])
                            V("tensor_tensor", osq[:c, :, :], o_sb[:c, :, :], o_sb[:c, :, :], reads=[o_sb], writes=[osq], op=ALU.mult)
                            V("tensor_reduce", oss[:c, :], osq[:c, :, :], reads=[osq], writes=[oss], axis=AX.X, op=ALU.add)
                            rsqrt(oss[:c, :], oss[:c, :], 1e-5, [oss], oss, scale=1.0 / 64)
                            V("tensor_tensor", o_sb[:c, :, :], o_sb[:c, :, :], oss[:c, :].unsqueeze(2).to_broadcast([c, 4, 64]),
                              reads=[o_sb, oss], writes=[o_sb], op=ALU.mult)
                            V("tensor_tensor", o_sb[:c, :, :], o_sb[:c, :, :], gng[:c, :].unsqueeze(1).to_broadcast([c, 4, 64]),
                              reads=[o_sb, gng], writes=[o_sb], op=ALU.mult)
                            V("tensor_tensor", o_sb[:c, :, :], o_sb[:c, :, :], z_sb[:c, :].rearrange("p (a b) -> p a b", b=64),
                              reads=[o_sb, z_sb], writes=[o_sb], op=ALU.mult)
                            dma(mix[c0:c0 + c, 768:1024], o_sb[:c, :, :].rearrange("p a b -> p (a b)"), [o_sb], [mix])
                        if t == nt - 1:
                            dst = ngdn_s[l].rearrange("h k v -> k h v") if is_sample else ngdn_p[l, j].rearrange("h k v -> k h v")
                            dma(dst, S_sb[:, :, :], [S_sb], [], is_out=True)
                    else:
                        V("memset", mix[:R, 768:1024], 0.0, reads=[], writes=[mix])

                    for kc in range(8):
                        pp = PS[kc // 4]
                        tr(pp[:, (kc % 4) * 128:(kc % 4) * 128 + R], mix[:R, kc * 128:(kc + 1) * 128], ident[:R, :R],
                           [mix, ident], [pp])
                    for hh in range(2):
                        V("tensor_copy", mixT[:, hh * 4:(hh + 1) * 4, :R],
                          PS[hh][:, :].rearrange("p (a b) -> p a b", b=128)[:, :, :R], reads=[PS[hh]], writes=[mixT])
                    for hh in range(2):
                        for kc in range(8):
                            mm(PS[2 + hh][:R, :], mixT[:, kc, :R], w_out_sb[:, kc, hh * 512:(hh + 1) * 512],
                               [mixT, w_out_sb], [PS[2 + hh]], start=(kc == 0), stop=(kc == 7))
                    residual_ln([PS[2][:R, :], PS[3][:R, :], PS[2], PS[3]], R, x_t)
                    store_x(j, t, R, is_sample, x_t)
                P.barrier()

        def phase_b(l, j, T_len, is_sample):
            nt = (T_len + 127) // 128
            with ExitStack() as sbk:
                def sbb(name, shape, dt=F32):
                    return sb(name, shape, dt, stack=sbk)
                wq_sb = sbb("wq_sb", [128, 8, 2048], BF16)
                keysT = sbb("keysT", [128, 16, 128])
                kraw = sbb("kraw", [128, 128])
                h2T = sbb("h2T", [128, 8, 128]); h2Tb = sbb("h2Tb", [128, 8, 128], BF16)
                h2 = sbb("h2", [128, D])
                qT = sbb("qT", [128, 16, 128])
                sc = sbb("sc", [128, 16, 128]); sc2 = sbb("sc2", [128, 16, 128])
                tops = sbb("tops", [128, 16, 16]); topi = sbb("topi", [128, 16, 16], U32); topf = sbb("topf", [128, 16, 16])
                cand = sc2; cand2 = sc
                bests = sbb("bests", [128, 8, 16]); besti = sbb("besti", [128, 8, 16], U32)
                af = sbb("af", [128, 8, 16]); bf = sbb("bf", [128, 8, 16])
                oh = qT
                idf = sbb("idf", [128, 8, 16]); idf2 = sbb("idf2", [128, 8, 16]); ids = sbb("ids", [128, 128], I32)
                gmax = sbb("gmax", [128, 8]); gsum = sbb("gsum", [128, 8]); gates = sbb("gates", [128, 8, 16])
                dots = sbb("dots", [128, 128]); wts = sbb("wts", [128, 128]); gt2 = sbb("gt2", [128, 128])
                NRING = 12
                gbuf = [sbb(f"gbuf{i}", [128, 2 * D], BF16) for i in range(NRING)]
                xg = sbb("xg", [128, 128])
                dcol = sbb("dcol", [128, 128]); wcol = sbb("wcol", [128, 128])
                junk = sbb("junk", [128, D], BF16)
                facc = sbb("facc", [128, D])
                dg = [sbb(f"dg{i}", [128, 128], BF16) for i in range(2)]

                Gt = sbb("Gt", [128, D])
                gate_tile(l, j, 40, Gt)
                for cb in range(16):
                    n0 = cb * 128
                    w = wst[cb % 2]
                    dma(w[:], peer_wq[l, :, n0:n0 + 128].rearrange("(kc p) n -> p kc n", p=128), [], [w])
                    V("tensor_copy", wq_sb[:, :, n0:n0 + 128], w[:], reads=[w], writes=[wq_sb], eng="pool")
                for c16 in range(16):
                    dma(kraw[:], peer_keys[l, c16], [], [kraw])
                    tr(PS[0][:, 0:128], kraw[:], ident[:], [kraw, ident], [PS[0]])
                    V("tensor_copy", keysT[:, c16, :], PS[0][:, 0:128], reads=[PS[0]], writes=[keysT])
                dma(lng[:], ln2_g[l:l + 1, :].partition_broadcast(128), [], [lng])
                dma(lnb[:], ln2_b[l:l + 1, :].partition_broadcast(128), [], [lnb])

                for t in range(nt):
                    R = min(128, T_len - t * 128)
                    load_x(j, t, R, 1, is_sample)
                    normalize(xh, x_t, R)
                    for kc in range(8):
                        pp = PS[kc // 4]
                        tr(pp[:, (kc % 4) * 128:(kc % 4) * 128 + R], xh[:R, kc * 128:(kc + 1) * 128], ident[:R, :R],
                           [xh, ident], [pp])
                    for kc in range(8):
                        pp = PS[kc // 4]
                        act(h2T[:, kc, :R], pp[:, (kc % 4) * 128:(kc % 4) * 128 + R], AF.Identity, [pp, modT], [h2T],
                            scale=modT[:, l, 32 + kc, j:j + 1], bias=modT[:, l, 24 + kc, j:j + 1])
                    V("tensor_copy", h2Tb[:, :, :R], h2T[:, :, :R], reads=[h2T], writes=[h2Tb])
                    for kc in range(8):
                        pp = PS[2 + kc // 4]
                        tr(pp[:R, (kc % 4) * 128:(kc % 4 + 1) * 128], h2T[:, kc, :R], ident[:, :], [h2T, ident], [pp])
                    for hh in range(2):
                        V("tensor_copy", h2[:R, hh * 512:(hh + 1) * 512], PS[2 + hh][:R, :], reads=[PS[2 + hh]], writes=[h2])
                    for c16 in range(16):
                        pq = PS[4 + (c16 % 2)]
                        for kc in range(8):
                            mm(pq[:, :R], wq_sb[:, kc, c16 * 128:(c16 + 1) * 128], h2Tb[:, kc, :R], [wq_sb, h2Tb], [pq],
                               start=(kc == 0), stop=(kc == 7))
                        act(qT[:, c16, :R], pq[:, :R], AF.Copy, [pq], [qT])
                    for c16 in range(16):
                        pq = PS[6 + (c16 // 4) % 2]
                        mm(pq[:R, (c16 % 4) * 128:(c16 % 4 + 1) * 128], qT[:, c16, :R], keysT[:, c16, :], [qT, keysT], [pq])
                        if c16 % 4 == 3:
                            V("tensor_copy", sc[:R, c16 - 3:c16 + 1, :].rearrange("p a b -> p (a b)"), pq[:R, :], reads=[pq], writes=[sc])
                    for c16 in range(16):
                        V("max", tops[:R, c16, 0:8], sc[:R, c16, :], reads=[sc], writes=[tops])
                        V("max_index", topi[:R, c16, 0:8], tops[:R, c16, 0:8], sc[:R, c16, :], reads=[tops, sc], writes=[topi])
                        V("match_replace", sc2[:R, c16, :], tops[:R, c16, 0:8], sc[:R, c16, :], -1e30, reads=[tops, sc], writes=[sc2])
                        V("max", tops[:R, c16, 8:16], sc2[:R, c16, :], reads=[sc2], writes=[tops])
                        V("max_index", topi[:R, c16, 8:16], tops[:R, c16, 8:16], sc2[:R, c16, :], reads=[tops, sc2], writes=[topi])
                    V("tensor_copy", topf[:R, :, :], topi[:R, :, :], reads=[topi], writes=[topf])
                    t4 = tops[:R, :, :].rearrange("p (h s) k -> p h s k", s=2)
                    f4 = topf[:R, :, :].rearrange("p (h s) k -> p h s k", s=2)
                    candv = cand[:R, :, :].rearrange("p (h s) k -> p h (s k)", s=2)
                    cand2v = cand2[:R, :, :].rearrange("p (h s) k -> p h (s k)", s=2)
                    ohv = oh[:R, :, :].rearrange("p (h a) (b c) -> p h a b c", a=2, c=16)
                    ohv = oh[:R, :, :].rearrange("p a b -> p (a b)").rearrange("p (h j a) -> p h j a", h=8, j=16)
                    c4 = candv.rearrange("p h (a b) -> p h a b", b=16)
                    V("tensor_tensor", c4, t4[:, :, 0, :].unsqueeze(3).to_broadcast([R, 8, 16, 16]),
                      t4[:, :, 1, :].unsqueeze(2).to_broadcast([R, 8, 16, 16]), reads=[tops], writes=[cand], op=ALU.add)
                    for hd in range(8):
                        V("max", bests[:R, hd, 0:8], candv[:, hd, :], reads=[cand], writes=[bests])
                        V("max_index", besti[:R, hd, 0:8], bests[:R, hd, 0:8], candv[:, hd, :], reads=[bests, cand], writes=[besti])
                        V("match_replace", cand2v[:, hd, :], bests[:R, hd, 0:8], candv[:, hd, :], -1e30, reads=[bests, cand], writes=[cand2])
                        V("max", bests[:R, hd, 8:16], cand2v[:, hd, :], reads=[cand2], writes=[bests])
                        V("max_index", besti[:R, hd, 8:16], bests[:R, hd, 8:16], cand2v[:, hd, :], reads=[bests, cand2], writes=[besti])
                    V("tensor_copy", af[:R, :, :], besti[:R, :, :], reads=[besti], writes=[af])
                    V("tensor_copy", bf[:R, :, :], af[:R, :, :], reads=[af], writes=[bf])
                    V("tensor_tensor", ohv, bf[:R, :, :].unsqueeze(3).to_broadcast([R, 8, 16, 16]),
                      thr16[:R, :].unsqueeze(1).unsqueeze(1).to_broadcast([R, 8, 16, 16]), reads=[bf, thr16], writes=[oh], op=ALU.is_ge)
                    V("tensor_reduce", af[:R, :, :], ohv, reads=[oh], writes=[af], axis=AX.X, op=ALU.add)
                    V("scalar_tensor_tensor", bf[:R, :, :], af[:R, :, :], -16.0, bf[:R, :, :], reads=[af, bf], writes=[bf],
                      op0=ALU.mult, op1=ALU.add)
                    io16 = iota_f[:R, 0:16].unsqueeze(1).unsqueeze(1).to_broadcast([R, 8, 16, 16])
                    for sel, half, dstf in ((af, 0, idf), (bf, 1, idf2)):
                        V("tensor_tensor", ohv, sel[:R, :, :].unsqueeze(3).to_broadcast([R, 8, 16, 16]), io16,
                          reads=[sel, iota_f], writes=[oh], op=ALU.is_equal)
                        V("tensor_tensor", ohv, ohv, f4[:, :, half, :].unsqueeze(2).to_broadcast([R, 8, 16, 16]),
                          reads=[oh, topf], writes=[oh], op=ALU.mult)
                        V("tensor_reduce", dstf[:R, :, :], ohv, reads=[oh], writes=[dstf], axis=AX.X, op=ALU.add)
                    V("scalar_tensor_tensor", idf[:R, :, :], idf[:R, :, :], 128.0, idf2[:R, :, :], reads=[idf, idf2], writes=[idf],
                      op0=ALU.mult, op1=ALU.add)
                    V("tensor_scalar_add", idf[:R, :, :], idf[:R, :, :], float(l * 16384), reads=[idf], writes=[idf])
                    V("tensor_copy", ids[:R, :], idf[:R, :, :].rearrange("p a b -> p (a b)"), reads=[idf], writes=[ids])
                    V("tensor_reduce", gmax[:R, :], bests[:R, :, :], reads=[bests], writes=[gmax], axis=AX.X, op=ALU.max)
                    V("tensor_tensor", gates[:R, :, :], bests[:R, :, :], gmax[:R, :].unsqueeze(2).to_broadcast([R, 8, 16]),
                      reads=[bests, gmax], writes=[gates], op=ALU.subtract)
                    act(gates[:R, :, :], gates[:R, :, :], AF.Exp, [gates], [gates])
                    V("tensor_reduce", gsum[:R, :], gates[:R, :, :], reads=[gates], writes=[gsum], axis=AX.X, op=ALU.add)
                    V("reciprocal", gsum[:R, :], gsum[:R, :], reads=[gsum], writes=[gsum])
                    V("tensor_tensor", gates[:R, :, :], gates[:R, :, :], gsum[:R, :].unsqueeze(2).to_broadcast([R, 8, 16]),
                      reads=[gates, gsum], writes=[gates], op=ALU.mult)
                    GS = 4
                    NG = 128 // GS
                    gflat = gates[:R, :, :].rearrange("p a b -> p (a b)")
                    GC = 2.0 * math.sqrt(2.0 / math.pi)

                    def issue(g):
                        for k in range(g * GS, (g + 1) * GS):
                            gb = gbuf[k % NRING]
                            P.op("pool", lambda e, gb=gb, k=k, R=R: e.indirect_dma_start(
                                out=gb[:R, :], out_offset=None, in_=euv[:, :],
                                in_offset=bass.IndirectOffsetOnAxis(ap=ids[:R, k:k + 1], axis=0)),
                                [ids], [gb], dma=True)

                    issue(0)
                    issue(1)
                    for g in range(NG):
                        if g + 2 < NG:
                            issue(g + 2)
                        k0 = g * GS
                        for k in range(k0, k0 + GS):
                            gb = gbuf[k % NRING]
                            V("scalar_tensor_tensor", junk[:R, :], gb[:R, 0:D], 1.0, h2[:R, :], reads=[gb, h2],
                              writes=[junk, dots], op0=ALU.mult, op1=ALU.mult, accum_out=dots[:R, k:k + 1])
                        ds = dots[:R, k0:k0 + GS]
                        tt = gt2[:R, k0:k0 + GS]
                        V("tensor_tensor", tt, ds, ds, reads=[dots], writes=[gt2], op=ALU.mult)
                        V("tensor_scalar", tt, tt, 0.044715, 1.0, reads=[gt2], writes=[gt2], op0=ALU.mult, op1=ALU.add)
                        V("tensor_tensor", tt, tt, ds, reads=[gt2, dots], writes=[gt2], op=ALU.mult)
                        act(tt, tt, AF.Sigmoid, [gt2], [gt2], scale=GC)
                        V("tensor_tensor", xg[:R, k0:k0 + GS], ds, gflat[:, k0:k0 + GS], reads=[dots, gates], writes=[xg], op=ALU.mult)
                        V("tensor_tensor", wts[:R, k0:k0 + GS], tt, xg[:R, k0:k0 + GS], reads=[gt2, xg], writes=[wts], op=ALU.mult)
                        for k in range(k0, k0 + GS):
                            gb = gbuf[k % NRING]
                            d = dg[k % 2]
                            V("tensor_scalar", d[:R, :R], identb[:R, :R], wts[:R, k:k + 1], None, reads=[identb, wts], writes=[d], op0=ALU.mult)
                            for hh in range(2):
                                mm(PS[6 + hh][:R, :], d[:R, :R], gb[:R, D + hh * 512:D + (hh + 1) * 512], [d, gb], [PS[6 + hh]],
                                   start=(k == 0), stop=(k == 127))
                    for hh in range(2):
                        V("tensor_tensor", facc[:R, hh * 512:(hh + 1) * 512], PS[6 + hh][:R, :], Gt[:R, hh * 512:(hh + 1) * 512],
                          reads=[PS[6 + hh], Gt], writes=[facc], op=ALU.mult)
                    residual_ln([facc[:R, 0:512], facc[:R, 512:1024], facc, facc], R, x_t)
                    store_x(j, t, R, is_sample, x_t)
                P.barrier()

        order = [NP] + list(range(NP))
        for j in order:
            is_sample = (j == NP)
            T_len = TS if is_sample else SEQ
            for l in range(L):
                phase_a(l, j, T_len, is_sample)
                if stages >= 5:
                    phase_b(l, j, T_len, is_sample)

        P.emit()
        print("n_ops", P.n_ops, {e: len(v) for e, v in P.ops.items()}, flush=True)
    return nc


WEIGHT_NAMES = ["w_ada", "b_ada", "w_in", "sgu_ln_g", "sgu_ln_b", "sgu_w", "sgu_b", "lam_q1", "lam_k1",
                "lam_q2", "lam_k2", "diff_norm_g", "conv_w", "gdn_a_log", "gdn_dt_bias", "gdn_norm_g",
                "w_out", "ln1_g", "ln1_b", "peer_wq", "peer_keys", "expert_u", "expert_v", "ln2_g", "ln2_b"]


def run(cfg, inputs, stages=99):
    L = cfg.DEPTH
    NP = cfg.BATCH // NCORES
    f = lambda a: np.ascontiguousarray(np.asarray(a, dtype=np.float32))
    W = {k: f(inputs[k]) for k in WEIGHT_NAMES}
    W["sgu_ln_g"] = W["sgu_ln_g"].reshape(L, 256)
    W["sgu_ln_b"] = W["sgu_ln_b"].reshape(L, 256)
    W["peer_keys"] = W["peer_keys"].reshape(L, 16, 128, 128)
    W["expert_u"] = W["expert_u"].reshape(L * 16384, cfg.D)
    W["expert_v"] = W["expert_v"].reshape(L * 16384, cfg.D)
    xp = f(inputs["x_prompt"]); xs = f(inputs["x_sample"])
    ck = f(inputs["cache_k"]); cv = f(inputs["cache_v"])
    sg = f(inputs["state_gdn"]); sc = f(inputs["state_conv"])
    cp = f(inputs["c_prompt"]); cs = f(inputs["c_sample"])
    in_maps = []
    for i in range(NCORES):
        m = dict(W)
        m["xp"] = xp[i * NP:(i + 1) * NP]
        m["xs"] = xs[i]
        m["cache_k"] = f(ck[:, i].reshape(L, cfg.PAST, 512))
        m["cache_v"] = f(cv[:, i].reshape(L, cfg.PAST, 512))
        m["state_gdn"] = f(sg[:, i])
        m["state_conv"] = f(sc[:, i])
        m["c_in"] = f(np.concatenate([cp[i * NP:(i + 1) * NP], cs[i:i + 1]], axis=0))
        in_maps.append(m)
    import time as _time
    _t0 = _time.time()
    nc = build(cfg, stages)
    print("build s", _time.time() - _t0, flush=True)
    _t0 = _time.time()
    import os as _os
    if _os.environ.get("K_TRACE"):
        res = run_bass_kernel_spmd(nc, in_maps, core_ids=list(range(NCORES)), trace=True)
        print("EXEC_TIME_NS", res.exec_time_ns, flush=True)
    else:
        res = run_bass_kernel_spmd(nc, in_maps, core_ids=list(range(NCORES)))
    print("run_spmd s", _time.time() - _t0, flush=True)
    R = res.results
    cat = lambda k, ax: np.concatenate([r[k] for r in R], axis=ax)
    stk = lambda k, ax: np.stack([r[k] for r in R], axis=ax)
    S = cfg.SEQ
    y_prompt = cat("y_p", 0)
    y_sample = stk("y_s", 0)
    nk_p = cat("nk_p", 1).reshape(L, cfg.BATCH, S, 4, 128)
    nv_p = cat("nv_p", 1).reshape(L, cfg.BATCH, S, 4, 128)
    ngdn_p = cat("ngdn_p", 1)
    nconv_p = cat("nconv_p", 1)
    nk_s = stk("nk_s", 1).reshape(L, cfg.DEC_BATCH, cfg.DEC_SEQ, 4, 128)
    nv_s = stk("nv_s", 1).reshape(L, cfg.DEC_BATCH, cfg.DEC_SEQ, 4, 128)
    ngdn_s = stk("ngdn_s", 1)
    nconv_s = stk("nconv_s", 1)
    nsgu_s = stk("nsgu_s", 1)
    return (y_prompt, y_sample, nk_p, nv_p, ngdn_p, nconv_p, nk_s, nv_s, ngdn_s, nconv_s, nsgu_s)


def kernel(**inputs):
    return run(Cfg(), inputs)
```

```python
import math
import numpy as np
import concourse.bass as bass
import concourse.mybir as mybir
from concourse.bass_utils import run_bass_kernel_spmd

F32 = mybir.dt.float32
BF16 = mybir.dt.bfloat16
I32 = mybir.dt.int32
U32 = mybir.dt.uint32
AF = mybir.ActivationFunctionType
ALU = mybir.AluOpType
AX = mybir.AxisListType

NCORES = 8


class Cfg:
    def __init__(self, **kw):
        self.D = 1024
        self.BATCH = 32
        self.SEQ = 2048
        self.DEPTH = 4
        self.DEC_BATCH = 8
        self.DEC_SEQ = 16
        self.PAST = 4096
        for k, v in kw.items():
            setattr(self, k, v)
        self.ALPHA = (2 * self.DEPTH) ** 0.25


class Res:
    __slots__ = ("name", "lw", "rd")

    def __init__(self, name=""):
        self.name = name
        self.lw = None
        self.rd = {}


class T:
    def __init__(self, handle, name, nres=1):
        self.h = handle
        self.name = name
        self.res = [Res(f"{name}.{i}") for i in range(nres)]

    def __getitem__(self, idx):
        return self.h[idx]

    @property
    def r(self):
        return self.res[0]


COMPUTE = ("pe", "act", "dve", "pool")
NDMASEM = 24


class Prog:
    def __init__(self, nc, stack):
        self.nc = nc
        self.stack = stack
        self.ops = {e: [] for e in ("pe", "act", "dve", "pool", "sp")}
        self.sem = {}
        for e in COMPUTE:
            self.sem[e] = stack.enter_context(nc.semaphore(f"s_{e}"))
        self.cnt = {e: 0 for e in COMPUTE}
        self.dsem = {}
        self.dcnt = {}
        self.dnext = {}
        for q in ("sp", "pool"):
            self.dsem[q] = [stack.enter_context(nc.semaphore(f"d_{q}{i}")) for i in range(NDMASEM)]
            self.dcnt[q] = [0] * NDMASEM
            self.dnext[q] = 0
        self.known = {e: {} for e in self.ops}
        self.out_tokens = []
        self.n_ops = 0

    def _need(self, eng, tok, waits):
        if tok is None:
            return
        key, val = tok[0], tok[1]
        if self.known[eng].get(key, 0) >= val:
            return
        self.known[eng][key] = val
        waits.append((key, val))

    def barrier(self):
        toks = []
        for c in COMPUTE:
            if self.cnt[c] > 0:
                toks.append((("c", c), self.cnt[c], c, False))
        for q in ("sp", "pool"):
            for slot in range(NDMASEM):
                if self.dcnt[q][slot] > 0:
                    toks.append((("d", q, slot), self.dcnt[q][slot], q, True))
        for e in self.ops:
            waits = []
            for tk in toks:
                self._need(e, tk, waits)
            if waits:
                self.ops[e].append((waits, None, None))

    def op(self, eng, fn, reads=(), writes=(), dma=False, is_out=False):
        waits = []
        for r in reads:
            rr = r.r if isinstance(r, T) else r
            if rr.lw is not None:
                if rr.lw[2] == eng and eng == "pe" and not dma and rr.lw[3] is False:
                    pass
                else:
                    self._need(eng, rr.lw, waits)
        for w in writes:
            ww = w.r if isinstance(w, T) else w
            if ww.lw is not None:
                if not (ww.lw[2] == eng and not dma and ww.lw[3] is False):
                    self._need(eng, ww.lw, waits)
            for tk in ww.rd.values():
                if not (tk[2] == eng and not dma and tk[3] is False):
                    self._need(eng, tk, waits)
        if dma:
            q = eng
            slot = self.dnext[q]
            self.dnext[q] = (slot + 1) % NDMASEM
            key = ("d", q, slot)
            prev = self.dcnt[q][slot]
            if prev > 0:
                self._need(eng, (key, prev, q, True), waits)
            self.dcnt[q][slot] = prev + 16
            tok = (key, prev + 16, q, True)
            inc = (self.dsem[q][slot], 16)
        else:
            self.cnt[eng] += 1
            tok = (("c", eng), self.cnt[eng], eng, False)
            inc = (self.sem[eng], 1)
        self.ops[eng].append((waits, fn, inc))
        for r in reads:
            rr = r.r if isinstance(r, T) else r
            rr.rd[tok[0]] = tok
        for w in writes:
            ww = w.r if isinstance(w, T) else w
            ww.lw = tok
            ww.rd = {}
        if is_out:
            self.out_tokens.append(tok)
        self.n_ops += 1
        return tok

    def semobj(self, key):
        if key[0] == "c":
            return self.sem[key[1]]
        return self.dsem[key[1]][key[2]]

    def emit(self):
        nc = self.nc
        final_waits = []
        for tok in self.out_tokens:
            self._need("sp", tok, final_waits)
        for q in ("sp", "pool"):
            for slot in range(NDMASEM):
                if self.dcnt[q][slot] > 0:
                    self._need("sp", (("d", q, slot), self.dcnt[q][slot], q, True), final_waits)
        engmap = {"pe": "tensor", "act": "scalar", "dve": "vector", "pool": "gpsimd", "sp": "sync"}
        with nc.Block() as block:
            for e, attr in engmap.items():
                ops = self.ops[e]
                extra = final_waits if e == "sp" else []

                def body(eng, ops=ops, extra=extra):
                    for waits, fn, inc in ops:
                        for key, val in waits:
                            eng.wait_ge(self.semobj(key), val)
                        if fn is None:
                            continue
                        ins = fn(eng)
                        ins.then_inc(inc[0], inc[1])
                    for key, val in extra:
                        eng.wait_ge(self.semobj(key), val)

                getattr(block, attr)(body)


def build(cfg, stages=99):
    from contextlib import ExitStack
    D, L = cfg.D, cfg.DEPTH
    NP = cfg.BATCH // NCORES
    SEQ, TS, PAST = cfg.SEQ, cfg.DEC_SEQ, cfg.PAST
    NS = NP + 1
    P_IN = 3080
    ALPHA = cfg.ALPHA
    SLOPES = [2.0 ** (-(8.0 / 4) * (h + 1)) for h in range(4)]
    NEG = -30000.0
    nc = bass.Bass("TRN2", target_bir_lowering=False)

    def din(name, shape, dt=F32):
        return nc.dram_tensor(name, list(shape), dt, kind="ExternalInput")

    def dout(name, shape, dt=F32):
        return nc.dram_tensor(name, list(shape), dt, kind="ExternalOutput")

    xp = din("xp", [NP, SEQ, D]); xs = din("xs", [TS, D])
    cache_k = din("cache_k", [L, PAST, 512]); cache_v = din("cache_v", [L, PAST, 512])
    state_gdn = din("state_gdn", [L, 4, 64, 64]); state_conv = din("state_conv", [L, 3, 768])
    c_in = din("c_in", [NS, D])
    w_ada = din("w_ada", [L, D, 6 * D]); b_ada = din("b_ada", [L, 6 * D])
    w_in = din("w_in", [L, D, P_IN])
    sgu_ln_g = din("sgu_ln_g", [L, 256]); sgu_ln_b = din("sgu_ln_b", [L, 256])
    sgu_w = din("sgu_w", [L, 4, 128, 128]); sgu_b = din("sgu_b", [L, 4, 128])
    lam_q1 = din("lam_q1", [L, 64]); lam_k1 = din("lam_k1", [L, 64])
    lam_q2 = din("lam_q2", [L, 64]); lam_k2 = din("lam_k2", [L, 64])
    diff_norm_g = din("diff_norm_g", [L, 128])
    conv_w = din("conv_w", [L, 4, 768])
    gdn_a_log = din("gdn_a_log", [L, 4]); gdn_dt_bias = din("gdn_dt_bias", [L, 4])
    gdn_norm_g = din("gdn_norm_g", [L, 64])
    w_out = din("w_out", [L, D, D])
    ln1_g = din("ln1_g", [L, D]); ln1_b = din("ln1_b", [L, D])
    peer_wq = din("peer_wq", [L, D, 2048])
    peer_keys = din("peer_keys", [L, 16, 128, 128])
    expert_u = din("expert_u", [L * 16384, D]); expert_v = din("expert_v", [L * 16384, D])
    ln2_g = din("ln2_g", [L, D]); ln2_b = din("ln2_b", [L, D])

    euv = nc.dram_tensor("euv_bf", [L * 16384, 2 * D], BF16, kind="Internal")
    y_p = dout("y_p", [NP, SEQ, D]); y_s = dout("y_s", [TS, D])
    nk_p = dout("nk_p", [L, NP, SEQ, 512]); nv_p = dout("nv_p", [L, NP, SEQ, 512])
    ngdn_p = dout("ngdn_p", [L, NP, 4, 64, 64]); nconv_p = dout("nconv_p", [L, NP, 3, 768])
    nk_s = dout("nk_s", [L, TS, 512]); nv_s = dout("nv_s", [L, TS, 512])
    ngdn_s = dout("ngdn_s", [L, 4, 64, 64]); nconv_s = dout("nconv_s", [L, 3, 768])
    nsgu_s = dout("nsgu_s", [L, TS, 256])

    with ExitStack() as st:
        P = Prog(nc, st)

        uid = [0]

        def sb(name, shape, dt=F32, stack=None):
            uid[0] += 1
            return T((stack or st).enter_context(nc.sbuf_tensor(f"{name}_{uid[0]}", list(shape), dt)), name)

        def ps(name, shape=(128, 512), dt=F32):
            return T(st.enter_context(nc.psum_tensor(name, list(shape), dt)), name)

        PS = [ps(f"ps{i}") for i in range(8)]

        def dma(out, in_, reads=(), writes=(), q="sp", is_out=False, **kw):
            P.op(q, lambda e: e.dma_start(out=out, in_=in_, **kw), reads, writes, dma=True, is_out=is_out)

        def mm(out, lhsT, rhs, reads, writes, start=True, stop=True, **kw):
            P.op("pe", lambda e: e.matmul(out, lhsT, rhs, start=start, stop=stop, **kw), reads, writes)

        def tr(out, in_, ident_ap, reads, writes):
            P.op("pe", lambda e: e.transpose(out, in_, ident_ap), reads, writes)

        def act(out, in_, func, reads, writes, **kw):
            P.op("act", lambda e: e.activation(out=out, in_=in_, func=func, **kw), reads, writes)

        def V(method, *args, reads=(), writes=(), eng="dve", **kw):
            P.op(eng, lambda e: getattr(e, method)(*args, **kw), reads, writes)

        def rsqrt(dst, src, eps, reads, dst_t, scale=1.0):
            V("tensor_scalar", dst, src, scale, eps, reads=reads, writes=[dst_t], op0=ALU.mult, op1=ALU.add)
            act(dst, dst, AF.Sqrt, [dst_t], [dst_t])
            V("reciprocal", dst, dst, reads=[dst_t], writes=[dst_t])

        def bcast_row(dram_row_ap, n):
            return dram_row_ap.partition_broadcast(128) if hasattr(dram_row_ap, "partition_broadcast") else dram_row_ap

        ident = sb("ident", [128, 128])
        identb = sb("identb", [128, 128], BF16)
        iota_p = sb("iota_p", [128, 1])
        iota_f = sb("iota_f", [128, 128])
        dif = sb("dif", [128, 128])
        triU = sb("triU", [128, 128])
        maskL = sb("maskL", [64, 64])
        maskLs = sb("maskLs", [64, 64])
        maskU = sb("maskU", [64, 64])
        ones64 = sb("ones64", [64, 64])
        ones_row = sb("ones_row", [1, 128])
        ones_rowb = sb("ones_rowb", [1, 128], BF16)
        kirow = sb("kirow", [1, 128], BF16)
        sloperow = sb("sloperow", [1, 4, 2, 128], BF16)
        nsqi = sb("nsqi", [1, 4, 2, 128], BF16)
        biasd = sb("biasd", [128, 4, 2, 128], BF16)
        tmpc = sb("tmpc", [128, 128])
        thr16 = sb("thr16", [128, 16])
        V("iota", iota_p[:], [[0, 1]], reads=[], writes=[iota_p], eng="pool", base=0, channel_multiplier=1,
          allow_small_or_imprecise_dtypes=True)
        V("iota", iota_f[:], [[1, 128]], reads=[], writes=[iota_f], eng="pool", base=0, channel_multiplier=0,
          allow_small_or_imprecise_dtypes=True)
        V("tensor_scalar", dif[:], iota_f[:], iota_p[:, 0:1], None, reads=[iota_f, iota_p], writes=[dif],
          op0=ALU.subtract)
        V("tensor_single_scalar", ident[:], dif[:], 0.0, reads=[dif], writes=[ident], op=ALU.is_equal)
        V("tensor_copy", identb[:], ident[:], reads=[ident], writes=[identb])
        V("tensor_single_scalar", triU[:], dif[:], 0.0, reads=[dif], writes=[triU], op=ALU.is_ge)
        V("tensor_single_scalar", maskU[:], dif[:64, :64], 0.0, reads=[dif], writes=[maskU], op=ALU.is_ge)
        V("tensor_single_scalar", maskL[:], dif[:64, :64], 0.0, reads=[dif], writes=[maskL], op=ALU.is_le)
        V("tensor_single_scalar", maskLs[:], dif[:64, :64], 0.0, reads=[dif], writes=[maskLs], op=ALU.is_lt)
        V("tensor_scalar", thr16[:], iota_f[:, 0:16], 1.0, 16.0, reads=[iota_f], writes=[thr16], op0=ALU.add, op1=ALU.mult)
        V("memset", ones64[:], 1.0, reads=[], writes=[ones64])
        V("memset", ones_row[:], 1.0, reads=[], writes=[ones_row])
        V("memset", ones_rowb[:], 1.0, reads=[], writes=[ones_rowb])
        V("tensor_copy", kirow[:], iota_f[0:1, :], reads=[iota_f], writes=[kirow])
        mk = sb("mk", [128, 128])
        V("tensor_single_scalar", mk[:], iota_f[:], 64.0, reads=[iota_f], writes=[mk], op=ALU.is_lt)
        V("tensor_single_scalar", tmpc[:], iota_p[:].to_broadcast([128, 128]), 64.0, reads=[iota_p], writes=[tmpc],
          op=ALU.is_ge)
        V("tensor_tensor", mk[:], mk[:], tmpc[:], reads=[mk, tmpc], writes=[mk], op=ALU.mult)
        V("tensor_single_scalar", mk[:], mk[:], NEG, reads=[mk], writes=[mk], op=ALU.mult)
        V("scalar_tensor_tensor", tmpc[:], dif[:], -1.0, dif[:], reads=[dif], writes=[tmpc], op0=ALU.mult, op1=ALU.max)
        NDD = max(PAST // 128, SEQ // 128) + 1
        kbias = sb("kbias", [128, 4, NDD])
        mk2 = sb("mk2", [128, 128])
        for h in range(4):
            for dd in range(NDD):
                V("tensor_scalar", kbias[:, h, dd:dd + 1], iota_p[:, 0:1], SLOPES[h], -SLOPES[h] * 128.0 * dd,
                  reads=[iota_p], writes=[kbias], op0=ALU.mult, op1=ALU.add)
            V("scalar_tensor_tensor", mk2[:], iota_f[:], SLOPES[h], mk[:], reads=[iota_f, mk], writes=[mk2],
              op0=ALU.mult, op1=ALU.add)
            for m in range(2):
                V("scalar_tensor_tensor", biasd[:, h, m, :], tmpc[:], -SLOPES[h], mk2[:], reads=[tmpc, mk2],
                  writes=[biasd], op0=ALU.mult, op1=ALU.add)

        modT = sb("modT", [128, L, 48, NS])
        with ExitStack() as s0:
            c_sb = sb("c_sb", [NS, D], stack=s0)
            cT = sb("cT", [128, 8, NS], stack=s0)
            wst = [sb(f"wst{i}", [128, 8, 512], stack=s0) for i in range(2)]
            brow = sb("brow", [1, 6 * D], stack=s0)
            dma(c_sb[:], c_in[:, :], [], [c_sb])
            act(c_sb[:], c_sb[:], AF.Silu, [c_sb], [c_sb])
            for kc in range(8):
                tr(PS[0][:, kc * NS:(kc + 1) * NS], c_sb[:NS, kc * 128:(kc + 1) * 128], ident[:NS, :NS],
                   [c_sb, ident], [PS[0]])
            V("tensor_copy", cT[:].rearrange("p a b -> p (a b)"), PS[0][:, 0:8 * NS], reads=[PS[0]], writes=[cT])
            for l in range(L):
                dma(brow[:], b_ada[l:l + 1, :], [], [brow])
                for cb in range(12):
                    w = wst[cb % 2]
                    dma(w[:], w_ada[l, :, cb * 512:(cb + 1) * 512].rearrange("(kc p) n -> p kc n", p=128), [], [w])
                    pt = PS[cb % 2]
                    for f4 in range(4):
                        fc = cb * 4 + f4
                        for kc in range(8):
                            mm(pt[:, f4 * NS:(f4 + 1) * NS], w[:, kc, f4 * 128:(f4 + 1) * 128], cT[:, kc, :],
                               [w, cT], [pt], start=(kc == 0), stop=False)
                        mm(pt[:, f4 * NS:(f4 + 1) * NS], brow[0:1, fc * 128:(fc + 1) * 128], ones_row[0:1, :NS],
                           [brow, ones_row], [pt], start=False, stop=True)
                    V("tensor_copy", modT[:, l, cb * 4:(cb + 1) * 4, :].rearrange("p a b -> p (a b)"),
                      pt[:, 0:4 * NS], reads=[pt], writes=[modT])
                for c0 in (8, 32):
                    V("tensor_scalar_add", modT[:, l, c0:c0 + 8, :], modT[:, l, c0:c0 + 8, :], 1.0,
                      reads=[modT], writes=[modT])
            P.barrier()

        with ExitStack() as sc:
            cin = [sb(f"cin{i}", [128, 4096], stack=sc) for i in range(2)]
            cout = [sb(f"cout{i}", [128, 4096], BF16, stack=sc) for i in range(2)]
            nchunk = L * 16384 // 512
            ci = 0
            for (src, half) in ((expert_u, 0), (expert_v, 1)):
                sv = src[:, :].rearrange("(c p r) d -> c p (r d)", p=128, r=4)
                dv = euv[:, half * D:(half + 1) * D].rearrange("(c p r) d -> c p r d", p=128, r=4)
                for c in range(nchunk):
                    a, b = cin[ci % 2], cout[ci % 2]
                    dma(a[:], sv[c], [], [a])
                    eng = ("act", "dve", "pool")[ci % 3]
                    if eng == "act":
                        act(b[:], a[:], AF.Copy, [a], [b])
                    else:
                        V("tensor_copy", b[:], a[:], reads=[a], writes=[b], eng=eng)
                    dma(dv[c], b[:].rearrange("p (r d) -> p r d", r=4), [b], [])
                    ci += 1
            P.barrier()

        wst = [sb(f"wstg{i}", [128, 8, 128]) for i in range(2)]
        x_t = sb("x_t", [128, D])
        xh = sb("xh", [128, D])
        stats = sb("stats", [128, 2, 6])
        mv = sb("mv", [128, 2])
        rstd = sb("rstd", [128, 1])
        lng = sb("lng", [128, D]); lnb = sb("lnb", [128, D])
        xres = {}

        def xr(j, t):
            return xres.setdefault((j, t), Res(f"x{j}.{t}"))

        kvres = {}

        def kvr(l, j, t):
            return kvres.setdefault((l, j, t), Res(f"kv{l}.{j}.{t}"))

        def ln_stats(src_ap, R, src_res):
            for i in range(2):
                V("bn_stats", stats[:R, i, :], src_ap[:, i * 512:(i + 1) * 512], reads=[src_res], writes=[stats])
            V("bn_aggr", mv[:R, :], stats[:R, :, :], reads=[stats], writes=[mv])
            rsqrt(rstd[:R, :], mv[:R, 1:2], 1e-5, [mv], rstd)

        def normalize(dst_t, src_t, R):
            ln_stats(src_t[:R, :], R, src_t)
            V("tensor_scalar", dst_t[:R, :], src_t[:R, :], mv[:R, 0:1], rstd[:R, 0:1], reads=[src_t, mv, rstd],
              writes=[dst_t], op0=ALU.subtract, op1=ALU.mult)

        def gate_tile(l, j, c0, Gt):
            for kc in range(8):
                pp = PS[kc // 4]
                V("tensor_copy", tmpc[:], modT[:, l, c0 + kc, j:j + 1].to_broadcast([128, 128]), reads=[modT], writes=[tmpc])
                mm(pp[:, (kc % 4) * 128:(kc % 4 + 1) * 128], tmpc[:], ident[:], [tmpc, ident], [pp])
            for hh in range(2):
                V("tensor_copy", Gt[:, hh * 512:(hh + 1) * 512], PS[hh][:, :], reads=[PS[hh]], writes=[Gt])

        def load_x(j, t, R, l, is_sample):
            if is_sample:
                src = xs[t * 128:t * 128 + R, :] if l == 0 else y_s[t * 128:t * 128 + R, :]
            else:
                src = xp[j, t * 128:t * 128 + R, :] if l == 0 else y_p[j, t * 128:t * 128 + R, :]
            dma(x_t[:R, :], src, [xr(j, t)], [x_t])

        def store_x(j, t, R, is_sample, src_t):
            dst = y_s[t * 128:t * 128 + R, :] if is_sample else y_p[j, t * 128:t * 128 + R, :]
            dma(dst, src_t[:R, :], [src_t], [xr(j, t)], is_out=True)

        def residual_ln(y_ps_list, R, dst_t):
            for hh in range(2):
                V("scalar_tensor_tensor", xh[:R, hh * 512:(hh + 1) * 512], x_t[:R, hh * 512:(hh + 1) * 512], ALPHA,
                  y_ps_list[hh], reads=[x_t] + [y_ps_list[2 + hh]], writes=[xh], op0=ALU.mult, op1=ALU.add)
            normalize(dst_t, xh, R)
            V("tensor_tensor", dst_t[:R, :], dst_t[:R, :], lng[:R, :], reads=[dst_t, lng], writes=[dst_t], op=ALU.mult)
            V("tensor_tensor", dst_t[:R, :], dst_t[:R, :], lnb[:R, :], reads=[dst_t, lnb], writes=[dst_t], op=ALU.add)

        def phase_a(l, j, T_len, is_sample):
            lam_init = 0.8 - 0.6 * math.exp(-0.3 * l)
            nt = (T_len + 127) // 128
            with ExitStack() as sa:
                def sba(name, shape, dt=F32):
                    return sb(name, shape, dt, stack=sa)
                w_in_sb = sba("w_in_sb", [128, 8, P_IN], BF16)
                w_out_sb = sba("w_out_sb", [128, 8, D], BF16)
                hT = sba("hT", [128, 8, 128], BF16)
                p_sb = sba("p_sb", [128, P_IN])
                mix = sba("mix", [128, D])
                mixT = sba("mixT", [128, 8, 128], BF16)
                wTs = sba("wTs", [128, 4, 128])
                bsT = sba("bsT", [128, 4])
                sg_g = sba("sg_g", [128, 256]); sg_b = sba("sg_b", [128, 256])
                uv = sba("uv", [128, 512]); gtmp = sba("gtmp", [128, 512])
                vst = sba("vst", [128, 4, 2]); vn = sba("vn", [128, 256]); vsq = sba("vsq", [128, 256])
                lamc = sba("lamc", [128, 4]); lamv = sba("lamv", [128, 4, 64]); nlam = sba("nlam", [128, 1])
                dng = sba("dng", [128, 128])
                Qblk = sba("Qblk", [128, 4, 2, 128], BF16)
                kst = [sba("kst0", [128, 512])] * 2
                vstg = [sba("vstg0", [128, 512])] * 2
                kTb = sba("kTb", [128, 4, 128], BF16)
                vb = sba("vb", [128, 4, 130], BF16)
                pT = sba("pT", [128, 2, 128], BF16)
                anum = sba("anum", [128, 8, 130]); aden = sba("aden", [128, 8])
                aout = sba("aout", [128, 4, 128]); asq = sba("asq", [128, 4, 128]); ass = sba("ass", [128, 4])
                cw = sba("cw", [64, 12, 4])
                negA = sba("negA", [64, 4]); dtb = sba("dtb", [64, 4]); gng = sba("gng", [64, 64])
                cbuf = sba("cbuf", [64, 12, 131]); cv = sba("cv", [64, 12, 128]); ctmp = sba("ctmp", [64, 12, 128])
                S_sb = sba("S_sb", [64, 4, 64])
                ktm = sba("ktm", [64, 4, 64]); vtm = sba("vtm", [64, 4, 64])
                gab = sba("gab", [64, 8]); gg = sba("gg", [64, 4]); beta = sba("beta", [64, 4])
                gB = sba("gB", [64, 4, 64]); gc_tm = sba("gc_tm", [64, 4]); Grow = sba("Grow", [64, 4, 64])
                D1 = sba("D1", [64, 4, 64]); E1 = sba("E1", [64, 4, 64]); E2 = sba("E2", [64, 4, 64])
                egc = sba("egc", [64, 4]); egrow = sba("egrow", [64, 4, 64]); kdf = sba("kdf", [64, 4])
                glc = sba("glc", [64, 4])
                Am = [sba(f"Am{i}", [64, 4, 64]) for i in range(2)]
                Bm = [sba(f"Bm{i}", [64, 4, 64]) for i in range(2)]
                XT = sba("XT", [64, 4, 64])
                Vb = gB; Kg = sba("Kg", [64, 4, 64]); kdec = sba("kdec", [64, 4, 64])
                u_sb = sba("u_sb", [64, 4, 64]); wT_sb = sba("wT_sb", [64, 4, 64])
                aqkT = sba("aqkT", [64, 4, 64]); qgT = sba("qgT", [64, 4, 64]); vnew = sba("vnew", [64, 4, 64])
                o_sb = E1; osq = D1; oss = sba("oss", [64, 4])
                z_sb = sba("z_sb", [64, 256])

                Gt = mix
                gate_tile(l, j, 16, Gt)
                for cb in range(25):
                    n0 = cb * 128
                    n1 = min(P_IN, n0 + 128)
                    w = wst[cb % 2]
                    dma(w[:, :, :n1 - n0], w_in[l, :, n0:n1].rearrange("(kc p) n -> p kc n", p=128), [], [w])
                    V("tensor_copy", w_in_sb[:, :, n0:n1], w[:, :, :n1 - n0], reads=[w], writes=[w_in_sb], eng="pool")
                for cb in range(8):
                    n0 = cb * 128
                    w = wst[(cb + 1) % 2]
                    dma(w[:], w_out[l, :, n0:n0 + 128].rearrange("(kc p) n -> p kc n", p=128), [], [w])
                    for kc in range(8):
                        V("tensor_tensor", w_out_sb[:, kc, n0:n0 + 128], w[:, kc, :], Gt[:, n0:n0 + 128],
                          reads=[w, Gt], writes=[w_out_sb], op=ALU.mult, eng="pool")
                dma(lng[:], ln1_g[l:l + 1, :].partition_broadcast(128), [], [lng])
                dma(lnb[:], ln1_b[l:l + 1, :].partition_broadcast(128), [], [lnb])
                wraw = p_sb
                dma(p_sb[:, 1536:2048].rearrange("p (g j) -> p g j", g=4), sgu_w[l].rearrange("g i j -> i g j"), [], [p_sb])
                for g in range(4):
                    tr(PS[2][:, g * 128:(g + 1) * 128], p_sb[:, 1536 + g * 128:1536 + (g + 1) * 128], ident[:], [p_sb, ident], [PS[2]])
                V("tensor_copy", wTs[:].rearrange("p a b -> p (a b)"), PS[2][:, :], reads=[PS[2]], writes=[wTs])
                V("memset", wTs[64:128, :, 0:64], 0.0, reads=[], writes=[wTs])
                dma(p_sb[:4, 2048:2176], sgu_b[l], [], [p_sb])
                tr(PS[3][:, 0:4], p_sb[:4, 2048:2176], ident[:4, :4], [p_sb, ident], [PS[3]])
                V("tensor_copy", bsT[:], PS[3][:, 0:4], reads=[PS[3]], writes=[bsT])
                dma(sg_g[:], sgu_ln_g[l:l + 1, :].partition_broadcast(128), [], [sg_g])
                dma(sg_b[:], sgu_ln_b[l:l + 1, :].partition_broadcast(128), [], [sg_b])
                for i, lq in enumerate((lam_q1, lam_k1, lam_q2, lam_k2)):
                    dma(lamv[:, i, :], lq[l:l + 1, :].partition_broadcast(128), [], [lamv])
                V("tensor_tensor", lamv[:, 0, :], lamv[:, 0, :], lamv[:, 1, :], reads=[lamv], writes=[lamv], op=ALU.mult)
                V("tensor_tensor", lamv[:, 2, :], lamv[:, 2, :], lamv[:, 3, :], reads=[lamv], writes=[lamv], op=ALU.mult)
                V("tensor_reduce", lamc[:, 0:1], lamv[:, 0, :], reads=[lamv], writes=[lamc], axis=AX.X, op=ALU.add)
                V("tensor_reduce", lamc[:, 1:2], lamv[:, 2, :], reads=[lamv], writes=[lamc], axis=AX.X, op=ALU.add)
                act(lamc[:, 0:2], lamc[:, 0:2], AF.Exp, [lamc], [lamc])
                V("tensor_tensor", nlam[:], lamc[:, 1:2], lamc[:, 0:1], reads=[lamc], writes=[nlam], op=ALU.subtract)
                V("tensor_scalar_add", nlam[:], nlam[:], -lam_init, reads=[nlam], writes=[nlam])
                dma(dng[:], diff_norm_g[l:l + 1, :].partition_broadcast(128), [], [dng])
                V("tensor_single_scalar", dng[:], dng[:], 1.0 - lam_init, reads=[dng], writes=[dng], op=ALU.mult)
                V("memset", Qblk[:], 0.0, reads=[], writes=[Qblk])
                V("memset", vb[:, :, 128:130], 1.0, reads=[], writes=[vb])
                dma(p_sb[:4, 0:768], conv_w[l], [], [p_sb])
                for g in range(12):
                    tr(PS[3][:64, 16 + g * 4:16 + g * 4 + 4], p_sb[:4, g * 64:(g + 1) * 64], ident[:4, :4],
                       [p_sb, ident], [PS[3]])
                V("tensor_copy", cw[:].rearrange("p a b -> p (a b)"), PS[3][:64, 16:64], reads=[PS[3]], writes=[cw])
                dma(negA[:], gdn_a_log[l:l + 1, :].partition_broadcast(64), [], [negA])
                act(negA[:], negA[:], AF.Exp, [negA], [negA])
                V("tensor_single_scalar", negA[:], negA[:], -1.0, reads=[negA], writes=[negA], op=ALU.mult)
                dma(dtb[:], gdn_dt_bias[l:l + 1, :].partition_broadcast(64), [], [dtb])
                dma(gng[:], gdn_norm_g[l:l + 1, :].partition_broadcast(64), [], [gng])
                if is_sample:
                    dma(S_sb[:], state_gdn[l].rearrange("h k v -> k h v"), [], [S_sb])
                    dma(p_sb[:3, 768:1536], state_conv[l], [], [p_sb])
                    for g in range(12):
                        tr(PS[3][:64, 64 + g * 3:64 + g * 3 + 3], p_sb[:3, 768 + g * 64:768 + (g + 1) * 64], ident[:3, :3],
                           [p_sb, ident], [PS[3]])
                    V("tensor_copy", cbuf[:, :, 0:3], PS[3][:64, 64:100].rearrange("p (a b) -> p a b", b=3),
                      reads=[PS[3]], writes=[cbuf])
                else:
                    V("memset", S_sb[:], 0.0, reads=[], writes=[S_sb])
                    V("memset", cbuf[:, :, 0:3], 0.0, reads=[], writes=[cbuf])

                def attn_block(k_ap, v_ap, kv_reads, nk, R, diag, delta, first):
                    for h in range(4):
                        tr(PS[4][:, h * 128:h * 128 + nk], k_ap[:, h * 128:(h + 1) * 128], ident[:nk, :nk],
                           kv_reads + [ident], [PS[4]])
                    act(kTb[:, :, :nk], PS[4][:, :].rearrange("p (a b) -> p a b", b=128)[:, :, :nk], AF.Copy,
                        [PS[4]], [kTb])
                    V("tensor_copy", vb[:nk, :, 0:128], v_ap.rearrange("p (h d) -> p h d", h=4), reads=kv_reads, writes=[vb])
                    dd = delta // 128
                    for h in range(4):
                        sc = PS[5 + (h % 2)]
                        scv = sc[:nk, 0:256].rearrange("p (m q) -> p m q", m=2)[:, :, :R]
                        if diag:
                            mm(scv, kTb[:, h, :nk], Qblk[:, h, :, :R], [kTb, Qblk], [sc], start=True, stop=False)
                            mm(scv, identb[:nk, :nk], biasd[:nk, h, :, :R], [identb, biasd], [sc], start=False, stop=True)
                            act(pT[:nk, :, :R], scv, AF.Exp, [sc], [pT])
                        else:
                            mm(scv, kTb[:, h, :nk], Qblk[:, h, :, :R], [kTb, Qblk], [sc], start=True, stop=True)
                            act(pT[:nk, :, :R], scv, AF.Exp, [sc, kbias], [pT], bias=kbias[:nk, h, dd:dd + 1])
                        pv = PS[7]
                        for m in range(2):
                            mm(pv[:R, m * 130:m * 130 + 129], pT[:nk, m, :R], vb[:nk, h, 0:129], [pT, vb], [pv])
                        pvv = pv[:R, 0:260].rearrange("p (m d) -> p m d", m=2)[:, :, 0:129]
                        if first:
                            V("tensor_copy", anum[:R, 2 * h:2 * h + 2, 0:129], pvv, reads=[pv], writes=[anum])
                        else:
                            V("tensor_tensor", anum[:R, 2 * h:2 * h + 2, 0:129], anum[:R, 2 * h:2 * h + 2, 0:129], pvv,
                              reads=[pv, anum], writes=[anum], op=ALU.add)

                for t in range(nt):
                    R = min(128, T_len - t * 128)
                    load_x(j, t, R, l, is_sample)
                    normalize(xh, x_t, R)
                    for kc in range(8):
                        pp = PS[kc // 4]
                        tr(pp[:, (kc % 4) * 128:(kc % 4) * 128 + R], xh[:R, kc * 128:(kc + 1) * 128], ident[:R, :R],
                           [xh, ident], [pp])
                    for kc in range(8):
                        pp = PS[kc // 4]
                        act(hT[:, kc, :R], pp[:, (kc % 4) * 128:(kc % 4) * 128 + R], AF.Identity, [pp, modT], [hT],
                            scale=modT[:, l, 8 + kc, j:j + 1], bias=modT[:, l, 0 + kc, j:j + 1])
                    for cb in range(7):
                        n0 = cb * 512
                        n1 = min(P_IN, n0 + 512)
                        pb = PS[2 + cb % 2]
                        for kc in range(8):
                            mm(pb[:R, :n1 - n0], hT[:, kc, :R], w_in_sb[:, kc, n0:n1], [hT, w_in_sb], [pb],
                               start=(kc == 0), stop=(kc == 7))
                        if cb % 2 == 0:
                            act(p_sb[:R, n0:n1], pb[:R, :n1 - n0], AF.Copy, [pb], [p_sb])
                        else:
                            V("tensor_copy", p_sb[:R, n0:n1], pb[:R, :n1 - n0], reads=[pb], writes=[p_sb])
                    kdst = nk_s[l, t * 128:t * 128 + R, :] if is_sample else nk_p[l, j, t * 128:t * 128 + R, :]
                    vdst = nv_s[l, t * 128:t * 128 + R, :] if is_sample else nv_p[l, j, t * 128:t * 128 + R, :]
                    dma(kdst, p_sb[:R, 1024:1536], [p_sb], [kvr(l, j, t)], is_out=True)
                    dma(vdst, p_sb[:R, 1536:2048], [p_sb], [kvr(l, j, t)], is_out=True)
                    if t == nt - 1:
                        dst = nconv_s[l, :, :] if is_sample else nconv_p[l, j, :, :]
                        dma(dst, p_sb[R - 3:R, 2048:2816], [p_sb], [], is_out=True)

                    if stages >= 2:
                        V("tensor_tensor", gtmp[:R, :], p_sb[:R, 0:512], p_sb[:R, 0:512], reads=[p_sb], writes=[gtmp], op=ALU.mult)
                        V("tensor_scalar", gtmp[:R, :], gtmp[:R, :], 0.044715, 1.0, reads=[gtmp], writes=[gtmp],
                          op0=ALU.mult, op1=ALU.add)
                        V("tensor_tensor", gtmp[:R, :], gtmp[:R, :], p_sb[:R, 0:512], reads=[gtmp, p_sb], writes=[gtmp], op=ALU.mult)
                        act(gtmp[:R, :], gtmp[:R, :], AF.Sigmoid, [gtmp], [gtmp], scale=2.0 * math.sqrt(2.0 / math.pi))
                        V("tensor_tensor", uv[:R, :], gtmp[:R, :], p_sb[:R, 0:512], reads=[gtmp, p_sb], writes=[uv], op=ALU.mult)
                        v3 = uv[:R, 256:512].rearrange("p (g d) -> p g d", g=4)
                        V("tensor_reduce", vst[:R, :, 0], v3, reads=[uv], writes=[vst], axis=AX.X, op=ALU.add)
                        V("tensor_tensor", vsq[:R, :], uv[:R, 256:512], uv[:R, 256:512], reads=[uv], writes=[vsq], op=ALU.mult)
                        V("tensor_reduce", vst[:R, :, 1], vsq[:R, :].rearrange("p (g d) -> p g d", g=4), reads=[vsq],
                          writes=[vst], axis=AX.X, op=ALU.add)
                        V("tensor_single_scalar", vst[:R, :, :], vst[:R, :, :], 1.0 / 64, reads=[vst], writes=[vst], op=ALU.mult)
                        V("tensor_tensor", vsq[:R, 0:4], vst[:R, :, 0], vst[:R, :, 0], reads=[vst], writes=[vsq], op=ALU.mult)
                        V("tensor_tensor", vst[:R, :, 1], vst[:R, :, 1], vsq[:R, 0:4], reads=[vst, vsq], writes=[vst], op=ALU.subtract)
                        rsqrt(vst[:R, :, 1], vst[:R, :, 1], 1e-5, [vst], vst)
                        vn3 = vn[:R, :].rearrange("p (g d) -> p g d", g=4)
                        V("tensor_tensor", vn3, v3, vst[:R, :, 0:1].to_broadcast([R, 4, 64]), reads=[uv, vst], writes=[vn], op=ALU.subtract)
                        V("tensor_tensor", vn3, vn3, vst[:R, :, 1:2].to_broadcast([R, 4, 64]), reads=[vn, vst], writes=[vn], op=ALU.mult)
                        V("tensor_tensor", vn[:R, :], vn[:R, :], sg_g[:R, :], reads=[vn, sg_g], writes=[vn], op=ALU.mult)
                        V("tensor_tensor", vn[:R, :], vn[:R, :], sg_b[:R, :], reads=[vn, sg_b], writes=[vn], op=ALU.add)
                        if is_sample:
                            dma(nsgu_s[l, t * 128:t * 128 + R, :], vn[:R, :], [vn], [], is_out=True)
                        for g in range(4):
                            mm(PS[0][:R, g * 64:(g + 1) * 64], wTs[:R, g, :R], vn[:R, g * 64:(g + 1) * 64], [wTs, vn], [PS[0]])
                        for g in range(4):
                            V("scalar_tensor_tensor", mix[:R, g * 64:(g + 1) * 64], PS[0][:R, g * 64:(g + 1) * 64],
                              bsT[:R, g:g + 1], uv[:R, g * 64:(g + 1) * 64], reads=[PS[0], bsT, uv], writes=[mix],
                              op0=ALU.add, op1=ALU.mult)
                    else:
                        V("memset", mix[:R, 0:256], 0.0, reads=[], writes=[mix])

                    if stages >= 3:
                        for h in range(4):
                            pq = PS[4 + (h % 2)]
                            for kc in range(8):
                                mm(pq[:, :R], w_in_sb[:, kc, 512 + h * 128:512 + (h + 1) * 128], hT[:, kc, :R],
                                   [w_in_sb, hT], [pq], start=(kc == 0), stop=(kc == 7))
                            act(Qblk[0:64, h, 0, :R], pq[0:64, :R], AF.Copy, [pq], [Qblk], scale=0.125)
                            act(Qblk[64:128, h, 1, :R], pq[64:128, :R], AF.Copy, [pq], [Qblk], scale=0.125)
                        qpos0 = (PAST if is_sample else 0) + t * 128
                        nblk = 0
                        if is_sample:
                            for kb in range(PAST // 128):
                                ks, vs = kst[nblk % 2], vstg[nblk % 2]
                                dma(ks[:], cache_k[l, kb * 128:(kb + 1) * 128, :], [], [ks])
                                dma(vs[:], cache_v[l, kb * 128:(kb + 1) * 128, :], [], [vs])
                                attn_block(ks[:, :], vs[:, :], [ks, vs], 128, R, False, qpos0 - kb * 128, nblk == 0)
                                nblk += 1
                        for kb in range(t):
                            ks, vs = kst[nblk % 2], vstg[nblk % 2]
                            ksrc = nk_s[l, kb * 128:(kb + 1) * 128, :] if is_sample else nk_p[l, j, kb * 128:(kb + 1) * 128, :]
                            vsrc = nv_s[l, kb * 128:(kb + 1) * 128, :] if is_sample else nv_p[l, j, kb * 128:(kb + 1) * 128, :]
                            dma(ks[:], ksrc, [kvr(l, j, kb)], [ks])
                            dma(vs[:], vsrc, [kvr(l, j, kb)], [vs])
                            attn_block(ks[:, :], vs[:, :], [ks, vs], 128, R, False, (t - kb) * 128, nblk == 0)
                            nblk += 1
                        attn_block(p_sb[:R, 1024:1536], p_sb[:R, 1536:2048], [p_sb], R, R, True, 0, nblk == 0)
                        V("reciprocal", aden[:R, :], anum[:R, :, 128], reads=[anum], writes=[aden])
                        V("tensor_tensor", anum[:R, :, 0:128], anum[:R, :, 0:128], aden[:R, :].unsqueeze(2).to_broadcast([R, 8, 128]),
                          reads=[anum, aden], writes=[anum], op=ALU.mult)
                        a4 = anum[:R, :, 0:128].rearrange("p (h m) d -> p h m d", m=2)
                        V("scalar_tensor_tensor", aout[:R, :, :], a4[:, :, 1, :], nlam[:R, 0:1], a4[:, :, 0, :],
                          reads=[anum, nlam], writes=[aout], op0=ALU.mult, op1=ALU.add)
                        V("tensor_tensor", asq[:R, :, :], aout[:R, :, :], aout[:R, :, :], reads=[aout], writes=[asq], op=ALU.mult)
                        V("tensor_reduce", ass[:R, :], asq[:R, :, :], reads=[asq], writes=[ass], axis=AX.X, op=ALU.add)
                        rsqrt(ass[:R, :], ass[:R, :], 1e-5, [ass], ass, scale=1.0 / 128)
                        V("tensor_tensor", aout[:R, :, :], aout[:R, :, :], ass[:R, :].unsqueeze(2).to_broadcast([R, 4, 128]),
                          reads=[aout, ass], writes=[aout], op=ALU.mult)
                        V("tensor_tensor", mix[:R, 256:768].rearrange("p (h d) -> p h d", h=4), aout[:R, :, :],
                          dng[:R, :].unsqueeze(1).to_broadcast([R, 4, 128]), reads=[aout, dng], writes=[mix], op=ALU.mult)
                    else:
                        V("memset", mix[:R, 256:768], 0.0, reads=[], writes=[mix])

                    if stages >= 4:
                        for g in range(12):
                            pg = PS[4 + (g // 4)]
                            col = 2048 + g * 64
                            for kc in range(8):
                                mm(pg[:64, (g % 4) * 128:(g % 4) * 128 + R], w_in_sb[:, kc, col:col + 64], hT[:, kc, :R],
                                   [w_in_sb, hT], [pg], start=(kc == 0), stop=(kc == 7))
                        for q3 in range(3):
                            act(cbuf[:, q3 * 4:(q3 + 1) * 4, 3:3 + R],
                                PS[4 + q3][:64, :].rearrange("p (a b) -> p a b", b=128)[:, :, :R], AF.Copy,
                                [PS[4 + q3]], [cbuf])
                        for jt in range(4):
                            wj = cw[:, :, jt:jt + 1].to_broadcast([64, 12, R])
                            if jt == 0:
                                V("tensor_tensor", cv[:, :, :R], cbuf[:, :, jt:jt + R], wj, reads=[cbuf, cw], writes=[cv], op=ALU.mult)
                            else:
                                V("tensor_tensor", ctmp[:, :, :R], cbuf[:, :, jt:jt + R], wj, reads=[cbuf, cw], writes=[ctmp], op=ALU.mult)
                                V("tensor_tensor", cv[:, :, :R], cv[:, :, :R], ctmp[:, :, :R], reads=[cv, ctmp], writes=[cv], op=ALU.add)
                        V("tensor_copy", ctmp[:, :, 0:3], cbuf[:, :, R:R + 3], reads=[cbuf], writes=[ctmp])
                        V("tensor_copy", cbuf[:, :, 0:3], ctmp[:, :, 0:3], reads=[ctmp], writes=[cbuf])
                        act(cv[:, :, :R], cv[:, :, :R], AF.Silu, [cv], [cv])
                        V("tensor_tensor", ctmp[:, 0:8, :R], cv[:, 0:8, :R], cv[:, 0:8, :R], reads=[cv], writes=[ctmp], op=ALU.mult)
                        for q2 in range(2):
                            mm(PS[4 + q2][:64, :].rearrange("p (a b) -> p a b", b=128)[:, :, :R], ones64[:, :],
                               ctmp[:, q2 * 4:(q2 + 1) * 4, :R], [ones64, ctmp], [PS[4 + q2]])
                            V("tensor_scalar_add", ctmp[:, q2 * 4:(q2 + 1) * 4, :R],
                              PS[4 + q2][:64, :].rearrange("p (a b) -> p a b", b=128)[:, :, :R], 1e-6,
                              reads=[PS[4 + q2]], writes=[ctmp])
                        act(ctmp[:, 0:8, :R], ctmp[:, 0:8, :R], AF.Sqrt, [ctmp], [ctmp])
                        V("reciprocal", ctmp[:, 0:8, :R], ctmp[:, 0:8, :R], reads=[ctmp], writes=[ctmp])
                        V("tensor_tensor", cv[:, 0:8, :R], cv[:, 0:8, :R], ctmp[:, 0:8, :R], reads=[cv, ctmp], writes=[cv], op=ALU.mult)
                        V("tensor_single_scalar", cv[:, 0:4, :R], cv[:, 0:4, :R], 0.125, reads=[cv], writes=[cv], op=ALU.mult)
                        for c0 in range(0, R, 64):
                            c = min(64, R - c0)
                            nsteps = max(0, int(math.ceil(math.log2(c))) - 1)
                            for g in range(8):
                                tr(PS[6][:c, g * 64:(g + 1) * 64], cv[:, 4 + g, c0:c0 + c], ident[:64, :64], [cv, ident], [PS[6]])
                            V("tensor_copy", ktm[:c, :, :], PS[6][:c, 0:256].rearrange("p (a b) -> p a b", b=64), reads=[PS[6]], writes=[ktm])
                            V("tensor_copy", vtm[:c, :, :], PS[6][:c, 256:512].rearrange("p (a b) -> p a b", b=64), reads=[PS[6]], writes=[vtm])
                            for kc in range(8):
                                mm(PS[7][:c, 0:8], hT[:, kc, c0:c0 + c], w_in_sb[:, kc, 3072:3080], [hT, w_in_sb], [PS[7]],
                                   start=(kc == 0), stop=(kc == 7))
                            for kc in range(8):
                                mm(PS[7][:c, 64:320], hT[:, kc, c0:c0 + c], w_in_sb[:, kc, 2816:3072], [hT, w_in_sb], [PS[7]],
                                   start=(kc == 0), stop=(kc == 7))
                            V("tensor_tensor", gab[:c, 0:4], PS[7][:c, 0:4], dtb[:c, :], reads=[PS[7], dtb], writes=[gab], op=ALU.add)
                            act(gab[:c, 0:4], gab[:c, 0:4], AF.Exp, [gab], [gab])
                            V("tensor_scalar_add", gab[:c, 0:4], gab[:c, 0:4], 1.0, reads=[gab], writes=[gab])
                            act(gab[:c, 0:4], gab[:c, 0:4], AF.Ln, [gab], [gab])
                            V("tensor_tensor", gg[:c, :], gab[:c, 0:4], negA[:c, :], reads=[gab, negA], writes=[gg], op=ALU.mult)
                            act(beta[:c, :], PS[7][:c, 4:8], AF.Sigmoid, [PS[7]], [beta])
                            act(z_sb[:c, :], PS[7][:c, 64:320], AF.Silu, [PS[7]], [z_sb])
                            V("tensor_copy", gB[:c, :, :], gg[:c, :].unsqueeze(2).to_broadcast([c, 4, 64]), reads=[gg], writes=[gB])
                            mm(PS[6][:c, 0:4], triU[:c, :c], gg[:c, :], [triU, gg], [PS[6]])
                            for h in range(4):
                                mm(PS[6][:64, 64 + h * 64:64 + h * 64 + c], gB[:c, h, :], triU[:c, :c], [gB, triU], [PS[6]])
                            V("tensor_copy", gc_tm[:c, :], PS[6][:c, 0:4], reads=[PS[6]], writes=[gc_tm])
                            V("tensor_copy", Grow[:, :, :c], PS[6][:64, 64:320].rearrange("p (a b) -> p a b", b=64)[:, :, :c],
                              reads=[PS[6]], writes=[Grow])
                            V("tensor_tensor", D1[:c, :, :c], gc_tm[:c, :].unsqueeze(2).to_broadcast([c, 4, c]), Grow[:c, :, :c],
                              reads=[gc_tm, Grow], writes=[D1], op=ALU.subtract)
                            V("tensor_single_scalar", E1[:c, :, :c], D1[:c, :, :c], 0.0, reads=[D1], writes=[E1], op=ALU.min)
                            act(E1[:c, :, :c], E1[:c, :, :c], AF.Exp, [E1], [E1])
                            V("tensor_scalar", E2[:c, :, :c], D1[:c, :, :c], -1.0, 0.0, reads=[D1], writes=[E2], op0=ALU.mult, op1=ALU.min)
                            act(E2[:c, :, :c], E2[:c, :, :c], AF.Exp, [E2], [E2])
                            act(egc[:c, :], gc_tm[:c, :], AF.Exp, [gc_tm], [egc])
                            act(egrow[:, :, :c], Grow[:, :, :c], AF.Exp, [Grow], [egrow])
                            act(glc[:, :], Grow[:, :, c - 1], AF.Exp, [Grow], [glc])
                            V("tensor_tensor", kdf[:c, :], Grow[:c, :, c - 1], gc_tm[:c, :], reads=[Grow, gc_tm], writes=[kdf], op=ALU.subtract)
                            act(kdf[:c, :], kdf[:c, :], AF.Exp, [kdf], [kdf])
                            for h in range(4):
                                mm(PS[4][:c, h * 64:h * 64 + c], cv[:, 4 + h, c0:c0 + c], cv[:, 4 + h, c0:c0 + c], [cv], [PS[4]])
                                mm(PS[4][:c, 256 + h * 64:256 + h * 64 + c], cv[:, 4 + h, c0:c0 + c], cv[:, h, c0:c0 + c], [cv], [PS[4]])
                            V("tensor_tensor", D1[:c, :, :c], E1[:c, :, :c], maskLs[:c, :c].unsqueeze(1).to_broadcast([c, 4, c]),
                              reads=[E1, maskLs], writes=[D1], op=ALU.mult)
                            A0, B0 = Am[0], Bm[0]
                            V("tensor_tensor", A0[:c, :, :c], PS[4][:c, 0:256].rearrange("p (a b) -> p a b", b=64)[:, :, :c],
                              D1[:c, :, :c], reads=[PS[4], D1], writes=[A0], op=ALU.mult)
                            V("tensor_tensor", A0[:c, :, :c], A0[:c, :, :c], beta[:c, :].unsqueeze(2).to_broadcast([c, 4, c]),
                              reads=[A0, beta], writes=[A0], op=ALU.mult)
                            V("tensor_tensor", E2[:c, :, :c], E2[:c, :, :c], maskU[:c, :c].unsqueeze(1).to_broadcast([c, 4, c]),
                              reads=[E2, maskU], writes=[E2], op=ALU.mult)
                            V("tensor_tensor", aqkT[:c, :, :c], PS[4][:c, 256:512].rearrange("p (a b) -> p a b", b=64)[:, :, :c],
                              E2[:c, :, :c], reads=[PS[4], E2], writes=[aqkT], op=ALU.mult)
                            for h in range(4):
                                tr(PS[5][:c, h * 64:h * 64 + c], A0[:c, h, :c], ident[:c, :c], [A0, ident], [PS[5]])
                            V("tensor_copy", B0[:c, :, :c], PS[5][:c, 0:256].rearrange("p (a b) -> p a b", b=64)[:, :, :c],
                              reads=[PS[5]], writes=[B0])
                            V("tensor_tensor", XT[:c, :, :c], ident[:c, :c].unsqueeze(1).to_broadcast([c, 4, c]), B0[:c, :, :c],
                              reads=[ident, B0], writes=[XT], op=ALU.subtract)
                            cur = 0
                            for n in range(1, nsteps + 1):
                                Ac, Bc, An, Bn = Am[cur], Bm[cur], Am[1 - cur], Bm[1 - cur]
                                for h in range(4):
                                    mm(PS[4][:c, h * 64:h * 64 + c], Bc[:c, h, :c], Ac[:c, h, :c], [Ac, Bc], [PS[4]])
                                    if n < nsteps:
                                        mm(PS[4][:c, 256 + h * 64:256 + h * 64 + c], Ac[:c, h, :c], Bc[:c, h, :c], [Ac, Bc], [PS[4]])
                                V("tensor_copy", An[:c, :, :c], PS[4][:c, 0:256].rearrange("p (a b) -> p a b", b=64)[:, :, :c],
                                  reads=[PS[4]], writes=[An])
                                if n < nsteps:
                                    V("tensor_copy", Bn[:c, :, :c], PS[4][:c, 256:512].rearrange("p (a b) -> p a b", b=64)[:, :, :c],
                                      reads=[PS[4]], writes=[Bn])
                                for h in range(4):
                                    mm(PS[5][:c, h * 64:h * 64 + c], An[:c, h, :c], XT[:c, h, :c], [An, XT], [PS[5]])
                                V("tensor_tensor", XT[:c, :, :c], XT[:c, :, :c], PS[5][:c, 0:256].rearrange("p (a b) -> p a b", b=64)[:, :, :c],
                                  reads=[XT, PS[5]], writes=[XT], op=ALU.add)
                                cur = 1 - cur
                            V("tensor_tensor", Vb[:c, :, :], vtm[:c, :, :], beta[:c, :].unsqueeze(2).to_broadcast([c, 4, 64]),
                              reads=[vtm, beta], writes=[Vb], op=ALU.mult)
                            V("tensor_tensor", Kg[:c, :, :], ktm[:c, :, :], beta[:c, :].unsqueeze(2).to_broadcast([c, 4, 64]),
                              reads=[ktm, beta], writes=[Kg], op=ALU.mult)
                            V("tensor_tensor", Kg[:c, :, :], Kg[:c, :, :], egc[:c, :].unsqueeze(2).to_broadcast([c, 4, 64]),
                              reads=[Kg, egc], writes=[Kg], op=ALU.mult)
                            V("tensor_tensor", kdec[:c, :, :], ktm[:c, :, :], kdf[:c, :].unsqueeze(2).to_broadcast([c, 4, 64]),
                              reads=[ktm, kdf], writes=[kdec], op=ALU.mult)
                            V("tensor_tensor", qgT[:, :, :c], cv[:, 0:4, c0:c0 + c], egrow[:, :, :c], reads=[cv, egrow], writes=[qgT], op=ALU.mult)
                            for h in range(4):
                                mm(PS[6][:c, h * 64:(h + 1) * 64], XT[:c, h, :c], Vb[:c, h, :], [XT, Vb], [PS[6]])
                                mm(PS[6][:64, 256 + h * 64:256 + h * 64 + c], Kg[:c, h, :], XT[:c, h, :c], [Kg, XT], [PS[6]])
                            V("tensor_copy", u_sb[:c, :, :], PS[6][:c, 0:256].rearrange("p (a b) -> p a b", b=64), reads=[PS[6]], writes=[u_sb])
                            V("tensor_copy", wT_sb[:, :, :c], PS[6][:64, 256:512].rearrange("p (a b) -> p a b", b=64)[:, :, :c],
                              reads=[PS[6]], writes=[wT_sb])
                            for h in range(4):
                                mm(PS[5][:c, 256 + h * 64:256 + (h + 1) * 64], wT_sb[:, h, :c], S_sb[:, h, :], [wT_sb, S_sb], [PS[5]])
                            V("tensor_tensor", vnew[:c, :, :], u_sb[:c, :, :], PS[5][:c, 256:512].rearrange("p (a b) -> p a b", b=64),
                              reads=[u_sb, PS[5]], writes=[vnew], op=ALU.subtract)
                            for h in range(4):
                                mm(PS[6][:c, h * 64:(h + 1) * 64], qgT[:, h, :c], S_sb[:, h, :], [qgT, S_sb], [PS[6]], start=True, stop=False)
                                mm(PS[6][:c, h * 64:(h + 1) * 64], aqkT[:c, h, :c], vnew[:c, h, :], [aqkT, vnew], [PS[6]], start=False, stop=True)
                            for h in range(4):
                                mm(PS[5][:64, h * 64:(h + 1) * 64], kdec[:c, h, :], vnew[:c, h, :], [kdec, vnew], [PS[5]])
                            V("tensor_tensor", S_sb[:, :, :], S_sb[:, :, :], glc[:, :].unsqueeze(2).to_broadcast([64, 4, 64]),
                              reads=[S_sb, glc], writes=[S_sb], op=ALU.mult)
                            V("tensor_tensor", S_sb[:, :, :], S_sb[:, :, :], PS[5][:64, 0:256].rearrange("p (a b) -> p a b", b=64),
                              reads=[S_sb, PS[5]], writes=[S_sb], op=ALU.add)
                            V("tensor_copy", o_sb[:c, :, :], PS[6][:c, 0:256].rearrange("p (a b) -> p a b", b=64), reads=[PS[6]], writes=[o_sb])
                            V("tensor_tensor", osq[:c, :, :], o_sb[:c, :, :], o_sb[:c, :, :], reads=[o_sb], writes=[osq], op=ALU.mult)
                            V("tensor_reduce", oss[:c, :], osq[:c, :, :], reads=[osq], writes=[oss], axis=AX.X, op=ALU.add)
                            rsqrt(oss[:c, :], oss[:c, :], 1e-5, [oss], oss, scale=1.0 / 64)
                            V("tensor_tensor", o_sb[:c, :, :], o_sb[:c, :, :], oss[:c, :].unsqueeze(2).to_broadcast([c, 4, 64]),
                              reads=[o_sb, oss], writes=[o_sb], op=ALU.mult)
                            V("tensor_tensor", o_sb[:c, :, :], o_sb[:c, :, :], gng[:c, :].unsqueeze(1).to_broadcast([c, 4, 64]),
                              reads=[o_sb, gng], writes=[o_sb], op=ALU.mult)
                            V("tensor_tensor", o_sb[:c, :, :], o_sb[:c, :, :], z_sb[:c, :].rearrange("p (a b) -> p a b", b=64),
                              reads=[o_sb, z_sb], writes=[o_sb], op=ALU.mult)
                            dma(mix[c0:c0 + c, 768:1024], o_sb[:c, :, :].rearrange("p a b -> p (a b)"), [o_sb], [mix])
                        if t == nt - 1:
                            dst = ngdn_s[l].rearrange("h k v -> k h v") if is_sample else ngdn_p[l, j].rearrange("h k v -> k h v")
                            dma(dst, S_sb[:, :, :], [S_sb], [], is_out=True)
                    else:
                        V("memset", mix[:R, 768:1024], 0.0, reads=[], writes=[mix])

                    for kc in range(8):
                        pp = PS[kc // 4]
                        tr(pp[:, (kc % 4) * 128:(kc % 4) * 128 + R], mix[:R, kc * 128:(kc + 1) * 128], ident[:R, :R],
                           [mix, ident], [pp])
                    for hh in range(2):
                        V("tensor_copy", mixT[:, hh * 4:(hh + 1) * 4, :R],
                          PS[hh][:, :].rearrange("p (a b) -> p a b", b=128)[:, :, :R], reads=[PS[hh]], writes=[mixT])
                    for hh in range(2):
                        for kc in range(8):
                            mm(PS[2 + hh][:R, :], mixT[:, kc, :R], w_out_sb[:, kc, hh * 512:(hh + 1) * 512],
                               [mixT, w_out_sb], [PS[2 + hh]], start=(kc == 0), stop=(kc == 7))
                    residual_ln([PS[2][:R, :], PS[3][:R, :], PS[2], PS[3]], R, x_t)
                    store_x(j, t, R, is_sample, x_t)
                P.barrier()

        def phase_b(l, j, T_len, is_sample):
            nt = (T_len + 127) // 128
            with ExitStack() as sbk:
                def sbb(name, shape, dt=F32):
                    return sb(name, shape, dt, stack=sbk)
                wq_sb = sbb("wq_sb", [128, 8, 2048], BF16)
                keysT = sbb("keysT", [128, 16, 128])
                kraw = sbb("kraw", [128, 128])
                h2T = sbb("h2T", [128, 8, 128]); h2Tb = sbb("h2Tb", [128, 8, 128], BF16)
                h2 = sbb("h2", [128, D])
                qT = sbb("qT", [128, 16, 128])
                sc = sbb("sc", [128, 16, 128]); sc2 = sbb("sc2", [128, 16, 128])
                tops = sbb("tops", [128, 16, 16]); topi = sbb("topi", [128, 16, 16], U32); topf = sbb("topf", [128, 16, 16])
                cand = sc2; cand2 = sc
                bests = sbb("bests", [128, 8, 16]); besti = sbb("besti", [128, 8, 16], U32)
                af = sbb("af", [128, 8, 16]); bf = sbb("bf", [128, 8, 16])
                oh = qT
                idf = sbb("idf", [128, 8, 16]); idf2 = sbb("idf2", [128, 8, 16]); ids = sbb("ids", [128, 128], I32)
                gmax = sbb("gmax", [128, 8]); gsum = sbb("gsum", [128, 8]); gates = sbb("gates", [128, 8, 16])
                dots = sbb("dots", [128, 128]); wts = sbb("wts", [128, 128]); gt2 = sbb("gt2", [128, 128])
                NRING = 12
                gbuf = [sbb(f"gbuf{i}", [128, 2 * D], BF16) for i in range(NRING)]
                xg = sbb("xg", [128, 128])
                dcol = sbb("dcol", [128, 128]); wcol = sbb("wcol", [128, 128])
                junk = sbb("junk", [128, D], BF16)
                facc = sbb("facc", [128, D])
                dg = [sbb(f"dg{i}", [128, 128], BF16) for i in range(2)]

                Gt = sbb("Gt", [128, D])
                gate_tile(l, j, 40, Gt)
                for cb in range(16):
                    n0 = cb * 128
                    w = wst[cb % 2]
                    dma(w[:], peer_wq[l, :, n0:n0 + 128].rearrange("(kc p) n -> p kc n", p=128), [], [w])
                    V("tensor_copy", wq_sb[:, :, n0:n0 + 128], w[:], reads=[w], writes=[wq_sb], eng="pool")
                for c16 in range(16):
                    dma(kraw[:], peer_keys[l, c16], [], [kraw])
                    tr(PS[0][:, 0:128], kraw[:], ident[:], [kraw, ident], [PS[0]])
                    V("tensor_copy", keysT[:, c16, :], PS[0][:, 0:128], reads=[PS[0]], writes=[keysT])
                dma(lng[:], ln2_g[l:l + 1, :].partition_broadcast(128), [], [lng])
                dma(lnb[:], ln2_b[l:l + 1, :].partition_broadcast(128), [], [lnb])

                for t in range(nt):
                    R = min(128, T_len - t * 128)
                    load_x(j, t, R, 1, is_sample)
                    normalize(xh, x_t, R)
                    for kc in range(8):
                        pp = PS[kc // 4]
                        tr(pp[:, (kc % 4) * 128:(kc % 4) * 128 + R], xh[:R, kc * 128:(kc + 1) * 128], ident[:R, :R],
                           [xh, ident], [pp])
                    for kc in range(8):
                        pp = PS[kc // 4]
                        act(h2T[:, kc, :R], pp[:, (kc % 4) * 128:(kc % 4) * 128 + R], AF.Identity, [pp, modT], [h2T],
                            scale=modT[:, l, 32 + kc, j:j + 1], bias=modT[:, l, 24 + kc, j:j + 1])
                    V("tensor_copy", h2Tb[:, :, :R], h2T[:, :, :R], reads=[h2T], writes=[h2Tb])
                    for kc in range(8):
                        pp = PS[2 + kc // 4]
                        tr(pp[:R, (kc % 4) * 128:(kc % 4 + 1) * 128], h2T[:, kc, :R], ident[:, :], [h2T, ident], [pp])
                    for hh in range(2):
                        V("tensor_copy", h2[:R, hh * 512:(hh + 1) * 512], PS[2 + hh][:R, :], reads=[PS[2 + hh]], writes=[h2])
                    for c16 in range(16):
                        pq = PS[4 + (c16 % 2)]
                        for kc in range(8):
                            mm(pq[:, :R], wq_sb[:, kc, c16 * 128:(c16 + 1) * 128], h2Tb[:, kc, :R], [wq_sb, h2Tb], [pq],
                               start=(kc == 0), stop=(kc == 7))
                        act(qT[:, c16, :R], pq[:, :R], AF.Copy, [pq], [qT])
                    for c16 in range(16):
                        pq = PS[6 + (c16 // 4) % 2]
                        mm(pq[:R, (c16 % 4) * 128:(c16 % 4 + 1) * 128], qT[:, c16, :R], keysT[:, c16, :], [qT, keysT], [pq])
                        if c16 % 4 == 3:
                            V("tensor_copy", sc[:R, c16 - 3:c16 + 1, :].rearrange("p a b -> p (a b)"), pq[:R, :], reads=[pq], writes=[sc])
                    for c16 in range(16):
                        V("max", tops[:R, c16, 0:8], sc[:R, c16, :], reads=[sc], writes=[tops])
                        V("max_index", topi[:R, c16, 0:8], tops[:R, c16, 0:8], sc[:R, c16, :], reads=[tops, sc], writes=[topi])
                        V("match_replace", sc2[:R, c16, :], tops[:R, c16, 0:8], sc[:R, c16, :], -1e30, reads=[tops, sc], writes=[sc2])
                        V("max", tops[:R, c16, 8:16], sc2[:R, c16, :], reads=[sc2], writes=[tops])
                        V("max_index", topi[:R, c16, 8:16], tops[:R, c16, 8:16], sc2[:R, c16, :], reads=[tops, sc2], writes=[topi])
                    V("tensor_copy", topf[:R, :, :], topi[:R, :, :], reads=[topi], writes=[topf])
                    t4 = tops[:R, :, :].rearrange("p (h s) k -> p h s k", s=2)
                    f4 = topf[:R, :, :].rearrange("p (h s) k -> p h s k", s=2)
                    candv = cand[:R, :, :].rearrange("p (h s) k -> p h (s k)", s=2)
                    cand2v = cand2[:R, :, :].rearrange("p (h s) k -> p h (s k)", s=2)
                    ohv = oh[:R, :, :].rearrange("p (h a) (b c) -> p h a b c", a=2, c=16)
                    ohv = oh[:R, :, :].rearrange("p a b -> p (a b)").rearrange("p (h j a) -> p h j a", h=8, j=16)
                    c4 = candv.rearrange("p h (a b) -> p h a b", b=16)
                    V("tensor_tensor", c4, t4[:, :, 0, :].unsqueeze(3).to_broadcast([R, 8, 16, 16]),
                      t4[:, :, 1, :].unsqueeze(2).to_broadcast([R, 8, 16, 16]), reads=[tops], writes=[cand], op=ALU.add)
                    for hd in range(8):
                        V("max", bests[:R, hd, 0:8], candv[:, hd, :], reads=[cand], writes=[bests])
                        V("max_index", besti[:R, hd, 0:8], bests[:R, hd, 0:8], candv[:, hd, :], reads=[bests, cand], writes=[besti])
                        V("match_replace", cand2v[:, hd, :], bests[:R, hd, 0:8], candv[:, hd, :], -1e30, reads=[bests, cand], writes=[cand2])
                        V("max", bests[:R, hd, 8:16], cand2v[:, hd, :], reads=[cand2], writes=[bests])
                        V("max_index", besti[:R, hd, 8:16], bests[:R, hd, 8:16], cand2v[:, hd, :], reads=[bests, cand2], writes=[besti])
                    V("tensor_copy", af[:R, :, :], besti[:R, :, :], reads=[besti], writes=[af])
                    V("tensor_copy", bf[:R, :, :], af[:R, :, :], reads=[af], writes=[bf])
                    V("tensor_tensor", ohv, bf[:R, :, :].unsqueeze(3).to_broadcast([R, 8, 16, 16]),
                      thr16[:R, :].unsqueeze(1).unsqueeze(1).to_broadcast([R, 8, 16, 16]), reads=[bf, thr16], writes=[oh], op=ALU.is_ge)
                    V("tensor_reduce", af[:R, :, :], ohv, reads=[oh], writes=[af], axis=AX.X, op=ALU.add)
                    V("scalar_tensor_tensor", bf[:R, :, :], af[:R, :, :], -16.0, bf[:R, :, :], reads=[af, bf], writes=[bf],
                      op0=ALU.mult, op1=ALU.add)
                    io16 = iota_f[:R, 0:16].unsqueeze(1).unsqueeze(1).to_broadcast([R, 8, 16, 16])
                    for sel, half, dstf in ((af, 0, idf), (bf, 1, idf2)):
                        V("tensor_tensor", ohv, sel[:R, :, :].unsqueeze(3).to_broadcast([R, 8, 16, 16]), io16,
                          reads=[sel, iota_f], writes=[oh], op=ALU.is_equal)
                        V("tensor_tensor", ohv, ohv, f4[:, :, half, :].unsqueeze(2).to_broadcast([R, 8, 16, 16]),
                          reads=[oh, topf], writes=[oh], op=ALU.mult)
                        V("tensor_reduce", dstf[:R, :, :], ohv, reads=[oh], writes=[dstf], axis=AX.X, op=ALU.add)
                    V("scalar_tensor_tensor", idf[:R, :, :], idf[:R, :, :], 128.0, idf2[:R, :, :], reads=[idf, idf2], writes=[idf],
                      op0=ALU.mult, op1=ALU.add)
                    V("tensor_scalar_add", idf[:R, :, :], idf[:R, :, :], float(l * 16384), reads=[idf], writes=[idf])
                    V("tensor_copy", ids[:R, :], idf[:R, :, :].rearrange("p a b -> p (a b)"), reads=[idf], writes=[ids])
                    V("tensor_reduce", gmax[:R, :], bests[:R, :, :], reads=[bests], writes=[gmax], axis=AX.X, op=ALU.max)
                    V("tensor_tensor", gates[:R, :, :], bests[:R, :, :], gmax[:R, :].unsqueeze(2).to_broadcast([R, 8, 16]),
                      reads=[bests, gmax], writes=[gates], op=ALU.subtract)
                    act(gates[:R, :, :], gates[:R, :, :], AF.Exp, [gates], [gates])
                    V("tensor_reduce", gsum[:R, :], gates[:R, :, :], reads=[gates], writes=[gsum], axis=AX.X, op=ALU.add)
                    V("reciprocal", gsum[:R, :], gsum[:R, :], reads=[gsum], writes=[gsum])
                    V("tensor_tensor", gates[:R, :, :], gates[:R, :, :], gsum[:R, :].unsqueeze(2).to_broadcast([R, 8, 16]),
                      reads=[gates, gsum], writes=[gates], op=ALU.mult)
                    GS = 4
                    NG = 128 // GS
                    gflat = gates[:R, :, :].rearrange("p a b -> p (a b)")
                    GC = 2.0 * math.sqrt(2.0 / math.pi)

                    def issue(g):
                        for k in range(g * GS, (g + 1) * GS):
                            gb = gbuf[k % NRING]
                            P.op("pool", lambda e, gb=gb, k=k, R=R: e.indirect_dma_start(
                                out=gb[:R, :], out_offset=None, in_=euv[:, :],
                                in_offset=bass.IndirectOffsetOnAxis(ap=ids[:R, k:k + 1], axis=0)),
                                [ids], [gb], dma=True)

                    issue(0)
                    issue(1)
                    for g in range(NG):
                        if g + 2 < NG:
                            issue(g + 2)
                        k0 = g * GS
                        for k in range(k0, k0 + GS):
                            gb = gbuf[k % NRING]
                            V("scalar_tensor_tensor", junk[:R, :], gb[:R, 0:D], 1.0, h2[:R, :], reads=[gb, h2],
                              writes=[junk, dots], op0=ALU.mult, op1=ALU.mult, accum_out=dots[:R, k:k + 1])
                        ds = dots[:R, k0:k0 + GS]
                        tt = gt2[:R, k0:k0 + GS]
                        V("tensor_tensor", tt, ds, ds, reads=[dots], writes=[gt2], op=ALU.mult)
                        V("tensor_scalar", tt, tt, 0.044715, 1.0, reads=[gt2], writes=[gt2], op0=ALU.mult, op1=ALU.add)
                        V("tensor_tensor", tt, tt, ds, reads=[gt2, dots], writes=[gt2], op=ALU.mult)
                        act(tt, tt, AF.Sigmoid, [gt2], [gt2], scale=GC)
                        V("tensor_tensor", xg[:R, k0:k0 + GS], ds, gflat[:, k0:k0 + GS], reads=[dots, gates], writes=[xg], op=ALU.mult)
                        V("tensor_tensor", wts[:R, k0:k0 + GS], tt, xg[:R, k0:k0 + GS], reads=[gt2, xg], writes=[wts], op=ALU.mult)
                        for k in range(k0, k0 + GS):
                            gb = gbuf[k % NRING]
                            d = dg[k % 2]
                            V("tensor_scalar", d[:R, :R], identb[:R, :R], wts[:R, k:k + 1], None, reads=[identb, wts], writes=[d], op0=ALU.mult)
                            for hh in range(2):
                                mm(PS[6 + hh][:R, :], d[:R, :R], gb[:R, D + hh * 512:D + (hh + 1) * 512], [d, gb], [PS[6 + hh]],
                                   start=(k == 0), stop=(k == 127))
                    for hh in range(2):
                        V("tensor_tensor", facc[:R, hh * 512:(hh + 1) * 512], PS[6 + hh][:R, :], Gt[:R, hh * 512:(hh + 1) * 512],
                          reads=[PS[6 + hh], Gt], writes=[facc], op=ALU.mult)
                    residual_ln([facc[:R, 0:512], facc[:R, 512:1024], facc, facc], R, x_t)
                    store_x(j, t, R, is_sample, x_t)
                P.barrier()

        order = [NP] + list(range(NP))
        for j in order:
            is_sample = (j == NP)
            T_len = TS if is_sample else SEQ
            for l in range(L):
                phase_a(l, j, T_len, is_sample)
                if stages >= 5:
                    phase_b(l, j, T_len, is_sample)

        P.emit()
        print("n_ops", P.n_ops, {e: len(v) for e, v in P.ops.items()}, flush=True)
    return nc


WEIGHT_NAMES = ["w_ada", "b_ada", "w_in", "sgu_ln_g", "sgu_ln_b", "sgu_w", "sgu_b", "lam_q1", "lam_k1",
                "lam_q2", "lam_k2", "diff_norm_g", "conv_w", "gdn_a_log", "gdn_dt_bias", "gdn_norm_g",
                "w_out", "ln1_g", "ln1_b", "peer_wq", "peer_keys", "expert_u", "expert_v", "ln2_g", "ln2_b"]


def run(cfg, inputs, stages=99):
    L = cfg.DEPTH
    NP = cfg.BATCH // NCORES
    f = lambda a: np.ascontiguousarray(np.asarray(a, dtype=np.float32))
    W = {k: f(inputs[k]) for k in WEIGHT_NAMES}
    W["sgu_ln_g"] = W["sgu_ln_g"].reshape(L, 256)
    W["sgu_ln_b"] = W["sgu_ln_b"].reshape(L, 256)
    W["peer_keys"] = W["peer_keys"].reshape(L, 16, 128, 128)
    W["expert_u"] = W["expert_u"].reshape(L * 16384, cfg.D)
    W["expert_v"] = W["expert_v"].reshape(L * 16384, cfg.D)
    xp = f(inputs["x_prompt"]); xs = f(inputs["x_sample"])
    ck = f(inputs["cache_k"]); cv = f(inputs["cache_v"])
    sg = f(inputs["state_gdn"]); sc = f(inputs["state_conv"])
    cp = f(inputs["c_prompt"]); cs = f(inputs["c_sample"])
    in_maps = []
    for i in range(NCORES):
        m = dict(W)
        m["xp"] = xp[i * NP:(i + 1) * NP]
        m["xs"] = xs[i]
        m["cache_k"] = f(ck[:, i].reshape(L, cfg.PAST, 512))
        m["cache_v"] = f(cv[:, i].reshape(L, cfg.PAST, 512))
        m["state_gdn"] = f(sg[:, i])
        m["state_conv"] = f(sc[:, i])
        m["c_in"] = f(np.concatenate([cp[i * NP:(i + 1) * NP], cs[i:i + 1]], axis=0))
        in_maps.append(m)
    import time as _time
    _t0 = _time.time()
    nc = build(cfg, stages)
    print("build s", _time.time() - _t0, flush=True)
    _t0 = _time.time()
    import os as _os
    if _os.environ.get("K_TRACE"):
        res = run_bass_kernel_spmd(nc, in_maps, core_ids=list(range(NCORES)), trace=True)
        print("EXEC_TIME_NS", res.exec_time_ns, flush=True)
    else:
        res = run_bass_kernel_spmd(nc, in_maps, core_ids=list(range(NCORES)))
    print("run_spmd s", _time.time() - _t0, flush=True)
    R = res.results
    cat = lambda k, ax: np.concatenate([r[k] for r in R], axis=ax)
    stk = lambda k, ax: np.stack([r[k] for r in R], axis=ax)
    S = cfg.SEQ
    y_prompt = cat("y_p", 0)
    y_sample = stk("y_s", 0)
    nk_p = cat("nk_p", 1).reshape(L, cfg.BATCH, S, 4, 128)
    nv_p = cat("nv_p", 1).reshape(L, cfg.BATCH, S, 4, 128)
    ngdn_p = cat("ngdn_p", 1)
    nconv_p = cat("nconv_p", 1)
    nk_s = stk("nk_s", 1).reshape(L, cfg.DEC_BATCH, cfg.DEC_SEQ, 4, 128)
    nv_s = stk("nv_s", 1).reshape(L, cfg.DEC_BATCH, cfg.DEC_SEQ, 4, 128)
    ngdn_s = stk("ngdn_s", 1)
    nconv_s = stk("nconv_s", 1)
    nsgu_s = stk("nsgu_s", 1)
    return (y_prompt, y_sample, nk_p, nv_p, ngdn_p, nconv_p, nk_s, nv_s, ngdn_s, nconv_s, nsgu_s)


def kernel(**inputs):
    return run(Cfg(), inputs)
```

```python
import math
import numpy as np
import concourse.bass as bass
import concourse.mybir as mybir
from concourse.bass_utils import run_bass_kernel_spmd

F32 = mybir.dt.float32
BF16 = mybir.dt.bfloat16
I32 = mybir.dt.int32
U32 = mybir.dt.uint32
AF = mybir.ActivationFunctionType
ALU = mybir.AluOpType
AX = mybir.AxisListType

NCORES = 8


class Cfg:
    def __init__(self, **kw):
        self.D = 1024
        self.BATCH = 32
        self.SEQ = 2048
        self.DEPTH = 4
        self.DEC_BATCH = 8
        self.DEC_SEQ = 16
        self.PAST = 4096
        for k, v in kw.items():
            setattr(self, k, v)
        self.ALPHA = (2 * self.DEPTH) ** 0.25


class Res:
    __slots__ = ("name", "lw", "rd")

    def __init__(self, name=""):
        self.name = name
        self.lw = None
        self.rd = {}


class T:
    def __init__(self, handle, name, nres=1):
        self.h = handle
        self.name = name
        self.res = [Res(f"{name}.{i}") for i in range(nres)]

    def __getitem__(self, idx):
        return self.h[idx]

    @property
    def r(self):
        return self.res[0]


COMPUTE = ("pe", "act", "dve", "pool")
NDMASEM = 24


class Prog:
    def __init__(self, nc, stack):
        self.nc = nc
        self.stack = stack
        self.ops = {e: [] for e in ("pe", "act", "dve", "pool", "sp")}
        self.sem = {}
        for e in COMPUTE:
            self.sem[e] = stack.enter_context(nc.semaphore(f"s_{e}"))
        self.cnt = {e: 0 for e in COMPUTE}
        self.dsem = {}
        self.dcnt = {}
        self.dnext = {}
        for q in ("sp", "pool"):
            self.dsem[q] = [stack.enter_context(nc.semaphore(f"d_{q}{i}")) for i in range(NDMASEM)]
            self.dcnt[q] = [0] * NDMASEM
            self.dnext[q] = 0
        self.known = {e: {} for e in self.ops}
        self.out_tokens = []
        self.n_ops = 0

    def _need(self, eng, tok, waits):
        if tok is None:
            return
        key, val = tok[0], tok[1]
        if self.known[eng].get(key, 0) >= val:
            return
        self.known[eng][key] = val
        waits.append((key, val))

    def barrier(self):
        toks = []
        for c in COMPUTE:
            if self.cnt[c] > 0:
                toks.append((("c", c), self.cnt[c], c, False))
        for q in ("sp", "pool"):
            for slot in range(NDMASEM):
                if self.dcnt[q][slot] > 0:
                    toks.append((("d", q, slot), self.dcnt[q][slot], q, True))
        for e in self.ops:
            waits = []
            for tk in toks:
                self._need(e, tk, waits)
            if waits:
                self.ops[e].append((waits, None, None))

    def op(self, eng, fn, reads=(), writes=(), dma=False, is_out=False):
        waits = []
        for r in reads:
            rr = r.r if isinstance(r, T) else r
            if rr.lw is not None:
                if rr.lw[2] == eng and eng == "pe" and not dma and rr.lw[3] is False:
                    pass
                else:
                    self._need(eng, rr.lw, waits)
        for w in writes:
            ww = w.r if isinstance(w, T) else w
            if ww.lw is not None:
                if not (ww.lw[2] == eng and not dma and ww.lw[3] is False):
                    self._need(eng, ww.lw, waits)
            for tk in ww.rd.values():
                if not (tk[2] == eng and not dma and tk[3] is False):
                    self._need(eng, tk, waits)
        if dma:
            q = eng
            slot = self.dnext[q]
            self.dnext[q] = (slot + 1) % NDMASEM
            key = ("d", q, slot)
            prev = self.dcnt[q][slot]
            if prev > 0:
                self._need(eng, (key, prev, q, True), waits)
            self.dcnt[q][slot] = prev + 16
            tok = (key, prev + 16, q, True)
            inc = (self.dsem[q][slot], 16)
        else:
            self.cnt[eng] += 1
            tok = (("c", eng), self.cnt[eng], eng, False)
            inc = (self.sem[eng], 1)
        self.ops[eng].append((waits, fn, inc))
        for r in reads:
            rr = r.r if isinstance(r, T) else r
            rr.rd[tok[0]] = tok
        for w in writes:
            ww = w.r if isinstance(w, T) else w
            ww.lw = tok
            ww.rd = {}
        if is_out:
            self.out_tokens.append(tok)
        self.n_ops += 1
        return tok

    def semobj(self, key):
        if key[0] == "c":
            return self.sem[key[1]]
        return self.dsem[key[1]][key[2]]

    def emit(self):
        nc = self.nc
        final_waits = []
        for tok in self.out_tokens:
            self._need("sp", tok, final_waits)
        for q in ("sp", "pool"):
            for slot in range(NDMASEM):
                if self.dcnt[q][slot] > 0:
                    self._need("sp", (("d", q, slot), self.dcnt[q][slot], q, True), final_waits)
        engmap = {"pe": "tensor", "act": "scalar", "dve": "vector", "pool": "gpsimd", "sp": "sync"}
        with nc.Block() as block:
            for e, attr in engmap.items():
                ops = self.ops[e]
                extra = final_waits if e == "sp" else []

                def body(eng, ops=ops, extra=extra):
                    for waits, fn, inc in ops:
                        for key, val in waits:
                            eng.wait_ge(self.semobj(key), val)
                        if fn is None:
                            continue
                        ins = fn(eng)
                        ins.then_inc(inc[0], inc[1])
                    for key, val in extra:
                        eng.wait_ge(self.semobj(key), val)

                getattr(block, attr)(body)


def build(cfg, stages=99):
    from contextlib import ExitStack
    D, L = cfg.D, cfg.DEPTH
    NP = cfg.BATCH // NCORES
    SEQ, TS, PAST = cfg.SEQ, cfg.DEC_SEQ, cfg.PAST
    NS = NP + 1
    P_IN = 3080
    ALPHA = cfg.ALPHA
    SLOPES = [2.0 ** (-(8.0 / 4) * (h + 1)) for h in range(4)]
    NEG = -30000.0
    nc = bass.Bass("TRN2", target_bir_lowering=False)

    def din(name, shape, dt=F32):
        return nc.dram_tensor(name, list(shape), dt, kind="ExternalInput")

    def dout(name, shape, dt=F32):
        return nc.dram_tensor(name, list(shape), dt, kind="ExternalOutput")

    xp = din("xp", [NP, SEQ, D]); xs = din("xs", [TS, D])
    cache_k = din("cache_k", [L, PAST, 512]); cache_v = din("cache_v", [L, PAST, 512])
    state_gdn = din("state_gdn", [L, 4, 64, 64]); state_conv = din("state_conv", [L, 3, 768])
    c_in = din("c_in", [NS, D])
    w_ada = din("w_ada", [L, D, 6 * D]); b_ada = din("b_ada", [L, 6 * D])
    w_in = din("w_in", [L, D, P_IN])
    sgu_ln_g = din("sgu_ln_g", [L, 256]); sgu_ln_b = din("sgu_ln_b", [L, 256])
    sgu_w = din("sgu_w", [L, 4, 128, 128]); sgu_b = din("sgu_b", [L, 4, 128])
    lam_q1 = din("lam_q1", [L, 64]); lam_k1 = din("lam_k1", [L, 64])
    lam_q2 = din("lam_q2", [L, 64]); lam_k2 = din("lam_k2", [L, 64])
    diff_norm_g = din("diff_norm_g", [L, 128])
    conv_w = din("conv_w", [L, 4, 768])
    gdn_a_log = din("gdn_a_log", [L, 4]); gdn_dt_bias = din("gdn_dt_bias", [L, 4])
    gdn_norm_g = din("gdn_norm_g", [L, 64])
    w_out = din("w_out", [L, D, D])
    ln1_g = din("ln1_g", [L, D]); ln1_b = din("ln1_b", [L, D])
    peer_wq = din("peer_wq", [L, D, 2048])
    peer_keys = din("peer_keys", [L, 16, 128, 128])
    expert_u = din("expert_u", [L * 16384, D]); expert_v = din("expert_v", [L * 16384, D])
    ln2_g = din("ln2_g", [L, D]); ln2_b = din("ln2_b", [L, D])

    euv = nc.dram_tensor("euv_bf", [L * 16384, 2 * D], BF16, kind="Internal")
    y_p = dout("y_p", [NP, SEQ, D]); y_s = dout("y_s", [TS, D])
    nk_p = dout("nk_p", [L, NP, SEQ, 512]); nv_p = dout("nv_p", [L, NP, SEQ, 512])
    ngdn_p = dout("ngdn_p", [L, NP, 4, 64, 64]); nconv_p = dout("nconv_p", [L, NP, 3, 768])
    nk_s = dout("nk_s", [L, TS, 512]); nv_s = dout("nv_s", [L, TS, 512])
    ngdn_s = dout("ngdn_s", [L, 4, 64, 64]); nconv_s = dout("nconv_s", [L, 3, 768])
    nsgu_s = dout("nsgu_s", [L, TS, 256])

    with ExitStack() as st:
        P = Prog(nc, st)

        uid = [0]

        def sb(name, shape, dt=F32, stack=None):
            uid[0] += 1
            return T((stack or st).enter_context(nc.sbuf_tensor(f"{name}_{uid[0]}", list(shape), dt)), name)

        def ps(name, shape=(128, 512), dt=F32):
            return T(st.enter_context(nc.psum_tensor(name, list(shape), dt)), name)

        PS = [ps(f"ps{i}") for i in range(8)]

        def dma(out, in_, reads=(), writes=(), q="sp", is_out=False, **kw):
            P.op(q, lambda e: e.dma_start(out=out, in_=in_, **kw), reads, writes, dma=True, is_out=is_out)

        def mm(out, lhsT, rhs, reads, writes, start=True, stop=True, **kw):
            P.op("pe", lambda e: e.matmul(out, lhsT, rhs, start=start, stop=stop, **kw), reads, writes)

        def tr(out, in_, ident_ap, reads, writes):
            P.op("pe", lambda e: e.transpose(out, in_, ident_ap), reads, writes)

        def act(out, in_, func, reads, writes, **kw):
            P.op("act", lambda e: e.activation(out=out, in_=in_, func=func, **kw), reads, writes)

        def V(method, *args, reads=(), writes=(), eng="dve", **kw):
            P.op(eng, lambda e: getattr(e, method)(*args, **kw), reads, writes)

        def rsqrt(dst, src, eps, reads, dst_t, scale=1.0):
            V("tensor_scalar", dst, src, scale, eps, reads=reads, writes=[dst_t], op0=ALU.mult, op1=ALU.add)
            act(dst, dst, AF.Sqrt, [dst_t], [dst_t])
            V("reciprocal", dst, dst, reads=[dst_t], writes=[dst_t])

        def bcast_row(dram_row_ap, n):
            return dram_row_ap.partition_broadcast(128) if hasattr(dram_row_ap, "partition_broadcast") else dram_row_ap

        ident = sb("ident", [128, 128])
        identb = sb("identb", [128, 128], BF16)
        iota_p = sb("iota_p", [128, 1])
        iota_f = sb("iota_f", [128, 128])
        dif = sb("dif", [128, 128])
        triU = sb("triU", [128, 128])
        maskL = sb("maskL", [64, 64])
        maskLs = sb("maskLs", [64, 64])
        maskU = sb("maskU", [64, 64])
        ones64 = sb("ones64", [64, 64])
        ones_row = sb("ones_row", [1, 128])
        ones_rowb = sb("ones_rowb", [1, 128], BF16)
        kirow = sb("kirow", [1, 128], BF16)
        sloperow = sb("sloperow", [1, 4, 2, 128], BF16)
        nsqi = sb("nsqi", [1, 4, 2, 128], BF16)
        biasd = sb("biasd", [128, 4, 2, 128], BF16)
        tmpc = sb("tmpc", [128, 128])
        thr16 = sb("thr16", [128, 16])
        V("iota", iota_p[:], [[0, 1]], reads=[], writes=[iota_p], eng="pool", base=0, channel_multiplier=1,
          allow_small_or_imprecise_dtypes=True)
        V("iota", iota_f[:], [[1, 128]], reads=[], writes=[iota_f], eng="pool", base=0, channel_multiplier=0,
          allow_small_or_imprecise_dtypes=True)
        V("tensor_scalar", dif[:], iota_f[:], iota_p[:, 0:1], None, reads=[iota_f, iota_p], writes=[dif],
          op0=ALU.subtract)
        V("tensor_single_scalar", ident[:], dif[:], 0.0, reads=[dif], writes=[ident], op=ALU.is_equal)
        V("tensor_copy", identb[:], ident[:], reads=[ident], writes=[identb])
        V("tensor_single_scalar", triU[:], dif[:], 0.0, reads=[dif], writes=[triU], op=ALU.is_ge)
        V("tensor_single_scalar", maskU[:], dif[:64, :64], 0.0, reads=[dif], writes=[maskU], op=ALU.is_ge)
        V("tensor_single_scalar", maskL[:], dif[:64, :64], 0.0, reads=[dif], writes=[maskL], op=ALU.is_le)
        V("tensor_single_scalar", maskLs[:], dif[:64, :64], 0.0, reads=[dif], writes=[maskLs], op=ALU.is_lt)
        V("tensor_scalar", thr16[:], iota_f[:, 0:16], 1.0, 16.0, reads=[iota_f], writes=[thr16], op0=ALU.add, op1=ALU.mult)
        V("memset", ones64[:], 1.0, reads=[], writes=[ones64])
        V("memset", ones_row[:], 1.0, reads=[], writes=[ones_row])
        V("memset", ones_rowb[:], 1.0, reads=[], writes=[ones_rowb])
        V("tensor_copy", kirow[:], iota_f[0:1, :], reads=[iota_f], writes=[kirow])
        mk = sb("mk", [128, 128])
        V("tensor_single_scalar", mk[:], iota_f[:], 64.0, reads=[iota_f], writes=[mk], op=ALU.is_lt)
        V("tensor_single_scalar", tmpc[:], iota_p[:].to_broadcast([128, 128]), 64.0, reads=[iota_p], writes=[tmpc],
          op=ALU.is_ge)
        V("tensor_tensor", mk[:], mk[:], tmpc[:], reads=[mk, tmpc], writes=[mk], op=ALU.mult)
        V("tensor_single_scalar", mk[:], mk[:], NEG, reads=[mk], writes=[mk], op=ALU.mult)
        V("scalar_tensor_tensor", tmpc[:], dif[:], -1.0, dif[:], reads=[dif], writes=[tmpc], op0=ALU.mult, op1=ALU.max)
        NDD = max(PAST // 128, SEQ // 128) + 1
        kbias = sb("kbias", [128, 4, NDD])
        mk2 = sb("mk2", [128, 128])
        for h in range(4):
            for dd in range(NDD):
                V("tensor_scalar", kbias[:, h, dd:dd + 1], iota_p[:, 0:1], SLOPES[h], -SLOPES[h] * 128.0 * dd,
                  reads=[iota_p], writes=[kbias], op0=ALU.mult, op1=ALU.add)
            V("scalar_tensor_tensor", mk2[:], iota_f[:], SLOPES[h], mk[:], reads=[iota_f, mk], writes=[mk2],
              op0=ALU.mult, op1=ALU.add)
            for m in range(2):
                V("scalar_tensor_tensor", biasd[:, h, m, :], tmpc[:], -SLOPES[h], mk2[:], reads=[tmpc, mk2],
                  writes=[biasd], op0=ALU.mult, op1=ALU.add)

        modT = sb("modT", [128, L, 48, NS])
        with ExitStack() as s0:
            c_sb = sb("c_sb", [NS, D], stack=s0)
            cT = sb("cT", [128, 8, NS], stack=s0)
            wst = [sb(f"wst{i}", [128, 8, 512], stack=s0) for i in range(2)]
            brow = sb("brow", [1, 6 * D], stack=s0)
            dma(c_sb[:], c_in[:, :], [], [c_sb])
            act(c_sb[:], c_sb[:], AF.Silu, [c_sb], [c_sb])
            for kc in range(8):
                tr(PS[0][:, kc * NS:(kc + 1) * NS], c_sb[:NS, kc * 128:(kc + 1) * 128], ident[:NS, :NS],
                   [c_sb, ident], [PS[0]])
            V("tensor_copy", cT[:].rearrange("p a b -> p (a b)"), PS[0][:, 0:8 * NS], reads=[PS[0]], writes=[cT])
            for l in range(L):
                dma(brow[:], b_ada[l:l + 1, :], [], [brow])
                for cb in range(12):
                    w = wst[cb % 2]
                    dma(w[:], w_ada[l, :, cb * 512:(cb + 1) * 512].rearrange("(kc p) n -> p kc n", p=128), [], [w])
                    pt = PS[cb % 2]
                    for f4 in range(4):
                        fc = cb * 4 + f4
                        for kc in range(8):
                            mm(pt[:, f4 * NS:(f4 + 1) * NS], w[:, kc, f4 * 128:(f4 + 1) * 128], cT[:, kc, :],
                               [w, cT], [pt], start=(kc == 0), stop=False)
                        mm(pt[:, f4 * NS:(f4 + 1) * NS], brow[0:1, fc * 128:(fc + 1) * 128], ones_row[0:1, :NS],
                           [brow, ones_row], [pt], start=False, stop=True)
                    V("tensor_copy", modT[:, l, cb * 4:(cb + 1) * 4, :].rearrange("p a b -> p (a b)"),
                      pt[:, 0:4 * NS], reads=[pt], writes=[modT])
                for c0 in (8, 32):
                    V("tensor_scalar_add", modT[:, l, c0:c0 + 8, :], modT[:, l, c0:c0 + 8, :], 1.0,
                      reads=[modT], writes=[modT])
            P.barrier()

        with ExitStack() as sc:
            cin = [sb(f"cin{i}", [128, 4096], stack=sc) for i in range(2)]
            cout = [sb(f"cout{i}", [128, 4096], BF16, stack=sc) for i in range(2)]
            nchunk = L * 16384 // 512
            ci = 0
            for (src, half) in ((expert_u, 0), (expert_v, 1)):
                sv = src[:, :].rearrange("(c p r) d -> c p (r d)", p=128, r=4)
                dv = euv[:, half * D:(half + 1) * D].rearrange("(c p r) d -> c p r d", p=128, r=4)
                for c in range(nchunk):
                    a, b = cin[ci % 2], cout[ci % 2]
                    dma(a[:], sv[c], [], [a])
                    eng = ("act", "dve", "pool")[ci % 3]
                    if eng == "act":
                        act(b[:], a[:], AF.Copy, [a], [b])
                    else:
                        V("tensor_copy", b[:], a[:], reads=[a], writes=[b], eng=eng)
                    dma(dv[c], b[:].rearrange("p (r d) -> p r d", r=4), [b], [])
                    ci += 1
            P.barrier()

        wst = [sb(f"wstg{i}", [128, 2, 512]) for i in range(2)]
        x_t = sb("x_t", [128, D])
        xh = sb("xh", [128, D])
        stats = sb("stats", [128, 2, 6])
        mv = sb("mv", [128, 2])
        rstd = sb("rstd", [128, 1])
        lng = sb("lng", [128, D]); lnb = sb("lnb", [128, D])
        xres = {}

        def xr(j, t):
            return xres.setdefault((j, t), Res(f"x{j}.{t}"))

        kvres = {}

        def kvr(l, j, t):
            return kvres.setdefault((l, j, t), Res(f"kv{l}.{j}.{t}"))

        def ln_stats(src_ap, R, src_res):
            for i in range(2):
                V("bn_stats", stats[:R, i, :], src_ap[:, i * 512:(i + 1) * 512], reads=[src_res], writes=[stats])
            V("bn_aggr", mv[:R, :], stats[:R, :, :], reads=[stats], writes=[mv])
            rsqrt(rstd[:R, :], mv[:R, 1:2], 1e-5, [mv], rstd)

        def normalize(dst_t, src_t, R):
            ln_stats(src_t[:R, :], R, src_t)
            V("tensor_scalar", dst_t[:R, :], src_t[:R, :], mv[:R, 0:1], rstd[:R, 0:1], reads=[src_t, mv, rstd],
              writes=[dst_t], op0=ALU.subtract, op1=ALU.mult)

        def gate_tile(l, j, c0, Gt):
            for kc in range(8):
                pp = PS[kc // 4]
                V("tensor_copy", tmpc[:], modT[:, l, c0 + kc, j:j + 1].to_broadcast([128, 128]), reads=[modT], writes=[tmpc])
                mm(pp[:, (kc % 4) * 128:(kc % 4 + 1) * 128], tmpc[:], ident[:], [tmpc, ident], [pp])
            for hh in range(2):
                V("tensor_copy", Gt[:, hh * 512:(hh + 1) * 512], PS[hh][:, :], reads=[PS[hh]], writes=[Gt])

        def load_x(j, t, R, l, is_sample):
            if is_sample:
                src = xs[t * 128:t * 128 + R, :] if l == 0 else y_s[t * 128:t * 128 + R, :]
            else:
                src = xp[j, t * 128:t * 128 + R, :] if l == 0 else y_p[j, t * 128:t * 128 + R, :]
            dma(x_t[:R, :], src, [xr(j, t)], [x_t])

        def store_x(j, t, R, is_sample, src_t):
            dst = y_s[t * 128:t * 128 + R, :] if is_sample else y_p[j, t * 128:t * 128 + R, :]
            dma(dst, src_t[:R, :], [src_t], [xr(j, t)], is_out=True)

        def residual_ln(y_ps_list, R, dst_t):
            for hh in range(2):
                V("scalar_tensor_tensor", xh[:R, hh * 512:(hh + 1) * 512], x_t[:R, hh * 512:(hh + 1) * 512], ALPHA,
                  y_ps_list[hh], reads=[x_t] + [y_ps_list[2 + hh]], writes=[xh], op0=ALU.mult, op1=ALU.add)
            normalize(dst_t, xh, R)
            V("tensor_tensor", dst_t[:R, :], dst_t[:R, :], lng[:R, :], reads=[dst_t, lng], writes=[dst_t], op=ALU.mult)
            V("tensor_tensor", dst_t[:R, :], dst_t[:R, :], lnb[:R, :], reads=[dst_t, lnb], writes=[dst_t], op=ALU.add)

        def phase_a(l, j, T_len, is_sample):
            lam_init = 0.8 - 0.6 * math.exp(-0.3 * l)
            nt = (T_len + 127) // 128
            with ExitStack() as sa:
                def sba(name, shape, dt=F32):
                    return sb(name, shape, dt, stack=sa)
                w_in_sb = sba("w_in_sb", [128, 8, P_IN], BF16)
                w_out_sb = sba("w_out_sb", [128, 8, D], BF16)
                hT = sba("hT", [128, 8, 128], BF16)
                p_sb = sba("p_sb", [128, P_IN])
                mix = sba("mix", [128, D])
                mixT = sba("mixT", [128, 8, 128], BF16)
                wTs = sba("wTs", [128, 4, 128])
                bsT = sba("bsT", [128, 4])
                sg_g = sba("sg_g", [128, 256]); sg_b = sba("sg_b", [128, 256])
                uv = sba("uv", [128, 512]); gtmp = sba("gtmp", [128, 512])
                vst = sba("vst", [128, 4, 2]); vn = sba("vn", [128, 256]); vsq = sba("vsq", [128, 256])
                lamc = sba("lamc", [128, 4]); lamv = sba("lamv", [128, 4, 64]); nlam = sba("nlam", [128, 1])
                dng = sba("dng", [128, 128])
                Qblk = sba("Qblk", [128, 4, 2, 128], BF16)
                kst = [sba("kst0", [128, 512])] * 2
                vstg = [sba("vstg0", [128, 512])] * 2
                kTb = sba("kTb", [128, 4, 128], BF16)
                vb = sba("vb", [128, 4, 130], BF16)
                pT = sba("pT", [128, 2, 128], BF16)
                anum = sba("anum", [128, 8, 130]); aden = sba("aden", [128, 8])
                aout = sba("aout", [128, 4, 128]); asq = sba("asq", [128, 4, 128]); ass = sba("ass", [128, 4])
                cw = sba("cw", [64, 12, 4])
                negA = sba("negA", [64, 4]); dtb = sba("dtb", [64, 4]); gng = sba("gng", [64, 64])
                cbuf = sba("cbuf", [64, 12, 131]); cv = sba("cv", [64, 12, 128]); ctmp = sba("ctmp", [64, 12, 128])
                S_sb = sba("S_sb", [64, 4, 64])
                ktm = sba("ktm", [64, 4, 64]); vtm = sba("vtm", [64, 4, 64])
                gab = sba("gab", [64, 8]); gg = sba("gg", [64, 4]); beta = sba("beta", [64, 4])
                gB = sba("gB", [64, 4, 64]); gc_tm = sba("gc_tm", [64, 4]); Grow = sba("Grow", [64, 4, 64])
                D1 = sba("D1", [64, 4, 64]); E1 = sba("E1", [64, 4, 64]); E2 = sba("E2", [64, 4, 64])
                egc = sba("egc", [64, 4]); egrow = sba("egrow", [64, 4, 64]); kdf = sba("kdf", [64, 4])
                glc = sba("glc", [64, 4])
                Am = [sba(f"Am{i}", [64, 4, 64]) for i in range(2)]
                Bm = [sba(f"Bm{i}", [64, 4, 64]) for i in range(2)]
                XT = sba("XT", [64, 4, 64])
                Vb = gB; Kg = sba("Kg", [64, 4, 64]); kdec = sba("kdec", [64, 4, 64])
                u_sb = sba("u_sb", [64, 4, 64]); wT_sb = sba("wT_sb", [64, 4, 64])
                aqkT = sba("aqkT", [64, 4, 64]); qgT = sba("qgT", [64, 4, 64]); vnew = sba("vnew", [64, 4, 64])
                o_sb = E1; osq = D1; oss = sba("oss", [64, 4])
                z_sb = sba("z_sb", [64, 256])

                Gt = mix
                gate_tile(l, j, 16, Gt)
                wi = 0
                for cb in range(7):
                    n0 = cb * 512
                    n1 = min(P_IN, n0 + 512)
                    for kp in range(4):
                        w = wst[wi % 2]
                        dma(w[:, :, :n1 - n0], w_in[l, kp * 256:(kp + 1) * 256, n0:n1].rearrange("(kc p) n -> p kc n", p=128), [], [w])
                        V("tensor_copy", w_in_sb[:, 2 * kp:2 * kp + 2, n0:n1], w[:, :, :n1 - n0], reads=[w], writes=[w_in_sb],
                          eng=("pool" if wi % 2 == 0 else "dve"))
                        wi += 1
                for cb in range(2):
                    n0 = cb * 512
                    for kp in range(4):
                        w = wst[wi % 2]
                        dma(w[:], w_out[l, kp * 256:(kp + 1) * 256, n0:n0 + 512].rearrange("(kc p) n -> p kc n", p=128), [], [w])
                        for k2 in range(2):
                            V("tensor_tensor", w_out_sb[:, 2 * kp + k2, n0:n0 + 512], w[:, k2, :], Gt[:, n0:n0 + 512],
                              reads=[w, Gt], writes=[w_out_sb], op=ALU.mult, eng=("pool" if wi % 2 == 0 else "dve"))
                        wi += 1
                dma(lng[:], ln1_g[l:l + 1, :].partition_broadcast(128), [], [lng])
                dma(lnb[:], ln1_b[l:l + 1, :].partition_broadcast(128), [], [lnb])
                wraw = p_sb
                dma(p_sb[:, 1536:2048].rearrange("p (g j) -> p g j", g=4), sgu_w[l].rearrange("g i j -> i g j"), [], [p_sb])
                for g in range(4):
                    tr(PS[2][:, g * 128:(g + 1) * 128], p_sb[:, 1536 + g * 128:1536 + (g + 1) * 128], ident[:], [p_sb, ident], [PS[2]])
                V("tensor_copy", wTs[:].rearrange("p a b -> p (a b)"), PS[2][:, :], reads=[PS[2]], writes=[wTs])
                V("memset", wTs[64:128, :, 0:64], 0.0, reads=[], writes=[wTs])
                dma(p_sb[:4, 2048:2176], sgu_b[l], [], [p_sb])
                tr(PS[3][:, 0:4], p_sb[:4, 2048:2176], ident[:4, :4], [p_sb, ident], [PS[3]])
                V("tensor_copy", bsT[:], PS[3][:, 0:4], reads=[PS[3]], writes=[bsT])
                dma(sg_g[:], sgu_ln_g[l:l + 1, :].partition_broadcast(128), [], [sg_g])
                dma(sg_b[:], sgu_ln_b[l:l + 1, :].partition_broadcast(128), [], [sg_b])
                for i, lq in enumerate((lam_q1, lam_k1, lam_q2, lam_k2)):
                    dma(lamv[:, i, :], lq[l:l + 1, :].partition_broadcast(128), [], [lamv])
                V("tensor_tensor", lamv[:, 0, :], lamv[:, 0, :], lamv[:, 1, :], reads=[lamv], writes=[lamv], op=ALU.mult)
                V("tensor_tensor", lamv[:, 2, :], lamv[:, 2, :], lamv[:, 3, :], reads=[lamv], writes=[lamv], op=ALU.mult)
                V("tensor_reduce", lamc[:, 0:1], lamv[:, 0, :], reads=[lamv], writes=[lamc], axis=AX.X, op=ALU.add)
                V("tensor_reduce", lamc[:, 1:2], lamv[:, 2, :], reads=[lamv], writes=[lamc], axis=AX.X, op=ALU.add)
                act(lamc[:, 0:2], lamc[:, 0:2], AF.Exp, [lamc], [lamc])
                V("tensor_tensor", nlam[:], lamc[:, 1:2], lamc[:, 0:1], reads=[lamc], writes=[nlam], op=ALU.subtract)
                V("tensor_scalar_add", nlam[:], nlam[:], -lam_init, reads=[nlam], writes=[nlam])
                dma(dng[:], diff_norm_g[l:l + 1, :].partition_broadcast(128), [], [dng])
                V("tensor_single_scalar", dng[:], dng[:], 1.0 - lam_init, reads=[dng], writes=[dng], op=ALU.mult)
                V("memset", Qblk[:], 0.0, reads=[], writes=[Qblk])
                V("memset", vb[:, :, 128:130], 1.0, reads=[], writes=[vb])
                dma(p_sb[:4, 0:768], conv_w[l], [], [p_sb])
                for g in range(12):
                    tr(PS[3][:64, 16 + g * 4:16 + g * 4 + 4], p_sb[:4, g * 64:(g + 1) * 64], ident[:4, :4],
                       [p_sb, ident], [PS[3]])
                V("tensor_copy", cw[:].rearrange("p a b -> p (a b)"), PS[3][:64, 16:64], reads=[PS[3]], writes=[cw])
                dma(negA[:], gdn_a_log[l:l + 1, :].partition_broadcast(64), [], [negA])
                act(negA[:], negA[:], AF.Exp, [negA], [negA])
                V("tensor_single_scalar", negA[:], negA[:], -1.0, reads=[negA], writes=[negA], op=ALU.mult)
                dma(dtb[:], gdn_dt_bias[l:l + 1, :].partition_broadcast(64), [], [dtb])
                dma(gng[:], gdn_norm_g[l:l + 1, :].partition_broadcast(64), [], [gng])
                if is_sample:
                    dma(S_sb[:], state_gdn[l].rearrange("h k v -> k h v"), [], [S_sb])
                    dma(p_sb[:3, 768:1536], state_conv[l], [], [p_sb])
                    for g in range(12):
                        tr(PS[3][:64, 64 + g * 3:64 + g * 3 + 3], p_sb[:3, 768 + g * 64:768 + (g + 1) * 64], ident[:3, :3],
                           [p_sb, ident], [PS[3]])
                    V("tensor_copy", cbuf[:, :, 0:3], PS[3][:64, 64:100].rearrange("p (a b) -> p a b", b=3),
                      reads=[PS[3]], writes=[cbuf])
                else:
                    V("memset", S_sb[:], 0.0, reads=[], writes=[S_sb])
                    V("memset", cbuf[:, :, 0:3], 0.0, reads=[], writes=[cbuf])

                def attn_block(k_ap, v_ap, kv_reads, nk, R, diag, delta, first):
                    for h in range(4):
                        tr(PS[4][:, h * 128:h * 128 + nk], k_ap[:, h * 128:(h + 1) * 128], ident[:nk, :nk],
                           kv_reads + [ident], [PS[4]])
                    act(kTb[:, :, :nk], PS[4][:, :].rearrange("p (a b) -> p a b", b=128)[:, :, :nk], AF.Copy,
                        [PS[4]], [kTb])
                    V("tensor_copy", vb[:nk, :, 0:128], v_ap.rearrange("p (h d) -> p h d", h=4), reads=kv_reads, writes=[vb])
                    dd = delta // 128
                    for h in range(4):
                        sc = PS[5 + (h % 2)]
                        scv = sc[:nk, 0:256].rearrange("p (m q) -> p m q", m=2)[:, :, :R]
                        if diag:
                            mm(scv, kTb[:, h, :nk], Qblk[:, h, :, :R], [kTb, Qblk], [sc], start=True, stop=False)
                            mm(scv, identb[:nk, :nk], biasd[:nk, h, :, :R], [identb, biasd], [sc], start=False, stop=True)
                            act(pT[:nk, :, :R], scv, AF.Exp, [sc], [pT])
                        else:
                            mm(scv, kTb[:, h, :nk], Qblk[:, h, :, :R], [kTb, Qblk], [sc], start=True, stop=True)
                            act(pT[:nk, :, :R], scv, AF.Exp, [sc, kbias], [pT], bias=kbias[:nk, h, dd:dd + 1])
                        pv = PS[7]
                        for m in range(2):
                            mm(pv[:R, m * 130:m * 130 + 129], pT[:nk, m, :R], vb[:nk, h, 0:129], [pT, vb], [pv])
                        pvv = pv[:R, 0:260].rearrange("p (m d) -> p m d", m=2)[:, :, 0:129]
                        if first:
                            V("tensor_copy", anum[:R, 2 * h:2 * h + 2, 0:129], pvv, reads=[pv], writes=[anum])
                        else:
                            V("tensor_tensor", anum[:R, 2 * h:2 * h + 2, 0:129], anum[:R, 2 * h:2 * h + 2, 0:129], pvv,
                              reads=[pv, anum], writes=[anum], op=ALU.add)

                for t in range(nt):
                    R = min(128, T_len - t * 128)
                    load_x(j, t, R, l, is_sample)
                    normalize(xh, x_t, R)
                    for kc in range(8):
                        pp = PS[kc // 4]
                        tr(pp[:, (kc % 4) * 128:(kc % 4) * 128 + R], xh[:R, kc * 128:(kc + 1) * 128], ident[:R, :R],
                           [xh, ident], [pp])
                    for kc in range(8):
                        pp = PS[kc // 4]
                        act(hT[:, kc, :R], pp[:, (kc % 4) * 128:(kc % 4) * 128 + R], AF.Identity, [pp, modT], [hT],
                            scale=modT[:, l, 8 + kc, j:j + 1], bias=modT[:, l, 0 + kc, j:j + 1])
                    cbs = [0, 2, 3] + ([4, 5] if t == nt - 1 else [])
                    for ci, cb in enumerate(cbs):
                        n0 = cb * 512
                        n1 = min(P_IN, n0 + 512)
                        pb = PS[2 + ci % 2]
                        for kc in range(8):
                            mm(pb[:R, :n1 - n0], hT[:, kc, :R], w_in_sb[:, kc, n0:n1], [hT, w_in_sb], [pb],
                               start=(kc == 0), stop=(kc == 7))
                        if ci % 2 == 0:
                            act(p_sb[:R, n0:n1], pb[:R, :n1 - n0], AF.Copy, [pb], [p_sb])
                        else:
                            V("tensor_copy", p_sb[:R, n0:n1], pb[:R, :n1 - n0], reads=[pb], writes=[p_sb])
                    kdst = nk_s[l, t * 128:t * 128 + R, :] if is_sample else nk_p[l, j, t * 128:t * 128 + R, :]
                    vdst = nv_s[l, t * 128:t * 128 + R, :] if is_sample else nv_p[l, j, t * 128:t * 128 + R, :]
                    dma(kdst, p_sb[:R, 1024:1536], [p_sb], [kvr(l, j, t)], is_out=True)
                    dma(vdst, p_sb[:R, 1536:2048], [p_sb], [kvr(l, j, t)], is_out=True)
                    if t == nt - 1:
                        dst = nconv_s[l, :, :] if is_sample else nconv_p[l, j, :, :]
                        dma(dst, p_sb[R - 3:R, 2048:2816], [p_sb], [], is_out=True)

                    if stages >= 2:
                        V("tensor_tensor", gtmp[:R, :], p_sb[:R, 0:512], p_sb[:R, 0:512], reads=[p_sb], writes=[gtmp], op=ALU.mult)
                        V("tensor_scalar", gtmp[:R, :], gtmp[:R, :], 0.044715, 1.0, reads=[gtmp], writes=[gtmp],
                          op0=ALU.mult, op1=ALU.add)
                        V("tensor_tensor", gtmp[:R, :], gtmp[:R, :], p_sb[:R, 0:512], reads=[gtmp, p_sb], writes=[gtmp], op=ALU.mult)
                        act(gtmp[:R, :], gtmp[:R, :], AF.Sigmoid, [gtmp], [gtmp], scale=2.0 * math.sqrt(2.0 / math.pi))
                        V("tensor_tensor", uv[:R, :], gtmp[:R, :], p_sb[:R, 0:512], reads=[gtmp, p_sb], writes=[uv], op=ALU.mult)
                        v3 = uv[:R, 256:512].rearrange("p (g d) -> p g d", g=4)
                        V("tensor_reduce", vst[:R, :, 0], v3, reads=[uv], writes=[vst], axis=AX.X, op=ALU.add)
                        V("tensor_tensor", vsq[:R, :], uv[:R, 256:512], uv[:R, 256:512], reads=[uv], writes=[vsq], op=ALU.mult)
                        V("tensor_reduce", vst[:R, :, 1], vsq[:R, :].rearrange("p (g d) -> p g d", g=4), reads=[vsq],
                          writes=[vst], axis=AX.X, op=ALU.add)
                        V("tensor_single_scalar", vst[:R, :, :], vst[:R, :, :], 1.0 / 64, reads=[vst], writes=[vst], op=ALU.mult)
                        V("tensor_tensor", vsq[:R, 0:4], vst[:R, :, 0], vst[:R, :, 0], reads=[vst], writes=[vsq], op=ALU.mult)
                        V("tensor_tensor", vst[:R, :, 1], vst[:R, :, 1], vsq[:R, 0:4], reads=[vst, vsq], writes=[vst], op=ALU.subtract)
                        rsqrt(vst[:R, :, 1], vst[:R, :, 1], 1e-5, [vst], vst)
                        vn3 = vn[:R, :].rearrange("p (g d) -> p g d", g=4)
                        V("tensor_tensor", vn3, v3, vst[:R, :, 0:1].to_broadcast([R, 4, 64]), reads=[uv, vst], writes=[vn], op=ALU.subtract)
                        V("tensor_tensor", vn3, vn3, vst[:R, :, 1:2].to_broadcast([R, 4, 64]), reads=[vn, vst], writes=[vn], op=ALU.mult)
                        V("tensor_tensor", vn[:R, :], vn[:R, :], sg_g[:R, :], reads=[vn, sg_g], writes=[vn], op=ALU.mult)
                        V("tensor_tensor", vn[:R, :], vn[:R, :], sg_b[:R, :], reads=[vn, sg_b], writes=[vn], op=ALU.add)
                        if is_sample:
                            dma(nsgu_s[l, t * 128:t * 128 + R, :], vn[:R, :], [vn], [], is_out=True)
                        for g in range(4):
                            mm(PS[0][:R, g * 64:(g + 1) * 64], wTs[:R, g, :R], vn[:R, g * 64:(g + 1) * 64], [wTs, vn], [PS[0]])
                        for g in range(4):
                            V("scalar_tensor_tensor", mix[:R, g * 64:(g + 1) * 64], PS[0][:R, g * 64:(g + 1) * 64],
                              bsT[:R, g:g + 1], uv[:R, g * 64:(g + 1) * 64], reads=[PS[0], bsT, uv], writes=[mix],
                              op0=ALU.add, op1=ALU.mult)
                    else:
                        V("memset", mix[:R, 0:256], 0.0, reads=[], writes=[mix])

                    if stages >= 3:
                        for h in range(4):
                            pq = PS[4 + (h % 2)]
                            for kc in range(8):
                                mm(pq[:, :R], w_in_sb[:, kc, 512 + h * 128:512 + (h + 1) * 128], hT[:, kc, :R],
                                   [w_in_sb, hT], [pq], start=(kc == 0), stop=(kc == 7))
                            act(Qblk[0:64, h, 0, :R], pq[0:64, :R], AF.Copy, [pq], [Qblk], scale=0.125)
                            act(Qblk[64:128, h, 1, :R], pq[64:128, :R], AF.Copy, [pq], [Qblk], scale=0.125)
                        qpos0 = (PAST if is_sample else 0) + t * 128
                        nblk = 0
                        if is_sample:
                            for kb in range(PAST // 128):
                                ks, vs = kst[nblk % 2], vstg[nblk % 2]
                                dma(ks[:], cache_k[l, kb * 128:(kb + 1) * 128, :], [], [ks])
                                dma(vs[:], cache_v[l, kb * 128:(kb + 1) * 128, :], [], [vs])
                                attn_block(ks[:, :], vs[:, :], [ks, vs], 128, R, False, qpos0 - kb * 128, nblk == 0)
                                nblk += 1
                        for kb in range(t):
                            ks, vs = kst[nblk % 2], vstg[nblk % 2]
                            ksrc = nk_s[l, kb * 128:(kb + 1) * 128, :] if is_sample else nk_p[l, j, kb * 128:(kb + 1) * 128, :]
                            vsrc = nv_s[l, kb * 128:(kb + 1) * 128, :] if is_sample else nv_p[l, j, kb * 128:(kb + 1) * 128, :]
                            dma(ks[:], ksrc, [kvr(l, j, kb)], [ks])
                            dma(vs[:], vsrc, [kvr(l, j, kb)], [vs])
                            attn_block(ks[:, :], vs[:, :], [ks, vs], 128, R, False, (t - kb) * 128, nblk == 0)
                            nblk += 1
                        attn_block(p_sb[:R, 1024:1536], p_sb[:R, 1536:2048], [p_sb], R, R, True, 0, nblk == 0)
                        V("reciprocal", aden[:R, :], anum[:R, :, 128], reads=[anum], writes=[aden])
                        V("tensor_tensor", anum[:R, :, 0:128], anum[:R, :, 0:128], aden[:R, :].unsqueeze(2).to_broadcast([R, 8, 128]),
                          reads=[anum, aden], writes=[anum], op=ALU.mult)
                        a4 = anum[:R, :, 0:128].rearrange("p (h m) d -> p h m d", m=2)
                        V("scalar_tensor_tensor", aout[:R, :, :], a4[:, :, 1, :], nlam[:R, 0:1], a4[:, :, 0, :],
                          reads=[anum, nlam], writes=[aout], op0=ALU.mult, op1=ALU.add)
                        V("tensor_tensor", asq[:R, :, :], aout[:R, :, :], aout[:R, :, :], reads=[aout], writes=[asq], op=ALU.mult)
                        V("tensor_reduce", ass[:R, :], asq[:R, :, :], reads=[asq], writes=[ass], axis=AX.X, op=ALU.add)
                        rsqrt(ass[:R, :], ass[:R, :], 1e-5, [ass], ass, scale=1.0 / 128)
                        V("tensor_tensor", aout[:R, :, :], aout[:R, :, :], ass[:R, :].unsqueeze(2).to_broadcast([R, 4, 128]),
                          reads=[aout, ass], writes=[aout], op=ALU.mult)
                        V("tensor_tensor", mix[:R, 256:768].rearrange("p (h d) -> p h d", h=4), aout[:R, :, :],
                          dng[:R, :].unsqueeze(1).to_broadcast([R, 4, 128]), reads=[aout, dng], writes=[mix], op=ALU.mult)
                    else:
                        V("memset", mix[:R, 256:768], 0.0, reads=[], writes=[mix])

                    if stages >= 4:
                        for g in range(12):
                            pg = PS[4 + (g // 4)]
                            col = 2048 + g * 64
                            for kc in range(8):
                                mm(pg[:64, (g % 4) * 128:(g % 4) * 128 + R], w_in_sb[:, kc, col:col + 64], hT[:, kc, :R],
                                   [w_in_sb, hT], [pg], start=(kc == 0), stop=(kc == 7))
                        for q3 in range(3):
                            act(cbuf[:, q3 * 4:(q3 + 1) * 4, 3:3 + R],
                                PS[4 + q3][:64, :].rearrange("p (a b) -> p a b", b=128)[:, :, :R], AF.Copy,
                                [PS[4 + q3]], [cbuf])
                        for jt in range(4):
                            wj = cw[:, :, jt:jt + 1].to_broadcast([64, 12, R])
                            if jt == 0:
                                V("tensor_tensor", cv[:, :, :R], cbuf[:, :, jt:jt + R], wj, reads=[cbuf, cw], writes=[cv], op=ALU.mult)
                            else:
                                V("tensor_tensor", ctmp[:, :, :R], cbuf[:, :, jt:jt + R], wj, reads=[cbuf, cw], writes=[ctmp], op=ALU.mult)
                                V("tensor_tensor", cv[:, :, :R], cv[:, :, :R], ctmp[:, :, :R], reads=[cv, ctmp], writes=[cv], op=ALU.add)
                        V("tensor_copy", ctmp[:, :, 0:3], cbuf[:, :, R:R + 3], reads=[cbuf], writes=[ctmp])
                        V("tensor_copy", cbuf[:, :, 0:3], ctmp[:, :, 0:3], reads=[ctmp], writes=[cbuf])
                        act(cv[:, :, :R], cv[:, :, :R], AF.Silu, [cv], [cv])
                        V("tensor_tensor", ctmp[:, 0:8, :R], cv[:, 0:8, :R], cv[:, 0:8, :R], reads=[cv], writes=[ctmp], op=ALU.mult)
                        for q2 in range(2):
                            mm(PS[4 + q2][:64, :].rearrange("p (a b) -> p a b", b=128)[:, :, :R], ones64[:, :],
                               ctmp[:, q2 * 4:(q2 + 1) * 4, :R], [ones64, ctmp], [PS[4 + q2]])
                            V("tensor_scalar_add", ctmp[:, q2 * 4:(q2 + 1) * 4, :R],
                              PS[4 + q2][:64, :].rearrange("p (a b) -> p a b", b=128)[:, :, :R], 1e-6,
                              reads=[PS[4 + q2]], writes=[ctmp])
                        act(ctmp[:, 0:8, :R], ctmp[:, 0:8, :R], AF.Sqrt, [ctmp], [ctmp])
                        V("reciprocal", ctmp[:, 0:8, :R], ctmp[:, 0:8, :R], reads=[ctmp], writes=[ctmp])
                        V("tensor_tensor", cv[:, 0:8, :R], cv[:, 0:8, :R], ctmp[:, 0:8, :R], reads=[cv, ctmp], writes=[cv], op=ALU.mult)
                        V("tensor_single_scalar", cv[:, 0:4, :R], cv[:, 0:4, :R], 0.125, reads=[cv], writes=[cv], op=ALU.mult)
                        for c0 in range(0, R, 64):
                            c = min(64, R - c0)
                            nsteps = max(0, int(math.ceil(math.log2(c))) - 1)
                            for g in range(8):
                                tr(PS[6][:c, g * 64:(g + 1) * 64], cv[:, 4 + g, c0:c0 + c], ident[:64, :64], [cv, ident], [PS[6]])
                            V("tensor_copy", ktm[:c, :, :], PS[6][:c, 0:256].rearrange("p (a b) -> p a b", b=64), reads=[PS[6]], writes=[ktm])
                            V("tensor_copy", vtm[:c, :, :], PS[6][:c, 256:512].rearrange("p (a b) -> p a b", b=64), reads=[PS[6]], writes=[vtm])
                            for kc in range(8):
                                mm(PS[7][:c, 0:8], hT[:, kc, c0:c0 + c], w_in_sb[:, kc, 3072:3080], [hT, w_in_sb], [PS[7]],
                                   start=(kc == 0), stop=(kc == 7))
                            for kc in range(8):
                                mm(PS[7][:c, 64:320], hT[:, kc, c0:c0 + c], w_in_sb[:, kc, 2816:3072], [hT, w_in_sb], [PS[7]],
                                   start=(kc == 0), stop=(kc == 7))
                            V("tensor_tensor", gab[:c, 0:4], PS[7][:c, 0:4], dtb[:c, :], reads=[PS[7], dtb], writes=[gab], op=ALU.add)
                            act(gab[:c, 0:4], gab[:c, 0:4], AF.Exp, [gab], [gab])
                            V("tensor_scalar_add", gab[:c, 0:4], gab[:c, 0:4], 1.0, reads=[gab], writes=[gab])
                            act(gab[:c, 0:4], gab[:c, 0:4], AF.Ln, [gab], [gab])
                            V("tensor_tensor", gg[:c, :], gab[:c, 0:4], negA[:c, :], reads=[gab, negA], writes=[gg], op=ALU.mult)
                            act(beta[:c, :], PS[7][:c, 4:8], AF.Sigmoid, [PS[7]], [beta])
                            act(z_sb[:c, :], PS[7][:c, 64:320], AF.Silu, [PS[7]], [z_sb])
                            V("tensor_copy", gB[:c, :, :], gg[:c, :].unsqueeze(2).to_broadcast([c, 4, 64]), reads=[gg], writes=[gB])
                            mm(PS[6][:c, 0:4], triU[:c, :c], gg[:c, :], [triU, gg], [PS[6]])
                            for h in range(4):
                                mm(PS[6][:64, 64 + h * 64:64 + h * 64 + c], gB[:c, h, :], triU[:c, :c], [gB, triU], [PS[6]])
                            V("tensor_copy", gc_tm[:c, :], PS[6][:c, 0:4], reads=[PS[6]], writes=[gc_tm])
                            V("tensor_copy", Grow[:, :, :c], PS[6][:64, 64:320].rearrange("p (a b) -> p a b", b=64)[:, :, :c],
                              reads=[PS[6]], writes=[Grow])
                            V("tensor_tensor", D1[:c, :, :c], gc_tm[:c, :].unsqueeze(2).to_broadcast([c, 4, c]), Grow[:c, :, :c],
                              reads=[gc_tm, Grow], writes=[D1], op=ALU.subtract)
                            V("tensor_single_scalar", E1[:c, :, :c], D1[:c, :, :c], 0.0, reads=[D1], writes=[E1], op=ALU.min)
                            act(E1[:c, :, :c], E1[:c, :, :c], AF.Exp, [E1], [E1])
                            V("tensor_scalar", E2[:c, :, :c], D1[:c, :, :c], -1.0, 0.0, reads=[D1], writes=[E2], op0=ALU.mult, op1=ALU.min)
                            act(E2[:c, :, :c], E2[:c, :, :c], AF.Exp, [E2], [E2])
                            act(egc[:c, :], gc_tm[:c, :], AF.Exp, [gc_tm], [egc])
                            act(egrow[:, :, :c], Grow[:, :, :c], AF.Exp, [Grow], [egrow])
                            act(glc[:, :], Grow[:, :, c - 1], AF.Exp, [Grow], [glc])
                            V("tensor_tensor", kdf[:c, :], Grow[:c, :, c - 1], gc_tm[:c, :], reads=[Grow, gc_tm], writes=[kdf], op=ALU.subtract)
                            act(kdf[:c, :], kdf[:c, :], AF.Exp, [kdf], [kdf])
                            for h in range(4):
                                mm(PS[4][:c, h * 64:h * 64 + c], cv[:, 4 + h, c0:c0 + c], cv[:, 4 + h, c0:c0 + c], [cv], [PS[4]])
                                mm(PS[4][:c, 256 + h * 64:256 + h * 64 + c], cv[:, 4 + h, c0:c0 + c], cv[:, h, c0:c0 + c], [cv], [PS[4]])
                            V("tensor_tensor", D1[:c, :, :c], E1[:c, :, :c], maskLs[:c, :c].unsqueeze(1).to_broadcast([c, 4, c]),
                              reads=[E1, maskLs], writes=[D1], op=ALU.mult)
                            A0, B0 = Am[0], Bm[0]
                            V("tensor_tensor", A0[:c, :, :c], PS[4][:c, 0:256].rearrange("p (a b) -> p a b", b=64)[:, :, :c],
                              D1[:c, :, :c], reads=[PS[4], D1], writes=[A0], op=ALU.mult)
                            V("tensor_tensor", A0[:c, :, :c], A0[:c, :, :c], beta[:c, :].unsqueeze(2).to_broadcast([c, 4, c]),
                              reads=[A0, beta], writes=[A0], op=ALU.mult)
                            V("tensor_tensor", E2[:c, :, :c], E2[:c, :, :c], maskU[:c, :c].unsqueeze(1).to_broadcast([c, 4, c]),
                              reads=[E2, maskU], writes=[E2], op=ALU.mult)
                            V("tensor_tensor", aqkT[:c, :, :c], PS[4][:c, 256:512].rearrange("p (a b) -> p a b", b=64)[:, :, :c],
                              E2[:c, :, :c], reads=[PS[4], E2], writes=[aqkT], op=ALU.mult)
                            for h in range(4):
                                tr(PS[5][:c, h * 64:h * 64 + c], A0[:c, h, :c], ident[:c, :c], [A0, ident], [PS[5]])
                            V("tensor_copy", B0[:c, :, :c], PS[5][:c, 0:256].rearrange("p (a b) -> p a b", b=64)[:, :, :c],
                              reads=[PS[5]], writes=[B0])
                            V("tensor_tensor", XT[:c, :, :c], ident[:c, :c].unsqueeze(1).to_broadcast([c, 4, c]), B0[:c, :, :c],
                              reads=[ident, B0], writes=[XT], op=ALU.subtract)
                            cur = 0
                            for n in range(1, nsteps + 1):
                                Ac, Bc, An, Bn = Am[cur], Bm[cur], Am[1 - cur], Bm[1 - cur]
                                for h in range(4):
                                    mm(PS[4][:c, h * 64:h * 64 + c], Bc[:c, h, :c], Ac[:c, h, :c], [Ac, Bc], [PS[4]])
                                    if n < nsteps:
                                        mm(PS[4][:c, 256 + h * 64:256 + h * 64 + c], Ac[:c, h, :c], Bc[:c, h, :c], [Ac, Bc], [PS[4]])
                                V("tensor_copy", An[:c, :, :c], PS[4][:c, 0:256].rearrange("p (a b) -> p a b", b=64)[:, :, :c],
                                  reads=[PS[4]], writes=[An])
                                if n < nsteps:
                                    V("tensor_copy", Bn[:c, :, :c], PS[4][:c, 256:512].rearrange("p (a b) -> p a b", b=64)[:, :, :c],
                                      reads=[PS[4]], writes=[Bn])
                                for h in range(4):
                                    mm(PS[5][:c, h * 64:h * 64 + c], An[:c, h, :c], XT[:c, h, :c], [An, XT], [PS[5]])
                                V("tensor_tensor", XT[:c, :, :c], XT[:c, :, :c], PS[5][:c, 0:256].rearrange("p (a b) -> p a b", b=64)[:, :, :c],
                                  reads=[XT, PS[5]], writes=[XT], op=ALU.add)
                                cur = 1 - cur
                            V("tensor_tensor", Vb[:c, :, :], vtm[:c, :, :], beta[:c, :].unsqueeze(2).to_broadcast([c, 4, 64]),
                              reads=[vtm, beta], writes=[Vb], op=ALU.mult)
                            V("tensor_tensor", Kg[:c, :, :], ktm[:c, :, :], beta[:c, :].unsqueeze(2).to_broadcast([c, 4, 64]),
                              reads=[ktm, beta], writes=[Kg], op=ALU.mult)
                            V("tensor_tensor", Kg[:c, :, :], Kg[:c, :, :], egc[:c, :].unsqueeze(2).to_broadcast([c, 4, 64]),
                              reads=[Kg, egc], writes=[Kg], op=ALU.mult)
                            V("tensor_tensor", kdec[:c, :, :], ktm[:c, :, :], kdf[:c, :].unsqueeze(2).to_broadcast([c, 4, 64]),
                              reads=[ktm, kdf], writes=[kdec], op=ALU.mult)
                            V("tensor_tensor", qgT[:, :, :c], cv[:, 0:4, c0:c0 + c], egrow[:, :, :c], reads=[cv, egrow], writes=[qgT], op=ALU.mult)
                            for h in range(4):
                                mm(PS[6][:c, h * 64:(h + 1) * 64], XT[:c, h, :c], Vb[:c, h, :], [XT, Vb], [PS[6]])
                                mm(PS[6][:64, 256 + h * 64:256 + h * 64 + c], Kg[:c, h, :], XT[:c, h, :c], [Kg, XT], [PS[6]])
                            V("tensor_copy", u_sb[:c, :, :], PS[6][:c, 0:256].rearrange("p (a b) -> p a b", b=64), reads=[PS[6]], writes=[u_sb])
                            V("tensor_copy", wT_sb[:, :, :c], PS[6][:64, 256:512].rearrange("p (a b) -> p a b", b=64)[:, :, :c],
                              reads=[PS[6]], writes=[wT_sb])
                            for h in range(4):
                                mm(PS[5][:c, 256 + h * 64:256 + (h + 1) * 64], wT_sb[:, h, :c], S_sb[:, h, :], [wT_sb, S_sb], [PS[5]])
                            V("tensor_tensor", vnew[:c, :, :], u_sb[:c, :, :], PS[5][:c, 256:512].rearrange("p (a b) -> p a b", b=64),
                              reads=[u_sb, PS[5]], writes=[vnew], op=ALU.subtract)
                            for h in range(4):
                                mm(PS[6][:c, h * 64:(h + 1) * 64], qgT[:, h, :c], S_sb[:, h, :], [qgT, S_sb], [PS[6]], start=True, stop=False)
                                mm(PS[6][:c, h * 64:(h + 1) * 64], aqkT[:c, h, :c], vnew[:c, h, :], [aqkT, vnew], [PS[6]], start=False, stop=True)
                            for h in range(4):
                                mm(PS[5][:64, h * 64:(h + 1) * 64], kdec[:c, h, :], vnew[:c, h, :], [kdec, vnew], [PS[5]])
                            V("tensor_tensor", S_sb[:, :, :], S_sb[:, :, :], glc[:, :].unsqueeze(2).to_broadcast([64, 4, 64]),
                              reads=[S_sb, glc], writes=[S_sb], op=ALU.mult)
                            V("tensor_tensor", S_sb[:, :, :], S_sb[:, :, :], PS[5][:64, 0:256].rearrange("p (a b) -> p a b", b=64),
                              reads=[S_sb, PS[5]], writes=[S_sb], op=ALU.add)
                            V("tensor_copy", o_sb[:c, :, :], PS[6][:c, 0:256].rearrange("p (a b) -> p a b", b=64), reads=[PS[6]], writes=[o_sb])
                            V("tensor_tensor", osq[:c, :, :], o_sb[:c, :, :], o_sb[:c, :, :], reads=[o_sb], writes=[osq], op=ALU.mult)
                            V("tensor_reduce", oss[:c, :], osq[:c, :, :], reads=[osq], writes=[oss], axis=AX.X, op=ALU.add)
                            rsqrt(oss[:c, :], oss[:c, :], 1e-5, [oss], oss, scale=1.0 / 64)
                            V("tensor_tensor", o_sb[:c, :, :], o_sb[:c, :, :], oss[:c, :].unsqueeze(2).to_broadcast([c, 4, 64]),
                              reads=[o_sb, oss], writes=[o_sb], op=ALU.mult)
                            V("tensor_tensor", o_sb[:c, :, :], o_sb[:c, :, :], gng[:c, :].unsqueeze(1).to_broadcast([c, 4, 64]),
                              reads=[o_sb, gng], writes=[o_sb], op=ALU.mult)
                            V("tensor_tensor", o_sb[:c, :, :], o_sb[:c, :, :], z_sb[:c, :].rearrange("p (a b) -> p a b", b=64),
                              reads=[o_sb, z_sb], writes=[o_sb], op=ALU.mult)
                            dma(mix[c0:c0 + c, 768:1024], o_sb[:c, :, :].rearrange("p a b -> p (a b)"), [o_sb], [mix])
                        if t == nt - 1:
                            dst = ngdn_s[l].rearrange("h k v -> k h v") if is_sample else ngdn_p[l, j].rearrange("h k v -> k h v")
                            dma(dst, S_sb[:, :, :], [S_sb], [], is_out=True)
                    else:
                        V("memset", mix[:R, 768:1024], 0.0, reads=[], writes=[mix])

                    for kc in range(8):
                        pp = PS[kc // 4]
                        tr(pp[:, (kc % 4) * 128:(kc % 4) * 128 + R], mix[:R, kc * 128:(kc + 1) * 128], ident[:R, :R],
                           [mix, ident], [pp])
                    for hh in range(2):
                        V("tensor_copy", mixT[:, hh * 4:(hh + 1) * 4, :R],
                          PS[hh][:, :].rearrange("p (a b) -> p a b", b=128)[:, :, :R], reads=[PS[hh]], writes=[mixT])
                    for hh in range(2):
                        for kc in range(8):
                            mm(PS[2 + hh][:R, :], mixT[:, kc, :R], w_out_sb[:, kc, hh * 512:(hh + 1) * 512],
                               [mixT, w_out_sb], [PS[2 + hh]], start=(kc == 0), stop=(kc == 7))
                    residual_ln([PS[2][:R, :], PS[3][:R, :], PS[2], PS[3]], R, x_t)
                    store_x(j, t, R, is_sample, x_t)
                P.barrier()

        def phase_b(l, j, T_len, is_sample):
            nt = (T_len + 127) // 128
            with ExitStack() as sbk:
                def sbb(name, shape, dt=F32):
                    return sb(name, shape, dt, stack=sbk)
                wq_sb = sbb("wq_sb", [128, 8, 2048], BF16)
                keysT = sbb("keysT", [128, 16, 128])
                kraw = sbb("kraw", [128, 128])
                h2T = sbb("h2T", [128, 8, 128]); h2Tb = sbb("h2Tb", [128, 8, 128], BF16)
                h2 = sbb("h2", [128, D])
                qT = sbb("qT", [128, 16, 128])
                sc = sbb("sc", [128, 16, 128]); sc2 = sbb("sc2", [128, 16, 128])
                tops = sbb("tops", [128, 16, 16]); topi = sbb("topi", [128, 16, 16], U32); topf = sbb("topf", [128, 16, 16])
                cand = sc2; cand2 = sc
                bests = sbb("bests", [128, 8, 16]); besti = sbb("besti", [128, 8, 16], U32)
                af = sbb("af", [128, 8, 16]); bf = sbb("bf", [128, 8, 16])
                oh = qT
                idf = sbb("idf", [128, 8, 16]); idf2 = sbb("idf2", [128, 8, 16]); ids = sbb("ids", [128, 128], I32)
                gmax = sbb("gmax", [128, 8]); gsum = sbb("gsum", [128, 8]); gates = sbb("gates", [128, 8, 16])
                dots = sbb("dots", [128, 128]); wts = sbb("wts", [128, 128]); gt2 = sbb("gt2", [128, 128])
                NRING = 12
                gbuf = [sbb(f"gbuf{i}", [128, 2 * D], BF16) for i in range(NRING)]
                xg = sbb("xg", [128, 128])
                dcol = sbb("dcol", [128, 128]); wcol = sbb("wcol", [128, 128])
                junk = sbb("junk", [128, D], BF16)
                facc = sbb("facc", [128, D])
                dg = [sbb(f"dg{i}", [128, 128], BF16) for i in range(2)]

                Gt = sbb("Gt", [128, D])
                gate_tile(l, j, 40, Gt)
                wi = 0
                for cb in range(4):
                    n0 = cb * 512
                    for kp in range(4):
                        w = wst[wi % 2]
                        dma(w[:], peer_wq[l, kp * 256:(kp + 1) * 256, n0:n0 + 512].rearrange("(kc p) n -> p kc n", p=128), [], [w])
                        V("tensor_copy", wq_sb[:, 2 * kp:2 * kp + 2, n0:n0 + 512], w[:], reads=[w], writes=[wq_sb],
                          eng=("pool" if wi % 2 == 0 else "dve"))
                        wi += 1
                for c16 in range(16):
                    dma(kraw[:], peer_keys[l, c16], [], [kraw])
                    tr(PS[0][:, 0:128], kraw[:], ident[:], [kraw, ident], [PS[0]])
                    V("tensor_copy", keysT[:, c16, :], PS[0][:, 0:128], reads=[PS[0]], writes=[keysT])
                dma(lng[:], ln2_g[l:l + 1, :].partition_broadcast(128), [], [lng])
                dma(lnb[:], ln2_b[l:l + 1, :].partition_broadcast(128), [], [lnb])

                for t in range(nt):
                    R = min(128, T_len - t * 128)
                    load_x(j, t, R, 1, is_sample)
                    normalize(xh, x_t, R)
                    for kc in range(8):
                        pp = PS[kc // 4]
                        tr(pp[:, (kc % 4) * 128:(kc % 4) * 128 + R], xh[:R, kc * 128:(kc + 1) * 128], ident[:R, :R],
                           [xh, ident], [pp])
                    for kc in range(8):
                        pp = PS[kc // 4]
                        act(h2T[:, kc, :R], pp[:, (kc % 4) * 128:(kc % 4) * 128 + R], AF.Identity, [pp, modT], [h2T],
                            scale=modT[:, l, 32 + kc, j:j + 1], bias=modT[:, l, 24 + kc, j:j + 1])
                    V("tensor_copy", h2Tb[:, :, :R], h2T[:, :, :R], reads=[h2T], writes=[h2Tb])
                    for kc in range(8):
                        pp = PS[2 + kc // 4]
                        tr(pp[:R, (kc % 4) * 128:(kc % 4 + 1) * 128], h2T[:, kc, :R], ident[:, :], [h2T, ident], [pp])
                    for hh in range(2):
                        V("tensor_copy", h2[:R, hh * 512:(hh + 1) * 512], PS[2 + hh][:R, :], reads=[PS[2 + hh]], writes=[h2])
                    for c16 in range(16):
                        pq = PS[4 + (c16 % 2)]
                        for kc in range(8):
                            mm(pq[:, :R], wq_sb[:, kc, c16 * 128:(c16 + 1) * 128], h2Tb[:, kc, :R], [wq_sb, h2Tb], [pq],
                               start=(kc == 0), stop=(kc == 7))
                        act(qT[:, c16, :R], pq[:, :R], AF.Copy, [pq], [qT])
                    for c16 in range(16):
                        pq = PS[6 + (c16 // 4) % 2]
                        mm(pq[:R, (c16 % 4) * 128:(c16 % 4 + 1) * 128], qT[:, c16, :R], keysT[:, c16, :], [qT, keysT], [pq])
                        if c16 % 4 == 3:
                            V("tensor_copy", sc[:R, c16 - 3:c16 + 1, :].rearrange("p a b -> p (a b)"), pq[:R, :], reads=[pq], writes=[sc])
                    for c16 in range(16):
                        V("max", tops[:R, c16, 0:8], sc[:R, c16, :], reads=[sc], writes=[tops])
                        V("max_index", topi[:R, c16, 0:8], tops[:R, c16, 0:8], sc[:R, c16, :], reads=[tops, sc], writes=[topi])
                        V("match_replace", sc2[:R, c16, :], tops[:R, c16, 0:8], sc[:R, c16, :], -1e30, reads=[tops, sc], writes=[sc2])
                        V("max", tops[:R, c16, 8:16], sc2[:R, c16, :], reads=[sc2], writes=[tops])
                        V("max_index", topi[:R, c16, 8:16], tops[:R, c16, 8:16], sc2[:R, c16, :], reads=[tops, sc2], writes=[topi])
                    V("tensor_copy", topf[:R, :, :], topi[:R, :, :], reads=[topi], writes=[topf])
                    t4 = tops[:R, :, :].rearrange("p (h s) k -> p h s k", s=2)
                    f4 = topf[:R, :, :].rearrange("p (h s) k -> p h s k", s=2)
                    candv = cand[:R, :, :].rearrange("p (h s) k -> p h (s k)", s=2)
                    cand2v = cand2[:R, :, :].rearrange("p (h s) k -> p h (s k)", s=2)
                    ohv = oh[:R, :, :].rearrange("p (h a) (b c) -> p h a b c", a=2, c=16)
                    ohv = oh[:R, :, :].rearrange("p a b -> p (a b)").rearrange("p (h j a) -> p h j a", h=8, j=16)
                    c4 = candv.rearrange("p h (a b) -> p h a b", b=16)
                    V("tensor_tensor", c4, t4[:, :, 0, :].unsqueeze(3).to_broadcast([R, 8, 16, 16]),
                      t4[:, :, 1, :].unsqueeze(2).to_broadcast([R, 8, 16, 16]), reads=[tops], writes=[cand], op=ALU.add)
                    for hd in range(8):
                        V("max", bests[:R, hd, 0:8], candv[:, hd, :], reads=[cand], writes=[bests])
                        V("max_index", besti[:R, hd, 0:8], bests[:R, hd, 0:8], candv[:, hd, :], reads=[bests, cand], writes=[besti])
                        V("match_replace", cand2v[:, hd, :], bests[:R, hd, 0:8], candv[:, hd, :], -1e30, reads=[bests, cand], writes=[cand2])
                        V("max", bests[:R, hd, 8:16], cand2v[:, hd, :], reads=[cand2], writes=[bests])
                        V("max_index", besti[:R, hd, 8:16], bests[:R, hd, 8:16], cand2v[:, hd, :], reads=[bests, cand2], writes=[besti])
                    V("tensor_copy", af[:R, :, :], besti[:R, :, :], reads=[besti], writes=[af])
                    V("tensor_copy", bf[:R, :, :], af[:R, :, :], reads=[af], writes=[bf])
                    V("tensor_tensor", ohv, bf[:R, :, :].unsqueeze(3).to_broadcast([R, 8, 16, 16]),
                      thr16[:R, :].unsqueeze(1).unsqueeze(1).to_broadcast([R, 8, 16, 16]), reads=[bf, thr16], writes=[oh], op=ALU.is_ge)
                    V("tensor_reduce", af[:R, :, :], ohv, reads=[oh], writes=[af], axis=AX.X, op=ALU.add)
                    V("scalar_tensor_tensor", bf[:R, :, :], af[:R, :, :], -16.0, bf[:R, :, :], reads=[af, bf], writes=[bf],
                      op0=ALU.mult, op1=ALU.add)
                    io16 = iota_f[:R, 0:16].unsqueeze(1).unsqueeze(1).to_broadcast([R, 8, 16, 16])
                    for sel, half, dstf in ((af, 0, idf), (bf, 1, idf2)):
                        V("tensor_tensor", ohv, sel[:R, :, :].unsqueeze(3).to_broadcast([R, 8, 16, 16]), io16,
                          reads=[sel, iota_f], writes=[oh], op=ALU.is_equal)
                        V("tensor_tensor", ohv, ohv, f4[:, :, half, :].unsqueeze(2).to_broadcast([R, 8, 16, 16]),
                          reads=[oh, topf], writes=[oh], op=ALU.mult)
                        V("tensor_reduce", dstf[:R, :, :], ohv, reads=[oh], writes=[dstf], axis=AX.X, op=ALU.add)
                    V("scalar_tensor_tensor", idf[:R, :, :], idf[:R, :, :], 128.0, idf2[:R, :, :], reads=[idf, idf2], writes=[idf],
                      op0=ALU.mult, op1=ALU.add)
                    V("tensor_scalar_add", idf[:R, :, :], idf[:R, :, :], float(l * 16384), reads=[idf], writes=[idf])
                    V("tensor_copy", ids[:R, :], idf[:R, :, :].rearrange("p a b -> p (a b)"), reads=[idf], writes=[ids])
                    V("tensor_reduce", gmax[:R, :], bests[:R, :, :], reads=[bests], writes=[gmax], axis=AX.X, op=ALU.max)
                    V("tensor_tensor", gates[:R, :, :], bests[:R, :, :], gmax[:R, :].unsqueeze(2).to_broadcast([R, 8, 16]),
                      reads=[bests, gmax], writes=[gates], op=ALU.subtract)
                    act(gates[:R, :, :], gates[:R, :, :], AF.Exp, [gates], [gates])
                    V("tensor_reduce", gsum[:R, :], gates[:R, :, :], reads=[gates], writes=[gsum], axis=AX.X, op=ALU.add)
                    V("reciprocal", gsum[:R, :], gsum[:R, :], reads=[gsum], writes=[gsum])
                    V("tensor_tensor", gates[:R, :, :], gates[:R, :, :], gsum[:R, :].unsqueeze(2).to_broadcast([R, 8, 16]),
                      reads=[gates, gsum], writes=[gates], op=ALU.mult)
                    GS = 4
                    NG = 128 // GS
                    gflat = gates[:R, :, :].rearrange("p a b -> p (a b)")
                    GC = 2.0 * math.sqrt(2.0 / math.pi)

                    def issue(g):
                        for k in range(g * GS, (g + 1) * GS):
                            gb = gbuf[k % NRING]
                            P.op("pool", lambda e, gb=gb, k=k, R=R: e.indirect_dma_start(
                                out=gb[:R, :], out_offset=None, in_=euv[:, :],
                                in_offset=bass.IndirectOffsetOnAxis(ap=ids[:R, k:k + 1], axis=0)),
                                [ids], [gb], dma=True)

                    issue(0)
                    issue(1)
                    for g in range(NG):
                        if g + 2 < NG:
                            issue(g + 2)
                        k0 = g * GS
                        for k in range(k0, k0 + GS):
                            gb = gbuf[k % NRING]
                            V("scalar_tensor_tensor", junk[:R, :], gb[:R, 0:D], 1.0, h2[:R, :], reads=[gb, h2],
                              writes=[junk, dots], op0=ALU.mult, op1=ALU.mult, accum_out=dots[:R, k:k + 1])
                        ds = dots[:R, k0:k0 + GS]
                        tt = gt2[:R, k0:k0 + GS]
                        V("tensor_tensor", tt, ds, ds, reads=[dots], writes=[gt2], op=ALU.mult)
                        V("tensor_scalar", tt, tt, 0.044715, 1.0, reads=[gt2], writes=[gt2], op0=ALU.mult, op1=ALU.add)
                        V("tensor_tensor", tt, tt, ds, reads=[gt2, dots], writes=[gt2], op=ALU.mult)
                        act(tt, tt, AF.Sigmoid, [gt2], [gt2], scale=GC)
                        V("tensor_tensor", xg[:R, k0:k0 + GS], ds, gflat[:, k0:k0 + GS], reads=[dots, gates], writes=[xg], op=ALU.mult)
                        V("tensor_tensor", wts[:R, k0:k0 + GS], tt, xg[:R, k0:k0 + GS], reads=[gt2, xg], writes=[wts], op=ALU.mult)
                        for k in range(k0, k0 + GS):
                            gb = gbuf[k % NRING]
                            d = dg[k % 2]
                            V("tensor_scalar", d[:R, :R], identb[:R, :R], wts[:R, k:k + 1], None, reads=[identb, wts], writes=[d], op0=ALU.mult)
                            for hh in range(2):
                                mm(PS[6 + hh][:R, :], d[:R, :R], gb[:R, D + hh * 512:D + (hh + 1) * 512], [d, gb], [PS[6 + hh]],
                                   start=(k == 0), stop=(k == 127))
                    for hh in range(2):
                        V("tensor_tensor", facc[:R, hh * 512:(hh + 1) * 512], PS[6 + hh][:R, :], Gt[:R, hh * 512:(hh + 1) * 512],
                          reads=[PS[6 + hh], Gt], writes=[facc], op=ALU.mult)
                    residual_ln([facc[:R, 0:512], facc[:R, 512:1024], facc, facc], R, x_t)
                    store_x(j, t, R, is_sample, x_t)
                P.barrier()

        order = [NP] + list(range(NP))
        for j in order:
            is_sample = (j == NP)
            T_len = TS if is_sample else SEQ
            for l in range(L):
                phase_a(l, j, T_len, is_sample)
                if stages >= 5:
                    phase_b(l, j, T_len, is_sample)

        P.emit()
        print("n_ops", P.n_ops, {e: len(v) for e, v in P.ops.items()}, flush=True)
    return nc


WEIGHT_NAMES = ["w_ada", "b_ada", "w_in", "sgu_ln_g", "sgu_ln_b", "sgu_w", "sgu_b", "lam_q1", "lam_k1",
                "lam_q2", "lam_k2", "diff_norm_g", "conv_w", "gdn_a_log", "gdn_dt_bias", "gdn_norm_g",
                "w_out", "ln1_g", "ln1_b", "peer_wq", "peer_keys", "expert_u", "expert_v", "ln2_g", "ln2_b"]


def run(cfg, inputs, stages=99):
    L = cfg.DEPTH
    NP = cfg.BATCH // NCORES
    f = lambda a: np.ascontiguousarray(np.asarray(a, dtype=np.float32))
    W = {k: f(inputs[k]) for k in WEIGHT_NAMES}
    W["sgu_ln_g"] = W["sgu_ln_g"].reshape(L, 256)
    W["sgu_ln_b"] = W["sgu_ln_b"].reshape(L, 256)
    W["peer_keys"] = W["peer_keys"].reshape(L, 16, 128, 128)
    W["expert_u"] = W["expert_u"].reshape(L * 16384, cfg.D)
    W["expert_v"] = W["expert_v"].reshape(L * 16384, cfg.D)
    xp = f(inputs["x_prompt"]); xs = f(inputs["x_sample"])
    ck = f(inputs["cache_k"]); cv = f(inputs["cache_v"])
    sg = f(inputs["state_gdn"]); sc = f(inputs["state_conv"])
    cp = f(inputs["c_prompt"]); cs = f(inputs["c_sample"])
    in_maps = []
    for i in range(NCORES):
        m = dict(W)
        m["xp"] = xp[i * NP:(i + 1) * NP]
        m["xs"] = xs[i]
        m["cache_k"] = f(ck[:, i].reshape(L, cfg.PAST, 512))
        m["cache_v"] = f(cv[:, i].reshape(L, cfg.PAST, 512))
        m["state_gdn"] = f(sg[:, i])
        m["state_conv"] = f(sc[:, i])
        m["c_in"] = f(np.concatenate([cp[i * NP:(i + 1) * NP], cs[i:i + 1]], axis=0))
        in_maps.append(m)
    import time as _time
    _t0 = _time.time()
    nc = build(cfg, stages)
    print("build s", _time.time() - _t0, flush=True)
    _t0 = _time.time()
    import os as _os
    if _os.environ.get("K_TRACE"):
        res = run_bass_kernel_spmd(nc, in_maps, core_ids=list(range(NCORES)), trace=True)
        print("EXEC_TIME_NS", res.exec_time_ns, flush=True)
    else:
        res = run_bass_kernel_spmd(nc, in_maps, core_ids=list(range(NCORES)))
    print("run_spmd s", _time.time() - _t0, flush=True)
    R = res.results
    cat = lambda k, ax: np.concatenate([r[k] for r in R], axis=ax)
    stk = lambda k, ax: np.stack([r[k] for r in R], axis=ax)
    S = cfg.SEQ
    y_prompt = cat("y_p", 0)
    y_sample = stk("y_s", 0)
    nk_p = cat("nk_p", 1).reshape(L, cfg.BATCH, S, 4, 128)
    nv_p = cat("nv_p", 1).reshape(L, cfg.BATCH, S, 4, 128)
    ngdn_p = cat("ngdn_p", 1)
    nconv_p = cat("nconv_p", 1)
    nk_s = stk("nk_s", 1).reshape(L, cfg.DEC_BATCH, cfg.DEC_SEQ, 4, 128)
    nv_s = stk("nv_s", 1).reshape(L, cfg.DEC_BATCH, cfg.DEC_SEQ, 4, 128)
    ngdn_s = stk("ngdn_s", 1)
    nconv_s = stk("nconv_s", 1)
    nsgu_s = stk("nsgu_s", 1)
    return (y_prompt, y_sample, nk_p, nv_p, ngdn_p, nconv_p, nk_s, nv_s, ngdn_s, nconv_s, nsgu_s)


def kernel(**inputs):
    return run(Cfg(), inputs)
```
